# Optimizing a Trainium2 kernel written in Bass

```python
import math, functools
import jax, jax.numpy as jnp
from jax import lax
import numpy as np

D_MODEL = 2048
BATCH = 2
SEQ = 16384
DEPTH = 2

GRID_W = 64
CTX_LEN = 256

HY_WIDTH = D_MODEL // 2
HY_ORDER = 2
HY_SHORT = 3
HY_BANDS = 16
HY_EMB = 1 + 2 * HY_BANDS
HY_FILTER_HIDDEN = 64
HY_FILTER_GAIN = 0.03
HY_MIN_DECAY = math.log(1e-2) / 1.5
HY_MAX_DECAY = math.log(1e-2) / 0.3
GLA_HEADS = 4
GLA_DK = D_MODEL // 4
GLA_DV = D_MODEL // 2
GLA_HEAD_K = GLA_DK // GLA_HEADS
GLA_HEAD_V = GLA_DV // GLA_HEADS
GLA_RANK = 16
GLA_TAU = 16.0
GLA_CHUNK = 64
AB_SIZES = ((HY_ORDER + 1) * HY_WIDTH, GLA_DK, GLA_DK, GLA_DV, GLA_DV, GLA_RANK, GLA_RANK)
AB_IN = sum(AB_SIZES)
AB_MIX = HY_WIDTH + GLA_DV
SSD_D_INNER = 2 * D_MODEL
SSD_HEADDIM = 64
SSD_HEADS = SSD_D_INNER // SSD_HEADDIM
SSD_GROUPS = 8
SSD_HPG = SSD_HEADS // SSD_GROUPS
SSD_STATE = 128
SSD_CONV = 3
SSD_CHUNK = 128
SSD_CONV_DIM = SSD_D_INNER + 2 * SSD_GROUPS * SSD_STATE
SSD_SIZES = (SSD_D_INNER, SSD_CONV_DIM, SSD_HEADS, SSD_HEADS)
SSD_IN = sum(SSD_SIZES)
N_EXPERTS = 16
N_EXPERT_GROUPS = 4
EXPERTS_PER_GROUP = N_EXPERTS // N_EXPERT_GROUPS
TOP_K = 2
D_EXPERT = D_MODEL // 2
MOE_BLOCK = 256
ALPHA = (2 * DEPTH) ** 0.25
BETA = (8 * DEPTH) ** -0.25
EPS = 1e-6

kernel_name = 'hybrid_diffusion_hyena_gla_ssd_moe'


def _split(t, sizes):
    return jnp.split(t, np.cumsum(sizes)[:-1].tolist(), axis=-1)


def _layer_norm(x, g, b):
    xf = x.astype(jnp.float32)
    xc = xf - jnp.mean(xf, -1, keepdims=True)
    var = jnp.mean(xc * xc, -1, keepdims=True)
    return (xc * lax.rsqrt(var + EPS) * g.astype(jnp.float32) + b.astype(jnp.float32)).astype(x.dtype)


def _rms_norm(x, g):
    xf = x.astype(jnp.float32)
    return xf * lax.rsqrt(jnp.mean(xf * xf, -1, keepdims=True) + EPS) * g.astype(jnp.float32)


def _dwconv1d(x, w, b):
    k, L = w.shape[0], x.shape[1]
    xp = jnp.pad(x, ((0, 0), (k // 2, k // 2), (0, 0)))
    return sum(xp[:, j:j + L] * w[j] for j in range(k)) + b


def _dwconv_grid(x, w, b):
    bsz, L, C = x.shape
    rows = L // GRID_W
    k = w.shape[0]
    p = k // 2
    xg = jnp.pad(x.reshape(bsz, rows, GRID_W, C), ((0, 0), (p, p), (p, p), (0, 0)))
    out = sum(xg[:, i:i + rows, j:j + GRID_W] * w[i, j] for i in range(k) for j in range(k))
    return (out + b).reshape(bsz, L, C)


def _hyena_spectra(L, w1, b1, fr1, w2, b2, fr2, w3):
    f32 = jnp.float32
    pos = jnp.arange(L, dtype=f32)[:, None]
    t = pos / max(L - 1, 1)
    bands = jnp.linspace(1e-4, HY_BANDS - 1, HY_BANDS, dtype=f32)
    ang = 2.0 * math.pi * bands * pos / L
    z = jnp.concatenate([t, jnp.cos(ang), -jnp.sin(ang)], axis=-1)
    hid = jnp.sin(fr1.astype(f32) * (z @ w1.astype(f32) + b1.astype(f32)))
    hid = jnp.sin(fr2.astype(f32) * (hid @ w2.astype(f32) + b2.astype(f32)))
    h = (hid @ w3.astype(f32)).reshape(L, HY_ORDER, 2, HY_WIDTH)
    deltas = jnp.abs(jnp.linspace(HY_MIN_DECAY, HY_MAX_DECAY, HY_WIDTH, dtype=f32))
    h = h * jnp.exp(-t * deltas)[:, None, None, :]
    causal, anti = h[:, :, 0], h[:, :, 1]
    full = jnp.concatenate([causal, jnp.zeros_like(causal[:1]), jnp.flip(anti[1:], axis=0)], axis=0)
    return jnp.fft.rfft(full, axis=0)


def _long_conv(u, spec, d):
    L = u.shape[1]
    uf = u.astype(jnp.float32)
    y = jnp.fft.irfft(jnp.fft.rfft(uf, n=2 * L, axis=1) * spec, n=2 * L, axis=1)[:, :L]
    return y + uf * d.astype(jnp.float32)


def _hyena_branch(u, conv_w, conv_b, f_w1, f_b1, f_fr1, f_w2, f_b2, f_fr2, f_w3, long_bias):
    L = u.shape[1]
    parts = jnp.split(_dwconv1d(u, conv_w, conv_b), HY_ORDER + 1, axis=-1)
    spec = _hyena_spectra(L, f_w1, f_b1, f_fr1, f_w2, f_b2, f_fr2, f_w3)
    y = parts[0].astype(jnp.float32)
    for o in range(HY_ORDER):
        y = parts[o + 1].astype(jnp.float32) * _long_conv(y, spec[:, o], long_bias[o])
    return y.astype(u.dtype)


def _gla_scan(q, k, v, g, s0):
    f32 = jnp.float32
    bsz, L = q.shape[:2]
    n = L // GLA_CHUNK

    def chunks(t):
        return jnp.moveaxis(t.astype(f32).reshape((bsz, n, GLA_CHUNK) + t.shape[2:]), 1, 0)

    causal = jnp.tril(jnp.ones((GLA_CHUNK, GLA_CHUNK), bool))[None, :, :, None, None]

    def step(state, inp):
        qc, kc, vc, gc = inp
        b = jnp.cumsum(gc, axis=1)
        decay = jnp.exp(jnp.where(causal, b[:, :, None] - b[:, None, :], -jnp.inf))
        att = jnp.einsum('bihk,bjhk,bijhk->bhij', qc, kc, decay)
        o = jnp.einsum('bhij,bjhv->bihv', att, vc) + jnp.einsum('bihk,bhkv->bihv', qc * jnp.exp(b), state)
        tail = jnp.exp(b[:, -1:] - b)
        state = jnp.exp(b[:, -1])[..., None] * state + jnp.einsum('bjhk,bjhv->bhkv', kc * tail, vc)
        return state, o

    state, o = lax.scan(step, s0, (chunks(q), chunks(k), chunks(v), chunks(g)))
    return jnp.moveaxis(o, 0, 1).reshape(bsz, L, GLA_HEADS, GLA_HEAD_V), state


def _ssd_scan(x, dt, bmat, cmat, s0, a_neg, d_skip):
    f32 = jnp.float32
    bsz, L = x.shape[:2]
    n = L // SSD_CHUNK
    x = x.astype(f32)
    a = dt * a_neg
    xdt = x * dt[..., None]

    def chunks(t):
        return jnp.moveaxis(t.astype(f32).reshape((bsz, n, SSD_CHUNK) + t.shape[2:]), 1, 0)

    causal = jnp.tril(jnp.ones((SSD_CHUNK, SSD_CHUNK), bool))[None, :, :, None, None]

    def step(state, inp):
        xc, ac, bc, cc = inp
        acs = jnp.cumsum(ac, axis=1)
        decay = jnp.exp(jnp.where(causal, acs[:, :, None] - acs[:, None, :], -jnp.inf))
        cb = jnp.einsum('bign,bjgn->bijg', cc, bc)
        y = jnp.einsum('bijg,bijgr,bjgrp->bigrp', cb, decay, xc)
        y = y + jnp.einsum('bign,bgrpn->bigrp', cc, state) * jnp.exp(acs)[..., None]
        tail = jnp.exp(acs[:, -1:] - acs)
        state = jnp.exp(acs[:, -1])[..., None, None] * state + jnp.einsum('bjgn,bjgr,bjgrp->bgrpn', bc, tail, xc)
        return state, y

    state, y = lax.scan(step, s0, (chunks(xdt), chunks(a), chunks(bmat), chunks(cmat)))
    y = jnp.moveaxis(y, 0, 1).reshape(x.shape) + d_skip[..., None] * x
    return y, state


def _two_way(scan_f, scan_b, ctx_f, ctx_b, lat_f, lat_b, s0):
    def flip(args):
        return tuple(jnp.flip(a, axis=1) for a in args)
    yc_f, st_f = scan_f(*ctx_f, s0)
    yc_b, st_b = scan_b(*flip(ctx_b), s0)
    yl_f, _ = scan_f(*lat_f, st_f)
    yl_b, _ = scan_b(*flip(lat_b), st_b)
    return yc_f + jnp.flip(yc_b, axis=1), yl_f + jnp.flip(yl_b, axis=1)


def _hyena_gla_mixer(h_ctx, h_lat, w_in, w_out, hy_p, gate_w2, gate_b, norm_g, need_ctx):
    f32 = jnp.float32

    def project(h):
        bsz, L = h.shape[:2]
        u, q, k, v, r, g1f, g1b = _split(h @ w_in, AB_SIZES)

        def log_gate(g1, d):
            z = (g1 @ gate_w2[d] + gate_b[d]).astype(f32)
            return (jax.nn.log_sigmoid(z) / GLA_TAU).reshape(bsz, L, GLA_HEADS, GLA_HEAD_K)

        q = q.reshape(bsz, L, GLA_HEADS, GLA_HEAD_K) * GLA_HEAD_K ** -0.5
        k = k.reshape(bsz, L, GLA_HEADS, GLA_HEAD_K)
        v = v.reshape(bsz, L, GLA_HEADS, GLA_HEAD_V)
        return u, (q, k, v), r, log_gate(g1f, 0), log_gate(g1b, 1)

    u_c, qkv_c, r_c, gf_c, gb_c = project(h_ctx)
    u_l, qkv_l, r_l, gf_l, gb_l = project(h_lat)
    s0 = jnp.zeros((h_lat.shape[0], GLA_HEADS, GLA_HEAD_K, GLA_HEAD_V), f32)
    o_c, o_l = _two_way(_gla_scan, _gla_scan, (*qkv_c, gf_c), (*qkv_c, gb_c), (*qkv_l, gf_l), (*qkv_l, gb_l), s0)

    def finish(u, o, r):
        bsz, L = u.shape[:2]
        o = _rms_norm(o, norm_g.reshape(GLA_HEADS, GLA_HEAD_V)).reshape(bsz, L, GLA_DV)
        o = (o * jax.nn.silu(r.astype(f32))).astype(u.dtype)
        return jnp.concatenate([_hyena_branch(u, *hy_p), o], axis=-1) @ w_out

    y_lat = finish(u_l, o_l, r_l)
    y_ctx = finish(u_c, o_c, r_c) if need_ctx else None
    return y_ctx, y_lat


def _mamba_mixer(h_ctx, h_lat, w_in, conv_w, conv_b, dt_bias, a_log, d_skip, norm_g, w_out, need_ctx):
    f32 = jnp.float32

    def project(h, conv):
        bsz, L = h.shape[:2]
        z, xbc, dt_f, dt_b = _split(h @ w_in, SSD_SIZES)
        xbc = jax.nn.silu(conv(xbc))
        xs, bm, cm = _split(xbc, (SSD_D_INNER, SSD_GROUPS * SSD_STATE, SSD_GROUPS * SSD_STATE))
        xs = xs.reshape(bsz, L, SSD_GROUPS, SSD_HPG, SSD_HEADDIM)
        bm = bm.reshape(bsz, L, SSD_GROUPS, SSD_STATE)
        cm = cm.reshape(bsz, L, SSD_GROUPS, SSD_STATE)
        dts = [jax.nn.softplus(t.astype(f32) + dt_bias[d].astype(f32)).reshape(bsz, L, SSD_GROUPS, SSD_HPG)
               for d, t in enumerate((dt_f, dt_b))]
        return z, (xs, dts[0], bm, cm), (xs, dts[1], bm, cm)

    z_c, ctx_f, ctx_b = project(h_ctx, lambda t: _dwconv1d(t, conv_w[SSD_CONV // 2], conv_b))
    z_l, lat_f, lat_b = project(h_lat, lambda t: _dwconv_grid(t, conv_w, conv_b))
    a_neg = -jnp.exp(a_log.astype(f32)).reshape(2, SSD_GROUPS, SSD_HPG)
    dsk = d_skip.astype(f32).reshape(2, SSD_GROUPS, SSD_HPG)
    scan_f = functools.partial(_ssd_scan, a_neg=a_neg[0], d_skip=dsk[0])
    scan_b = functools.partial(_ssd_scan, a_neg=a_neg[1], d_skip=dsk[1])
    s0 = jnp.zeros((h_lat.shape[0], SSD_GROUPS, SSD_HPG, SSD_HEADDIM, SSD_STATE), f32)
    y_c, y_l = _two_way(scan_f, scan_b, ctx_f, ctx_b, lat_f, lat_b, s0)

    def finish(y, z):
        bsz, L = z.shape[:2]
        y = y.reshape(bsz, L, SSD_D_INNER) * jax.nn.silu(z.astype(f32))
        y = _rms_norm(y.reshape(bsz, L, SSD_GROUPS, SSD_D_INNER // SSD_GROUPS), norm_g.reshape(SSD_GROUPS, -1))
        return y.reshape(bsz, L, SSD_D_INNER).astype(z.dtype) @ w_out

    y_lat = finish(y_l, z_l)
    y_ctx = finish(y_c, z_c) if need_ctx else None
    return y_ctx, y_lat


def _moe(h, router_w, router_b, w_gate, w_up, w_down):
    f32 = jnp.float32
    t = h.shape[0]
    scores = jax.nn.sigmoid(h.astype(f32) @ router_w.astype(f32))
    sel = (scores + router_b.astype(f32)).reshape(t, N_EXPERT_GROUPS, EXPERTS_PER_GROUP)
    group = jnp.argmax(lax.top_k(sel, 2)[0].sum(-1), axis=-1)
    in_group = jnp.take_along_axis(sel, group[:, None, None], axis=1)[:, 0]
    experts = group[:, None] * EXPERTS_PER_GROUP + lax.top_k(in_group, TOP_K)[1]
    gates = jnp.take_along_axis(scores, experts, axis=1)
    gates = gates / jnp.sum(gates, -1, keepdims=True)
    e_flat = experts.reshape(-1)
    tk = e_flat.shape[0]
    order = jnp.argsort(e_flat)
    e_s = e_flat[order]
    tok_s = order // TOP_K
    g_s = gates.reshape(-1)[order]
    counts = jnp.bincount(e_flat, length=N_EXPERTS)
    padded = (counts + MOE_BLOCK - 1) // MOE_BLOCK * MOE_BLOCK
    starts = jnp.cumsum(counts) - counts
    pstarts = jnp.cumsum(padded) - padded
    dest = pstarts[e_s] + jnp.arange(tk) - starts[e_s]
    n_blocks = -(-tk // MOE_BLOCK) + N_EXPERTS
    buf = jnp.zeros((n_blocks * MOE_BLOCK, h.shape[1]), h.dtype).at[dest].set(h[tok_s])
    block_expert = jnp.minimum(
        jnp.searchsorted(pstarts + padded, jnp.arange(n_blocks) * MOE_BLOCK, side='right'), N_EXPERTS - 1)

    def expert_block(args):
        xb, e = args
        return (jax.nn.silu(xb @ w_gate[e]) * (xb @ w_up[e])) @ w_down[e]

    out_blocks = lax.map(expert_block, (buf.reshape(n_blocks, MOE_BLOCK, -1), block_expert))
    rows = (out_blocks.reshape(n_blocks * MOE_BLOCK, -1)[dest] * g_s[:, None]).astype(h.dtype)
    return jnp.zeros_like(h).at[tok_s].add(rows)


def setup_inputs(seed: int = 0) -> dict:
    key = jax.random.key(seed)
    keys = iter(jax.random.split(key, 48))

    def nrm(shape, scale):
        return scale * jax.random.normal(next(keys), shape, jnp.float32)

    ne, no = (DEPTH + 1) // 2, DEPTH // 2
    fh = HY_FILTER_HIDDEN
    dt0 = jnp.exp(jax.random.uniform(next(keys), (no, 2, SSD_HEADS), jnp.float32, math.log(1e-3), math.log(1e-1)))
    a0 = jax.random.uniform(next(keys), (no, 2, SSD_HEADS), jnp.float32, 1.0, 16.0)
    return {
        'x': nrm((BATCH, SEQ, D_MODEL), 1.0),
        'c': nrm((BATCH, D_MODEL), 1.0),
        'ctx': nrm((BATCH, CTX_LEN, D_MODEL), 1.0),
        'c_ctx': nrm((D_MODEL,), 1.0),
        'router_w': nrm((D_MODEL, N_EXPERTS), D_MODEL ** -0.5),
        'router_b': nrm((N_EXPERTS,), 0.01),
        'mod_w': nrm((DEPTH, D_MODEL, 6 * D_MODEL), 0.5 * D_MODEL ** -0.5),
        'mod_b': nrm((DEPTH, 6 * D_MODEL), 0.02),
        'ln_g': 1.0 + nrm((DEPTH, 2, D_MODEL), 0.02),
        'ln_b': nrm((DEPTH, 2, D_MODEL), 0.02),
        'exp_w_gate': nrm((DEPTH, N_EXPERTS, D_MODEL, D_EXPERT), D_MODEL ** -0.5),
        'exp_w_up': nrm((DEPTH, N_EXPERTS, D_MODEL, D_EXPERT), D_MODEL ** -0.5),
        'exp_w_down': nrm((DEPTH, N_EXPERTS, D_EXPERT, D_MODEL), BETA * D_EXPERT ** -0.5),
        'ab_w_in': nrm((ne, D_MODEL, AB_IN), D_MODEL ** -0.5),
        'ab_w_out': nrm((ne, AB_MIX, D_MODEL), BETA * AB_MIX ** -0.5),
        'hy_conv_w': nrm((ne, HY_SHORT, (HY_ORDER + 1) * HY_WIDTH), HY_SHORT ** -0.5),
        'hy_conv_b': nrm((ne, (HY_ORDER + 1) * HY_WIDTH), 0.02),
        'hy_f_w1': nrm((ne, HY_EMB, fh), HY_EMB ** -0.5),
        'hy_f_b1': nrm((ne, fh), 0.02),
        'hy_f_fr1': 1.0 + nrm((ne, fh), 0.1),
        'hy_f_w2': nrm((ne, fh, fh), fh ** -0.5),
        'hy_f_b2': nrm((ne, fh), 0.02),
        'hy_f_fr2': 1.0 + nrm((ne, fh), 0.1),
        'hy_f_w3': nrm((ne, fh, HY_ORDER * 2 * HY_WIDTH), HY_FILTER_GAIN * fh ** -0.5),
        'hy_long_bias': nrm((ne, HY_ORDER, HY_WIDTH), 1.0),
        'gla_gate_w2': nrm((ne, 2, GLA_RANK, GLA_DK), GLA_RANK ** -0.5),
        'gla_gate_b': nrm((ne, 2, GLA_DK), 0.5),
        'gla_norm_g': 1.0 + nrm((ne, GLA_DV), 0.02),
        'ssd_w_in': nrm((no, D_MODEL, SSD_IN), D_MODEL ** -0.5),
        'ssd_conv_w': nrm((no, SSD_CONV, SSD_CONV, SSD_CONV_DIM), 1.0 / SSD_CONV),
        'ssd_conv_b': nrm((no, SSD_CONV_DIM), 0.02),
        'ssd_dt_bias': dt0 + jnp.log(-jnp.expm1(-dt0)),
        'ssd_a_log': jnp.log(a0),
        'ssd_d': 1.0 + nrm((no, 2, SSD_HEADS), 0.1),
        'ssd_norm_g': 1.0 + nrm((no, SSD_D_INNER), 0.02),
        'ssd_w_out': nrm((no, SSD_D_INNER, D_MODEL), BETA * SSD_D_INNER ** -0.5),
    }


def reference(x, c, ctx, c_ctx, router_w, router_b, mod_w, mod_b, ln_g, ln_b,
              exp_w_gate, exp_w_up, exp_w_down, ab_w_in, ab_w_out,
              hy_conv_w, hy_conv_b, hy_f_w1, hy_f_b1, hy_f_fr1, hy_f_w2, hy_f_b2, hy_f_fr2, hy_f_w3, hy_long_bias,
              gla_gate_w2, gla_gate_b, gla_norm_g,
              ssd_w_in, ssd_conv_w, ssd_conv_b, ssd_dt_bias, ssd_a_log, ssd_d, ssd_norm_g, ssd_w_out):
    n_lat = x.shape[0] * x.shape[1]
    for i in range(DEPTH):
        last = i == DEPTH - 1
        j = i // 2
        mod_l = jnp.split((jax.nn.silu(c) @ mod_w[i] + mod_b[i])[:, None, :], 6, axis=-1)
        mod_c = jnp.split(jax.nn.silu(c_ctx) @ mod_w[i] + mod_b[i], 6, axis=-1)
        h_l = x * (1 + mod_l[1]) + mod_l[0]
        h_c = ctx * (1 + mod_c[1]) + mod_c[0]
        if i % 2 == 0:
            hy_p = (hy_conv_w[j], hy_conv_b[j], hy_f_w1[j], hy_f_b1[j], hy_f_fr1[j], hy_f_w2[j], hy_f_b2[j],
                    hy_f_fr2[j], hy_f_w3[j], hy_long_bias[j])
            y_c, y_l = _hyena_gla_mixer(h_c, h_l, ab_w_in[j], ab_w_out[j], hy_p, gla_gate_w2[j], gla_gate_b[j],
                                        gla_norm_g[j], not last)
        else:
            y_c, y_l = _mamba_mixer(h_c, h_l, ssd_w_in[j], ssd_conv_w[j], ssd_conv_b[j], ssd_dt_bias[j],
                                    ssd_a_log[j], ssd_d[j], ssd_norm_g[j], ssd_w_out[j], not last)
        x = _layer_norm(ALPHA * x + mod_l[2] * y_l, ln_g[i, 0], ln_b[i, 0])
        tokens = (x * (1 + mod_l[4]) + mod_l[3]).reshape(n_lat, D_MODEL)
        if not last:
            ctx = _layer_norm(ALPHA * ctx + mod_c[2] * y_c, ln_g[i, 0], ln_b[i, 0])
            h_c = ctx * (1 + mod_c[4]) + mod_c[3]
            tokens = jnp.concatenate([tokens, h_c.reshape(-1, D_MODEL)], axis=0)
        y = _moe(tokens, router_w, router_b, exp_w_gate[i], exp_w_up[i], exp_w_down[i])
        x = _layer_norm(ALPHA * x + mod_l[5] * y[:n_lat].reshape(x.shape), ln_g[i, 1], ln_b[i, 1])
        if not last:
            ctx = _layer_norm(ALPHA * ctx + mod_c[5] * y[n_lat:].reshape(ctx.shape), ln_g[i, 1], ln_b[i, 1])
    return x
```

```python
import math
from contextlib import ExitStack
import numpy as np
import concourse.bass as bass
import concourse.mybir as mybir
from concourse.bass_utils import run_bass_kernel_spmd

F32 = mybir.dt.float32
BF16 = mybir.dt.bfloat16
AF = mybir.ActivationFunctionType
ALU = mybir.AluOpType
AX = mybir.AxisListType

D = 2048
NE = 16
DEXP = 1024
ALPHA = (2 * 2) ** 0.25
EPS = 1e-6
SEM_LIMIT = 30000


class Buf:
    def __init__(self, t=None, name=""):
        self.t = t
        self.name = name
        self.w = None
        self.r = []

    def __getitem__(self, idx):
        return self.t[idx]


class Prog:
    def __init__(self):
        self.nc = bass.Bass("TRN2", target_bir_lowering=False)
        nc = self.nc
        self.E = {"pe": nc.tensor, "dve": nc.vector, "act": nc.scalar, "pool": nc.gpsimd, "sp": nc.sync}
        self.nsem = 0
        self.sem = {}
        self.cnt = {}
        for e in self.E:
            self._new_eng_sem(e)
        self.waited = {e: {} for e in self.E}
        self.dsem = []
        self.dcnt = []
        for i in range(12):
            self.dsem.append(self._alloc_sem())
            self.dcnt.append(0)
        self.drr = 0
        self.ninstr = 0
        self.uid = 0

    def _alloc_sem(self):
        self.nsem += 1
        return (self.nsem, self.nc.alloc_semaphore(f"s{self.nsem}"))

    def _new_eng_sem(self, e):
        self.sem[e] = self._alloc_sem()
        self.cnt[e] = 0

    def sb(self, shape, dt=F32, name=None):
        self.uid += 1
        return Buf(self.nc.alloc_sbuf_tensor(name or f"sb{self.uid}", list(shape), dt), name or f"sb{self.uid}")

    def ps(self, name=None):
        self.uid += 1
        return Buf(self.nc.alloc_psum_tensor(name or f"ps{self.uid}", [128, 512], F32), name or f"ps{self.uid}")

    def dram(self, name, shape, dt=F32, kind="Internal"):
        return Buf(self.nc.dram_tensor(name, list(shape), dt, kind=kind).ap(), name)

    def _wait(self, eng, tickets):
        wd = self.waited[eng]
        need = {}
        for tk in tickets:
            if tk is None:
                continue
            (sid, sh), v = tk
            if wd.get(sid, 0) >= v:
                continue
            if sid not in need or need[sid][1] < v:
                need[sid] = (sh, v)
        for sid, (sh, v) in need.items():
            self.E[eng].wait_ge(sh, v)
            wd[sid] = v
            self.ninstr += 1

    def _deps(self, r, w):
        t = []
        for b in r:
            t.append(b.w)
        for b in w:
            t.append(b.w)
            t.extend(b.r)
        return t

    def _commit(self, tk, r, w):
        for b in r:
            b.r.append(tk)
            if len(b.r) > 24:
                b.r = b.r[-24:] if False else b.r
        for b in w:
            b.w = tk
            b.r = []

    def op(self, eng, fn, r=(), w=()):
        self._wait(eng, self._deps(r, w))
        if self.cnt[eng] >= SEM_LIMIT:
            self._new_eng_sem(eng)
        ins = fn(self.E[eng])
        self.cnt[eng] += 1
        s = self.sem[eng]
        ins.then_inc(s[1], 1)
        tk = (s, self.cnt[eng])
        self._commit(tk, r, w)
        self.ninstr += 1
        return tk

    def dma(self, q, out, in_, r=(), w=(), **kw):
        i = self.drr
        self.drr = (self.drr + 1) % len(self.dsem)
        s = self.dsem[i]
        self._wait(q, self._deps(r, w) + [(s, self.dcnt[i])] if self.dcnt[i] else self._deps(r, w))
        if self.dcnt[i] >= SEM_LIMIT * 16:
            self.dsem[i] = self._alloc_sem()
            self.dcnt[i] = 0
            s = self.dsem[i]
        ins = self.E[q].dma_start(out=out, in_=in_, **kw)
        self.dcnt[i] += 16
        ins.then_inc(s[1], 16)
        tk = (s, self.dcnt[i])
        self._commit(tk, r, w)
        self.ninstr += 1
        return tk

    def barrier(self):
        tks = [(self.sem[e], self.cnt[e]) for e in self.E if self.cnt[e]]
        tks += [(self.dsem[i], self.dcnt[i]) for i in range(len(self.dsem)) if self.dcnt[i]]
        for e in self.E:
            self._wait(e, tks)

    def sbs(self, stack, shape, dt=F32):
        self.uid += 1
        return Buf(stack.enter_context(self.nc.sbuf_tensor(f"sc{self.uid}", list(shape), dt)), f"sc{self.uid}")

    def finish(self, bufs):
        self._wait("sp", [b.w for b in bufs])


def make_ident(P, dt=F32):
    ident = P.sb([128, 128], dt, "ident_" + str(dt))
    P.op("pool", lambda e: e.memset(ident[:], 0.0), w=[ident])
    P.op("pool", lambda e: e.affine_select(out=ident[:], in_=ident[:], pattern=[[-1, 128]],
                                           compare_op=ALU.not_equal, fill=1.0, base=0, channel_multiplier=1),
         r=[ident], w=[ident])
    return ident


def bc_ap(ap1d, n):
    return bass.AP(ap1d.tensor, ap1d.offset, [[0, 128], [1, n]])


def layer_norm_tile(P, n, pre, tmp, gbc, bbc, st):
    P.op("dve", lambda e: e.reduce_sum(out=st[:n, 0:1], in_=pre[:n, :], axis=AX.X), r=[pre], w=[st])
    P.op("dve", lambda e: e.tensor_scalar(out=st[:n, 1:2], in0=st[:n, 0:1], scalar1=-1.0 / D, scalar2=None,
                                          op0=ALU.mult), r=[st], w=[st])
    P.op("act", lambda e: e.activation(out=tmp[:n, :], in_=pre[:n, :], func=AF.Square, bias=st[:n, 1:2], scale=1.0),
         r=[pre, st], w=[tmp])
    P.op("dve", lambda e: e.reduce_sum(out=st[:n, 2:3], in_=tmp[:n, :], axis=AX.X), r=[tmp], w=[st])
    P.op("dve", lambda e: e.tensor_scalar(out=st[:n, 2:3], in0=st[:n, 2:3], scalar1=1.0 / D, scalar2=EPS,
                                          op0=ALU.mult, op1=ALU.add), r=[st], w=[st])
    P.op("act", lambda e: e.activation(out=st[:n, 2:3], in_=st[:n, 2:3], func=AF.Sqrt), r=[st], w=[st])
    P.op("dve", lambda e: e.reciprocal(out=st[:n, 3:4], in_=st[:n, 2:3]), r=[st], w=[st])
    P.op("dve", lambda e: e.tensor_scalar(out=pre[:n, :], in0=pre[:n, :], scalar1=st[:n, 1:2], scalar2=st[:n, 3:4],
                                          op0=ALU.add, op1=ALU.mult), r=[pre, st], w=[pre])
    P.op("dve", lambda e: e.tensor_tensor(out=pre[:n, :], in0=pre[:n, :], in1=gbc[:n, :], op=ALU.mult),
         r=[pre, gbc], w=[pre])
    P.op("dve", lambda e: e.tensor_tensor(out=pre[:n, :], in0=pre[:n, :], in1=bbc[:n, :], op=ALU.add),
         r=[pre, bbc], w=[pre])


def build_post(T_l, T_c, KC, NEXP=NE):
    P = Prog()
    T = T_l + T_c
    IN = "ExternalInput"
    yT = P.dram("yT", [KC * 128, T], F32, IN)
    xres = P.dram("xres", [T, D], F32, IN)
    w_out = P.dram("w_out", [KC * 128, D], F32, IN)
    modrow = P.dram("modrow", [2, 6, D], F32, IN)
    modcol = P.dram("modcol", [2, 128, 6 * 16], F32, IN)
    lng = P.dram("lng", [2, D], F32, IN)
    lnb = P.dram("lnb", [2, D], F32, IN)
    rw = P.dram("rw", [D, NE], F32, IN)
    rb = P.dram("rb", [NE], F32, IN)
    wg = P.dram("wg", [NEXP, D, DEXP], F32, IN)
    wu = P.dram("wu", [NEXP, D, DEXP], F32, IN)
    wd = P.dram("wd", [NEXP, DEXP, D], F32, IN)
    ident_d = P.dram("ident", [128, 128], F32, IN)
    sel_d = P.dram("sel", [16, 16 * 128], F32, IN)
    xout = P.dram("xout", [T, D], F32, "ExternalOutput")
    x1_d = P.dram("x1_d", [T, D], F32)
    tokT_d = P.dram("tokT_d", [D, T], F32)

    sb, ps = P.sb, P.ps
    ident = sb([128, 128]); sel = sb([16, 16 * 128])
    rws = sb([128, 16, NE]); rbb = sb([128, NE])
    mcol = [sb([128, 96]) for _ in range(2)]
    sc2p1 = [sb([128, 16]) for _ in range(2)]
    bcA = sb([128, D]); bcB = sb([128, D]); bcC = sb([128, D])
    xt = sb([128, D]); tmp = sb([128, D]); st = sb([128, 4])
    ytile = sb([128, KC, 128], BF16)
    wring = [sb([128, D], BF16) for _ in range(2)]
    tokT32 = sb([128, 16, 128])
    GT = sb([16, T])
    PS = [ps() for _ in range(8)]
    sm = {k: sb([128, 16]) for k in ["sc", "sel", "eq", "s2", "t2", "msk", "gs"]}
    sm4 = {k: sb([128, 4]) for k in ["m1", "m2", "gs", "gm"]}
    sm1 = {k: sb([128, 1]) for k in ["gmax", "den"]}

    def ld(q, dst, src, wbuf, rbuf=()):
        P.dma(q, dst, src, r=list(rbuf), w=[wbuf])

    ld("sp", ident[:], ident_d[:, :], ident, [ident_d])
    ld("sp", sel[:], sel_d[:, :], sel, [sel_d])
    ld("sp", rws[:], rw.t.rearrange("(kc p) e -> p kc e", p=128), rws, [rw])
    ld("sp", rbb[:], bc_ap(rb.t, NE), rbb, [rb])
    for s in range(2):
        ld("sp", mcol[s][:], modcol[s, :, :], mcol[s], [modcol])
        P.op("dve", lambda e, s=s: e.tensor_scalar(out=sc2p1[s][:], in0=mcol[s][:, 64:80], scalar1=1.0, scalar2=None,
                                                    op0=ALU.add), r=[mcol[s]], w=[sc2p1[s]])
    ld("sp", bcB[:], bc_ap(lng[0, :], D), bcB, [lng])
    ld("sp", bcC[:], bc_ap(lnb[0, :], D), bcC, [lnb])

    tiles = [(t0, 128, 0) for t0 in range(0, T_l, 128)]
    if T_c:
        tiles.append((T_l, T_c, 1))
    cur_set = [-1]

    for ti, (t0, n, s) in enumerate(tiles):
        if cur_set[0] != s:
            ld("sp", bcA[:], bc_ap(modrow[s, 2, :], D), bcA, [modrow])
            cur_set[0] = s
        ld("pool", ytile[:, :, :n], yT.t[:, t0:t0 + n].rearrange("(kc p) t -> p kc t", p=128), ytile, [yT])
        ld("sp", xt[:n, :], xres[t0:t0 + n, :], xt, [xres])
        for kc in range(KC):
            ws = wring[kc % 2]
            ld("pool", ws[:], w_out[kc * 128:(kc + 1) * 128, :], ws, [w_out])
            for j in range(4):
                P.op("pe", lambda e, j=j, kc=kc, ws=ws: e.matmul(PS[j][:n, :], ytile[:, kc, :n], ws[:, j * 512:(j + 1) * 512],
                                                                start=(kc == 0), stop=(kc == KC - 1)),
                     r=[ytile, ws], w=[PS[j]])
        for j in range(4):
            P.op("dve", lambda e, j=j: e.tensor_tensor(out=tmp[:n, j * 512:(j + 1) * 512], in0=PS[j][:n, :],
                                                      in1=bcA[:n, j * 512:(j + 1) * 512], op=ALU.mult),
                 r=[PS[j], bcA], w=[tmp])
        P.op("dve", lambda e: e.scalar_tensor_tensor(out=xt[:n, :], in0=xt[:n, :], scalar=ALPHA, in1=tmp[:n, :],
                                                     op0=ALU.mult, op1=ALU.add), r=[xt, tmp], w=[xt])
        layer_norm_tile(P, n, xt, tmp, bcB, bcC, st)
        ld("sp", x1_d[t0:t0 + n, :], xt[:n, :], x1_d, [xt])
        for kc in range(16):
            P.op("pe", lambda e, kc=kc: e.transpose(PS[4 + kc // 4][:, (kc % 4) * 128:(kc % 4) * 128 + n],
                                                    xt[:n, kc * 128:(kc + 1) * 128], ident[:n, :n]),
                 r=[xt, ident], w=[PS[4 + kc // 4]])
        for kc in range(16):
            P.op("act", lambda e, kc=kc: e.activation(out=tokT32[:, kc, :n],
                                                      in_=PS[4 + kc // 4][:, (kc % 4) * 128:(kc % 4) * 128 + n],
                                                      func=AF.Identity, bias=mcol[s][:, 48 + kc:49 + kc],
                                                      scale=sc2p1[s][:, kc:kc + 1]),
                 r=[PS[4 + kc // 4], mcol[s], sc2p1[s]], w=[tokT32])
        ld("sp", tokT_d.t[:, t0:t0 + n].rearrange("(kc p) t -> p kc t", p=128), tokT32[:, :, :n], tokT_d, [tokT32])
        for kc in range(16):
            P.op("pe", lambda e, kc=kc: e.matmul(PS[0][:n, 0:NE], tokT32[:, kc, :n], rws[:, kc, :],
                                                 start=(kc == 0), stop=(kc == 15)), r=[tokT32, rws], w=[PS[0]])
        sc, sl, eq, s2, t2, msk, gsel = (sm[k] for k in ["sc", "sel", "eq", "s2", "t2", "msk", "gs"])
        m1, m2, gs, gm = (sm4[k] for k in ["m1", "m2", "gs", "gm"])
        gmax, den = sm1["gmax"], sm1["den"]
        v3 = lambda b: b[:n, :].rearrange("p (g e) -> p g e", g=4)
        b4 = lambda b: b[:n, :].unsqueeze(2).to_broadcast([n, 4, 4])
        P.op("act", lambda e: e.activation(out=sc[:n, :], in_=PS[0][:n, 0:NE], func=AF.Sigmoid), r=[PS[0]], w=[sc])
        P.op("dve", lambda e: e.tensor_tensor(out=sl[:n, :], in0=sc[:n, :], in1=rbb[:n, :], op=ALU.add), r=[sc, rbb], w=[sl])
        P.op("dve", lambda e: e.tensor_reduce(out=m1[:n, :], in_=v3(sl), axis=AX.X, op=ALU.max), r=[sl], w=[m1])
        P.op("dve", lambda e: e.tensor_tensor(out=v3(eq), in0=v3(sl), in1=b4(m1), op=ALU.is_equal), r=[sl, m1], w=[eq])
        P.op("dve", lambda e: e.scalar_tensor_tensor(out=s2[:n, :], in0=eq[:n, :], scalar=-1e9, in1=sl[:n, :],
                                                     op0=ALU.mult, op1=ALU.add), r=[eq, sl], w=[s2])
        P.op("dve", lambda e: e.tensor_reduce(out=m2[:n, :], in_=v3(s2), axis=AX.X, op=ALU.max), r=[s2], w=[m2])
        P.op("dve", lambda e: e.tensor_tensor(out=gs[:n, :], in0=m1[:n, :], in1=m2[:n, :], op=ALU.add), r=[m1, m2], w=[gs])
        P.op("dve", lambda e: e.tensor_reduce(out=gmax[:n, :], in_=gs[:n, :], axis=AX.X, op=ALU.max), r=[gs], w=[gmax])
        P.op("dve", lambda e: e.tensor_scalar(out=gm[:n, :], in0=gs[:n, :], scalar1=gmax[:n, 0:1], scalar2=None,
                                              op0=ALU.is_equal), r=[gs, gmax], w=[gm])
        P.op("dve", lambda e: e.tensor_tensor(out=v3(t2), in0=v3(sl), in1=b4(m2), op=ALU.is_ge), r=[sl, m2], w=[t2])
        P.op("dve", lambda e: e.tensor_tensor(out=v3(msk), in0=v3(t2), in1=b4(gm), op=ALU.mult), r=[t2, gm], w=[msk])
        P.op("dve", lambda e: e.tensor_tensor(out=gsel[:n, :], in0=sc[:n, :], in1=msk[:n, :], op=ALU.mult), r=[sc, msk], w=[gsel])
        P.op("dve", lambda e: e.reduce_sum(out=den[:n, :], in_=gsel[:n, :], axis=AX.X), r=[gsel], w=[den])
        P.op("dve", lambda e: e.reciprocal(out=den[:n, :], in_=den[:n, :]), r=[den], w=[den])
        P.op("dve", lambda e: e.tensor_scalar(out=gsel[:n, :], in0=gsel[:n, :], scalar1=den[:n, 0:1], scalar2=None,
                                              op0=ALU.mult), r=[gsel, den], w=[gsel])
        P.op("pe", lambda e: e.transpose(PS[1][0:16, 0:n], gsel[:n, :], ident[:n, :n]), r=[gsel, ident], w=[PS[1]])
        P.op("act", lambda e: e.activation(out=GT[:, t0:t0 + n], in_=PS[1][0:16, 0:n], func=AF.Copy), r=[PS[1]], w=[GT])

    tb = sb([128, 16, 512], BF16)
    acc = sb([128, 16, 512])
    hT = sb([128, 8, 512], BF16)
    sg = [sb([128, 512]) for _ in range(2)]
    t1 = [sb([128, 512]) for _ in range(2)]
    gb = sb([128, 512])
    wgs = [sb([128, 16, 128], BF16) for _ in range(2)]
    wus = [sb([128, 16, 128], BF16) for _ in range(2)]
    wds = sb([128, 8, D], BF16)
    ld("sp", bcB[:], bc_ap(lng[1, :], D), bcB, [lng])
    ld("sp", bcC[:], bc_ap(lnb[1, :], D), bcC, [lnb])
    blocks = [(b0, 512, 0) for b0 in range(0, T_l, 512)]
    if T_c:
        blocks.append((T_l, T_c, 1))
    pc = 0
    for (b0, nb, s) in blocks:
        ld("pool", tb[:, :, :nb], tokT_d.t[:, b0:b0 + nb].rearrange("(kc p) t -> p kc t", p=128), tb, [tokT_d])
        for ex in range(NEXP):
            ld("pool", wds[:], wd.t[ex].rearrange("(hc p) d -> p hc d", p=128), wds, [wd])
            P.op("pe", lambda e, ex=ex: e.matmul(PS[2][:, :nb], sel[:, ex * 128:(ex + 1) * 128], GT[:, b0:b0 + nb],
                                                 start=True, stop=True), r=[sel, GT], w=[PS[2]])
            P.op("act", lambda e: e.activation(out=gb[:, :nb], in_=PS[2][:, :nb], func=AF.Copy), r=[PS[2]], w=[gb])
            for hc in range(8):
                a, b = wgs[pc % 2], wus[pc % 2]
                sgi, t1i = sg[pc % 2], t1[pc % 2]
                pg, pu = PS[(pc % 2) * 2], PS[(pc % 2) * 2 + 1]
                pc += 1
                ld("pool", a[:], wg.t[ex][:, hc * 128:(hc + 1) * 128].rearrange("(kc p) h -> p kc h", p=128), a, [wg])
                ld("pool", b[:], wu.t[ex][:, hc * 128:(hc + 1) * 128].rearrange("(kc p) h -> p kc h", p=128), b, [wu])
                for kc in range(16):
                    P.op("pe", lambda e, kc=kc, a=a, pg=pg: e.matmul(pg[:, :nb], a[:, kc, :], tb[:, kc, :nb], start=(kc == 0),
                                                                   stop=(kc == 15)), r=[a, tb], w=[pg])
                for kc in range(16):
                    P.op("pe", lambda e, kc=kc, b=b, pu=pu: e.matmul(pu[:, :nb], b[:, kc, :], tb[:, kc, :nb], start=(kc == 0),
                                                                   stop=(kc == 15)), r=[b, tb], w=[pu])
                P.op("act", lambda e, pg=pg, sgi=sgi: e.activation(out=sgi[:, :nb], in_=pg[:, :nb], func=AF.Silu), r=[pg], w=[sgi])
                P.op("dve", lambda e, pu=pu, t1i=t1i: e.tensor_tensor(out=t1i[:, :nb], in0=pu[:, :nb], in1=gb[:, :nb], op=ALU.mult),
                     r=[pu, gb], w=[t1i])
                P.op("dve", lambda e, hc=hc, sgi=sgi, t1i=t1i: e.tensor_tensor(out=hT[:, hc, :nb], in0=sgi[:, :nb], in1=t1i[:, :nb],
                                                                              op=ALU.mult), r=[sgi, t1i], w=[hT])
            for dc in range(16):
                pd = PS[4 + dc % 4]
                for hc in range(8):
                    P.op("pe", lambda e, dc=dc, hc=hc, pd=pd: e.matmul(pd[:, :nb], wds[:, hc, dc * 128:(dc + 1) * 128], hT[:, hc, :nb],
                                                                     start=(hc == 0), stop=(hc == 7)), r=[wds, hT], w=[pd])
                if ex == 0:
                    P.op("act", lambda e, dc=dc, pd=pd: e.activation(out=acc[:, dc, :nb], in_=pd[:, :nb], func=AF.Copy), r=[pd], w=[acc])
                else:
                    P.op("dve", lambda e, dc=dc, pd=pd: e.tensor_tensor(out=acc[:, dc, :nb], in0=acc[:, dc, :nb], in1=pd[:, :nb],
                                                                       op=ALU.add), r=[pd, acc], w=[acc])
        for dc in range(16):
            P.op("dve", lambda e, dc=dc: e.tensor_scalar(out=acc[:, dc, :nb], in0=acc[:, dc, :nb], scalar1=mcol[s][:, 80 + dc:81 + dc],
                                                        scalar2=None, op0=ALU.mult), r=[acc, mcol[s]], w=[acc])
        for j0 in range(0, nb, 128):
            n = min(128, nb - j0)
            t0 = b0 + j0
            for dc in range(16):
                P.op("pe", lambda e, dc=dc: e.transpose(PS[dc // 4][:n, (dc % 4) * 128:(dc % 4 + 1) * 128],
                                                        acc[:, dc, j0:j0 + n], ident[:, :]), r=[acc, ident], w=[PS[dc // 4]])
            ld("sp", xt[:n, :], x1_d[t0:t0 + n, :], xt, [x1_d])
            for j in range(4):
                P.op("dve", lambda e, j=j: e.scalar_tensor_tensor(out=xt[:n, j * 512:(j + 1) * 512], in0=xt[:n, j * 512:(j + 1) * 512],
                                                                 scalar=ALPHA, in1=PS[j][:n, :], op0=ALU.mult, op1=ALU.add),
                     r=[xt, PS[j]], w=[xt])
            layer_norm_tile(P, n, xt, tmp, bcB, bcC, st)
            ld("sp", xout[t0:t0 + n, :], xt[:n, :], xout, [xt])
    P.finish([xout])
    return P


def build_mod():
    P = Prog()
    IN = "ExternalInput"
    NCOL = 1536
    cT = P.dram("cT", [128, 16 * 3], F32, IN)
    w = P.dram("w", [2, D, NCOL], F32, IN)
    b = P.dram("b", [2, NCOL], F32, IN)
    out = P.dram("out", [2, 3, NCOL], F32, "ExternalOutput")
    ct = P.sb([128, 16, 3]); sg = P.sb([128, 16, 3])
    ring = [P.sb([128, NCOL]) for _ in range(3)]
    bb = P.sb([3, NCOL]); res = P.sb([3, NCOL])
    PS = [P.ps() for _ in range(3)]
    P.dma("sp", ct[:].rearrange("p k r -> p (k r)"), cT[:, :], r=[cT], w=[ct])
    P.op("act", lambda e: e.activation(out=sg[:], in_=ct[:], func=AF.Sigmoid), r=[ct], w=[sg])
    P.op("dve", lambda e: e.tensor_tensor(out=sg[:], in0=sg[:], in1=ct[:], op=ALU.mult), r=[sg, ct], w=[sg])
    i = 0
    for l in range(2):
        P.dma("sp", bb[:], bass.AP(b.t.tensor, b[l, :].offset, [[0, 3], [1, NCOL]]), r=[b], w=[bb])
        for kc in range(16):
            ws = ring[i % 3]; i += 1
            P.dma("sp", ws[:], w[l, kc * 128:(kc + 1) * 128, :], r=[w], w=[ws])
            for j in range(3):
                P.op("pe", lambda e, j=j, kc=kc, ws=ws: e.matmul(PS[j][0:3, :], sg[:, kc, :], ws[:, j * 512:(j + 1) * 512],
                                                                start=(kc == 0), stop=(kc == 15)), r=[sg, ws], w=[PS[j]])
        for j in range(3):
            P.op("dve", lambda e, j=j: e.tensor_tensor(out=res[:, j * 512:(j + 1) * 512], in0=PS[j][0:3, :],
                                                      in1=bb[:, j * 512:(j + 1) * 512], op=ALU.add), r=[PS[j], bb], w=[res])
        P.dma("sp", out[l, :, :], res[:], r=[res], w=[out])
    P.finish([out])
    return P


def build_ssd(LAT=16384, CTX=256, NU=2):
    P = Prog()
    IN = "ExternalInput"
    TB = CTX + LAT
    NCH = 6
    xT = P.dram("xT", [D, TB], F32, IN)
    modcol = P.dram("modcol", [2, 128, 32], F32, IN)
    wch = P.dram("wch", [NU, D, 768], F32, IN)
    wtm = P.dram("wtm", [NU, D, 528], F32, IN)
    cw = P.dram("cw", [NU, 128, NCH * 10], F32, IN)
    hp = P.dram("hp", [NU, 48], F32, IN)
    ng = P.dram("ng", [NU, 512], F32, IN)
    consts = P.dram("consts", [6, 128, 128], F32, IN)
    ymix = P.dram("ymix", [NU, LAT, 512], F32, "ExternalOutput")
    xbc_d = P.dram("xbc_d", [768, TB], F32)
    xc_d = P.dram("xc_d", [768, TB], F32)
    z_d = P.dram("z_d", [TB, 528], F32)
    yf_d = P.dram("yf_d", [LAT, 512], F32)
    sb = P.sb
    C = [sb([128, 128]) for _ in range(6)]
    for i in range(6):
        P.dma("sp", C[i][:], consts[i, :, :], r=[consts], w=[C[i]])
    ident, ones, TriF, TriB, SF, SB_ = C
    mcol = [sb([128, 32]) for _ in range(2)]
    scp1 = [sb([128, 16]) for _ in range(2)]
    for s in range(2):
        P.dma("sp", mcol[s][:], modcol[s, :, :], r=[modcol], w=[mcol[s]])
        P.op("dve", lambda e, s=s: e.tensor_scalar(out=scp1[s][:], in0=mcol[s][:, 16:32], scalar1=1.0, scalar2=None,
                                                    op0=ALU.add), r=[mcol[s]], w=[scp1[s]])
    PS = [P.ps() for _ in range(8)]
    wchs = sb([128, 16, 768], BF16)
    wtms = sb([128, 16, 528], BF16)
    xin32 = sb([128, 16, 512])
    hT = sb([128, 16, 512], BF16)
    stage = [sb([128, 528]) for _ in range(2)]
    cws = sb([128, NCH * 10])
    hpb = sb([128, 48]); aneg = sb([128, 16]); dsum = sb([128, 8]); ngb = sb([128, 512])
    R = min(32, LAT // 64)
    cin = sb([128, (R + 2) * 64]); cout = sb([128, max(R * 64, CTX)])

    for u in range(NU):
        P.dma("pool", wchs[:], wch.t[u].rearrange("(kc p) c -> p kc c", p=128), r=[wch], w=[wchs])
        P.dma("pool", wtms[:], wtm.t[u].rearrange("(kc p) c -> p kc c", p=128), r=[wtm], w=[wtms])
        P.dma("sp", cws[:], cw[u, :, :], r=[cw], w=[cws])
        P.dma("sp", hpb[:], bass.AP(hp.t.tensor, hp[u, :].offset, [[0, 128], [1, 48]]), r=[hp], w=[hpb])
        P.dma("sp", ngb[:], bass.AP(ng.t.tensor, ng[u, :].offset, [[0, 128], [1, 512]]), r=[ng], w=[ngb])
        P.op("act", lambda e: e.activation(out=aneg[:], in_=hpb[:, 16:32], func=AF.Exp), r=[hpb], w=[aneg])
        P.op("dve", lambda e: e.tensor_scalar(out=aneg[:], in0=aneg[:], scalar1=-1.0, scalar2=None, op0=ALU.mult), r=[aneg], w=[aneg])
        P.op("dve", lambda e: e.tensor_tensor(out=dsum[:], in0=hpb[:, 32:40], in1=hpb[:, 40:48], op=ALU.add), r=[hpb], w=[dsum])
        blocks = [(0, CTX, 1)] + [(CTX + i * 512, 512, 0) for i in range(LAT // 512)]
        si = 0
        for (t0, nb, s) in blocks:
            P.dma("sp", xin32[:, :, :nb], xT.t[:, t0:t0 + nb].rearrange("(kc p) t -> p kc t", p=128), r=[xT], w=[xin32])
            for kc in range(16):
                P.op("act", lambda e, kc=kc: e.activation(out=hT[:, kc, :nb], in_=xin32[:, kc, :nb], func=AF.Identity,
                                                          bias=mcol[s][:, kc:kc + 1], scale=scp1[s][:, kc:kc + 1]),
                     r=[xin32, mcol[s], scp1[s]], w=[hT])
            for m in range(NCH):
                pp = PS[m % 4]
                for kc in range(16):
                    P.op("pe", lambda e, m=m, kc=kc, pp=pp: e.matmul(pp[:, :nb], wchs[:, kc, m * 128:(m + 1) * 128], hT[:, kc, :nb],
                                                                   start=(kc == 0), stop=(kc == 15)), r=[wchs, hT], w=[pp])
                sg = stage[si % 2]; si += 1
                P.op("act", lambda e, pp=pp, sg=sg: e.activation(out=sg[:, :nb], in_=pp[:, :nb], func=AF.Copy), r=[pp], w=[sg])
                P.dma("sp", xbc_d[m * 128:(m + 1) * 128, t0:t0 + nb], sg[:, :nb], r=[sg], w=[xbc_d])
            for j0 in range(0, nb, 128):
                pz, pd = PS[4 + (j0 // 128) % 2 * 2], PS[5 + (j0 // 128) % 2 * 2]
                for kc in range(16):
                    P.op("pe", lambda e, kc=kc, pz=pz: e.matmul(pz[:, :], hT[:, kc, j0:j0 + 128], wtms[:, kc, 0:512],
                                                              start=(kc == 0), stop=(kc == 15)), r=[wtms, hT], w=[pz])
                for kc in range(16):
                    P.op("pe", lambda e, kc=kc, pd=pd: e.matmul(pd[:, 0:16], hT[:, kc, j0:j0 + 128], wtms[:, kc, 512:528],
                                                              start=(kc == 0), stop=(kc == 15)), r=[wtms, hT], w=[pd])
                sg = stage[si % 2]; si += 1
                P.op("act", lambda e, pz=pz, sg=sg: e.activation(out=sg[:, 0:512], in_=pz[:, :], func=AF.Copy), r=[pz], w=[sg])
                P.op("dve", lambda e, pd=pd, sg=sg: e.tensor_copy(out=sg[:, 512:528], in_=pd[:, 0:16]), r=[pd], w=[sg])
                P.dma("sp", z_d[t0 + j0:t0 + j0 + 128, :], sg[:, :], r=[sg], w=[z_d])
        for m in range(NCH):
            wv = lambda k: cws[:, m * 10 + k:m * 10 + k + 1]
            P.dma("sp", cin[:, 0:CTX], xbc_d[m * 128:(m + 1) * 128, 0:CTX], r=[xbc_d], w=[cin])
            P.op("dve", lambda e: e.tensor_scalar(out=cout[:, 0:CTX], in0=cin[:, 0:CTX], scalar1=wv(4), scalar2=wv(9),
                                                  op0=ALU.mult, op1=ALU.add), r=[cin, cws], w=[cout])
            P.op("dve", lambda e: e.scalar_tensor_tensor(out=cout[:, 1:CTX], in0=cin[:, 0:CTX - 1], scalar=wv(3), in1=cout[:, 1:CTX],
                                                         op0=ALU.mult, op1=ALU.add), r=[cin, cws, cout], w=[cout])
            P.op("dve", lambda e: e.scalar_tensor_tensor(out=cout[:, 0:CTX - 1], in0=cin[:, 1:CTX], scalar=wv(5), in1=cout[:, 0:CTX - 1],
                                                         op0=ALU.mult, op1=ALU.add), r=[cin, cws, cout], w=[cout])
            P.op("act", lambda e: e.activation(out=cout[:, 0:CTX], in_=cout[:, 0:CTX], func=AF.Silu), r=[cout], w=[cout])
            P.dma("sp", xc_d[m * 128:(m + 1) * 128, 0:CTX], cout[:, 0:CTX], r=[cout], w=[xc_d])
            NR = LAT // 64
            for r0 in range(0, NR, R):
                lo, hi = r0 - 1, r0 + R + 1
                if lo < 0:
                    P.op("dve", lambda e: e.memset(cin[:, 0:64], 0.0), w=[cin])
                if hi > NR:
                    P.op("dve", lambda e: e.memset(cin[:, (R + 1) * 64:(R + 2) * 64], 0.0), w=[cin])
                a, b_ = max(lo, 0), min(hi, NR)
                P.dma("sp", cin[:, (a - lo) * 64:(b_ - lo) * 64], xbc_d[m * 128:(m + 1) * 128, CTX + a * 64:CTX + b_ * 64],
                      r=[xbc_d], w=[cin])
                ci3 = cin[:, :].rearrange("p (r c) -> p r c", c=64)
                co3 = cout[:, :].rearrange("p (r c) -> p r c", c=64)
                P.op("dve", lambda e: e.tensor_scalar(out=co3[:, :, :], in0=ci3[:, 1:R + 1, :], scalar1=wv(4), scalar2=wv(9),
                                                      op0=ALU.mult, op1=ALU.add), r=[cin, cws], w=[cout])
                for i in range(3):
                    for j in range(3):
                        if i == 1 and j == 1:
                            continue
                        if j == 0:
                            o_, i_ = co3[:, :, 1:64], ci3[:, i:i + R, 0:63]
                        elif j == 1:
                            o_, i_ = co3[:, :, :], ci3[:, i:i + R, :]
                        else:
                            o_, i_ = co3[:, :, 0:63], ci3[:, i:i + R, 1:64]
                        P.op("dve", lambda e, o_=o_, i_=i_, k=i * 3 + j: e.scalar_tensor_tensor(out=o_, in0=i_, scalar=wv(k), in1=o_,
                                                                                           op0=ALU.mult, op1=ALU.add),
                             r=[cin, cws, cout], w=[cout])
                P.op("act", lambda e: e.activation(out=cout[:, :], in_=cout[:, :], func=AF.Silu), r=[cout], w=[cout])
                P.dma("sp", xc_d[m * 128:(m + 1) * 128, CTX + r0 * 64:CTX + (r0 + R) * 64], cout[:, :], r=[cout], w=[xc_d])
        ssd_scan(P, u, PS, (ident, ones, TriF, TriB, SF, SB_), xc_d, z_d, yf_d, ymix, hpb, aneg, dsum, ngb, LAT, CTX)
    P.finish([ymix])
    return P


def ssd_scan(P, u, PS, consts, xc_d, z_d, yf_d, ymix, hpb, aneg, dsum, ngb, LAT, CTX):
    ident, ones, TriF, TriB, SF, SB_ = consts
    sb = P.sb
    if not hasattr(P, "_ssd_bufs"):
        B = {}
        B["CT"] = sb([128, 128]); B["BT"] = sb([128, 128]); B["CTb"] = sb([128, 128], BF16); B["BTb"] = sb([128, 128], BF16)
        B["xcm"] = sb([128, 4, 128]); B["xtm"] = sb([128, 512]); B["Btm"] = sb([128, 128], BF16)
        B["zt"] = sb([128, 528]); B["dt"] = sb([128, 8]); B["a"] = sb([128, 8]); B["e8"] = sb([128, 8])
        B["acs"] = sb([128, 8]); B["eacs"] = sb([128, 8]); B["tail"] = sb([128, 8]); B["etot"] = sb([128, 8])
        B["xdt"] = sb([128, 512], BF16); B["xdtt"] = sb([128, 512], BF16)
        B["cbm"] = sb([128, 128]); B["lh"] = [sb([128, 128]) for _ in range(2)]; B["L"] = [sb([128, 128]) for _ in range(2)]
        B["M"] = [sb([128, 128], BF16) for _ in range(2)]
        B["S"] = sb([128, 512]); B["Sb"] = sb([128, 512], BF16); B["tS"] = sb([128, 512])
        B["y"] = sb([128, 512]); B["yf"] = sb([128, 512]); B["sz"] = sb([128, 512]); B["st"] = sb([128, 4])
        P._ssd_bufs = B
    B = P._ssd_bufs
    CT, BT, CTb, BTb, xcm, xtm, Btm, zt = (B[k] for k in ["CT", "BT", "CTb", "BTb", "xcm", "xtm", "Btm", "zt"])
    dt, a, e8, acs, eacs, tail, etot = (B[k] for k in ["dt", "a", "e8", "acs", "eacs", "tail", "etot"])
    xdt, xdtt, cbm, S, Sb, tS, y, yf, sz, st = (B[k] for k in ["xdt", "xdtt", "cbm", "S", "Sb", "tS", "y", "yf", "sz", "st"])
    bc8 = lambda t: t[:, 0:8].unsqueeze(2).to_broadcast([128, 8, 64])
    v3 = lambda t: t[:, :].rearrange("p (h q) -> p h q", h=8)
    nctx, nlat = CTX // 128, LAT // 128
    for d in range(2):
        Tri, SM = (TriF, SF) if d == 0 else (TriB, SB_)
        P.op("dve", lambda e: e.memset(S[:], 0.0), w=[S])
        P.op("dve", lambda e: e.memset(Sb[:], 0.0), w=[Sb])
        order = list(range(nctx)) + [nctx + i for i in range(nlat)]
        if d == 1:
            order = list(range(nctx))[::-1] + [nctx + i for i in range(nlat)][::-1]
        for ci, c in enumerate(order):
            p0 = c * 128
            lat = c >= nctx
            l0 = p0 - CTX
            last_state = (ci == len(order) - 1)
            P.dma("sp", CT[:], xc_d[640:768, p0:p0 + 128], r=[xc_d], w=[CT])
            P.dma("sp", BT[:], xc_d[512:640, p0:p0 + 128], r=[xc_d], w=[BT])
            P.dma("sp", xcm[:], xc_d.t[0:512, p0:p0 + 128].rearrange("(m p) t -> p m t", p=128), r=[xc_d], w=[xcm])
            P.dma("sp", zt[:], z_d[p0:p0 + 128, :], r=[z_d], w=[zt])
            P.op("act", lambda e: e.activation(out=CTb[:], in_=CT[:], func=AF.Copy), r=[CT], w=[CTb])
            P.op("act", lambda e: e.activation(out=BTb[:], in_=BT[:], func=AF.Copy), r=[BT], w=[BTb])
            for m in range(4):
                P.op("pe", lambda e, m=m: e.transpose(PS[0][:, m * 128:(m + 1) * 128], xcm[:, m, :], ident[:, :]), r=[xcm, ident], w=[PS[0]])
            P.op("act", lambda e: e.activation(out=xtm[:], in_=PS[0][:, :], func=AF.Copy), r=[PS[0]], w=[xtm])
            P.op("pe", lambda e: e.transpose(PS[1][:, 0:128], BT[:, :], ident[:, :]), r=[BT, ident], w=[PS[1]])
            P.op("act", lambda e: e.activation(out=Btm[:], in_=PS[1][:, 0:128], func=AF.Copy), r=[PS[1]], w=[Btm])
            P.op("dve", lambda e: e.tensor_tensor(out=e8[:], in0=zt[:, 512 + d * 8:520 + d * 8], in1=hpb[:, d * 8:d * 8 + 8], op=ALU.add),
                 r=[zt, hpb], w=[e8])
            P.op("act", lambda e: e.activation(out=e8[:], in_=e8[:], func=AF.Exp), r=[e8], w=[e8])
            P.op("act", lambda e: e.activation(out=dt[:], in_=e8[:], func=AF.Ln, bias=1.0, scale=1.0), r=[e8], w=[dt])
            P.op("dve", lambda e: e.tensor_tensor(out=a[:], in0=dt[:], in1=aneg[:, d * 8:d * 8 + 8], op=ALU.mult), r=[dt, aneg], w=[a])
            P.op("pe", lambda e: e.matmul(PS[2][:, 0:8], Tri[:, :], a[:, :], start=True, stop=True), r=[Tri, a], w=[PS[2]])
            P.op("pe", lambda e: e.matmul(PS[2][:, 8:16], ones[:, :], a[:, :], start=True, stop=True), r=[ones, a], w=[PS[2]])
            P.op("act", lambda e: e.activation(out=acs[:], in_=PS[2][:, 0:8], func=AF.Copy), r=[PS[2]], w=[acs])
            P.op("act", lambda e: e.activation(out=eacs[:], in_=PS[2][:, 0:8], func=AF.Exp), r=[PS[2]], w=[eacs])
            P.op("act", lambda e: e.activation(out=etot[:], in_=PS[2][:, 8:16], func=AF.Exp), r=[PS[2]], w=[etot])
            P.op("dve", lambda e: e.tensor_tensor(out=tail[:], in0=PS[2][:, 8:16], in1=acs[:], op=ALU.subtract), r=[PS[2], acs], w=[tail])
            P.op("act", lambda e: e.activation(out=tail[:], in_=tail[:], func=AF.Exp), r=[tail], w=[tail])
            P.op("dve", lambda e: e.tensor_tensor(out=v3(xdt), in0=v3(xtm), in1=bc8(dt), op=ALU.mult), r=[xtm, dt], w=[xdt])
            P.op("dve", lambda e: e.tensor_tensor(out=v3(xdtt), in0=v3(xdt), in1=bc8(tail), op=ALU.mult), r=[xdt, tail], w=[xdtt])
            if lat:
                P.op("pe", lambda e: e.matmul(PS[3][:, 0:128], BTb[:, :], CTb[:, :], start=True, stop=True), r=[BTb, CTb], w=[PS[3]])
                P.op("dve", lambda e: e.tensor_tensor(out=cbm[:], in0=PS[3][:, 0:128], in1=Tri[:, :], op=ALU.mult), r=[PS[3], Tri], w=[cbm])
                for h in range(8):
                    lh, L, M = B["lh"][h % 2], B["L"][h % 2], B["M"][h % 2]
                    pdf = PS[4 + h % 2]
                    P.op("dve", lambda e, h=h, lh=lh: e.tensor_scalar(out=lh[:], in0=SM[:, :], scalar1=a[:, h:h + 1], scalar2=None,
                                                                    op0=ALU.mult), r=[SM, a], w=[lh])
                    P.op("pe", lambda e, lh=lh, pdf=pdf: e.matmul(pdf[:, 0:128], lh[:, :], Tri[:, :], start=True, stop=True), r=[lh, Tri], w=[pdf])
                    P.op("act", lambda e, L=L, pdf=pdf: e.activation(out=L[:], in_=pdf[:, 0:128], func=AF.Exp), r=[pdf], w=[L])
                    P.op("dve", lambda e, L=L, M=M: e.tensor_tensor(out=M[:], in0=L[:], in1=cbm[:], op=ALU.mult), r=[L, cbm], w=[M])
                    P.op("pe", lambda e, h=h, M=M: e.matmul(PS[6][:, h * 64:(h + 1) * 64], M[:, :], xdt[:, h * 64:(h + 1) * 64],
                                                          start=True, stop=True), r=[M, xdt], w=[PS[6]])
                P.op("pe", lambda e: e.matmul(PS[7][:, :], CTb[:, :], Sb[:, :], start=True, stop=True), r=[CTb, Sb], w=[PS[7]])
                P.op("dve", lambda e: e.tensor_tensor(out=v3(y), in0=PS[7][:, :].rearrange("p (h q) -> p h q", h=8), in1=bc8(eacs), op=ALU.mult),
                     r=[PS[7], eacs], w=[y])
                P.op("dve", lambda e: e.tensor_tensor(out=y[:], in0=y[:], in1=PS[6][:, :], op=ALU.add), r=[y, PS[6]], w=[y])
                if d == 0:
                    P.dma("sp", yf_d[l0:l0 + 128, :], y[:], r=[y], w=[yf_d])
                else:
                    P.dma("sp", yf[:], yf_d[l0:l0 + 128, :], r=[yf_d], w=[yf])
                    P.op("dve", lambda e: e.tensor_tensor(out=y[:], in0=y[:], in1=yf[:], op=ALU.add), r=[y, yf], w=[y])
                    P.op("dve", lambda e: e.tensor_tensor(out=v3(yf), in0=v3(xtm), in1=bc8(dsum), op=ALU.mult), r=[xtm, dsum], w=[yf])
                    P.op("dve", lambda e: e.tensor_tensor(out=y[:], in0=y[:], in1=yf[:], op=ALU.add), r=[y, yf], w=[y])
                    P.op("act", lambda e: e.activation(out=sz[:], in_=zt[:, 0:512], func=AF.Silu), r=[zt], w=[sz])
                    P.op("dve", lambda e: e.tensor_tensor(out=y[:], in0=y[:], in1=sz[:], op=ALU.mult), r=[y, sz], w=[y])
                    P.op("act", lambda e: e.activation(out=sz[:], in_=y[:], func=AF.Square), r=[y], w=[sz])
                    P.op("dve", lambda e: e.reduce_sum(out=st[:, 0:1], in_=sz[:], axis=AX.X), r=[sz], w=[st])
                    P.op("dve", lambda e: e.tensor_scalar(out=st[:, 0:1], in0=st[:, 0:1], scalar1=1.0 / 512, scalar2=EPS,
                                                          op0=ALU.mult, op1=ALU.add), r=[st], w=[st])
                    P.op("act", lambda e: e.activation(out=st[:, 0:1], in_=st[:, 0:1], func=AF.Sqrt), r=[st], w=[st])
                    P.op("dve", lambda e: e.reciprocal(out=st[:, 1:2], in_=st[:, 0:1]), r=[st], w=[st])
                    P.op("dve", lambda e: e.scalar_tensor_tensor(out=y[:], in0=y[:], scalar=st[:, 1:2], in1=ngb[:], op0=ALU.mult,
                                                                 op1=ALU.mult), r=[y, st, ngb], w=[y])
                    P.dma("sp", ymix[u, l0:l0 + 128, :], y[:], r=[y], w=[ymix])
            if not last_state:
                P.op("pe", lambda e: e.matmul(PS[3][:, :], Btm[:, :], xdtt[:, :], start=True, stop=True), r=[Btm, xdtt], w=[PS[3]])
                P.op("dve", lambda e: e.tensor_tensor(out=v3(tS), in0=v3(S), in1=bc8(etot), op=ALU.mult), r=[S, etot], w=[tS])
                P.op("dve", lambda e: e.tensor_tensor(out=S[:], in0=tS[:], in1=PS[3][:, :], op=ALU.add), r=[tS, PS[3]], w=[S])
                P.op("act", lambda e: e.activation(out=Sb[:], in_=S[:], func=AF.Copy), r=[S], w=[Sb])


def build_mix0(LAT=16384, CTX=256):
    P = Prog()
    IN = "ExternalInput"
    TB = CTX + LAT
    xT = P.dram("xT", [2, D, TB], F32, IN)
    modcol = P.dram("modcol", [3, 128, 32], F32, IN)
    whc = P.dram("whc", [D, 384], F32, IN)
    wgc = P.dram("wgc", [D, 288], F32, IN)
    wgt = P.dram("wgt", [D, 512], F32, IN)
    hcw = P.dram("hcw", [128, 12], F32, IN)
    hlb = P.dram("hlb", [128, 2], F32, IN)
    fw1 = P.dram("fw1", [33, 64], F32, IN)
    fcol = P.dram("fcol", [64, 4], F32, IN)
    fw2 = P.dram("fw2", [64, 64], F32, IN)
    fw3 = P.dram("fw3", [64, 512], F32, IN)
    zl = P.dram("zl", [33, LAT], F32, IN); zc = P.dram("zc", [33, CTX], F32, IN)
    El = P.dram("El", [128, LAT], F32, IN); Ec = P.dram("Ec", [128, CTX], F32, IN)
    gw2 = P.dram("gw2", [2, 16, 128], F32, IN)
    gbc = P.dram("gbc", [128, 2], F32, IN)
    gng = P.dram("gng", [256], F32, IN)
    consts = P.dram("consts", [4, 128, 512], F32, IN)
    hy_out = P.dram("hy_out", [2, 128, TB], F32, "ExternalOutput")
    gla_out = P.dram("gla_out", [TB, 256], F32, "ExternalOutput")
    hu_d = P.dram("hu_d", [2, 384, TB], F32)
    gq_d = P.dram("gq_d", [256, TB], F32)
    gg_d = P.dram("gg_d", [2, 16, TB], F32)
    gvr_d = P.dram("gvr_d", [TB, 512], F32)
    of_d = P.dram("of_d", [TB, 256], F32)
    hf_d = {CTX: P.dram("hf_c", [4, 128, CTX], F32), LAT: P.dram("hf_l", [4, 128, LAT], F32)}
    sb = P.sb
    ident = sb([128, 128]); mF = sb([64, 64]); mB = sb([64, 64]); rmask = sb([128, 512])
    P.dma("sp", ident[:], consts[0, :, 0:128], r=[consts], w=[ident])
    P.dma("sp", mF[:], consts[1, 0:64, 0:64], r=[consts], w=[mF])
    P.dma("sp", mB[:], consts[2, 0:64, 0:64], r=[consts], w=[mB])
    P.dma("sp", rmask[:], consts[3, :, :], r=[consts], w=[rmask])
    mcol = [sb([128, 32]) for _ in range(3)]
    scp1 = [sb([128, 16]) for _ in range(3)]
    for s in range(3):
        P.dma("sp", mcol[s][:], modcol[s, :, :], r=[modcol], w=[mcol[s]])
        P.op("dve", lambda e, s=s: e.tensor_scalar(out=scp1[s][:], in0=mcol[s][:, 16:32], scalar1=1.0, scalar2=None,
                                                    op0=ALU.add), r=[mcol[s]], w=[scp1[s]])
    PS = [P.ps() for _ in range(8)]
    blocks = [(0, CTX)] + [(CTX + i * 512, 512) for i in range(LAT // 512)]

    with ExitStack() as stk:
        whs = P.sbs(stk, [128, 16, 384], BF16); wgs = P.sbs(stk, [128, 16, 288], BF16); wts = P.sbs(stk, [128, 16, 512], BF16)
        xin32 = P.sbs(stk, [128, 16, 512]); hT = P.sbs(stk, [128, 16, 512], BF16)
        stage = [P.sbs(stk, [128, 512]) for _ in range(3)]
        P.dma("pool", whs[:], whc.t.rearrange("(kc p) c -> p kc c", p=128), r=[whc], w=[whs])
        P.dma("pool", wgs[:], wgc.t.rearrange("(kc p) c -> p kc c", p=128), r=[wgc], w=[wgs])
        P.dma("pool", wts[:], wgt.t.rearrange("(kc p) c -> p kc c", p=128), r=[wgt], w=[wts])
        si = 0
        for bi in range(2):
            for (t0, nb) in blocks:
                s = 2 if t0 < CTX else bi
                P.dma("sp", xin32[:, :, :nb], xT.t[bi, :, t0:t0 + nb].rearrange("(kc p) t -> p kc t", p=128), r=[xT], w=[xin32])
                for kc in range(16):
                    P.op("act", lambda e, kc=kc, s=s: e.activation(out=hT[:, kc, :nb], in_=xin32[:, kc, :nb], func=AF.Identity,
                                                                   bias=mcol[s][:, kc:kc + 1], scale=scp1[s][:, kc:kc + 1]),
                         r=[xin32, mcol[s], scp1[s]], w=[hT])
                jobs = [(whs, m * 128, 128, hu_d, (bi, slice(m * 128, (m + 1) * 128))) for m in range(3)]
                if bi == 0:
                    jobs += [(wgs, m * 128, 128, gq_d, (slice(m * 128, (m + 1) * 128),)) for m in range(2)]
                    jobs += [(wgs, 256 + dd * 16, 16, gg_d, (dd, slice(0, 16))) for dd in range(2)]
                for ji, (wsb, c0, mw, dst, idx) in enumerate(jobs):
                    pp = PS[ji % 4]
                    for kc in range(16):
                        P.op("pe", lambda e, kc=kc, pp=pp, wsb=wsb, c0=c0, mw=mw: e.matmul(pp[:mw, :nb], wsb[:, kc, c0:c0 + mw], hT[:, kc, :nb],
                                                                                        start=(kc == 0), stop=(kc == 15)), r=[wsb, hT], w=[pp])
                    sg = stage[si % 3]; si += 1
                    P.op("act", lambda e, pp=pp, sg=sg, mw=mw: e.activation(out=sg[:mw, :nb], in_=pp[:mw, :nb], func=AF.Copy), r=[pp], w=[sg])
                    P.dma("sp", dst.t[idx + (slice(t0, t0 + nb),)], sg[:mw, :nb], r=[sg], w=[dst])
                if bi == 0:
                    for j0 in range(0, nb, 128):
                        pz = PS[4 + (j0 // 128) % 4]
                        for kc in range(16):
                            P.op("pe", lambda e, kc=kc, pz=pz, j0=j0: e.matmul(pz[:, :], hT[:, kc, j0:j0 + 128], wts[:, kc, :],
                                                                             start=(kc == 0), stop=(kc == 15)), r=[wts, hT], w=[pz])
                        sg = stage[si % 3]; si += 1
                        P.op("act", lambda e, pz=pz, sg=sg: e.activation(out=sg[:, :], in_=pz[:, :], func=AF.Copy), r=[pz], w=[sg])
                        P.dma("sp", gvr_d[t0 + j0:t0 + j0 + 128, :], sg[:, :], r=[sg], w=[gvr_d])
        P.barrier()

    with ExitStack() as stk:
        w1s = P.sbs(stk, [33, 64]); w2s = P.sbs(stk, [64, 64]); w3s = P.sbs(stk, [64, 512]); fc = P.sbs(stk, [64, 4])
        cws = P.sbs(stk, [128, 12]); lbs = P.sbs(stk, [128, 2])
        for dst, src in [(w1s, fw1), (w2s, fw2), (w3s, fw3), (fc, fcol), (cws, hcw), (lbs, hlb)]:
            P.dma("sp", dst[:], src[:, :], r=[src], w=[dst])
        zb = P.sbs(stk, [33, 512]); h1 = P.sbs(stk, [64, 512]); h2 = P.sbs(stk, [64, 512]); eb = P.sbs(stk, [128, 512])
        hst = [P.sbs(stk, [128, 512]) for _ in range(2)]
        rr = P.sbs(stk, [64, 512])
        si = 0
        for (L, zsrc, Esrc) in [(CTX, zc, Ec), (LAT, zl, El)]:
            for p0 in range(0, L, 512):
                nb = min(512, L - p0)
                P.dma("sp", zb[:, :nb], zsrc[:, p0:p0 + nb], r=[zsrc], w=[zb])
                P.dma("sp", eb[:, :nb], Esrc[:, p0:p0 + nb], r=[Esrc], w=[eb])
                for (wsb, kdim, src, dst, bcol, fcolm) in [(w1s, 33, zb, h1, 0, 1), (w2s, 64, h1, h2, 2, 3)]:
                    P.op("pe", lambda e, wsb=wsb, kdim=kdim, src=src: e.matmul(PS[0][:64, :nb], wsb[:kdim, :], src[:kdim, :nb], start=True, stop=True),
                         r=[wsb, src], w=[PS[0]])
                    P.op("dve", lambda e, dst=dst, bcol=bcol, fcolm=fcolm: e.tensor_scalar(out=dst[:, :nb], in0=PS[0][:64, :nb], scalar1=fc[:, bcol:bcol + 1],
                                                                                         scalar2=fc[:, fcolm:fcolm + 1], op0=ALU.add, op1=ALU.mult),
                         r=[PS[0], fc], w=[dst])
                    P.op("dve", lambda e, dst=dst: e.tensor_scalar(out=rr[:, :nb], in0=dst[:, :nb], scalar1=1.0 / (2 * math.pi), scalar2=12582912.0,
                                                                   op0=ALU.mult, op1=ALU.add), r=[dst], w=[rr])
                    P.op("dve", lambda e, dst=dst: e.tensor_scalar(out=rr[:, :nb], in0=rr[:, :nb], scalar1=12582912.0, scalar2=-2 * math.pi,
                                                                   op0=ALU.subtract, op1=ALU.mult), r=[rr], w=[rr])
                    P.op("dve", lambda e, dst=dst: e.tensor_tensor(out=dst[:, :nb], in0=dst[:, :nb], in1=rr[:, :nb], op=ALU.add), r=[dst, rr], w=[dst])
                    P.op("act", lambda e, dst=dst: e.activation(out=dst[:, :nb], in_=dst[:, :nb], func=AF.Sin), r=[dst], w=[dst])
                for od in range(4):
                    P.op("pe", lambda e, od=od: e.matmul(PS[1 + od % 2][:, :nb], w3s[:, od * 128:(od + 1) * 128], h2[:, :nb], start=True, stop=True),
                         r=[w3s, h2], w=[PS[1 + od % 2]])
                    sg = hst[si % 2]; si += 1
                    P.op("dve", lambda e, od=od, sg=sg: e.tensor_tensor(out=sg[:, :nb], in0=PS[1 + od % 2][:, :nb], in1=eb[:, :nb], op=ALU.mult),
                         r=[PS[1 + od % 2], eb], w=[sg])
                    P.dma("sp", hf_d[L][od, :, p0:p0 + nb], sg[:, :nb], r=[sg], w=[hf_d[L]])
        LB = min(2048, LAT)
        u = P.sbs(stk, [128, LAT]); acc = P.sbs(stk, [128, LAT])
        raw = P.sbs(stk, [128, LB + 2]); xg = P.sbs(stk, [128, LB])
        hring = [P.sbs(stk, [128, LB]) for _ in range(2)]

        def short_conv(bi, ti, s0, L, dstbuf, mul_into=None):
            wv = lambda k: cws[:, ti * 4 + k:ti * 4 + k + 1]
            for b0 in range(0, L, LB):
                n = min(LB, L - b0)
                lo, hi = b0 - 1, b0 + n + 1
                if lo < 0:
                    P.op("dve", lambda e: e.memset(raw[:, 0:1], 0.0), w=[raw])
                if hi > L:
                    P.op("dve", lambda e: e.memset(raw[:, n + 1:n + 2], 0.0), w=[raw])
                a, b_ = max(lo, 0), min(hi, L)
                P.dma("sp", raw[:, a - lo:b_ - lo], hu_d[bi, ti * 128:(ti + 1) * 128, s0 + a:s0 + b_], r=[hu_d], w=[raw])
                tgt = dstbuf[:, b0:b0 + n] if mul_into is None else xg[:, :n]
                tb_ = dstbuf if mul_into is None else xg
                P.op("dve", lambda e, tgt=tgt: e.tensor_scalar(out=tgt, in0=raw[:, 1:n + 1], scalar1=wv(1), scalar2=wv(3), op0=ALU.mult, op1=ALU.add),
                     r=[raw, cws], w=[tb_])
                P.op("dve", lambda e, tgt=tgt: e.scalar_tensor_tensor(out=tgt, in0=raw[:, 0:n], scalar=wv(0), in1=tgt, op0=ALU.mult, op1=ALU.add),
                     r=[raw, cws, tb_], w=[tb_])
                P.op("dve", lambda e, tgt=tgt: e.scalar_tensor_tensor(out=tgt, in0=raw[:, 2:n + 2], scalar=wv(2), in1=tgt, op0=ALU.mult, op1=ALU.add),
                     r=[raw, cws, tb_], w=[tb_])
                if mul_into is not None:
                    P.op("dve", lambda e: e.tensor_tensor(out=mul_into[:, b0:b0 + n], in0=mul_into[:, b0:b0 + n], in1=xg[:, :n], op=ALU.mult),
                         r=[mul_into, xg], w=[mul_into])

        hi_ = 0
        for bi in range(2):
            for (s0, L) in [(0, CTX), (CTX, LAT)]:
                cur, nxt = u, acc
                short_conv(bi, 0, s0, L, cur)
                for o in range(2):
                    P.op("dve", lambda e, cur=cur, nxt=nxt, o=o: e.tensor_scalar(out=nxt[:, 0:L], in0=cur[:, 0:L], scalar1=lbs[:, o:o + 1], scalar2=None,
                                                                                op0=ALU.mult), r=[cur, lbs], w=[nxt])
                    for dr in range(2):
                        for l0 in range(0, L, LB):
                            n = min(LB, L - l0)
                            hb = hring[hi_ % 2]; hi_ += 1
                            P.dma("sp", hb[:, :n], hf_d[L][o * 2 + dr, :, l0:l0 + n], r=[hf_d[L]], w=[hb])
                            for m in range(l0, l0 + n):
                                if dr == 1 and m == 0:
                                    continue
                                if dr == 0:
                                    o_, i_ = nxt[:, m:L], cur[:, 0:L - m]
                                else:
                                    o_, i_ = nxt[:, 0:L - m], cur[:, m:L]
                                P.op("dve", lambda e, o_=o_, i_=i_, hb=hb, mm=m - l0: e.scalar_tensor_tensor(out=o_, in0=i_, scalar=hb[:, mm:mm + 1], in1=o_,
                                                                                                        op0=ALU.mult, op1=ALU.add),
                                     r=[cur, hb, nxt], w=[nxt])
                    short_conv(bi, o + 1, s0, L, None, mul_into=nxt)
                    cur, nxt = nxt, cur
                P.dma("sp", hy_out[bi, :, s0:s0 + L], cur[:, 0:L], r=[cur], w=[hy_out])
        P.barrier()

    w2s = [sb([16, 128]) for _ in range(2)]
    gb = sb([128, 2]); ngb = sb([64, 256])
    for dd in range(2):
        P.dma("sp", w2s[dd][:], gw2[dd, :, :], r=[gw2], w=[w2s[dd]])
    P.dma("sp", gb[:], gbc[:, :], r=[gbc], w=[gb])
    P.op("dve", lambda e: e.tensor_scalar(out=gb[:], in0=gb[:], scalar1=-1.0, scalar2=None, op0=ALU.mult), r=[gb], w=[gb])
    P.dma("sp", ngb[:], bass.AP(gng.t.tensor, gng.t.offset, [[0, 64], [1, 256]]), r=[gng], w=[ngb])
    g1 = sb([16, 512]); qk = sb([128, 2, 512]); g = sb([128, 512]); bb = sb([128, 512]); t5 = sb([128, 512])
    ebb = sb([128, 512]); qt = sb([128, 512], BF16); kt = sb([128, 512], BF16); ktl = sb([128, 512]); ebl = sb([128, 8])
    vr = sb([64, 512]); vb = sb([64, 256], BF16); attb = sb([64, 64], BF16); ktT = sb([64, 128], BF16)
    S = sb([128, 256]); Sb = sb([128, 256], BF16); o_ = sb([64, 256]); of = sb([64, 256]); sq = sb([64, 256]); st = sb([64, 2])
    QS = 128 ** -0.5
    for d in range(2):
        msk = mF if d == 0 else mB
        P.op("dve", lambda e: e.memset(S[:], 0.0), w=[S])
        P.op("dve", lambda e: e.memset(Sb[:], 0.0), w=[Sb])
        blks = [blocks[0]] + blocks[1:] if d == 0 else [blocks[0]] + blocks[1:][::-1]
        for (t0, nb) in blks:
            nch = nb // 64
            P.dma("sp", g1[:, :nb], gg_d[d, :, t0:t0 + nb], r=[gg_d], w=[g1])
            P.dma("sp", qk[:, :, :nb], gq_d.t[:, t0:t0 + nb].rearrange("(m p) t -> p m t", p=128), r=[gq_d], w=[qk])
            P.op("pe", lambda e: e.matmul(PS[0][:, :nb], w2s[d][:, :], g1[:, :nb], start=True, stop=True), r=[w2s[d], g1], w=[PS[0]])
            P.op("act", lambda e: e.activation(out=g[:, :nb], in_=PS[0][:, :nb], func=AF.Exp, bias=gb[:, d:d + 1], scale=-1.0), r=[PS[0], gb], w=[g])
            P.op("act", lambda e: e.activation(out=g[:, :nb], in_=g[:, :nb], func=AF.Ln, bias=1.0, scale=1.0), r=[g], w=[g])
            P.op("dve", lambda e: e.tensor_scalar(out=g[:, :nb], in0=g[:, :nb], scalar1=-1.0 / 16, scalar2=None, op0=ALU.mult), r=[g], w=[g])
            P.op("dve", lambda e: e.tensor_tensor_scan(out=bb[:, :nb], data0=rmask[:, :nb], data1=g[:, :nb], initial=0.0, op0=ALU.mult, op1=ALU.add),
                 r=[rmask, g], w=[bb])
            b3 = lambda t: t[:, :nb].rearrange("p (c q) -> p c q", q=64)
            if d == 1:
                P.op("dve", lambda e: e.tensor_tensor(out=t5[:, :nb], in0=g[:, :nb], in1=bb[:, :nb], op=ALU.subtract), r=[g, bb], w=[t5])
                P.op("dve", lambda e: e.tensor_tensor(out=b3(bb), in0=b3(t5), in1=b3(bb)[:, :, 63:64].to_broadcast([128, nch, 64]), op=ALU.add),
                     r=[t5, bb], w=[bb])
            lastcol = 63 if d == 0 else 0
            P.op("act", lambda e: e.activation(out=ebl[:, :nch], in_=b3(bb)[:, :, lastcol], func=AF.Exp), r=[bb], w=[ebl])
            P.op("dve", lambda e: e.tensor_tensor(out=b3(t5), in0=b3(bb)[:, :, lastcol:lastcol + 1].to_broadcast([128, nch, 64]), in1=b3(bb), op=ALU.subtract),
                 r=[bb], w=[t5])
            P.op("act", lambda e: e.activation(out=t5[:, :nb], in_=t5[:, :nb], func=AF.Exp), r=[t5], w=[t5])
            P.op("dve", lambda e: e.tensor_tensor(out=ktl[:, :nb], in0=qk[:, 1, :nb], in1=t5[:, :nb], op=ALU.mult), r=[qk, t5], w=[ktl])
            P.op("act", lambda e: e.activation(out=ebb[:, :nb], in_=bb[:, :nb], func=AF.Exp), r=[bb], w=[ebb])
            P.op("dve", lambda e: e.scalar_tensor_tensor(out=qt[:, :nb], in0=qk[:, 0, :nb], scalar=QS, in1=ebb[:, :nb], op0=ALU.mult, op1=ALU.mult),
                 r=[qk, ebb], w=[qt])
            P.op("act", lambda e: e.activation(out=ebb[:, :nb], in_=bb[:, :nb], func=AF.Exp, scale=-1.0), r=[bb], w=[ebb])
            P.op("dve", lambda e: e.tensor_tensor(out=kt[:, :nb], in0=qk[:, 1, :nb], in1=ebb[:, :nb], op=ALU.mult), r=[qk, ebb], w=[kt])
            chs = list(range(nch)) if d == 0 else list(range(nch))[::-1]
            for c in chs:
                c0 = c * 64
                p0 = t0 + c0
                P.dma("sp", vr[:], gvr_d[p0:p0 + 64, :], r=[gvr_d], w=[vr])
                P.op("act", lambda e: e.activation(out=vb[:], in_=vr[:, 0:256], func=AF.Copy), r=[vr], w=[vb])
                P.op("pe", lambda e: e.matmul(PS[1][:64, 0:64], kt[:, c0:c0 + 64], qt[:, c0:c0 + 64], start=True, stop=True), r=[kt, qt], w=[PS[1]])
                P.op("dve", lambda e: e.tensor_tensor(out=attb[:], in0=PS[1][:64, 0:64], in1=msk[:], op=ALU.mult), r=[PS[1], msk], w=[attb])
                P.op("pe", lambda e: e.matmul(PS[2][:64, 0:256], attb[:, :], vb[:, :], start=True, stop=False), r=[attb, vb], w=[PS[2]])
                P.op("pe", lambda e: e.matmul(PS[2][:64, 0:256], qt[:, c0:c0 + 64], Sb[:, :], start=False, stop=True), r=[qt, Sb], w=[PS[2]])
                if d == 0:
                    P.op("act", lambda e: e.activation(out=o_[:], in_=PS[2][:64, 0:256], func=AF.Copy), r=[PS[2]], w=[o_])
                    P.dma("sp", of_d[p0:p0 + 64, :], o_[:], r=[o_], w=[of_d])
                else:
                    P.dma("sp", of[:], of_d[p0:p0 + 64, :], r=[of_d], w=[of])
                    P.op("dve", lambda e: e.tensor_tensor(out=o_[:], in0=of[:], in1=PS[2][:64, 0:256], op=ALU.add), r=[of, PS[2]], w=[o_])
                    P.op("act", lambda e: e.activation(out=sq[:], in_=o_[:], func=AF.Square), r=[o_], w=[sq])
                    P.op("dve", lambda e: e.reduce_sum(out=st[:, 0:1], in_=sq[:], axis=AX.X), r=[sq], w=[st])
                    P.op("dve", lambda e: e.tensor_scalar(out=st[:, 0:1], in0=st[:, 0:1], scalar1=1.0 / 256, scalar2=EPS, op0=ALU.mult, op1=ALU.add),
                         r=[st], w=[st])
                    P.op("act", lambda e: e.activation(out=st[:, 0:1], in_=st[:, 0:1], func=AF.Sqrt), r=[st], w=[st])
                    P.op("dve", lambda e: e.reciprocal(out=st[:, 1:2], in_=st[:, 0:1]), r=[st], w=[st])
                    P.op("dve", lambda e: e.scalar_tensor_tensor(out=o_[:], in0=o_[:], scalar=st[:, 1:2], in1=ngb[:], op0=ALU.mult, op1=ALU.mult),
                         r=[o_, st, ngb], w=[o_])
                    P.op("act", lambda e: e.activation(out=sq[:], in_=vr[:, 256:512], func=AF.Silu), r=[vr], w=[sq])
                    P.op("dve", lambda e: e.tensor_tensor(out=o_[:], in0=o_[:], in1=sq[:], op=ALU.mult), r=[o_, sq], w=[o_])
                    P.dma("sp", gla_out[p0:p0 + 64, :], o_[:], r=[o_], w=[gla_out])
                P.op("pe", lambda e: e.transpose(PS[3][:64, 0:128], ktl[:, c0:c0 + 64], ident[:, :]), r=[ktl, ident], w=[PS[3]])
                P.op("act", lambda e: e.activation(out=ktT[:], in_=PS[3][:64, 0:128], func=AF.Copy), r=[PS[3]], w=[ktT])
                P.op("pe", lambda e: e.matmul(PS[4][:, 0:256], ktT[:, :], vb[:, :], start=True, stop=True), r=[ktT, vb], w=[PS[4]])
                P.op("dve", lambda e, c=c: e.scalar_tensor_tensor(out=S[:], in0=S[:], scalar=ebl[:, c:c + 1], in1=PS[4][:, 0:256], op0=ALU.mult, op1=ALU.add),
                     r=[S, ebl, PS[4]], w=[S])
                P.op("act", lambda e: e.activation(out=Sb[:], in_=S[:], func=AF.Copy), r=[S], w=[Sb])
    P.finish([hy_out, gla_out])
    return P


HY_MIN_DECAY = math.log(1e-2) / 1.5
HY_MAX_DECAY = math.log(1e-2) / 0.3


def _colpack(vecs):
    a = np.stack(vecs, 0).reshape(len(vecs), 16, 128)
    return np.ascontiguousarray(a.transpose(2, 0, 1).reshape(128, len(vecs) * 16)).astype(np.float32)


def _hy_tables(L):
    pos = np.arange(L, dtype=np.float64)[None, :]
    t = pos / max(L - 1, 1)
    bands = np.linspace(1e-4, 15, 16, dtype=np.float32).astype(np.float64)[:, None]
    ang = 2.0 * math.pi * bands * pos / L
    z = np.concatenate([t, np.cos(ang), -np.sin(ang)], 0).astype(np.float32)
    deltas = np.abs(np.linspace(HY_MIN_DECAY, HY_MAX_DECAY, 1024, dtype=np.float32)).astype(np.float64)
    E = np.exp(-t * deltas[:, None]).astype(np.float32)
    return z, E


def mix0_consts():
    c = np.zeros((4, 128, 512), np.float32)
    k = np.arange(128)[:, None]; i = np.arange(128)[None, :]
    c[0, :, :128] = np.eye(128); c[1, :, :128] = (k <= i); c[2, :, :128] = (k >= i)
    c[3] = 1.0; c[3, :, ::64] = 0.0
    return c


def mix0_core_inputs(ci, xTb, modl, modc, W, tabs):
    b, head, ct = ci // 4, ci % 4, ci
    w_in = W["ab_w_in"]
    sl = lambda s, n: w_in[:, s:s + n]
    (zc, Ec), (zl, El) = tabs
    w3 = W["hy_f_w3"]
    return dict(
        xT=np.stack([xTb[b], xTb[1 - b]]),
        modcol=np.stack([_colpack([modl[b][0], modl[b][1]]), _colpack([modl[1 - b][0], modl[1 - b][1]]), _colpack([modc[0], modc[1]])]),
        whc=np.ascontiguousarray(np.concatenate([sl(ct * 128, 128), sl(1024 + ct * 128, 128), sl(2048 + ct * 128, 128)], 1)),
        wgc=np.ascontiguousarray(np.concatenate([sl(3072 + head * 128, 128), sl(3584 + head * 128, 128), sl(6144, 32)], 1)),
        wgt=np.ascontiguousarray(np.concatenate([sl(4096 + head * 256, 256), sl(5120 + head * 256, 256)], 1)),
        hcw=np.ascontiguousarray(np.stack([np.concatenate([W["hy_conv_w"][:, ti * 1024 + ct * 128:ti * 1024 + (ct + 1) * 128],
                                                           W["hy_conv_b"][None, ti * 1024 + ct * 128:ti * 1024 + (ct + 1) * 128]], 0).T
                                           for ti in range(3)], 1).reshape(128, 12)),
        hlb=np.ascontiguousarray(W["hy_long_bias"][:, ct * 128:(ct + 1) * 128].T),
        fw1=W["hy_f_w1"], fcol=np.ascontiguousarray(np.stack([W["hy_f_b1"], W["hy_f_fr1"], W["hy_f_b2"], W["hy_f_fr2"]], 1)),
        fw2=W["hy_f_w2"],
        fw3=np.ascontiguousarray(np.concatenate([w3[:, o * 2048 + dr * 1024 + ct * 128:o * 2048 + dr * 1024 + (ct + 1) * 128]
                                                 for o in range(2) for dr in range(2)], 1)),
        zl=zl, zc=zc, El=np.ascontiguousarray(El[ct * 128:(ct + 1) * 128]), Ec=np.ascontiguousarray(Ec[ct * 128:(ct + 1) * 128]),
        gw2=np.ascontiguousarray(W["gla_gate_w2"][:, :, head * 128:(head + 1) * 128]),
        gbc=np.ascontiguousarray(W["gla_gate_b"][:, head * 128:(head + 1) * 128].T),
        gng=np.ascontiguousarray(W["gla_norm_g"][head * 256:(head + 1) * 256]),
        consts=mix0_consts(),
    )


def ssd_unit_inputs(g, w_in, conv_w, conv_b, dt_bias, a_log, d_skip, norm_g):
    xs = 4096 + g * 512; bs = 4096 + 4096 + g * 128; cs = 4096 + 4096 + 1024 + g * 128
    wch = np.concatenate([w_in[:, xs:xs + 512], w_in[:, bs:bs + 128], w_in[:, cs:cs + 128]], 1)
    wtm = np.concatenate([w_in[:, g * 512:(g + 1) * 512], w_in[:, 10240 + g * 8:10240 + g * 8 + 8], w_in[:, 10304 + g * 8:10304 + g * 8 + 8]], 1)
    cidx = np.concatenate([np.arange(g * 512, (g + 1) * 512), 4096 + g * 128 + np.arange(128), 4096 + 1024 + g * 128 + np.arange(128)])
    cwf = np.concatenate([conv_w.reshape(9, 6144)[:, cidx], conv_b[None, cidx]], 0)
    cw = np.ascontiguousarray(cwf.reshape(10, 6, 128).transpose(2, 1, 0).reshape(128, 60))
    hp = np.concatenate([dt_bias[:, g * 8:(g + 1) * 8].reshape(-1), a_log[:, g * 8:(g + 1) * 8].reshape(-1), d_skip[:, g * 8:(g + 1) * 8].reshape(-1)])
    return wch, wtm, cw, hp.astype(np.float32), norm_g[g * 512:(g + 1) * 512]


def ssd_consts():
    k = np.arange(128)[:, None]; i = np.arange(128)[None, :]
    return np.stack([np.eye(128), np.ones((128, 128)), k <= i, k >= i, k > i, k < i]).astype(np.float32)


def _run(P, in_maps):
    in_maps = [{k: np.ascontiguousarray(v, dtype=np.float32) for k, v in m.items()} for m in in_maps]
    res = run_bass_kernel_spmd(P.nc, in_maps, core_ids=list(range(len(in_maps))))
    return res.results


def _post_launch(KC, T_l, T_c, yT_list, xres_list, modl, modc, w_out, lng, lnb, rw, rb, wg, wu, wd):
    ident = np.eye(128, dtype=np.float32)
    sel = np.zeros((16, 16, 128), np.float32)
    for e in range(16):
        sel[e, e, :] = 1
    sel = sel.reshape(16, 2048)
    P = build_post(T_l, T_c, KC)
    ins = []
    for ci in range(8):
        b = ci // 4
        ins.append(dict(yT=yT_list[ci], xres=xres_list[ci], w_out=w_out,
                        modrow=np.stack([modl[b], modc]), modcol=np.stack([_colpack(list(modl[b])), _colpack(list(modc))]),
                        lng=lng, lnb=lnb, rw=rw, rb=rb, wg=wg, wu=wu, wd=wd, ident=ident, sel=sel))
    return [r["xout"] for r in _run(P, ins)]


def kernel(**I):
    I = {k: np.asarray(v, dtype=np.float32) for k, v in I.items()}
    x, c, ctx, c_ctx = I["x"], I["c"], I["ctx"], I["c_ctx"]
    LAT, CTX = x.shape[1], ctx.shape[1]
    TB = LAT + CTX
    TPC = LAT // 4
    CPC = CTX // 4
    c_all = np.concatenate([c, c_ctx[None]], 0)
    cT = np.ascontiguousarray(c_all.reshape(3, 16, 128).transpose(2, 1, 0).reshape(128, 48))
    res = _run(build_mod(), [dict(cT=cT, w=I["mod_w"][:, :, i * 1536:(i + 1) * 1536], b=I["mod_b"][:, i * 1536:(i + 1) * 1536]) for i in range(8)])
    mod = np.concatenate([r["out"] for r in res], -1)
    modl = [[mod[l, b].reshape(6, D) for b in range(2)] for l in range(2)]
    modc = [mod[l, 2].reshape(6, D) for l in range(2)]
    xTb = [np.ascontiguousarray(np.concatenate([ctx[b], x[b]], 0).T) for b in range(2)]
    W0 = {k: I[k][0] for k in ["ab_w_in", "hy_conv_w", "hy_conv_b", "hy_f_w1", "hy_f_b1", "hy_f_fr1", "hy_f_w2", "hy_f_b2", "hy_f_fr2",
                               "hy_f_w3", "hy_long_bias", "gla_gate_w2", "gla_gate_b", "gla_norm_g"]}
    tabs = (_hy_tables(CTX), _hy_tables(LAT))
    res = _run(build_mix0(LAT, CTX), [mix0_core_inputs(ci, xTb, modl[0], modc[0], W0, tabs) for ci in range(8)])
    yT0 = [np.zeros((D, TB), np.float32) for _ in range(2)]
    for ci in range(8):
        b, head, ct = ci // 4, ci % 4, ci
        yT0[b][ct * 128:(ct + 1) * 128] = res[ci]["hy_out"][0]
        yT0[1 - b][ct * 128:(ct + 1) * 128] = res[ci]["hy_out"][1]
        yT0[b][1024 + head * 256:1024 + (head + 1) * 256] = res[ci]["gla_out"].T
    del xTb, res
    yl, xl = [], []
    for ci in range(8):
        b, j = ci // 4, ci % 4
        yl.append(np.concatenate([yT0[b][:, CTX + j * TPC:CTX + (j + 1) * TPC], yT0[b][:, j * CPC:(j + 1) * CPC]], 1))
        xl.append(np.concatenate([x[b, j * TPC:(j + 1) * TPC], ctx[b, j * CPC:(j + 1) * CPC]], 0))
    outs = _post_launch(16, TPC, CPC, yl, xl, modl[0], modc[0], I["ab_w_out"][0], I["ln_g"][0], I["ln_b"][0], I["router_w"], I["router_b"],
                        I["exp_w_gate"][0], I["exp_w_up"][0], I["exp_w_down"][0])
    x1 = np.zeros_like(x); ctx1 = np.zeros_like(ctx)
    for ci in range(8):
        b, j = ci // 4, ci % 4
        x1[b, j * TPC:(j + 1) * TPC] = outs[ci][:TPC]
        ctx1[b, j * CPC:(j + 1) * CPC] = outs[ci][TPC:]
    del yT0, yl, xl, outs
    xT1 = [np.ascontiguousarray(np.concatenate([ctx1[b], x1[b]], 0).T) for b in range(2)]
    cst = ssd_consts()
    ins = []
    for ci in range(8):
        b, gp = ci // 4, ci % 4
        us = [ssd_unit_inputs(2 * gp + u, I["ssd_w_in"][0], I["ssd_conv_w"][0], I["ssd_conv_b"][0], I["ssd_dt_bias"][0], I["ssd_a_log"][0],
                              I["ssd_d"][0], I["ssd_norm_g"][0]) for u in range(2)]
        ins.append(dict(xT=xT1[b], modcol=np.stack([_colpack([modl[1][b][0], modl[1][b][1]]), _colpack([modc[1][0], modc[1][1]])]),
                        wch=np.stack([u_[0] for u_ in us]), wtm=np.stack([u_[1] for u_ in us]), cw=np.stack([u_[2] for u_ in us]),
                        hp=np.stack([u_[3] for u_ in us]), ng=np.stack([u_[4] for u_ in us]), consts=cst))
    res = _run(build_ssd(LAT, CTX, 2), ins)
    yT1 = [np.zeros((4096, LAT), np.float32) for _ in range(2)]
    for ci in range(8):
        b, gp = ci // 4, ci % 4
        for u in range(2):
            g = 2 * gp + u
            yT1[b][g * 512:(g + 1) * 512] = res[ci]["ymix"][u].T
    del xT1, ins, res
    yl, xl = [], []
    for ci in range(8):
        b, j = ci // 4, ci % 4
        yl.append(yT1[b][:, j * TPC:(j + 1) * TPC])
        xl.append(x1[b, j * TPC:(j + 1) * TPC])
    outs = _post_launch(32, TPC, 0, yl, xl, modl[1], modc[1], I["ssd_w_out"][0], I["ln_g"][1], I["ln_b"][1], I["router_w"], I["router_b"],
                        I["exp_w_gate"][1], I["exp_w_up"][1], I["exp_w_down"][1])
    out = np.zeros_like(x)
    for ci in range(8):
        b, j = ci // 4, ci % 4
        out[b, j * TPC:(j + 1) * TPC] = outs[ci]
    return out
```

```python
import math
from contextlib import ExitStack
import numpy as np
import concourse.bass as bass
import concourse.mybir as mybir
from concourse.bass_utils import run_bass_kernel_spmd

F32 = mybir.dt.float32
BF16 = mybir.dt.bfloat16
AF = mybir.ActivationFunctionType
ALU = mybir.AluOpType
AX = mybir.AxisListType

D = 2048
NE = 16
DEXP = 1024
ALPHA = (2 * 2) ** 0.25
EPS = 1e-6
SEM_LIMIT = 30000


class Buf:
    def __init__(self, t=None, name=""):
        self.t = t
        self.name = name
        self.w = None
        self.r = []

    def __getitem__(self, idx):
        return self.t[idx]


class Prog:
    def __init__(self):
        self.nc = bass.Bass("TRN2", target_bir_lowering=False)
        nc = self.nc
        self.E = {"pe": nc.tensor, "dve": nc.vector, "act": nc.scalar, "pool": nc.gpsimd, "sp": nc.sync}
        self.nsem = 0
        self.sem = {}
        self.cnt = {}
        for e in self.E:
            self._new_eng_sem(e)
        self.waited = {e: {} for e in self.E}
        self.dsem = []
        self.dcnt = []
        for i in range(12):
            self.dsem.append(self._alloc_sem())
            self.dcnt.append(0)
        self.drr = 0
        self.ninstr = 0
        self.uid = 0

    def _alloc_sem(self):
        self.nsem += 1
        return (self.nsem, self.nc.alloc_semaphore(f"s{self.nsem}"))

    def _new_eng_sem(self, e):
        self.sem[e] = self._alloc_sem()
        self.cnt[e] = 0

    def sb(self, shape, dt=F32, name=None):
        self.uid += 1
        return Buf(self.nc.alloc_sbuf_tensor(name or f"sb{self.uid}", list(shape), dt), name or f"sb{self.uid}")

    def ps(self, name=None):
        self.uid += 1
        return Buf(self.nc.alloc_psum_tensor(name or f"ps{self.uid}", [128, 512], F32), name or f"ps{self.uid}")

    def dram(self, name, shape, dt=F32, kind="Internal"):
        return Buf(self.nc.dram_tensor(name, list(shape), dt, kind=kind).ap(), name)

    def _wait(self, eng, tickets):
        wd = self.waited[eng]
        need = {}
        for tk in tickets:
            if tk is None:
                continue
            (sid, sh), v = tk
            if wd.get(sid, 0) >= v:
                continue
            if sid not in need or need[sid][1] < v:
                need[sid] = (sh, v)
        for sid, (sh, v) in need.items():
            self.E[eng].wait_ge(sh, v)
            wd[sid] = v
            self.ninstr += 1

    def _deps(self, r, w):
        t = []
        for b in r:
            t.append(b.w)
        for b in w:
            t.append(b.w)
            t.extend(b.r)
        return t

    def _commit(self, tk, r, w):
        for b in r:
            b.r.append(tk)
            if len(b.r) > 24:
                b.r = b.r[-24:] if False else b.r
        for b in w:
            b.w = tk
            b.r = []

    def op(self, eng, fn, r=(), w=()):
        self._wait(eng, self._deps(r, w))
        if self.cnt[eng] >= SEM_LIMIT:
            self._new_eng_sem(eng)
        ins = fn(self.E[eng])
        self.cnt[eng] += 1
        s = self.sem[eng]
        ins.then_inc(s[1], 1)
        tk = (s, self.cnt[eng])
        self._commit(tk, r, w)
        self.ninstr += 1
        return tk

    def dma(self, q, out, in_, r=(), w=(), **kw):
        i = self.drr
        self.drr = (self.drr + 1) % len(self.dsem)
        s = self.dsem[i]
        self._wait(q, self._deps(r, w) + [(s, self.dcnt[i])] if self.dcnt[i] else self._deps(r, w))
        if self.dcnt[i] >= SEM_LIMIT * 16:
            self.dsem[i] = self._alloc_sem()
            self.dcnt[i] = 0
            s = self.dsem[i]
        ins = self.E[q].dma_start(out=out, in_=in_, **kw)
        self.dcnt[i] += 16
        ins.then_inc(s[1], 16)
        tk = (s, self.dcnt[i])
        self._commit(tk, r, w)
        self.ninstr += 1
        return tk

    def barrier(self):
        tks = [(self.sem[e], self.cnt[e]) for e in self.E if self.cnt[e]]
        tks += [(self.dsem[i], self.dcnt[i]) for i in range(len(self.dsem)) if self.dcnt[i]]
        for e in self.E:
            self._wait(e, tks)

    def sbs(self, stack, shape, dt=F32):
        self.uid += 1
        return Buf(stack.enter_context(self.nc.sbuf_tensor(f"sc{self.uid}", list(shape), dt)), f"sc{self.uid}")

    def finish(self, bufs):
        self._wait("sp", [b.w for b in bufs])


def make_ident(P, dt=F32):
    ident = P.sb([128, 128], dt, "ident_" + str(dt))
    P.op("pool", lambda e: e.memset(ident[:], 0.0), w=[ident])
    P.op("pool", lambda e: e.affine_select(out=ident[:], in_=ident[:], pattern=[[-1, 128]],
                                           compare_op=ALU.not_equal, fill=1.0, base=0, channel_multiplier=1),
         r=[ident], w=[ident])
    return ident


def bc_ap(ap1d, n):
    return bass.AP(ap1d.tensor, ap1d.offset, [[0, 128], [1, n]])


def layer_norm_tile(P, n, pre, tmp, gbc, bbc, st):
    P.op("dve", lambda e: e.reduce_sum(out=st[:n, 0:1], in_=pre[:n, :], axis=AX.X), r=[pre], w=[st])
    P.op("dve", lambda e: e.tensor_scalar(out=st[:n, 1:2], in0=st[:n, 0:1], scalar1=-1.0 / D, scalar2=None,
                                          op0=ALU.mult), r=[st], w=[st])
    P.op("act", lambda e: e.activation(out=tmp[:n, :], in_=pre[:n, :], func=AF.Square, bias=st[:n, 1:2], scale=1.0),
         r=[pre, st], w=[tmp])
    P.op("dve", lambda e: e.reduce_sum(out=st[:n, 2:3], in_=tmp[:n, :], axis=AX.X), r=[tmp], w=[st])
    P.op("dve", lambda e: e.tensor_scalar(out=st[:n, 2:3], in0=st[:n, 2:3], scalar1=1.0 / D, scalar2=EPS,
                                          op0=ALU.mult, op1=ALU.add), r=[st], w=[st])
    P.op("act", lambda e: e.activation(out=st[:n, 2:3], in_=st[:n, 2:3], func=AF.Sqrt), r=[st], w=[st])
    P.op("dve", lambda e: e.reciprocal(out=st[:n, 3:4], in_=st[:n, 2:3]), r=[st], w=[st])
    P.op("dve", lambda e: e.tensor_scalar(out=pre[:n, :], in0=pre[:n, :], scalar1=st[:n, 1:2], scalar2=st[:n, 3:4],
                                          op0=ALU.add, op1=ALU.mult), r=[pre, st], w=[pre])
    P.op("dve", lambda e: e.tensor_tensor(out=pre[:n, :], in0=pre[:n, :], in1=gbc[:n, :], op=ALU.mult),
         r=[pre, gbc], w=[pre])
    P.op("dve", lambda e: e.tensor_tensor(out=pre[:n, :], in0=pre[:n, :], in1=bbc[:n, :], op=ALU.add),
         r=[pre, bbc], w=[pre])


def build_post(T_l, T_c, KC, NEXP=NE):
    P = Prog()
    T = T_l + T_c
    IN = "ExternalInput"
    yT = P.dram("yT", [KC * 128, T], F32, IN)
    xres = P.dram("xres", [T, D], F32, IN)
    w_out = P.dram("w_out", [KC * 128, D], F32, IN)
    modrow = P.dram("modrow", [2, 6, D], F32, IN)
    modcol = P.dram("modcol", [2, 128, 6 * 16], F32, IN)
    lng = P.dram("lng", [2, D], F32, IN)
    lnb = P.dram("lnb", [2, D], F32, IN)
    rw = P.dram("rw", [D, NE], F32, IN)
    rb = P.dram("rb", [NE], F32, IN)
    wg = P.dram("wg", [NEXP, D, DEXP], F32, IN)
    wu = P.dram("wu", [NEXP, D, DEXP], F32, IN)
    wd = P.dram("wd", [NEXP, DEXP, D], F32, IN)
    ident_d = P.dram("ident", [128, 128], F32, IN)
    sel_d = P.dram("sel", [16, 16 * 128], F32, IN)
    xout = P.dram("xout", [T, D], F32, "ExternalOutput")
    x1_d = P.dram("x1_d", [T, D], F32)
    tokT_d = P.dram("tokT_d", [D, T], F32)

    sb, ps = P.sb, P.ps
    ident = sb([128, 128]); sel = sb([16, 16 * 128])
    rws = sb([128, 16, NE]); rbb = sb([128, NE])
    mcol = [sb([128, 96]) for _ in range(2)]
    sc2p1 = [sb([128, 16]) for _ in range(2)]
    bcA = sb([128, D]); bcB = sb([128, D]); bcC = sb([128, D])
    xt = sb([128, D]); tmp = sb([128, D]); st = sb([128, 4])
    ytile = sb([128, KC, 128], BF16)
    wring = [sb([128, D], BF16) for _ in range(2)]
    tokT32 = sb([128, 16, 128])
    GT = sb([16, T])
    PS = [ps() for _ in range(8)]
    sm = {k: sb([128, 16]) for k in ["sc", "sel", "eq", "s2", "t2", "msk", "gs"]}
    sm4 = {k: sb([128, 4]) for k in ["m1", "m2", "gs", "gm"]}
    sm1 = {k: sb([128, 1]) for k in ["gmax", "den"]}

    def ld(q, dst, src, wbuf, rbuf=()):
        P.dma(q, dst, src, r=list(rbuf), w=[wbuf])

    ld("sp", ident[:], ident_d[:, :], ident, [ident_d])
    ld("sp", sel[:], sel_d[:, :], sel, [sel_d])
    ld("sp", rws[:], rw.t.rearrange("(kc p) e -> p kc e", p=128), rws, [rw])
    ld("sp", rbb[:], bc_ap(rb.t, NE), rbb, [rb])
    for s in range(2):
        ld("sp", mcol[s][:], modcol[s, :, :], mcol[s], [modcol])
        P.op("dve", lambda e, s=s: e.tensor_scalar(out=sc2p1[s][:], in0=mcol[s][:, 64:80], scalar1=1.0, scalar2=None,
                                                    op0=ALU.add), r=[mcol[s]], w=[sc2p1[s]])
    ld("sp", bcB[:], bc_ap(lng[0, :], D), bcB, [lng])
    ld("sp", bcC[:], bc_ap(lnb[0, :], D), bcC, [lnb])

    tiles = [(t0, 128, 0) for t0 in range(0, T_l, 128)]
    if T_c:
        tiles.append((T_l, T_c, 1))
    cur_set = [-1]

    for ti, (t0, n, s) in enumerate(tiles):
        if cur_set[0] != s:
            ld("sp", bcA[:], bc_ap(modrow[s, 2, :], D), bcA, [modrow])
            cur_set[0] = s
        ld("pool", ytile[:, :, :n], yT.t[:, t0:t0 + n].rearrange("(kc p) t -> p kc t", p=128), ytile, [yT])
        ld("sp", xt[:n, :], xres[t0:t0 + n, :], xt, [xres])
        for kc in range(KC):
            ws = wring[kc % 2]
            ld("pool", ws[:], w_out[kc * 128:(kc + 1) * 128, :], ws, [w_out])
            for j in range(4):
                P.op("pe", lambda e, j=j, kc=kc, ws=ws: e.matmul(PS[j][:n, :], ytile[:, kc, :n], ws[:, j * 512:(j + 1) * 512],
                                                                start=(kc == 0), stop=(kc == KC - 1)),
                     r=[ytile, ws], w=[PS[j]])
        for j in range(4):
            P.op("dve", lambda e, j=j: e.tensor_tensor(out=tmp[:n, j * 512:(j + 1) * 512], in0=PS[j][:n, :],
                                                      in1=bcA[:n, j * 512:(j + 1) * 512], op=ALU.mult),
                 r=[PS[j], bcA], w=[tmp])
        P.op("dve", lambda e: e.scalar_tensor_tensor(out=xt[:n, :], in0=xt[:n, :], scalar=ALPHA, in1=tmp[:n, :],
                                                     op0=ALU.mult, op1=ALU.add), r=[xt, tmp], w=[xt])
        layer_norm_tile(P, n, xt, tmp, bcB, bcC, st)
        ld("sp", x1_d[t0:t0 + n, :], xt[:n, :], x1_d, [xt])
        for kc in range(16):
            P.op("pe", lambda e, kc=kc: e.transpose(PS[4 + kc // 4][:, (kc % 4) * 128:(kc % 4) * 128 + n],
                                                    xt[:n, kc * 128:(kc + 1) * 128], ident[:n, :n]),
                 r=[xt, ident], w=[PS[4 + kc // 4]])
        for kc in range(16):
            P.op("act", lambda e, kc=kc: e.activation(out=tokT32[:, kc, :n],
                                                      in_=PS[4 + kc // 4][:, (kc % 4) * 128:(kc % 4) * 128 + n],
                                                      func=AF.Identity, bias=mcol[s][:, 48 + kc:49 + kc],
                                                      scale=sc2p1[s][:, kc:kc + 1]),
                 r=[PS[4 + kc // 4], mcol[s], sc2p1[s]], w=[tokT32])
        ld("sp", tokT_d.t[:, t0:t0 + n].rearrange("(kc p) t -> p kc t", p=128), tokT32[:, :, :n], tokT_d, [tokT32])
        for kc in range(16):
            P.op("pe", lambda e, kc=kc: e.matmul(PS[0][:n, 0:NE], tokT32[:, kc, :n], rws[:, kc, :],
                                                 start=(kc == 0), stop=(kc == 15)), r=[tokT32, rws], w=[PS[0]])
        sc, sl, eq, s2, t2, msk, gsel = (sm[k] for k in ["sc", "sel", "eq", "s2", "t2", "msk", "gs"])
        m1, m2, gs, gm = (sm4[k] for k in ["m1", "m2", "gs", "gm"])
        gmax, den = sm1["gmax"], sm1["den"]
        v3 = lambda b: b[:n, :].rearrange("p (g e) -> p g e", g=4)
        b4 = lambda b: b[:n, :].unsqueeze(2).to_broadcast([n, 4, 4])
        P.op("act", lambda e: e.activation(out=sc[:n, :], in_=PS[0][:n, 0:NE], func=AF.Sigmoid), r=[PS[0]], w=[sc])
        P.op("dve", lambda e: e.tensor_tensor(out=sl[:n, :], in0=sc[:n, :], in1=rbb[:n, :], op=ALU.add), r=[sc, rbb], w=[sl])
        P.op("dve", lambda e: e.tensor_reduce(out=m1[:n, :], in_=v3(sl), axis=AX.X, op=ALU.max), r=[sl], w=[m1])
        P.op("dve", lambda e: e.tensor_tensor(out=v3(eq), in0=v3(sl), in1=b4(m1), op=ALU.is_equal), r=[sl, m1], w=[eq])
        P.op("dve", lambda e: e.scalar_tensor_tensor(out=s2[:n, :], in0=eq[:n, :], scalar=-1e9, in1=sl[:n, :],
                                                     op0=ALU.mult, op1=ALU.add), r=[eq, sl], w=[s2])
        P.op("dve", lambda e: e.tensor_reduce(out=m2[:n, :], in_=v3(s2), axis=AX.X, op=ALU.max), r=[s2], w=[m2])
        P.op("dve", lambda e: e.tensor_tensor(out=gs[:n, :], in0=m1[:n, :], in1=m2[:n, :], op=ALU.add), r=[m1, m2], w=[gs])
        P.op("dve", lambda e: e.tensor_reduce(out=gmax[:n, :], in_=gs[:n, :], axis=AX.X, op=ALU.max), r=[gs], w=[gmax])
        P.op("dve", lambda e: e.tensor_scalar(out=gm[:n, :], in0=gs[:n, :], scalar1=gmax[:n, 0:1], scalar2=None,
                                              op0=ALU.is_equal), r=[gs, gmax], w=[gm])
        P.op("dve", lambda e: e.tensor_tensor(out=v3(t2), in0=v3(sl), in1=b4(m2), op=ALU.is_ge), r=[sl, m2], w=[t2])
        P.op("dve", lambda e: e.tensor_tensor(out=v3(msk), in0=v3(t2), in1=b4(gm), op=ALU.mult), r=[t2, gm], w=[msk])
        P.op("dve", lambda e: e.tensor_tensor(out=gsel[:n, :], in0=sc[:n, :], in1=msk[:n, :], op=ALU.mult), r=[sc, msk], w=[gsel])
        P.op("dve", lambda e: e.reduce_sum(out=den[:n, :], in_=gsel[:n, :], axis=AX.X), r=[gsel], w=[den])
        P.op("dve", lambda e: e.reciprocal(out=den[:n, :], in_=den[:n, :]), r=[den], w=[den])
        P.op("dve", lambda e: e.tensor_scalar(out=gsel[:n, :], in0=gsel[:n, :], scalar1=den[:n, 0:1], scalar2=None,
                                              op0=ALU.mult), r=[gsel, den], w=[gsel])
        P.op("pe", lambda e: e.transpose(PS[1][0:16, 0:n], gsel[:n, :], ident[:n, :n]), r=[gsel, ident], w=[PS[1]])
        P.op("act", lambda e: e.activation(out=GT[:, t0:t0 + n], in_=PS[1][0:16, 0:n], func=AF.Copy), r=[PS[1]], w=[GT])

    tb = sb([128, 16, 512], BF16)
    acc = sb([128, 16, 512])
    hT = sb([128, 8, 512], BF16)
    sg = [sb([128, 512]) for _ in range(2)]
    t1 = [sb([128, 512]) for _ in range(2)]
    gb = sb([128, 512])
    wgs = [sb([128, 16, 128], BF16) for _ in range(2)]
    wus = [sb([128, 16, 128], BF16) for _ in range(2)]
    wds = sb([128, 8, D], BF16)
    ld("sp", bcB[:], bc_ap(lng[1, :], D), bcB, [lng])
    ld("sp", bcC[:], bc_ap(lnb[1, :], D), bcC, [lnb])
    blocks = [(b0, 512, 0) for b0 in range(0, T_l, 512)]
    if T_c:
        blocks.append((T_l, T_c, 1))
    pc = 0
    for (b0, nb, s) in blocks:
        ld("pool", tb[:, :, :nb], tokT_d.t[:, b0:b0 + nb].rearrange("(kc p) t -> p kc t", p=128), tb, [tokT_d])
        for ex in range(NEXP):
            ld("pool", wds[:], wd.t[ex].rearrange("(hc p) d -> p hc d", p=128), wds, [wd])
            P.op("pe", lambda e, ex=ex: e.matmul(PS[2][:, :nb], sel[:, ex * 128:(ex + 1) * 128], GT[:, b0:b0 + nb],
                                                 start=True, stop=True), r=[sel, GT], w=[PS[2]])
            P.op("act", lambda e: e.activation(out=gb[:, :nb], in_=PS[2][:, :nb], func=AF.Copy), r=[PS[2]], w=[gb])
            for hc in range(8):
                a, b = wgs[pc % 2], wus[pc % 2]
                sgi, t1i = sg[pc % 2], t1[pc % 2]
                pg, pu = PS[(pc % 2) * 2], PS[(pc % 2) * 2 + 1]
                pc += 1
                ld("pool", a[:], wg.t[ex][:, hc * 128:(hc + 1) * 128].rearrange("(kc p) h -> p kc h", p=128), a, [wg])
                ld("pool", b[:], wu.t[ex][:, hc * 128:(hc + 1) * 128].rearrange("(kc p) h -> p kc h", p=128), b, [wu])
                for kc in range(16):
                    P.op("pe", lambda e, kc=kc, a=a, pg=pg: e.matmul(pg[:, :nb], a[:, kc, :], tb[:, kc, :nb], start=(kc == 0),
                                                                   stop=(kc == 15)), r=[a, tb], w=[pg])
                for kc in range(16):
                    P.op("pe", lambda e, kc=kc, b=b, pu=pu: e.matmul(pu[:, :nb], b[:, kc, :], tb[:, kc, :nb], start=(kc == 0),
                                                                   stop=(kc == 15)), r=[b, tb], w=[pu])
                P.op("act", lambda e, pg=pg, sgi=sgi: e.activation(out=sgi[:, :nb], in_=pg[:, :nb], func=AF.Silu), r=[pg], w=[sgi])
                P.op("dve", lambda e, pu=pu, t1i=t1i: e.tensor_tensor(out=t1i[:, :nb], in0=pu[:, :nb], in1=gb[:, :nb], op=ALU.mult),
                     r=[pu, gb], w=[t1i])
                P.op("dve", lambda e, hc=hc, sgi=sgi, t1i=t1i: e.tensor_tensor(out=hT[:, hc, :nb], in0=sgi[:, :nb], in1=t1i[:, :nb],
                                                                              op=ALU.mult), r=[sgi, t1i], w=[hT])
            for dc in range(16):
                pd = PS[4 + dc % 4]
                for hc in range(8):
                    P.op("pe", lambda e, dc=dc, hc=hc, pd=pd: e.matmul(pd[:, :nb], wds[:, hc, dc * 128:(dc + 1) * 128], hT[:, hc, :nb],
                                                                     start=(hc == 0), stop=(hc == 7)), r=[wds, hT], w=[pd])
                if ex == 0:
                    P.op("act", lambda e, dc=dc, pd=pd: e.activation(out=acc[:, dc, :nb], in_=pd[:, :nb], func=AF.Copy), r=[pd], w=[acc])
                else:
                    P.op("dve", lambda e, dc=dc, pd=pd: e.tensor_tensor(out=acc[:, dc, :nb], in0=acc[:, dc, :nb], in1=pd[:, :nb],
                                                                       op=ALU.add), r=[pd, acc], w=[acc])
        for dc in range(16):
            P.op("dve", lambda e, dc=dc: e.tensor_scalar(out=acc[:, dc, :nb], in0=acc[:, dc, :nb], scalar1=mcol[s][:, 80 + dc:81 + dc],
                                                        scalar2=None, op0=ALU.mult), r=[acc, mcol[s]], w=[acc])
        for j0 in range(0, nb, 128):
            n = min(128, nb - j0)
            t0 = b0 + j0
            for dc in range(16):
                P.op("pe", lambda e, dc=dc: e.transpose(PS[dc // 4][:n, (dc % 4) * 128:(dc % 4 + 1) * 128],
                                                        acc[:, dc, j0:j0 + n], ident[:, :]), r=[acc, ident], w=[PS[dc // 4]])
            ld("sp", xt[:n, :], x1_d[t0:t0 + n, :], xt, [x1_d])
            for j in range(4):
                P.op("dve", lambda e, j=j: e.scalar_tensor_tensor(out=xt[:n, j * 512:(j + 1) * 512], in0=xt[:n, j * 512:(j + 1) * 512],
                                                                 scalar=ALPHA, in1=PS[j][:n, :], op0=ALU.mult, op1=ALU.add),
                     r=[xt, PS[j]], w=[xt])
            layer_norm_tile(P, n, xt, tmp, bcB, bcC, st)
            ld("sp", xout[t0:t0 + n, :], xt[:n, :], xout, [xt])
    P.finish([xout])
    return P


def build_mod():
    P = Prog()
    IN = "ExternalInput"
    NCOL = 1536
    cT = P.dram("cT", [128, 16 * 3], F32, IN)
    w = P.dram("w", [2, D, NCOL], F32, IN)
    b = P.dram("b", [2, NCOL], F32, IN)
    out = P.dram("out", [2, 3, NCOL], F32, "ExternalOutput")
    ct = P.sb([128, 16, 3]); sg = P.sb([128, 16, 3])
    ring = [P.sb([128, NCOL]) for _ in range(3)]
    bb = P.sb([3, NCOL]); res = P.sb([3, NCOL])
    PS = [P.ps() for _ in range(3)]
    P.dma("sp", ct[:].rearrange("p k r -> p (k r)"), cT[:, :], r=[cT], w=[ct])
    P.op("act", lambda e: e.activation(out=sg[:], in_=ct[:], func=AF.Sigmoid), r=[ct], w=[sg])
    P.op("dve", lambda e: e.tensor_tensor(out=sg[:], in0=sg[:], in1=ct[:], op=ALU.mult), r=[sg, ct], w=[sg])
    i = 0
    for l in range(2):
        P.dma("sp", bb[:], bass.AP(b.t.tensor, b[l, :].offset, [[0, 3], [1, NCOL]]), r=[b], w=[bb])
        for kc in range(16):
            ws = ring[i % 3]; i += 1
            P.dma("sp", ws[:], w[l, kc * 128:(kc + 1) * 128, :], r=[w], w=[ws])
            for j in range(3):
                P.op("pe", lambda e, j=j, kc=kc, ws=ws: e.matmul(PS[j][0:3, :], sg[:, kc, :], ws[:, j * 512:(j + 1) * 512],
                                                                start=(kc == 0), stop=(kc == 15)), r=[sg, ws], w=[PS[j]])
        for j in range(3):
            P.op("dve", lambda e, j=j: e.tensor_tensor(out=res[:, j * 512:(j + 1) * 512], in0=PS[j][0:3, :],
                                                      in1=bb[:, j * 512:(j + 1) * 512], op=ALU.add), r=[PS[j], bb], w=[res])
        P.dma("sp", out[l, :, :], res[:], r=[res], w=[out])
    P.finish([out])
    return P


def build_ssd(LAT=16384, CTX=256, NU=2):
    P = Prog()
    IN = "ExternalInput"
    TB = CTX + LAT
    NCH = 6
    xT = P.dram("xT", [D, TB], F32, IN)
    modcol = P.dram("modcol", [2, 128, 32], F32, IN)
    wch = P.dram("wch", [NU, D, 768], F32, IN)
    wtm = P.dram("wtm", [NU, D, 528], F32, IN)
    cw = P.dram("cw", [NU, 128, NCH * 10], F32, IN)
    hp = P.dram("hp", [NU, 48], F32, IN)
    ng = P.dram("ng", [NU, 512], F32, IN)
    consts = P.dram("consts", [6, 128, 128], F32, IN)
    ymix = P.dram("ymix", [NU, LAT, 512], F32, "ExternalOutput")
    xbc_d = P.dram("xbc_d", [768, TB], F32)
    xc_d = P.dram("xc_d", [768, TB], F32)
    z_d = P.dram("z_d", [TB, 528], F32)
    yf_d = P.dram("yf_d", [LAT, 512], F32)
    sb = P.sb
    C = [sb([128, 128]) for _ in range(6)]
    for i in range(6):
        P.dma("sp", C[i][:], consts[i, :, :], r=[consts], w=[C[i]])
    ident, ones, TriF, TriB, SF, SB_ = C
    mcol = [sb([128, 32]) for _ in range(2)]
    scp1 = [sb([128, 16]) for _ in range(2)]
    for s in range(2):
        P.dma("sp", mcol[s][:], modcol[s, :, :], r=[modcol], w=[mcol[s]])
        P.op("dve", lambda e, s=s: e.tensor_scalar(out=scp1[s][:], in0=mcol[s][:, 16:32], scalar1=1.0, scalar2=None,
                                                    op0=ALU.add), r=[mcol[s]], w=[scp1[s]])
    PS = [P.ps() for _ in range(8)]
    wchs = sb([128, 16, 768], BF16)
    wtms = sb([128, 16, 528], BF16)
    xin32 = sb([128, 16, 512])
    hT = sb([128, 16, 512], BF16)
    stage = [sb([128, 528]) for _ in range(2)]
    cws = sb([128, NCH * 10])
    hpb = sb([128, 48]); aneg = sb([128, 16]); dsum = sb([128, 8]); ngb = sb([128, 512])
    R = min(32, LAT // 64)
    cin = sb([128, (R + 2) * 64]); cout = sb([128, max(R * 64, CTX)])

    for u in range(NU):
        P.dma("pool", wchs[:], wch.t[u].rearrange("(kc p) c -> p kc c", p=128), r=[wch], w=[wchs])
        P.dma("pool", wtms[:], wtm.t[u].rearrange("(kc p) c -> p kc c", p=128), r=[wtm], w=[wtms])
        P.dma("sp", cws[:], cw[u, :, :], r=[cw], w=[cws])
        P.dma("sp", hpb[:], bass.AP(hp.t.tensor, hp[u, :].offset, [[0, 128], [1, 48]]), r=[hp], w=[hpb])
        P.dma("sp", ngb[:], bass.AP(ng.t.tensor, ng[u, :].offset, [[0, 128], [1, 512]]), r=[ng], w=[ngb])
        P.op("act", lambda e: e.activation(out=aneg[:], in_=hpb[:, 16:32], func=AF.Exp), r=[hpb], w=[aneg])
        P.op("dve", lambda e: e.tensor_scalar(out=aneg[:], in0=aneg[:], scalar1=-1.0, scalar2=None, op0=ALU.mult), r=[aneg], w=[aneg])
        P.op("dve", lambda e: e.tensor_tensor(out=dsum[:], in0=hpb[:, 32:40], in1=hpb[:, 40:48], op=ALU.add), r=[hpb], w=[dsum])
        blocks = [(0, CTX, 1)] + [(CTX + i * 512, 512, 0) for i in range(LAT // 512)]
        si = 0
        for (t0, nb, s) in blocks:
            P.dma("sp", xin32[:, :, :nb], xT.t[:, t0:t0 + nb].rearrange("(kc p) t -> p kc t", p=128), r=[xT], w=[xin32])
            for kc in range(16):
                P.op("act", lambda e, kc=kc: e.activation(out=hT[:, kc, :nb], in_=xin32[:, kc, :nb], func=AF.Identity,
                                                          bias=mcol[s][:, kc:kc + 1], scale=scp1[s][:, kc:kc + 1]),
                     r=[xin32, mcol[s], scp1[s]], w=[hT])
            for m in range(NCH):
                pp = PS[m % 4]
                for kc in range(16):
                    P.op("pe", lambda e, m=m, kc=kc, pp=pp: e.matmul(pp[:, :nb], wchs[:, kc, m * 128:(m + 1) * 128], hT[:, kc, :nb],
                                                                   start=(kc == 0), stop=(kc == 15)), r=[wchs, hT], w=[pp])
                sg = stage[si % 2]; si += 1
                P.op("act", lambda e, pp=pp, sg=sg: e.activation(out=sg[:, :nb], in_=pp[:, :nb], func=AF.Copy), r=[pp], w=[sg])
                P.dma("sp", xbc_d[m * 128:(m + 1) * 128, t0:t0 + nb], sg[:, :nb], r=[sg], w=[xbc_d])
            for j0 in range(0, nb, 128):
                pz, pd = PS[4 + (j0 // 128) % 2 * 2], PS[5 + (j0 // 128) % 2 * 2]
                for kc in range(16):
                    P.op("pe", lambda e, kc=kc, pz=pz: e.matmul(pz[:, :], hT[:, kc, j0:j0 + 128], wtms[:, kc, 0:512],
                                                              start=(kc == 0), stop=(kc == 15)), r=[wtms, hT], w=[pz])
                for kc in range(16):
                    P.op("pe", lambda e, kc=kc, pd=pd: e.matmul(pd[:, 0:16], hT[:, kc, j0:j0 + 128], wtms[:, kc, 512:528],
                                                              start=(kc == 0), stop=(kc == 15)), r=[wtms, hT], w=[pd])
                sg = stage[si % 2]; si += 1
                P.op("act", lambda e, pz=pz, sg=sg: e.activation(out=sg[:, 0:512], in_=pz[:, :], func=AF.Copy), r=[pz], w=[sg])
                P.op("dve", lambda e, pd=pd, sg=sg: e.tensor_copy(out=sg[:, 512:528], in_=pd[:, 0:16]), r=[pd], w=[sg])
                P.dma("sp", z_d[t0 + j0:t0 + j0 + 128, :], sg[:, :], r=[sg], w=[z_d])
        for m in range(NCH):
            wv = lambda k: cws[:, m * 10 + k:m * 10 + k + 1]
            P.dma("sp", cin[:, 0:CTX], xbc_d[m * 128:(m + 1) * 128, 0:CTX], r=[xbc_d], w=[cin])
            P.op("dve", lambda e: e.tensor_scalar(out=cout[:, 0:CTX], in0=cin[:, 0:CTX], scalar1=wv(4), scalar2=wv(9),
                                                  op0=ALU.mult, op1=ALU.add), r=[cin, cws], w=[cout])
            P.op("dve", lambda e: e.scalar_tensor_tensor(out=cout[:, 1:CTX], in0=cin[:, 0:CTX - 1], scalar=wv(3), in1=cout[:, 1:CTX],
                                                         op0=ALU.mult, op1=ALU.add), r=[cin, cws, cout], w=[cout])
            P.op("dve", lambda e: e.scalar_tensor_tensor(out=cout[:, 0:CTX - 1], in0=cin[:, 1:CTX], scalar=wv(5), in1=cout[:, 0:CTX - 1],
                                                         op0=ALU.mult, op1=ALU.add), r=[cin, cws, cout], w=[cout])
            P.op("act", lambda e: e.activation(out=cout[:, 0:CTX], in_=cout[:, 0:CTX], func=AF.Silu), r=[cout], w=[cout])
            P.dma("sp", xc_d[m * 128:(m + 1) * 128, 0:CTX], cout[:, 0:CTX], r=[cout], w=[xc_d])
            NR = LAT // 64
            for r0 in range(0, NR, R):
                lo, hi = r0 - 1, r0 + R + 1
                if lo < 0:
                    P.op("dve", lambda e: e.memset(cin[:, 0:64], 0.0), w=[cin])
                if hi > NR:
                    P.op("dve", lambda e: e.memset(cin[:, (R + 1) * 64:(R + 2) * 64], 0.0), w=[cin])
                a, b_ = max(lo, 0), min(hi, NR)
                P.dma("sp", cin[:, (a - lo) * 64:(b_ - lo) * 64], xbc_d[m * 128:(m + 1) * 128, CTX + a * 64:CTX + b_ * 64],
                      r=[xbc_d], w=[cin])
                ci3 = cin[:, :].rearrange("p (r c) -> p r c", c=64)
                co3 = cout[:, :].rearrange("p (r c) -> p r c", c=64)
                P.op("dve", lambda e: e.tensor_scalar(out=co3[:, :, :], in0=ci3[:, 1:R + 1, :], scalar1=wv(4), scalar2=wv(9),
                                                      op0=ALU.mult, op1=ALU.add), r=[cin, cws], w=[cout])
                for i in range(3):
                    for j in range(3):
                        if i == 1 and j == 1:
                            continue
                        if j == 0:
                            o_, i_ = co3[:, :, 1:64], ci3[:, i:i + R, 0:63]
                        elif j == 1:
                            o_, i_ = co3[:, :, :], ci3[:, i:i + R, :]
                        else:
                            o_, i_ = co3[:, :, 0:63], ci3[:, i:i + R, 1:64]
                        P.op("dve", lambda e, o_=o_, i_=i_, k=i * 3 + j: e.scalar_tensor_tensor(out=o_, in0=i_, scalar=wv(k), in1=o_,
                                                                                           op0=ALU.mult, op1=ALU.add),
                             r=[cin, cws, cout], w=[cout])
                P.op("act", lambda e: e.activation(out=cout[:, :], in_=cout[:, :], func=AF.Silu), r=[cout], w=[cout])
                P.dma("sp", xc_d[m * 128:(m + 1) * 128, CTX + r0 * 64:CTX + (r0 + R) * 64], cout[:, :], r=[cout], w=[xc_d])
        ssd_scan(P, u, PS, (ident, ones, TriF, TriB, SF, SB_), xc_d, z_d, yf_d, ymix, hpb, aneg, dsum, ngb, LAT, CTX)
    P.finish([ymix])
    return P


def ssd_scan(P, u, PS, consts, xc_d, z_d, yf_d, ymix, hpb, aneg, dsum, ngb, LAT, CTX):
    ident, ones, TriF, TriB, SF, SB_ = consts
    sb = P.sb
    if not hasattr(P, "_ssd_bufs"):
        B = {}
        B["CT"] = sb([128, 128]); B["BT"] = sb([128, 128]); B["CTb"] = sb([128, 128], BF16); B["BTb"] = sb([128, 128], BF16)
        B["xcm"] = sb([128, 4, 128]); B["xtm"] = sb([128, 512]); B["Btm"] = sb([128, 128], BF16)
        B["zt"] = sb([128, 528]); B["dt"] = sb([128, 8]); B["a"] = sb([128, 8]); B["e8"] = sb([128, 8])
        B["acs"] = sb([128, 8]); B["eacs"] = sb([128, 8]); B["tail"] = sb([128, 8]); B["etot"] = sb([128, 8])
        B["xdt"] = sb([128, 512], BF16); B["xdtt"] = sb([128, 512], BF16)
        B["cbm"] = sb([128, 128]); B["lh"] = [sb([128, 128]) for _ in range(2)]; B["L"] = [sb([128, 128]) for _ in range(2)]
        B["M"] = [sb([128, 128], BF16) for _ in range(2)]
        B["S"] = sb([128, 512]); B["Sb"] = sb([128, 512], BF16); B["tS"] = sb([128, 512])
        B["y"] = sb([128, 512]); B["yf"] = sb([128, 512]); B["sz"] = sb([128, 512]); B["st"] = sb([128, 4])
        P._ssd_bufs = B
    B = P._ssd_bufs
    CT, BT, CTb, BTb, xcm, xtm, Btm, zt = (B[k] for k in ["CT", "BT", "CTb", "BTb", "xcm", "xtm", "Btm", "zt"])
    dt, a, e8, acs, eacs, tail, etot = (B[k] for k in ["dt", "a", "e8", "acs", "eacs", "tail", "etot"])
    xdt, xdtt, cbm, S, Sb, tS, y, yf, sz, st = (B[k] for k in ["xdt", "xdtt", "cbm", "S", "Sb", "tS", "y", "yf", "sz", "st"])
    bc8 = lambda t: t[:, 0:8].unsqueeze(2).to_broadcast([128, 8, 64])
    v3 = lambda t: t[:, :].rearrange("p (h q) -> p h q", h=8)
    nctx, nlat = CTX // 128, LAT // 128
    for d in range(2):
        Tri, SM = (TriF, SF) if d == 0 else (TriB, SB_)
        P.op("dve", lambda e: e.memset(S[:], 0.0), w=[S])
        P.op("dve", lambda e: e.memset(Sb[:], 0.0), w=[Sb])
        order = list(range(nctx)) + [nctx + i for i in range(nlat)]
        if d == 1:
            order = list(range(nctx))[::-1] + [nctx + i for i in range(nlat)][::-1]
        for ci, c in enumerate(order):
            p0 = c * 128
            lat = c >= nctx
            l0 = p0 - CTX
            last_state = (ci == len(order) - 1)
            P.dma("sp", CT[:], xc_d[640:768, p0:p0 + 128], r=[xc_d], w=[CT])
            P.dma("sp", BT[:], xc_d[512:640, p0:p0 + 128], r=[xc_d], w=[BT])
            P.dma("sp", xcm[:], xc_d.t[0:512, p0:p0 + 128].rearrange("(m p) t -> p m t", p=128), r=[xc_d], w=[xcm])
            P.dma("sp", zt[:], z_d[p0:p0 + 128, :], r=[z_d], w=[zt])
            P.op("act", lambda e: e.activation(out=CTb[:], in_=CT[:], func=AF.Copy), r=[CT], w=[CTb])
            P.op("act", lambda e: e.activation(out=BTb[:], in_=BT[:], func=AF.Copy), r=[BT], w=[BTb])
            for m in range(4):
                P.op("pe", lambda e, m=m: e.transpose(PS[0][:, m * 128:(m + 1) * 128], xcm[:, m, :], ident[:, :]), r=[xcm, ident], w=[PS[0]])
            P.op("act", lambda e: e.activation(out=xtm[:], in_=PS[0][:, :], func=AF.Copy), r=[PS[0]], w=[xtm])
            P.op("pe", lambda e: e.transpose(PS[1][:, 0:128], BT[:, :], ident[:, :]), r=[BT, ident], w=[PS[1]])
            P.op("act", lambda e: e.activation(out=Btm[:], in_=PS[1][:, 0:128], func=AF.Copy), r=[PS[1]], w=[Btm])
            P.op("dve", lambda e: e.tensor_tensor(out=e8[:], in0=zt[:, 512 + d * 8:520 + d * 8], in1=hpb[:, d * 8:d * 8 + 8], op=ALU.add),
                 r=[zt, hpb], w=[e8])
            P.op("act", lambda e: e.activation(out=e8[:], in_=e8[:], func=AF.Exp), r=[e8], w=[e8])
            P.op("act", lambda e: e.activation(out=dt[:], in_=e8[:], func=AF.Ln, bias=1.0, scale=1.0), r=[e8], w=[dt])
            P.op("dve", lambda e: e.tensor_tensor(out=a[:], in0=dt[:], in1=aneg[:, d * 8:d * 8 + 8], op=ALU.mult), r=[dt, aneg], w=[a])
            P.op("pe", lambda e: e.matmul(PS[2][:, 0:8], Tri[:, :], a[:, :], start=True, stop=True), r=[Tri, a], w=[PS[2]])
            P.op("pe", lambda e: e.matmul(PS[2][:, 8:16], ones[:, :], a[:, :], start=True, stop=True), r=[ones, a], w=[PS[2]])
            P.op("act", lambda e: e.activation(out=acs[:], in_=PS[2][:, 0:8], func=AF.Copy), r=[PS[2]], w=[acs])
            P.op("act", lambda e: e.activation(out=eacs[:], in_=PS[2][:, 0:8], func=AF.Exp), r=[PS[2]], w=[eacs])
            P.op("act", lambda e: e.activation(out=etot[:], in_=PS[2][:, 8:16], func=AF.Exp), r=[PS[2]], w=[etot])
            P.op("dve", lambda e: e.tensor_tensor(out=tail[:], in0=PS[2][:, 8:16], in1=acs[:], op=ALU.subtract), r=[PS[2], acs], w=[tail])
            P.op("act", lambda e: e.activation(out=tail[:], in_=tail[:], func=AF.Exp), r=[tail], w=[tail])
            P.op("dve", lambda e: e.tensor_tensor(out=v3(xdt), in0=v3(xtm), in1=bc8(dt), op=ALU.mult), r=[xtm, dt], w=[xdt])
            P.op("dve", lambda e: e.tensor_tensor(out=v3(xdtt), in0=v3(xdt), in1=bc8(tail), op=ALU.mult), r=[xdt, tail], w=[xdtt])
            if lat:
                P.op("pe", lambda e: e.matmul(PS[3][:, 0:128], BTb[:, :], CTb[:, :], start=True, stop=True), r=[BTb, CTb], w=[PS[3]])
                P.op("dve", lambda e: e.tensor_tensor(out=cbm[:], in0=PS[3][:, 0:128], in1=Tri[:, :], op=ALU.mult), r=[PS[3], Tri], w=[cbm])
                for h in range(8):
                    lh, L, M = B["lh"][h % 2], B["L"][h % 2], B["M"][h % 2]
                    pdf = PS[4 + h % 2]
                    P.op("dve", lambda e, h=h, lh=lh: e.tensor_scalar(out=lh[:], in0=SM[:, :], scalar1=a[:, h:h + 1], scalar2=None,
                                                                    op0=ALU.mult), r=[SM, a], w=[lh])
                    P.op("pe", lambda e, lh=lh, pdf=pdf: e.matmul(pdf[:, 0:128], lh[:, :], Tri[:, :], start=True, stop=True), r=[lh, Tri], w=[pdf])
                    P.op("act", lambda e, L=L, pdf=pdf: e.activation(out=L[:], in_=pdf[:, 0:128], func=AF.Exp), r=[pdf], w=[L])
                    P.op("dve", lambda e, L=L, M=M: e.tensor_tensor(out=M[:], in0=L[:], in1=cbm[:], op=ALU.mult), r=[L, cbm], w=[M])
                    P.op("pe", lambda e, h=h, M=M: e.matmul(PS[6][:, h * 64:(h + 1) * 64], M[:, :], xdt[:, h * 64:(h + 1) * 64],
                                                          start=True, stop=True), r=[M, xdt], w=[PS[6]])
                P.op("pe", lambda e: e.matmul(PS[7][:, :], CTb[:, :], Sb[:, :], start=True, stop=True), r=[CTb, Sb], w=[PS[7]])
                P.op("dve", lambda e: e.tensor_tensor(out=v3(y), in0=PS[7][:, :].rearrange("p (h q) -> p h q", h=8), in1=bc8(eacs), op=ALU.mult),
                     r=[PS[7], eacs], w=[y])
                P.op("dve", lambda e: e.tensor_tensor(out=y[:], in0=y[:], in1=PS[6][:, :], op=ALU.add), r=[y, PS[6]], w=[y])
                if d == 0:
                    P.dma("sp", yf_d[l0:l0 + 128, :], y[:], r=[y], w=[yf_d])
                else:
                    P.dma("sp", yf[:], yf_d[l0:l0 + 128, :], r=[yf_d], w=[yf])
                    P.op("dve", lambda e: e.tensor_tensor(out=y[:], in0=y[:], in1=yf[:], op=ALU.add), r=[y, yf], w=[y])
                    P.op("dve", lambda e: e.tensor_tensor(out=v3(yf), in0=v3(xtm), in1=bc8(dsum), op=ALU.mult), r=[xtm, dsum], w=[yf])
                    P.op("dve", lambda e: e.tensor_tensor(out=y[:], in0=y[:], in1=yf[:], op=ALU.add), r=[y, yf], w=[y])
                    P.op("act", lambda e: e.activation(out=sz[:], in_=zt[:, 0:512], func=AF.Silu), r=[zt], w=[sz])
                    P.op("dve", lambda e: e.tensor_tensor(out=y[:], in0=y[:], in1=sz[:], op=ALU.mult), r=[y, sz], w=[y])
                    P.op("act", lambda e: e.activation(out=sz[:], in_=y[:], func=AF.Square), r=[y], w=[sz])
                    P.op("dve", lambda e: e.reduce_sum(out=st[:, 0:1], in_=sz[:], axis=AX.X), r=[sz], w=[st])
                    P.op("dve", lambda e: e.tensor_scalar(out=st[:, 0:1], in0=st[:, 0:1], scalar1=1.0 / 512, scalar2=EPS,
                                                          op0=ALU.mult, op1=ALU.add), r=[st], w=[st])
                    P.op("act", lambda e: e.activation(out=st[:, 0:1], in_=st[:, 0:1], func=AF.Sqrt), r=[st], w=[st])
                    P.op("dve", lambda e: e.reciprocal(out=st[:, 1:2], in_=st[:, 0:1]), r=[st], w=[st])
                    P.op("dve", lambda e: e.scalar_tensor_tensor(out=y[:], in0=y[:], scalar=st[:, 1:2], in1=ngb[:], op0=ALU.mult,
                                                                 op1=ALU.mult), r=[y, st, ngb], w=[y])
                    P.dma("sp", ymix[u, l0:l0 + 128, :], y[:], r=[y], w=[ymix])
            if not last_state:
                P.op("pe", lambda e: e.matmul(PS[3][:, :], Btm[:, :], xdtt[:, :], start=True, stop=True), r=[Btm, xdtt], w=[PS[3]])
                P.op("dve", lambda e: e.tensor_tensor(out=v3(tS), in0=v3(S), in1=bc8(etot), op=ALU.mult), r=[S, etot], w=[tS])
                P.op("dve", lambda e: e.tensor_tensor(out=S[:], in0=tS[:], in1=PS[3][:, :], op=ALU.add), r=[tS, PS[3]], w=[S])
                P.op("act", lambda e: e.activation(out=Sb[:], in_=S[:], func=AF.Copy), r=[S], w=[Sb])


def build_mix0(LAT=16384, CTX=256, fft=None):
    P = Prog()
    IN = "ExternalInput"
    TB = CTX + LAT
    if fft is None:
        fft = (LAT == 16384)
    xT = P.dram("xT", [2, D, TB], F32, IN)
    modcol = P.dram("modcol", [3, 128, 32], F32, IN)
    whc = P.dram("whc", [D, 384], F32, IN)
    wgc = P.dram("wgc", [D, 288], F32, IN)
    wgt = P.dram("wgt", [D, 512], F32, IN)
    hcw = P.dram("hcw", [128, 12], F32, IN)
    hlb = P.dram("hlb", [128, 2], F32, IN)
    fw1 = P.dram("fw1", [33, 64], F32, IN)
    fcol = P.dram("fcol", [64, 4], F32, IN)
    fw2 = P.dram("fw2", [64, 64], F32, IN)
    fw3 = P.dram("fw3", [64, 512], F32, IN)
    zl = P.dram("zl", [33, LAT], F32, IN); zc = P.dram("zc", [33, CTX], F32, IN)
    El = P.dram("El", [128, LAT], F32, IN); Ec = P.dram("Ec", [128, CTX], F32, IN)
    gw2 = P.dram("gw2", [2, 16, 128], F32, IN)
    gbc = P.dram("gbc", [128, 2], F32, IN)
    gng = P.dram("gng", [256], F32, IN)
    consts = P.dram("consts", [4, 128, 512], F32, IN)
    hy_out = P.dram("hy_out", [2, 128, TB], F32, "ExternalOutput")
    gla_out = P.dram("gla_out", [TB, 256], F32, "ExternalOutput")
    hu_d = P.dram("hu_d", [2, 384, TB], F32)
    gq_d = P.dram("gq_d", [256, TB], F32)
    gg_d = P.dram("gg_d", [2, 16, TB], F32)
    gvr_d = P.dram("gvr_d", [TB, 512], F32)
    of_d = P.dram("of_d", [TB, 256], F32)
    hf_d = {CTX: P.dram("hf_c", [4, 128, CTX], F32)}
    if fft:
        zr = P.dram("zr", [33, LAT], F32, IN); Er = P.dram("Er", [128, LAT], F32, IN)
        fF1 = P.dram("fF1", [128, 256], F32, IN); fTW = P.dram("fTW", [128, 512], F32, IN); fCS = P.dram("fCS", [128, 1024], F32, IN)
        fNSC = P.dram("fNSC", [128, 1024], F32, IN); fTWI = P.dram("fTWI", [128, 512], F32, IN); fC1S = P.dram("fC1S", [128, 128], F32, IN)
        hfull_d = P.dram("hfull_d", [2, 128, 2 * LAT], F32)
        Hr_d = P.dram("Hr_d", [2, 2, 128, 128, 128], F32); Hi_d = P.dram("Hi_d", [2, 2, 128, 128, 128], F32)
        cur_d = [P.dram("cur0_d", [128, LAT], F32), P.dram("cur1_d", [128, LAT], F32)]
        conv_d = P.dram("conv_d", [128, LAT], F32)
    else:
        hf_d[LAT] = P.dram("hf_l", [4, 128, LAT], F32)
    sb = P.sb
    ident = sb([128, 128]); mF = sb([64, 64]); mB = sb([64, 64]); rmask = sb([128, 512])
    P.dma("sp", ident[:], consts[0, :, 0:128], r=[consts], w=[ident])
    P.dma("sp", mF[:], consts[1, 0:64, 0:64], r=[consts], w=[mF])
    P.dma("sp", mB[:], consts[2, 0:64, 0:64], r=[consts], w=[mB])
    P.dma("sp", rmask[:], consts[3, :, :], r=[consts], w=[rmask])
    mcol = [sb([128, 32]) for _ in range(3)]
    scp1 = [sb([128, 16]) for _ in range(3)]
    for s in range(3):
        P.dma("sp", mcol[s][:], modcol[s, :, :], r=[modcol], w=[mcol[s]])
        P.op("dve", lambda e, s=s: e.tensor_scalar(out=scp1[s][:], in0=mcol[s][:, 16:32], scalar1=1.0, scalar2=None,
                                                    op0=ALU.add), r=[mcol[s]], w=[scp1[s]])
    PS = [P.ps() for _ in range(8)]
    blocks = [(0, CTX)] + [(CTX + i * 512, 512) for i in range(LAT // 512)]

    with ExitStack() as stk:
        whs = P.sbs(stk, [128, 16, 384], BF16); wgs = P.sbs(stk, [128, 16, 288], BF16); wts = P.sbs(stk, [128, 16, 512], BF16)
        xin32 = P.sbs(stk, [128, 16, 512]); hT = P.sbs(stk, [128, 16, 512], BF16)
        stage = [P.sbs(stk, [128, 512]) for _ in range(3)]
        P.dma("pool", whs[:], whc.t.rearrange("(kc p) c -> p kc c", p=128), r=[whc], w=[whs])
        P.dma("pool", wgs[:], wgc.t.rearrange("(kc p) c -> p kc c", p=128), r=[wgc], w=[wgs])
        P.dma("pool", wts[:], wgt.t.rearrange("(kc p) c -> p kc c", p=128), r=[wgt], w=[wts])
        si = 0
        for bi in range(2):
            for (t0, nb) in blocks:
                s = 2 if t0 < CTX else bi
                P.dma("sp", xin32[:, :, :nb], xT.t[bi, :, t0:t0 + nb].rearrange("(kc p) t -> p kc t", p=128), r=[xT], w=[xin32])
                for kc in range(16):
                    P.op("act", lambda e, kc=kc, s=s: e.activation(out=hT[:, kc, :nb], in_=xin32[:, kc, :nb], func=AF.Identity,
                                                                   bias=mcol[s][:, kc:kc + 1], scale=scp1[s][:, kc:kc + 1]),
                         r=[xin32, mcol[s], scp1[s]], w=[hT])
                jobs = [(whs, m * 128, 128, hu_d, (bi, slice(m * 128, (m + 1) * 128))) for m in range(3)]
                if bi == 0:
                    jobs += [(wgs, m * 128, 128, gq_d, (slice(m * 128, (m + 1) * 128),)) for m in range(2)]
                    jobs += [(wgs, 256 + dd * 16, 16, gg_d, (dd, slice(0, 16))) for dd in range(2)]
                for ji, (wsb, c0, mw, dst, idx) in enumerate(jobs):
                    pp = PS[ji % 4]
                    for kc in range(16):
                        P.op("pe", lambda e, kc=kc, pp=pp, wsb=wsb, c0=c0, mw=mw: e.matmul(pp[:mw, :nb], wsb[:, kc, c0:c0 + mw], hT[:, kc, :nb],
                                                                                        start=(kc == 0), stop=(kc == 15)), r=[wsb, hT], w=[pp])
                    sg = stage[si % 3]; si += 1
                    P.op("act", lambda e, pp=pp, sg=sg, mw=mw: e.activation(out=sg[:mw, :nb], in_=pp[:mw, :nb], func=AF.Copy), r=[pp], w=[sg])
                    P.dma("sp", dst.t[idx + (slice(t0, t0 + nb),)], sg[:mw, :nb], r=[sg], w=[dst])
                if bi == 0:
                    for j0 in range(0, nb, 128):
                        pz = PS[4 + (j0 // 128) % 4]
                        for kc in range(16):
                            P.op("pe", lambda e, kc=kc, pz=pz, j0=j0: e.matmul(pz[:, :], hT[:, kc, j0:j0 + 128], wts[:, kc, :],
                                                                             start=(kc == 0), stop=(kc == 15)), r=[wts, hT], w=[pz])
                        sg = stage[si % 3]; si += 1
                        P.op("act", lambda e, pz=pz, sg=sg: e.activation(out=sg[:, :], in_=pz[:, :], func=AF.Copy), r=[pz], w=[sg])
                        P.dma("sp", gvr_d[t0 + j0:t0 + j0 + 128, :], sg[:, :], r=[sg], w=[gvr_d])
        P.barrier()

    with ExitStack() as stk:
        w1s = P.sbs(stk, [33, 64]); w2s = P.sbs(stk, [64, 64]); w3s = P.sbs(stk, [64, 512]); fc = P.sbs(stk, [64, 4])
        cws = P.sbs(stk, [128, 12]); lbs = P.sbs(stk, [128, 2])
        for dst, src in [(w1s, fw1), (w2s, fw2), (w3s, fw3), (fc, fcol), (cws, hcw), (lbs, hlb)]:
            P.dma("sp", dst[:], src[:, :], r=[src], w=[dst])
        zb = P.sbs(stk, [33, 512]); h1 = P.sbs(stk, [64, 512]); h2 = P.sbs(stk, [64, 512]); eb = P.sbs(stk, [128, 512])
        hst = [P.sbs(stk, [128, 512]) for _ in range(2)]
        rr = P.sbs(stk, [64, 512])
        si = 0
        gen = [(CTX, zc, Ec, [(od, hf_d[CTX], (od,), 0) for od in range(4)])]
        if fft:
            gen.append((LAT, zl, El, [(0, hfull_d, (0,), 0), (2, hfull_d, (1,), 0)]))
            gen.append((LAT, zr, Er, [(1, hfull_d, (0,), LAT), (3, hfull_d, (1,), LAT)]))
        else:
            gen.append((LAT, zl, El, [(od, hf_d[LAT], (od,), 0) for od in range(4)]))
        for (L, zsrc, Esrc, ods) in gen:
            for p0 in range(0, L, 512):
                nb = min(512, L - p0)
                P.dma("sp", zb[:, :nb], zsrc[:, p0:p0 + nb], r=[zsrc], w=[zb])
                P.dma("sp", eb[:, :nb], Esrc[:, p0:p0 + nb], r=[Esrc], w=[eb])
                for (wsb, kdim, src, dst, bcol, fcolm) in [(w1s, 33, zb, h1, 0, 1), (w2s, 64, h1, h2, 2, 3)]:
                    P.op("pe", lambda e, wsb=wsb, kdim=kdim, src=src: e.matmul(PS[0][:64, :nb], wsb[:kdim, :], src[:kdim, :nb], start=True, stop=True),
                         r=[wsb, src], w=[PS[0]])
                    P.op("dve", lambda e, dst=dst, bcol=bcol, fcolm=fcolm: e.tensor_scalar(out=dst[:, :nb], in0=PS[0][:64, :nb], scalar1=fc[:, bcol:bcol + 1],
                                                                                         scalar2=fc[:, fcolm:fcolm + 1], op0=ALU.add, op1=ALU.mult),
                         r=[PS[0], fc], w=[dst])
                    P.op("dve", lambda e, dst=dst: e.tensor_scalar(out=rr[:, :nb], in0=dst[:, :nb], scalar1=1.0 / (2 * math.pi), scalar2=12582912.0,
                                                                   op0=ALU.mult, op1=ALU.add), r=[dst], w=[rr])
                    P.op("dve", lambda e, dst=dst: e.tensor_scalar(out=rr[:, :nb], in0=rr[:, :nb], scalar1=12582912.0, scalar2=-2 * math.pi,
                                                                   op0=ALU.subtract, op1=ALU.mult), r=[rr], w=[rr])
                    P.op("dve", lambda e, dst=dst: e.tensor_tensor(out=dst[:, :nb], in0=dst[:, :nb], in1=rr[:, :nb], op=ALU.add), r=[dst, rr], w=[dst])
                    P.op("act", lambda e, dst=dst: e.activation(out=dst[:, :nb], in_=dst[:, :nb], func=AF.Sin), r=[dst], w=[dst])
                for (od, dbuf, didx, coff) in ods:
                    P.op("pe", lambda e, od=od: e.matmul(PS[1 + od % 2][:, :nb], w3s[:, od * 128:(od + 1) * 128], h2[:, :nb], start=True, stop=True),
                         r=[w3s, h2], w=[PS[1 + od % 2]])
                    sg = hst[si % 2]; si += 1
                    P.op("dve", lambda e, od=od, sg=sg: e.tensor_tensor(out=sg[:, :nb], in0=PS[1 + od % 2][:, :nb], in1=eb[:, :nb], op=ALU.mult),
                         r=[PS[1 + od % 2], eb], w=[sg])
                    P.dma("sp", dbuf.t[didx + (slice(None), slice(coff + p0, coff + p0 + nb))], sg[:, :nb], r=[sg], w=[dbuf])
        LD = CTX if fft else LAT
        LB = min(2048, LAT)
        u = P.sbs(stk, [128, LD]); acc = P.sbs(stk, [128, LD])
        raw = P.sbs(stk, [128, LB + 2]); xg = P.sbs(stk, [128, LB])
        hring = [P.sbs(stk, [128, min(LB, LD)]) for _ in range(2)]

        def short_conv_block(bi, ti, s0, L, b0, n, tgt, tb_):
            wv = lambda k: cws[:, ti * 4 + k:ti * 4 + k + 1]
            lo, hi = b0 - 1, b0 + n + 1
            if lo < 0:
                P.op("dve", lambda e: e.memset(raw[:, 0:1], 0.0), w=[raw])
            if hi > L:
                P.op("dve", lambda e: e.memset(raw[:, n + 1:n + 2], 0.0), w=[raw])
            a, b_ = max(lo, 0), min(hi, L)
            P.dma("sp", raw[:, a - lo:b_ - lo], hu_d[bi, ti * 128:(ti + 1) * 128, s0 + a:s0 + b_], r=[hu_d], w=[raw])
            P.op("dve", lambda e: e.tensor_scalar(out=tgt, in0=raw[:, 1:n + 1], scalar1=wv(1), scalar2=wv(3), op0=ALU.mult, op1=ALU.add),
                 r=[raw, cws], w=[tb_])
            P.op("dve", lambda e: e.scalar_tensor_tensor(out=tgt, in0=raw[:, 0:n], scalar=wv(0), in1=tgt, op0=ALU.mult, op1=ALU.add),
                 r=[raw, cws, tb_], w=[tb_])
            P.op("dve", lambda e: e.scalar_tensor_tensor(out=tgt, in0=raw[:, 2:n + 2], scalar=wv(2), in1=tgt, op0=ALU.mult, op1=ALU.add),
                 r=[raw, cws, tb_], w=[tb_])

        def short_conv(bi, ti, s0, L, dstbuf, mul_into=None):
            for b0 in range(0, L, LB):
                n = min(LB, L - b0)
                if mul_into is None:
                    short_conv_block(bi, ti, s0, L, b0, n, dstbuf[:, b0:b0 + n], dstbuf)
                else:
                    short_conv_block(bi, ti, s0, L, b0, n, xg[:, :n], xg)
                    P.op("dve", lambda e: e.tensor_tensor(out=mul_into[:, b0:b0 + n], in0=mul_into[:, b0:b0 + n], in1=xg[:, :n], op=ALU.mult),
                         r=[mul_into, xg], w=[mul_into])

        if fft:
            F1 = P.sbs(stk, [128, 256]); TW = P.sbs(stk, [128, 4, 128]); CS = P.sbs(stk, [128, 2, 512]); NSC = P.sbs(stk, [128, 2, 512])
            TWI = P.sbs(stk, [128, 2, 256]); C1S = P.sbs(stk, [128, 2, 64])
            for dst, src in [(F1, fF1), (TW, fTW), (CS, fCS), (NSC, fNSC), (TWI, fTWI), (C1S, fC1S)]:
                P.dma("sp", dst[:].rearrange("p a b -> p (a b)") if len(dst.t.shape) == 3 else dst[:], src[:, :], r=[src], w=[dst])
            X = P.sbs(stk, [128, 4, 256]); Apr = P.sbs(stk, [128, 2, 4, 128]); Api = P.sbs(stk, [128, 2, 4, 128])
            tm1 = P.sbs(stk, [128, 512]); tm2 = P.sbs(stk, [128, 512])
            Hr = P.sbs(stk, [128, 2, 4, 128]); Hi = P.sbs(stk, [128, 2, 4, 128])
            Yr = P.sbs(stk, [128, 2, 4, 128]); Yi = P.sbs(stk, [128, 2, 4, 128])
            Br = P.sbs(stk, [128, 4, 256]); Bi = P.sbs(stk, [128, 4, 256]); yt = P.sbs(stk, [64, 4, 256])
            cb = P.sbs(stk, [128, LB]); vb2 = P.sbs(stk, [128, LB])

            def cmul(outr, outi, ar, ai, br, bi_, tv, rb, wbr, wbi):
                t1, t2 = tv(tm1), tv(tm2)
                P.op("dve", lambda e: e.tensor_tensor(out=t1, in0=ar, in1=br, op=ALU.mult), r=rb, w=[tm1])
                P.op("dve", lambda e: e.tensor_tensor(out=t2, in0=ai, in1=bi_, op=ALU.mult), r=rb, w=[tm2])
                P.op("dve", lambda e: e.tensor_tensor(out=outr, in0=t1, in1=t2, op=ALU.subtract), r=[tm1, tm2], w=[wbr])
                P.op("dve", lambda e: e.tensor_tensor(out=t1, in0=ar, in1=bi_, op=ALU.mult), r=rb, w=[tm1])
                P.op("dve", lambda e: e.tensor_tensor(out=t2, in0=ai, in1=br, op=ALU.mult), r=rb, w=[tm2])
                P.op("dve", lambda e: e.tensor_tensor(out=outi, in0=t1, in1=t2, op=ALU.add), r=[tm1, tm2], w=[wbi])

            def fft_pass(src_d, src_ap, K, o, mode, dst_d=None):
                for gi in range(32):
                    ch0 = gi * 4
                    P.dma("sp", X[:K, :, :], src_ap[ch0:ch0 + 4, 0:K * 256].rearrange("c (n1 n2) -> n1 c n2", n2=256), r=[src_d], w=[X])
                    for c in range(4):
                        for h in range(2):
                            P.op("pe", lambda e, c=c, h=h: e.matmul(PS[c][:, h * 256:(h + 1) * 256], X[:K, c, h * 128:(h + 1) * 128], F1[:K, :],
                                                                    start=True, stop=True), r=[X, F1], w=[PS[c]])
                    tv3 = lambda t: t[:, 0:256].rearrange("p (h k) -> p h k", h=2)
                    for c in range(4):
                        bank = PS[c][:, :].rearrange("p (h r k) -> p h r k", h=2, r=2)
                        cmul(Apr[:, :, c, :], Api[:, :, c, :], bank[:, :, 0, :], bank[:, :, 1, :], TW[:, 0:2, :], TW[:, 2:4, :], tv3,
                             [PS[c], TW], Apr, Api)
                    fl = lambda ap: ap.rearrange("p c k -> p (c k)")
                    for q in range(2):
                        PR, PI = PS[4 + q * 2], PS[5 + q * 2]
                        seq_r = [(CS[:, h, q * 128:(q + 1) * 128], Apr[:, h, :, :]) for h in range(2)] + \
                                [(CS[:, h, 256 + q * 128:256 + (q + 1) * 128], Api[:, h, :, :]) for h in range(2)]
                        seq_i = [(CS[:, h, q * 128:(q + 1) * 128], Api[:, h, :, :]) for h in range(2)] + \
                                [(NSC[:, h, q * 128:(q + 1) * 128], Apr[:, h, :, :]) for h in range(2)]
                        for (pp, seq) in [(PR, seq_r), (PI, seq_i)]:
                            for k, (lt, rh) in enumerate(seq):
                                P.op("pe", lambda e, pp=pp, lt=lt, rh=rh, k=k: e.matmul(pp[:, :], lt, fl(rh), start=(k == 0), stop=(k == 3)),
                                     r=[CS, NSC, Apr, Api], w=[pp])
                    if mode == "filter":
                        for q in range(2):
                            P.op("act", lambda e, q=q: e.activation(out=fl(Yr[:, q, :, :]), in_=PS[4 + q * 2][:, :], func=AF.Copy), r=[PS[4 + q * 2]], w=[Yr])
                            P.op("act", lambda e, q=q: e.activation(out=fl(Yi[:, q, :, :]), in_=PS[5 + q * 2][:, :], func=AF.Copy), r=[PS[5 + q * 2]], w=[Yi])
                        P.dma("sp", Hr_d.t[o, :, :, ch0:ch0 + 4, :].rearrange("q p c k -> p q c k"), Yr[:, :, :, :], r=[Yr], w=[Hr_d])
                        P.dma("sp", Hi_d.t[o, :, :, ch0:ch0 + 4, :].rearrange("q p c k -> p q c k"), Yi[:, :, :, :], r=[Yi], w=[Hi_d])
                        continue
                    P.dma("sp", Hr[:, :, :, :], Hr_d.t[o, :, :, ch0:ch0 + 4, :].rearrange("q p c k -> p q c k"), r=[Hr_d], w=[Hr])
                    P.dma("sp", Hi[:, :, :, :], Hi_d.t[o, :, :, ch0:ch0 + 4, :].rearrange("q p c k -> p q c k"), r=[Hi_d], w=[Hi])
                    for q in range(2):
                        cmul(fl(Yr[:, q, :, :]), fl(Yi[:, q, :, :]), PS[4 + q * 2][:, :], PS[5 + q * 2][:, :], fl(Hr[:, q, :, :]), fl(Hi[:, q, :, :]),
                             lambda t: t[:, :], [PS[4 + q * 2], PS[5 + q * 2], Hr, Hi], Yr, Yi)
                    for c in range(4):
                        seq = []
                        for q in range(2):
                            seq += [(Yr[:, q, c, :], CS[:, q, :]), (Yi[:, q, c, :], NSC[:, q, :])]
                        for k, (lt, rh) in enumerate(seq):
                            P.op("pe", lambda e, c=c, lt=lt, rh=rh, k=k: e.matmul(PS[c][:, :], lt, rh, start=(k == 0), stop=(k == 3)),
                                 r=[Yr, Yi, CS, NSC], w=[PS[c]])
                    for c in range(4):
                        cmul(Br[:, c, :], Bi[:, c, :], PS[c][:, 0:256], PS[c][:, 256:512], TWI[:, 0, :], TWI[:, 1, :], lambda t: t[:, 0:256],
                             [PS[c], TWI], Br, Bi)
                    fl2 = lambda ap: ap.rearrange("p c m -> p (c m)")
                    for pr in range(2):
                        py = PS[4 + pr]
                        P.op("pe", lambda e, pr=pr, py=py: e.matmul(py[:64, :], C1S[:, 0, :], fl2(Br[:, 2 * pr:2 * pr + 2, :]), start=True, stop=False),
                             r=[C1S, Br], w=[py])
                        P.op("pe", lambda e, pr=pr, py=py: e.matmul(py[:64, :], C1S[:, 1, :], fl2(Bi[:, 2 * pr:2 * pr + 2, :]), start=False, stop=True),
                             r=[C1S, Bi], w=[py])
                        P.op("act", lambda e, pr=pr, py=py: e.activation(out=fl2(yt[:, 2 * pr:2 * pr + 2, :]), in_=py[:64, :], func=AF.Copy), r=[py], w=[yt])
                    P.dma("sp", dst_d.t[ch0:ch0 + 4, :].rearrange("c (n1 n2) -> n1 c n2", n2=256), yt[:, :, :], r=[yt], w=[dst_d])

            for o in range(2):
                fft_pass(hfull_d, hfull_d.t[o], 128, o, "filter")

        hi_ = 0
        for bi in range(2):
            for (s0, L) in [(0, CTX), (CTX, LAT)]:
                if fft and L == LAT:
                    for b0 in range(0, L, LB):
                        short_conv_block(bi, 0, s0, L, b0, LB, xg[:, :LB], xg)
                        P.dma("sp", cur_d[0][:, b0:b0 + LB], xg[:, :LB], r=[xg], w=[cur_d[0]])
                    for o in range(2):
                        fft_pass(cur_d[o % 2], cur_d[o % 2].t, 64, o, "conv", conv_d)
                        dst = cur_d[(o + 1) % 2]
                        for b0 in range(0, L, LB):
                            P.dma("sp", cb[:, :], cur_d[o % 2][:, b0:b0 + LB], r=[cur_d[o % 2]], w=[cb])
                            P.dma("sp", vb2[:, :], conv_d[:, b0:b0 + LB], r=[conv_d], w=[vb2])
                            P.op("dve", lambda e, o=o: e.scalar_tensor_tensor(out=cb[:, :], in0=cb[:, :], scalar=lbs[:, o:o + 1], in1=vb2[:, :],
                                                                             op0=ALU.mult, op1=ALU.add), r=[cb, lbs, vb2], w=[cb])
                            short_conv_block(bi, o + 1, s0, L, b0, LB, xg[:, :LB], xg)
                            P.op("dve", lambda e: e.tensor_tensor(out=cb[:, :], in0=cb[:, :], in1=xg[:, :LB], op=ALU.mult), r=[cb, xg], w=[cb])
                            if o == 0:
                                P.dma("sp", dst[:, b0:b0 + LB], cb[:, :], r=[cb], w=[dst])
                            else:
                                P.dma("sp", hy_out[bi, :, s0 + b0:s0 + b0 + LB], cb[:, :], r=[cb], w=[hy_out])
                    continue
                cur, nxt = u, acc
                short_conv(bi, 0, s0, L, cur)
                for o in range(2):
                    P.op("dve", lambda e, cur=cur, nxt=nxt, o=o: e.tensor_scalar(out=nxt[:, 0:L], in0=cur[:, 0:L], scalar1=lbs[:, o:o + 1], scalar2=None,
                                                                                op0=ALU.mult), r=[cur, lbs], w=[nxt])
                    for dr in range(2):
                        for l0 in range(0, L, LB):
                            n = min(LB, L - l0)
                            hb = hring[hi_ % 2]; hi_ += 1
                            P.dma("sp", hb[:, :n], hf_d[L][o * 2 + dr, :, l0:l0 + n], r=[hf_d[L]], w=[hb])
                            for m in range(l0, l0 + n):
                                if dr == 1 and m == 0:
                                    continue
                                if dr == 0:
                                    o_, i_ = nxt[:, m:L], cur[:, 0:L - m]
                                else:
                                    o_, i_ = nxt[:, 0:L - m], cur[:, m:L]
                                P.op("dve", lambda e, o_=o_, i_=i_, hb=hb, mm=m - l0: e.scalar_tensor_tensor(out=o_, in0=i_, scalar=hb[:, mm:mm + 1], in1=o_,
                                                                                                        op0=ALU.mult, op1=ALU.add),
                                     r=[cur, hb, nxt], w=[nxt])
                    short_conv(bi, o + 1, s0, L, None, mul_into=nxt)
                    cur, nxt = nxt, cur
                P.dma("sp", hy_out[bi, :, s0:s0 + L], cur[:, 0:L], r=[cur], w=[hy_out])
        P.barrier()

    w2s = [sb([16, 128]) for _ in range(2)]
    gb = sb([128, 2]); ngb = sb([64, 256])
    for dd in range(2):
        P.dma("sp", w2s[dd][:], gw2[dd, :, :], r=[gw2], w=[w2s[dd]])
    P.dma("sp", gb[:], gbc[:, :], r=[gbc], w=[gb])
    P.op("dve", lambda e: e.tensor_scalar(out=gb[:], in0=gb[:], scalar1=-1.0, scalar2=None, op0=ALU.mult), r=[gb], w=[gb])
    P.dma("sp", ngb[:], bass.AP(gng.t.tensor, gng.t.offset, [[0, 64], [1, 256]]), r=[gng], w=[ngb])
    g1 = sb([16, 512]); qk = sb([128, 2, 512]); g = sb([128, 512]); bb = sb([128, 512]); t5 = sb([128, 512])
    ebb = sb([128, 512]); qt = sb([128, 512], BF16); kt = sb([128, 512], BF16); ktl = sb([128, 512]); ebl = sb([128, 8])
    vr = sb([64, 512]); vb = sb([64, 256], BF16); attb = sb([64, 64], BF16); ktT = sb([64, 128], BF16)
    S = sb([128, 256]); Sb = sb([128, 256], BF16); o_ = sb([64, 256]); of = sb([64, 256]); sq = sb([64, 256]); st = sb([64, 2])
    QS = 128 ** -0.5
    for d in range(2):
        msk = mF if d == 0 else mB
        P.op("dve", lambda e: e.memset(S[:], 0.0), w=[S])
        P.op("dve", lambda e: e.memset(Sb[:], 0.0), w=[Sb])
        blks = [blocks[0]] + blocks[1:] if d == 0 else [blocks[0]] + blocks[1:][::-1]
        for (t0, nb) in blks:
            nch = nb // 64
            P.dma("sp", g1[:, :nb], gg_d[d, :, t0:t0 + nb], r=[gg_d], w=[g1])
            P.dma("sp", qk[:, :, :nb], gq_d.t[:, t0:t0 + nb].rearrange("(m p) t -> p m t", p=128), r=[gq_d], w=[qk])
            P.op("pe", lambda e: e.matmul(PS[0][:, :nb], w2s[d][:, :], g1[:, :nb], start=True, stop=True), r=[w2s[d], g1], w=[PS[0]])
            P.op("act", lambda e: e.activation(out=g[:, :nb], in_=PS[0][:, :nb], func=AF.Exp, bias=gb[:, d:d + 1], scale=-1.0), r=[PS[0], gb], w=[g])
            P.op("act", lambda e: e.activation(out=g[:, :nb], in_=g[:, :nb], func=AF.Ln, bias=1.0, scale=1.0), r=[g], w=[g])
            P.op("dve", lambda e: e.tensor_scalar(out=g[:, :nb], in0=g[:, :nb], scalar1=-1.0 / 16, scalar2=None, op0=ALU.mult), r=[g], w=[g])
            P.op("dve", lambda e: e.tensor_tensor_scan(out=bb[:, :nb], data0=rmask[:, :nb], data1=g[:, :nb], initial=0.0, op0=ALU.mult, op1=ALU.add),
                 r=[rmask, g], w=[bb])
            b3 = lambda t: t[:, :nb].rearrange("p (c q) -> p c q", q=64)
            if d == 1:
                P.op("dve", lambda e: e.tensor_tensor(out=t5[:, :nb], in0=g[:, :nb], in1=bb[:, :nb], op=ALU.subtract), r=[g, bb], w=[t5])
                P.op("dve", lambda e: e.tensor_tensor(out=b3(bb), in0=b3(t5), in1=b3(bb)[:, :, 63:64].to_broadcast([128, nch, 64]), op=ALU.add),
                     r=[t5, bb], w=[bb])
            lastcol = 63 if d == 0 else 0
            P.op("act", lambda e: e.activation(out=ebl[:, :nch], in_=b3(bb)[:, :, lastcol], func=AF.Exp), r=[bb], w=[ebl])
            P.op("dve", lambda e: e.tensor_tensor(out=b3(t5), in0=b3(bb)[:, :, lastcol:lastcol + 1].to_broadcast([128, nch, 64]), in1=b3(bb), op=ALU.subtract),
                 r=[bb], w=[t5])
            P.op("act", lambda e: e.activation(out=t5[:, :nb], in_=t5[:, :nb], func=AF.Exp), r=[t5], w=[t5])
            P.op("dve", lambda e: e.tensor_tensor(out=ktl[:, :nb], in0=qk[:, 1, :nb], in1=t5[:, :nb], op=ALU.mult), r=[qk, t5], w=[ktl])
            P.op("act", lambda e: e.activation(out=ebb[:, :nb], in_=bb[:, :nb], func=AF.Exp), r=[bb], w=[ebb])
            P.op("dve", lambda e: e.scalar_tensor_tensor(out=qt[:, :nb], in0=qk[:, 0, :nb], scalar=QS, in1=ebb[:, :nb], op0=ALU.mult, op1=ALU.mult),
                 r=[qk, ebb], w=[qt])
            P.op("act", lambda e: e.activation(out=ebb[:, :nb], in_=bb[:, :nb], func=AF.Exp, scale=-1.0), r=[bb], w=[ebb])
            P.op("dve", lambda e: e.tensor_tensor(out=kt[:, :nb], in0=qk[:, 1, :nb], in1=ebb[:, :nb], op=ALU.mult), r=[qk, ebb], w=[kt])
            chs = list(range(nch)) if d == 0 else list(range(nch))[::-1]
            for c in chs:
                c0 = c * 64
                p0 = t0 + c0
                P.dma("sp", vr[:], gvr_d[p0:p0 + 64, :], r=[gvr_d], w=[vr])
                P.op("act", lambda e: e.activation(out=vb[:], in_=vr[:, 0:256], func=AF.Copy), r=[vr], w=[vb])
                P.op("pe", lambda e: e.matmul(PS[1][:64, 0:64], kt[:, c0:c0 + 64], qt[:, c0:c0 + 64], start=True, stop=True), r=[kt, qt], w=[PS[1]])
                P.op("dve", lambda e: e.tensor_tensor(out=attb[:], in0=PS[1][:64, 0:64], in1=msk[:], op=ALU.mult), r=[PS[1], msk], w=[attb])
                P.op("pe", lambda e: e.matmul(PS[2][:64, 0:256], attb[:, :], vb[:, :], start=True, stop=False), r=[attb, vb], w=[PS[2]])
                P.op("pe", lambda e: e.matmul(PS[2][:64, 0:256], qt[:, c0:c0 + 64], Sb[:, :], start=False, stop=True), r=[qt, Sb], w=[PS[2]])
                if d == 0:
                    P.op("act", lambda e: e.activation(out=o_[:], in_=PS[2][:64, 0:256], func=AF.Copy), r=[PS[2]], w=[o_])
                    P.dma("sp", of_d[p0:p0 + 64, :], o_[:], r=[o_], w=[of_d])
                else:
                    P.dma("sp", of[:], of_d[p0:p0 + 64, :], r=[of_d], w=[of])
                    P.op("dve", lambda e: e.tensor_tensor(out=o_[:], in0=of[:], in1=PS[2][:64, 0:256], op=ALU.add), r=[of, PS[2]], w=[o_])
                    P.op("act", lambda e: e.activation(out=sq[:], in_=o_[:], func=AF.Square), r=[o_], w=[sq])
                    P.op("dve", lambda e: e.reduce_sum(out=st[:, 0:1], in_=sq[:], axis=AX.X), r=[sq], w=[st])
                    P.op("dve", lambda e: e.tensor_scalar(out=st[:, 0:1], in0=st[:, 0:1], scalar1=1.0 / 256, scalar2=EPS, op0=ALU.mult, op1=ALU.add),
                         r=[st], w=[st])
                    P.op("act", lambda e: e.activation(out=st[:, 0:1], in_=st[:, 0:1], func=AF.Sqrt), r=[st], w=[st])
                    P.op("dve", lambda e: e.reciprocal(out=st[:, 1:2], in_=st[:, 0:1]), r=[st], w=[st])
                    P.op("dve", lambda e: e.scalar_tensor_tensor(out=o_[:], in0=o_[:], scalar=st[:, 1:2], in1=ngb[:], op0=ALU.mult, op1=ALU.mult),
                         r=[o_, st, ngb], w=[o_])
                    P.op("act", lambda e: e.activation(out=sq[:], in_=vr[:, 256:512], func=AF.Silu), r=[vr], w=[sq])
                    P.op("dve", lambda e: e.tensor_tensor(out=o_[:], in0=o_[:], in1=sq[:], op=ALU.mult), r=[o_, sq], w=[o_])
                    P.dma("sp", gla_out[p0:p0 + 64, :], o_[:], r=[o_], w=[gla_out])
                P.op("pe", lambda e: e.transpose(PS[3][:64, 0:128], ktl[:, c0:c0 + 64], ident[:, :]), r=[ktl, ident], w=[PS[3]])
                P.op("act", lambda e: e.activation(out=ktT[:], in_=PS[3][:64, 0:128], func=AF.Copy), r=[PS[3]], w=[ktT])
                P.op("pe", lambda e: e.matmul(PS[4][:, 0:256], ktT[:, :], vb[:, :], start=True, stop=True), r=[ktT, vb], w=[PS[4]])
                P.op("dve", lambda e, c=c: e.scalar_tensor_tensor(out=S[:], in0=S[:], scalar=ebl[:, c:c + 1], in1=PS[4][:, 0:256], op0=ALU.mult, op1=ALU.add),
                     r=[S, ebl, PS[4]], w=[S])
                P.op("act", lambda e: e.activation(out=Sb[:], in_=S[:], func=AF.Copy), r=[S], w=[Sb])
    P.finish([hy_out, gla_out])
    return P


HY_MIN_DECAY = math.log(1e-2) / 1.5
HY_MAX_DECAY = math.log(1e-2) / 0.3


def _colpack(vecs):
    a = np.stack(vecs, 0).reshape(len(vecs), 16, 128)
    return np.ascontiguousarray(a.transpose(2, 0, 1).reshape(128, len(vecs) * 16)).astype(np.float32)


def _hy_tables(L):
    pos = np.arange(L, dtype=np.float64)[None, :]
    t = pos / max(L - 1, 1)
    bands = np.linspace(1e-4, 15, 16, dtype=np.float32).astype(np.float64)[:, None]
    ang = 2.0 * math.pi * bands * pos / L
    z = np.concatenate([t, np.cos(ang), -np.sin(ang)], 0).astype(np.float32)
    deltas = np.abs(np.linspace(HY_MIN_DECAY, HY_MAX_DECAY, 1024, dtype=np.float32)).astype(np.float64)
    E = np.exp(-t * deltas[:, None]).astype(np.float32)
    return z, E


def fft_consts():
    N = 32768
    p = np.arange(128, dtype=np.float64)[:, None]
    k128 = np.arange(128, dtype=np.float64)[None, :]
    a1 = 2 * np.pi * p * k128 / 128
    F1 = np.concatenate([np.cos(a1), -np.sin(a1)], 1)
    n2 = (np.arange(2)[None, :, None] * 128 + p[:, :, None])
    at = 2 * np.pi * n2 * np.arange(128)[None, None, :] / N
    TW = np.concatenate([np.cos(at), -np.sin(at)], 1).reshape(128, 512)
    a2 = 2 * np.pi * n2 * np.arange(256)[None, None, :] / 256
    CS = np.concatenate([np.cos(a2), np.sin(a2)], 2).reshape(128, 1024)
    NSC = np.concatenate([-np.sin(a2), np.cos(a2)], 2).reshape(128, 1024)
    ai = 2 * np.pi * p * np.arange(256, dtype=np.float64)[None, :] / N
    TWI = np.concatenate([np.cos(ai), np.sin(ai)], 1)
    a3 = 2 * np.pi * p * np.arange(64, dtype=np.float64)[None, :] / 128
    C1S = np.concatenate([np.cos(a3), -np.sin(a3)], 1) / N
    f = lambda a: np.ascontiguousarray(a, dtype=np.float32)
    return dict(fF1=f(F1), fTW=f(TW), fCS=f(CS), fNSC=f(NSC), fTWI=f(TWI), fC1S=f(C1S))


def _hy_tables_rev(L):
    pos = (L - np.arange(L, dtype=np.float64))[None, :]
    t = pos / max(L - 1, 1)
    bands = np.linspace(1e-4, 15, 16, dtype=np.float32).astype(np.float64)[:, None]
    ang = 2.0 * math.pi * bands * pos / L
    z = np.concatenate([t, np.cos(ang), -np.sin(ang)], 0).astype(np.float32)
    deltas = np.abs(np.linspace(HY_MIN_DECAY, HY_MAX_DECAY, 1024, dtype=np.float32)).astype(np.float64)
    E = np.exp(-t * deltas[:, None]).astype(np.float32)
    E[:, 0] = 0.0
    return z, E


def mix0_consts():
    c = np.zeros((4, 128, 512), np.float32)
    k = np.arange(128)[:, None]; i = np.arange(128)[None, :]
    c[0, :, :128] = np.eye(128); c[1, :, :128] = (k <= i); c[2, :, :128] = (k >= i)
    c[3] = 1.0; c[3, :, ::64] = 0.0
    return c


def mix0_core_inputs(ci, xTb, modl, modc, W, tabs, rev=None):
    b, head, ct = ci // 4, ci % 4, ci
    w_in = W["ab_w_in"]
    sl = lambda s, n: w_in[:, s:s + n]
    (zc, Ec), (zl, El) = tabs
    w3 = W["hy_f_w3"]
    extra = {}
    if rev is not None:
        zr, Er = rev
        extra = dict(zr=zr, Er=np.ascontiguousarray(Er[ct * 128:(ct + 1) * 128]), **fft_consts())
    return dict(
        **extra,
        xT=np.stack([xTb[b], xTb[1 - b]]),
        modcol=np.stack([_colpack([modl[b][0], modl[b][1]]), _colpack([modl[1 - b][0], modl[1 - b][1]]), _colpack([modc[0], modc[1]])]),
        whc=np.ascontiguousarray(np.concatenate([sl(ct * 128, 128), sl(1024 + ct * 128, 128), sl(2048 + ct * 128, 128)], 1)),
        wgc=np.ascontiguousarray(np.concatenate([sl(3072 + head * 128, 128), sl(3584 + head * 128, 128), sl(6144, 32)], 1)),
        wgt=np.ascontiguousarray(np.concatenate([sl(4096 + head * 256, 256), sl(5120 + head * 256, 256)], 1)),
        hcw=np.ascontiguousarray(np.stack([np.concatenate([W["hy_conv_w"][:, ti * 1024 + ct * 128:ti * 1024 + (ct + 1) * 128],
                                                           W["hy_conv_b"][None, ti * 1024 + ct * 128:ti * 1024 + (ct + 1) * 128]], 0).T
                                           for ti in range(3)], 1).reshape(128, 12)),
        hlb=np.ascontiguousarray(W["hy_long_bias"][:, ct * 128:(ct + 1) * 128].T),
        fw1=W["hy_f_w1"], fcol=np.ascontiguousarray(np.stack([W["hy_f_b1"], W["hy_f_fr1"], W["hy_f_b2"], W["hy_f_fr2"]], 1)),
        fw2=W["hy_f_w2"],
        fw3=np.ascontiguousarray(np.concatenate([w3[:, o * 2048 + dr * 1024 + ct * 128:o * 2048 + dr * 1024 + (ct + 1) * 128]
                                                 for o in range(2) for dr in range(2)], 1)),
        zl=zl, zc=zc, El=np.ascontiguousarray(El[ct * 128:(ct + 1) * 128]), Ec=np.ascontiguousarray(Ec[ct * 128:(ct + 1) * 128]),
        gw2=np.ascontiguousarray(W["gla_gate_w2"][:, :, head * 128:(head + 1) * 128]),
        gbc=np.ascontiguousarray(W["gla_gate_b"][:, head * 128:(head + 1) * 128].T),
        gng=np.ascontiguousarray(W["gla_norm_g"][head * 256:(head + 1) * 256]),
        consts=mix0_consts(),
    )


def ssd_unit_inputs(g, w_in, conv_w, conv_b, dt_bias, a_log, d_skip, norm_g):
    xs = 4096 + g * 512; bs = 4096 + 4096 + g * 128; cs = 4096 + 4096 + 1024 + g * 128
    wch = np.concatenate([w_in[:, xs:xs + 512], w_in[:, bs:bs + 128], w_in[:, cs:cs + 128]], 1)
    wtm = np.concatenate([w_in[:, g * 512:(g + 1) * 512], w_in[:, 10240 + g * 8:10240 + g * 8 + 8], w_in[:, 10304 + g * 8:10304 + g * 8 + 8]], 1)
    cidx = np.concatenate([np.arange(g * 512, (g + 1) * 512), 4096 + g * 128 + np.arange(128), 4096 + 1024 + g * 128 + np.arange(128)])
    cwf = np.concatenate([conv_w.reshape(9, 6144)[:, cidx], conv_b[None, cidx]], 0)
    cw = np.ascontiguousarray(cwf.reshape(10, 6, 128).transpose(2, 1, 0).reshape(128, 60))
    hp = np.concatenate([dt_bias[:, g * 8:(g + 1) * 8].reshape(-1), a_log[:, g * 8:(g + 1) * 8].reshape(-1), d_skip[:, g * 8:(g + 1) * 8].reshape(-1)])
    return wch, wtm, cw, hp.astype(np.float32), norm_g[g * 512:(g + 1) * 512]


def ssd_consts():
    k = np.arange(128)[:, None]; i = np.arange(128)[None, :]
    return np.stack([np.eye(128), np.ones((128, 128)), k <= i, k >= i, k > i, k < i]).astype(np.float32)


def _run(P, in_maps):
    in_maps = [{k: np.ascontiguousarray(v, dtype=np.float32) for k, v in m.items()} for m in in_maps]
    res = run_bass_kernel_spmd(P.nc, in_maps, core_ids=list(range(len(in_maps))))
    return res.results


def _post_launch(KC, T_l, T_c, yT_list, xres_list, modl, modc, w_out, lng, lnb, rw, rb, wg, wu, wd):
    ident = np.eye(128, dtype=np.float32)
    sel = np.zeros((16, 16, 128), np.float32)
    for e in range(16):
        sel[e, e, :] = 1
    sel = sel.reshape(16, 2048)
    P = build_post(T_l, T_c, KC)
    ins = []
    for ci in range(8):
        b = ci // 4
        ins.append(dict(yT=yT_list[ci], xres=xres_list[ci], w_out=w_out,
                        modrow=np.stack([modl[b], modc]), modcol=np.stack([_colpack(list(modl[b])), _colpack(list(modc))]),
                        lng=lng, lnb=lnb, rw=rw, rb=rb, wg=wg, wu=wu, wd=wd, ident=ident, sel=sel))
    return [r["xout"] for r in _run(P, ins)]


def kernel(**I):
    I = {k: np.asarray(v, dtype=np.float32) for k, v in I.items()}
    x, c, ctx, c_ctx = I["x"], I["c"], I["ctx"], I["c_ctx"]
    LAT, CTX = x.shape[1], ctx.shape[1]
    TB = LAT + CTX
    TPC = LAT // 4
    CPC = CTX // 4
    c_all = np.concatenate([c, c_ctx[None]], 0)
    cT = np.ascontiguousarray(c_all.reshape(3, 16, 128).transpose(2, 1, 0).reshape(128, 48))
    res = _run(build_mod(), [dict(cT=cT, w=I["mod_w"][:, :, i * 1536:(i + 1) * 1536], b=I["mod_b"][:, i * 1536:(i + 1) * 1536]) for i in range(8)])
    mod = np.concatenate([r["out"] for r in res], -1)
    modl = [[mod[l, b].reshape(6, D) for b in range(2)] for l in range(2)]
    modc = [mod[l, 2].reshape(6, D) for l in range(2)]
    xTb = [np.ascontiguousarray(np.concatenate([ctx[b], x[b]], 0).T) for b in range(2)]
    W0 = {k: I[k][0] for k in ["ab_w_in", "hy_conv_w", "hy_conv_b", "hy_f_w1", "hy_f_b1", "hy_f_fr1", "hy_f_w2", "hy_f_b2", "hy_f_fr2",
                               "hy_f_w3", "hy_long_bias", "gla_gate_w2", "gla_gate_b", "gla_norm_g"]}
    tabs = (_hy_tables(CTX), _hy_tables(LAT))
    use_fft = (LAT == 16384)
    rev = _hy_tables_rev(LAT) if use_fft else None
    res = _run(build_mix0(LAT, CTX, use_fft), [mix0_core_inputs(ci, xTb, modl[0], modc[0], W0, tabs, rev) for ci in range(8)])
    yT0 = [np.zeros((D, TB), np.float32) for _ in range(2)]
    for ci in range(8):
        b, head, ct = ci // 4, ci % 4, ci
        yT0[b][ct * 128:(ct + 1) * 128] = res[ci]["hy_out"][0]
        yT0[1 - b][ct * 128:(ct + 1) * 128] = res[ci]["hy_out"][1]
        yT0[b][1024 + head * 256:1024 + (head + 1) * 256] = res[ci]["gla_out"].T
    del xTb, res
    yl, xl = [], []
    for ci in range(8):
        b, j = ci // 4, ci % 4
        yl.append(np.concatenate([yT0[b][:, CTX + j * TPC:CTX + (j + 1) * TPC], yT0[b][:, j * CPC:(j + 1) * CPC]], 1))
        xl.append(np.concatenate([x[b, j * TPC:(j + 1) * TPC], ctx[b, j * CPC:(j + 1) * CPC]], 0))
    outs = _post_launch(16, TPC, CPC, yl, xl, modl[0], modc[0], I["ab_w_out"][0], I["ln_g"][0], I["ln_b"][0], I["router_w"], I["router_b"],
                        I["exp_w_gate"][0], I["exp_w_up"][0], I["exp_w_down"][0])
    x1 = np.zeros_like(x); ctx1 = np.zeros_like(ctx)
    for ci in range(8):
        b, j = ci // 4, ci % 4
        x1[b, j * TPC:(j + 1) * TPC] = outs[ci][:TPC]
        ctx1[b, j * CPC:(j + 1) * CPC] = outs[ci][TPC:]
    del yT0, yl, xl, outs
    xT1 = [np.ascontiguousarray(np.concatenate([ctx1[b], x1[b]], 0).T) for b in range(2)]
    cst = ssd_consts()
    ins = []
    for ci in range(8):
        b, gp = ci // 4, ci % 4
        us = [ssd_unit_inputs(2 * gp + u, I["ssd_w_in"][0], I["ssd_conv_w"][0], I["ssd_conv_b"][0], I["ssd_dt_bias"][0], I["ssd_a_log"][0],
                              I["ssd_d"][0], I["ssd_norm_g"][0]) for u in range(2)]
        ins.append(dict(xT=xT1[b], modcol=np.stack([_colpack([modl[1][b][0], modl[1][b][1]]), _colpack([modc[1][0], modc[1][1]])]),
                        wch=np.stack([u_[0] for u_ in us]), wtm=np.stack([u_[1] for u_ in us]), cw=np.stack([u_[2] for u_ in us]),
                        hp=np.stack([u_[3] for u_ in us]), ng=np.stack([u_[4] for u_ in us]), consts=cst))
    res = _run(build_ssd(LAT, CTX, 2), ins)
    yT1 = [np.zeros((4096, LAT), np.float32) for _ in range(2)]
    for ci in range(8):
        b, gp = ci // 4, ci % 4
        for u in range(2):
            g = 2 * gp + u
            yT1[b][g * 512:(g + 1) * 512] = res[ci]["ymix"][u].T
    del xT1, ins, res
    yl, xl = [], []
    for ci in range(8):
        b, j = ci // 4, ci % 4
        yl.append(yT1[b][:, j * TPC:(j + 1) * TPC])
        xl.append(x1[b, j * TPC:(j + 1) * TPC])
    outs = _post_launch(32, TPC, 0, yl, xl, modl[1], modc[1], I["ssd_w_out"][0], I["ln_g"][1], I["ln_b"][1], I["router_w"], I["router_b"],
                        I["exp_w_gate"][1], I["exp_w_up"][1], I["exp_w_down"][1])
    out = np.zeros_like(x)
    for ci in range(8):
        b, j = ci // 4, ci % 4
        out[b, j * TPC:(j + 1) * TPC] = outs[ci]
    return out
```

```python
import math
from contextlib import ExitStack
import numpy as np
import concourse.bass as bass
import concourse.mybir as mybir
from concourse.bass_utils import run_bass_kernel_spmd

F32 = mybir.dt.float32
BF16 = mybir.dt.bfloat16
AF = mybir.ActivationFunctionType
ALU = mybir.AluOpType
AX = mybir.AxisListType

D = 2048
NE = 16
DEXP = 1024
ALPHA = (2 * 2) ** 0.25
EPS = 1e-6
SEM_LIMIT = 30000


class Buf:
    def __init__(self, t=None, name=""):
        self.t = t
        self.name = name
        self.w = None
        self.r = []

    def __getitem__(self, idx):
        return self.t[idx]


class Prog:
    def __init__(self):
        self.nc = bass.Bass("TRN2", target_bir_lowering=False)
        nc = self.nc
        self.E = {"pe": nc.tensor, "dve": nc.vector, "act": nc.scalar, "pool": nc.gpsimd, "sp": nc.sync}
        self.nsem = 0
        self.sem = {}
        self.cnt = {}
        for e in self.E:
            self._new_eng_sem(e)
        self.waited = {e: {} for e in self.E}
        self.dsem = []
        self.dcnt = []
        for i in range(12):
            self.dsem.append(self._alloc_sem())
            self.dcnt.append(0)
        self.drr = 0
        self.ninstr = 0
        self.uid = 0

    def _alloc_sem(self):
        self.nsem += 1
        return (self.nsem, self.nc.alloc_semaphore(f"s{self.nsem}"))

    def _new_eng_sem(self, e):
        self.sem[e] = self._alloc_sem()
        self.cnt[e] = 0

    def sb(self, shape, dt=F32, name=None):
        self.uid += 1
        return Buf(self.nc.alloc_sbuf_tensor(name or f"sb{self.uid}", list(shape), dt), name or f"sb{self.uid}")

    def ps(self, name=None):
        self.uid += 1
        return Buf(self.nc.alloc_psum_tensor(name or f"ps{self.uid}", [128, 512], F32), name or f"ps{self.uid}")

    def dram(self, name, shape, dt=F32, kind="Internal"):
        return Buf(self.nc.dram_tensor(name, list(shape), dt, kind=kind).ap(), name)

    def _wait(self, eng, tickets):
        wd = self.waited[eng]
        need = {}
        for tk in tickets:
            if tk is None:
                continue
            (sid, sh), v = tk
            if wd.get(sid, 0) >= v:
                continue
            if sid not in need or need[sid][1] < v:
                need[sid] = (sh, v)
        for sid, (sh, v) in need.items():
            self.E[eng].wait_ge(sh, v)
            wd[sid] = v
            self.ninstr += 1

    def _deps(self, r, w):
        t = []
        for b in r:
            t.append(b.w)
        for b in w:
            t.append(b.w)
            t.extend(b.r)
        return t

    def _commit(self, tk, r, w):
        for b in r:
            b.r.append(tk)
            if len(b.r) > 24:
                b.r = b.r[-24:] if False else b.r
        for b in w:
            b.w = tk
            b.r = []

    def op(self, eng, fn, r=(), w=()):
        self._wait(eng, self._deps(r, w))
        if self.cnt[eng] >= SEM_LIMIT:
            self._new_eng_sem(eng)
        ins = fn(self.E[eng])
        self.cnt[eng] += 1
        s = self.sem[eng]
        ins.then_inc(s[1], 1)
        tk = (s, self.cnt[eng])
        self._commit(tk, r, w)
        self.ninstr += 1
        return tk

    def dma(self, q, out, in_, r=(), w=(), **kw):
        i = self.drr
        self.drr = (self.drr + 1) % len(self.dsem)
        s = self.dsem[i]
        self._wait(q, self._deps(r, w) + [(s, self.dcnt[i])] if self.dcnt[i] else self._deps(r, w))
        if self.dcnt[i] >= SEM_LIMIT * 16:
            self.dsem[i] = self._alloc_sem()
            self.dcnt[i] = 0
            s = self.dsem[i]
        ins = self.E[q].dma_start(out=out, in_=in_, **kw)
        self.dcnt[i] += 16
        ins.then_inc(s[1], 16)
        tk = (s, self.dcnt[i])
        self._commit(tk, r, w)
        self.ninstr += 1
        return tk

    def barrier(self):
        tks = [(self.sem[e], self.cnt[e]) for e in self.E if self.cnt[e]]
        tks += [(self.dsem[i], self.dcnt[i]) for i in range(len(self.dsem)) if self.dcnt[i]]
        for e in self.E:
            self._wait(e, tks)

    def sbs(self, stack, shape, dt=F32):
        self.uid += 1
        return Buf(stack.enter_context(self.nc.sbuf_tensor(f"sc{self.uid}", list(shape), dt)), f"sc{self.uid}")

    def finish(self, bufs):
        self._wait("sp", [b.w for b in bufs])


def make_ident(P, dt=F32):
    ident = P.sb([128, 128], dt, "ident_" + str(dt))
    P.op("pool", lambda e: e.memset(ident[:], 0.0), w=[ident])
    P.op("pool", lambda e: e.affine_select(out=ident[:], in_=ident[:], pattern=[[-1, 128]],
                                           compare_op=ALU.not_equal, fill=1.0, base=0, channel_multiplier=1),
         r=[ident], w=[ident])
    return ident


def bc_ap(ap1d, n):
    return bass.AP(ap1d.tensor, ap1d.offset, [[0, 128], [1, n]])


def layer_norm_tile(P, n, pre, tmp, gbc, bbc, st):
    P.op("dve", lambda e: e.reduce_sum(out=st[:n, 0:1], in_=pre[:n, :], axis=AX.X), r=[pre], w=[st])
    P.op("dve", lambda e: e.tensor_scalar(out=st[:n, 1:2], in0=st[:n, 0:1], scalar1=-1.0 / D, scalar2=None,
                                          op0=ALU.mult), r=[st], w=[st])
    P.op("act", lambda e: e.activation(out=tmp[:n, :], in_=pre[:n, :], func=AF.Square, bias=st[:n, 1:2], scale=1.0),
         r=[pre, st], w=[tmp])
    P.op("dve", lambda e: e.reduce_sum(out=st[:n, 2:3], in_=tmp[:n, :], axis=AX.X), r=[tmp], w=[st])
    P.op("dve", lambda e: e.tensor_scalar(out=st[:n, 2:3], in0=st[:n, 2:3], scalar1=1.0 / D, scalar2=EPS,
                                          op0=ALU.mult, op1=ALU.add), r=[st], w=[st])
    P.op("act", lambda e: e.activation(out=st[:n, 2:3], in_=st[:n, 2:3], func=AF.Sqrt), r=[st], w=[st])
    P.op("dve", lambda e: e.reciprocal(out=st[:n, 3:4], in_=st[:n, 2:3]), r=[st], w=[st])
    P.op("dve", lambda e: e.tensor_scalar(out=pre[:n, :], in0=pre[:n, :], scalar1=st[:n, 1:2], scalar2=st[:n, 3:4],
                                          op0=ALU.add, op1=ALU.mult), r=[pre, st], w=[pre])
    P.op("dve", lambda e: e.tensor_tensor(out=pre[:n, :], in0=pre[:n, :], in1=gbc[:n, :], op=ALU.mult),
         r=[pre, gbc], w=[pre])
    P.op("dve", lambda e: e.tensor_tensor(out=pre[:n, :], in0=pre[:n, :], in1=bbc[:n, :], op=ALU.add),
         r=[pre, bbc], w=[pre])


def build_post(T_l, T_c, KC, NEXP=NE):
    P = Prog()
    T = T_l + T_c
    IN = "ExternalInput"
    yT = P.dram("yT", [KC * 128, T], F32, IN)
    xres = P.dram("xres", [T, D], F32, IN)
    w_out = P.dram("w_out", [KC * 128, D], F32, IN)
    modrow = P.dram("modrow", [2, 6, D], F32, IN)
    modcol = P.dram("modcol", [2, 128, 6 * 16], F32, IN)
    lng = P.dram("lng", [2, D], F32, IN)
    lnb = P.dram("lnb", [2, D], F32, IN)
    rw = P.dram("rw", [D, NE], F32, IN)
    rb = P.dram("rb", [NE], F32, IN)
    wg = P.dram("wg", [NEXP, D, DEXP], F32, IN)
    wu = P.dram("wu", [NEXP, D, DEXP], F32, IN)
    wd = P.dram("wd", [NEXP, DEXP, D], F32, IN)
    ident_d = P.dram("ident", [128, 128], F32, IN)
    sel_d = P.dram("sel", [16, 16 * 128], F32, IN)
    xout = P.dram("xout", [T, D], F32, "ExternalOutput")
    x1_d = P.dram("x1_d", [T, D], F32)
    tokT_d = P.dram("tokT_d", [D, T], F32)

    sb, ps = P.sb, P.ps
    ident = sb([128, 128]); sel = sb([16, 16 * 128])
    rws = sb([128, 16, NE]); rbb = sb([128, NE])
    mcol = [sb([128, 96]) for _ in range(2)]
    sc2p1 = [sb([128, 16]) for _ in range(2)]
    bcB = sb([128, D]); bcC = sb([128, D])
    xt = sb([128, D]); tmp = sb([128, D]); st = sb([128, 4])
    PS = [ps() for _ in range(8)]
    sm = {k: sb([128, 16]) for k in ["sc", "sel", "eq", "s2", "t2", "msk", "gs"]}
    sm4 = {k: sb([128, 4]) for k in ["m1", "m2", "gs", "gm"]}
    sm1 = {k: sb([128, 1]) for k in ["gmax", "den"]}
    stk1 = ExitStack()
    bcA = P.sbs(stk1, [128, D])
    ytile = P.sbs(stk1, [128, KC, 128], BF16)
    wring = [P.sbs(stk1, [128, D], BF16) for _ in range(4)]
    tokT32 = P.sbs(stk1, [128, 16, 128])
    gtt = P.sbs(stk1, [16, 128])
    gt_d = P.dram("gt_d", [16, T], F32)
    wob_d = P.dram("wob_d", [KC, 128, D], BF16)
    for kc in range(KC):
        P.dma("pool", wob_d[kc, :, :], w_out[kc * 128:(kc + 1) * 128, :], r=[w_out], w=[wob_d])

    def ld(q, dst, src, wbuf, rbuf=()):
        P.dma(q, dst, src, r=list(rbuf), w=[wbuf])

    ld("sp", ident[:], ident_d[:, :], ident, [ident_d])
    ld("sp", sel[:], sel_d[:, :], sel, [sel_d])
    ld("sp", rws[:], rw.t.rearrange("(kc p) e -> p kc e", p=128), rws, [rw])
    ld("sp", rbb[:], bc_ap(rb.t, NE), rbb, [rb])
    for s in range(2):
        ld("sp", mcol[s][:], modcol[s, :, :], mcol[s], [modcol])
        P.op("dve", lambda e, s=s: e.tensor_scalar(out=sc2p1[s][:], in0=mcol[s][:, 64:80], scalar1=1.0, scalar2=None,
                                                    op0=ALU.add), r=[mcol[s]], w=[sc2p1[s]])
    ld("sp", bcB[:], bc_ap(lng[0, :], D), bcB, [lng])
    ld("sp", bcC[:], bc_ap(lnb[0, :], D), bcC, [lnb])

    tiles = [(t0, 128, 0) for t0 in range(0, T_l, 128)]
    if T_c:
        tiles.append((T_l, T_c, 1))
    cur_set = [-1]

    for ti, (t0, n, s) in enumerate(tiles):
        if cur_set[0] != s:
            ld("sp", bcA[:], bc_ap(modrow[s, 2, :], D), bcA, [modrow])
            cur_set[0] = s
        ld("pool", ytile[:, :, :n], yT.t[:, t0:t0 + n].rearrange("(kc p) t -> p kc t", p=128), ytile, [yT])
        ld("sp", xt[:n, :], xres[t0:t0 + n, :], xt, [xres])
        for kc in range(KC):
            ws = wring[kc % 4]
            ld("sp" if kc % 2 == 0 else "act", ws[:], wob_d[kc, :, :], ws, [wob_d])
            for j in range(4):
                P.op("pe", lambda e, j=j, kc=kc, ws=ws: e.matmul(PS[j][:n, :], ytile[:, kc, :n], ws[:, j * 512:(j + 1) * 512],
                                                                start=(kc == 0), stop=(kc == KC - 1)),
                     r=[ytile, ws], w=[PS[j]])
        for j in range(4):
            P.op("dve", lambda e, j=j: e.tensor_tensor(out=tmp[:n, j * 512:(j + 1) * 512], in0=PS[j][:n, :],
                                                      in1=bcA[:n, j * 512:(j + 1) * 512], op=ALU.mult),
                 r=[PS[j], bcA], w=[tmp])
        P.op("dve", lambda e: e.scalar_tensor_tensor(out=xt[:n, :], in0=xt[:n, :], scalar=ALPHA, in1=tmp[:n, :],
                                                     op0=ALU.mult, op1=ALU.add), r=[xt, tmp], w=[xt])
        layer_norm_tile(P, n, xt, tmp, bcB, bcC, st)
        ld("sp", x1_d[t0:t0 + n, :], xt[:n, :], x1_d, [xt])
        for kc in range(16):
            P.op("pe", lambda e, kc=kc: e.transpose(PS[4 + kc // 4][:, (kc % 4) * 128:(kc % 4) * 128 + n],
                                                    xt[:n, kc * 128:(kc + 1) * 128], ident[:n, :n]),
                 r=[xt, ident], w=[PS[4 + kc // 4]])
        for kc in range(16):
            P.op("act", lambda e, kc=kc: e.activation(out=tokT32[:, kc, :n],
                                                      in_=PS[4 + kc // 4][:, (kc % 4) * 128:(kc % 4) * 128 + n],
                                                      func=AF.Identity, bias=mcol[s][:, 48 + kc:49 + kc],
                                                      scale=sc2p1[s][:, kc:kc + 1]),
                 r=[PS[4 + kc // 4], mcol[s], sc2p1[s]], w=[tokT32])
        ld("sp", tokT_d.t[:, t0:t0 + n].rearrange("(kc p) t -> p kc t", p=128), tokT32[:, :, :n], tokT_d, [tokT32])
        for kc in range(16):
            P.op("pe", lambda e, kc=kc: e.matmul(PS[0][:n, 0:NE], tokT32[:, kc, :n], rws[:, kc, :],
                                                 start=(kc == 0), stop=(kc == 15)), r=[tokT32, rws], w=[PS[0]])
        sc, sl, eq, s2, t2, msk, gsel = (sm[k] for k in ["sc", "sel", "eq", "s2", "t2", "msk", "gs"])
        m1, m2, gs, gm = (sm4[k] for k in ["m1", "m2", "gs", "gm"])
        gmax, den = sm1["gmax"], sm1["den"]
        v3 = lambda b: b[:n, :].rearrange("p (g e) -> p g e", g=4)
        b4 = lambda b: b[:n, :].unsqueeze(2).to_broadcast([n, 4, 4])
        P.op("act", lambda e: e.activation(out=sc[:n, :], in_=PS[0][:n, 0:NE], func=AF.Sigmoid), r=[PS[0]], w=[sc])
        P.op("dve", lambda e: e.tensor_tensor(out=sl[:n, :], in0=sc[:n, :], in1=rbb[:n, :], op=ALU.add), r=[sc, rbb], w=[sl])
        P.op("dve", lambda e: e.tensor_reduce(out=m1[:n, :], in_=v3(sl), axis=AX.X, op=ALU.max), r=[sl], w=[m1])
        P.op("dve", lambda e: e.tensor_tensor(out=v3(eq), in0=v3(sl), in1=b4(m1), op=ALU.is_equal), r=[sl, m1], w=[eq])
        P.op("dve", lambda e: e.scalar_tensor_tensor(out=s2[:n, :], in0=eq[:n, :], scalar=-1e9, in1=sl[:n, :],
                                                     op0=ALU.mult, op1=ALU.add), r=[eq, sl], w=[s2])
        P.op("dve", lambda e: e.tensor_reduce(out=m2[:n, :], in_=v3(s2), axis=AX.X, op=ALU.max), r=[s2], w=[m2])
        P.op("dve", lambda e: e.tensor_tensor(out=gs[:n, :], in0=m1[:n, :], in1=m2[:n, :], op=ALU.add), r=[m1, m2], w=[gs])
        P.op("dve", lambda e: e.tensor_reduce(out=gmax[:n, :], in_=gs[:n, :], axis=AX.X, op=ALU.max), r=[gs], w=[gmax])
        P.op("dve", lambda e: e.tensor_scalar(out=gm[:n, :], in0=gs[:n, :], scalar1=gmax[:n, 0:1], scalar2=None,
                                              op0=ALU.is_equal), r=[gs, gmax], w=[gm])
        P.op("dve", lambda e: e.tensor_tensor(out=v3(t2), in0=v3(sl), in1=b4(m2), op=ALU.is_ge), r=[sl, m2], w=[t2])
        P.op("dve", lambda e: e.tensor_tensor(out=v3(msk), in0=v3(t2), in1=b4(gm), op=ALU.mult), r=[t2, gm], w=[msk])
        P.op("dve", lambda e: e.tensor_tensor(out=gsel[:n, :], in0=sc[:n, :], in1=msk[:n, :], op=ALU.mult), r=[sc, msk], w=[gsel])
        P.op("dve", lambda e: e.reduce_sum(out=den[:n, :], in_=gsel[:n, :], axis=AX.X), r=[gsel], w=[den])
        P.op("dve", lambda e: e.reciprocal(out=den[:n, :], in_=den[:n, :]), r=[den], w=[den])
        P.op("dve", lambda e: e.tensor_scalar(out=gsel[:n, :], in0=gsel[:n, :], scalar1=den[:n, 0:1], scalar2=None,
                                              op0=ALU.mult), r=[gsel, den], w=[gsel])
        P.op("pe", lambda e: e.transpose(PS[1][0:16, 0:n], gsel[:n, :], ident[:n, :n]), r=[gsel, ident], w=[PS[1]])
        P.op("act", lambda e: e.activation(out=gtt[:, 0:n], in_=PS[1][0:16, 0:n], func=AF.Copy), r=[PS[1]], w=[gtt])
        ld("sp", gt_d[:, t0:t0 + n], gtt[:, 0:n], gt_d, [gtt])

    P.barrier()
    stk1.close()
    stk2 = ExitStack()
    tb = P.sbs(stk2, [128, 16, 512], BF16)
    acc = P.sbs(stk2, [128, 16, 512])
    hTs = [P.sbs(stk2, [128, 8, 512], BF16) for _ in range(2)]
    sg = [P.sbs(stk2, [128, 512]) for _ in range(2)]
    t1 = [P.sbs(stk2, [128, 512]) for _ in range(2)]
    gb = P.sbs(stk2, [128, 512])
    GTb = P.sbs(stk2, [16, 512])
    wgs = [P.sbs(stk2, [128, 16, 128], BF16) for _ in range(2)]
    wus = [P.sbs(stk2, [128, 16, 128], BF16) for _ in range(2)]
    wdss = [P.sbs(stk2, [128, 8, D], BF16) for _ in range(2)]
    wgb_d = P.dram("wgb_d", [NEXP, 8, 128, 16 * 128], BF16)
    wub_d = P.dram("wub_d", [NEXP, 8, 128, 16 * 128], BF16)
    wdb_d = P.dram("wdb_d", [NEXP, 128, 8 * D], BF16)
    for ex in range(NEXP):
        for hc in range(8):
            P.dma("pool", wgb_d.t[ex, hc].rearrange("p (kc h) -> p kc h", h=128),
                  wg.t[ex][:, hc * 128:(hc + 1) * 128].rearrange("(kc p) h -> p kc h", p=128), r=[wg], w=[wgb_d])
            P.dma("pool", wub_d.t[ex, hc].rearrange("p (kc h) -> p kc h", h=128),
                  wu.t[ex][:, hc * 128:(hc + 1) * 128].rearrange("(kc p) h -> p kc h", p=128), r=[wu], w=[wub_d])
        P.dma("pool", wdb_d.t[ex].rearrange("p (hc d) -> p hc d", d=D), wd.t[ex].rearrange("(hc p) d -> p hc d", p=128), r=[wd], w=[wdb_d])
    ld("sp", bcB[:], bc_ap(lng[1, :], D), bcB, [lng])
    ld("sp", bcC[:], bc_ap(lnb[1, :], D), bcC, [lnb])
    blocks = [(b0, 512, 0) for b0 in range(0, T_l, 512)]
    if T_c:
        blocks.append((T_l, T_c, 1))
    pc = 0
    for (b0, nb, s) in blocks:
        ld("pool", tb[:, :, :nb], tokT_d.t[:, b0:b0 + nb].rearrange("(kc p) t -> p kc t", p=128), tb, [tokT_d])
        ld("sp", GTb[:, :nb], gt_d[:, b0:b0 + nb], GTb, [gt_d])
        for ex in range(NEXP):
            hT = hTs[ex % 2]; wds = wdss[ex % 2]
            ld("sp", wds[:].rearrange("p hc d -> p (hc d)"), wdb_d[ex, :, :], wds, [wdb_d])
            P.op("pe", lambda e, ex=ex: e.matmul(PS[2][:, :nb], sel[:, ex * 128:(ex + 1) * 128], GTb[:, :nb],
                                                 start=True, stop=True), r=[sel, GTb], w=[PS[2]])
            P.op("act", lambda e: e.activation(out=gb[:, :nb], in_=PS[2][:, :nb], func=AF.Copy), r=[PS[2]], w=[gb])
            for hc in range(8):
                a, b = wgs[pc % 2], wus[pc % 2]
                sgi, t1i = sg[pc % 2], t1[pc % 2]
                pg, pu = PS[(pc % 2) * 2], PS[(pc % 2) * 2 + 1]
                pc += 1
                ld("sp", a[:].rearrange("p kc h -> p (kc h)"), wgb_d[ex, hc, :, :], a, [wgb_d])
                ld("act", b[:].rearrange("p kc h -> p (kc h)"), wub_d[ex, hc, :, :], b, [wub_d])
                for kc in range(16):
                    P.op("pe", lambda e, kc=kc, a=a, pg=pg: e.matmul(pg[:, :nb], a[:, kc, :], tb[:, kc, :nb], start=(kc == 0),
                                                                   stop=(kc == 15)), r=[a, tb], w=[pg])
                for kc in range(16):
                    P.op("pe", lambda e, kc=kc, b=b, pu=pu: e.matmul(pu[:, :nb], b[:, kc, :], tb[:, kc, :nb], start=(kc == 0),
                                                                   stop=(kc == 15)), r=[b, tb], w=[pu])
                P.op("act", lambda e, pg=pg, sgi=sgi: e.activation(out=sgi[:, :nb], in_=pg[:, :nb], func=AF.Silu), r=[pg], w=[sgi])
                P.op("dve", lambda e, pu=pu, t1i=t1i: e.tensor_tensor(out=t1i[:, :nb], in0=pu[:, :nb], in1=gb[:, :nb], op=ALU.mult),
                     r=[pu, gb], w=[t1i])
                P.op("dve", lambda e, hc=hc, sgi=sgi, t1i=t1i, hT=hT: e.tensor_tensor(out=hT[:, hc, :nb], in0=sgi[:, :nb], in1=t1i[:, :nb],
                                                                              op=ALU.mult), r=[sgi, t1i], w=[hT])
            for dc in range(16):
                pd = PS[4 + dc % 4]
                for hc in range(8):
                    P.op("pe", lambda e, dc=dc, hc=hc, pd=pd, wds=wds, hT=hT: e.matmul(pd[:, :nb], wds[:, hc, dc * 128:(dc + 1) * 128], hT[:, hc, :nb],
                                                                     start=(hc == 0), stop=(hc == 7)), r=[wds, hT], w=[pd])
                if ex == 0:
                    P.op("act", lambda e, dc=dc, pd=pd: e.activation(out=acc[:, dc, :nb], in_=pd[:, :nb], func=AF.Copy), r=[pd], w=[acc])
                else:
                    P.op("dve", lambda e, dc=dc, pd=pd: e.tensor_tensor(out=acc[:, dc, :nb], in0=acc[:, dc, :nb], in1=pd[:, :nb],
                                                                       op=ALU.add), r=[pd, acc], w=[acc])
        for dc in range(16):
            P.op("dve", lambda e, dc=dc: e.tensor_scalar(out=acc[:, dc, :nb], in0=acc[:, dc, :nb], scalar1=mcol[s][:, 80 + dc:81 + dc],
                                                        scalar2=None, op0=ALU.mult), r=[acc, mcol[s]], w=[acc])
        for j0 in range(0, nb, 128):
            n = min(128, nb - j0)
            t0 = b0 + j0
            for dc in range(16):
                P.op("pe", lambda e, dc=dc: e.transpose(PS[dc // 4][:n, (dc % 4) * 128:(dc % 4 + 1) * 128],
                                                        acc[:, dc, j0:j0 + n], ident[:, :]), r=[acc, ident], w=[PS[dc // 4]])
            ld("sp", xt[:n, :], x1_d[t0:t0 + n, :], xt, [x1_d])
            for j in range(4):
                P.op("dve", lambda e, j=j: e.scalar_tensor_tensor(out=xt[:n, j * 512:(j + 1) * 512], in0=xt[:n, j * 512:(j + 1) * 512],
                                                                 scalar=ALPHA, in1=PS[j][:n, :], op0=ALU.mult, op1=ALU.add),
                     r=[xt, PS[j]], w=[xt])
            layer_norm_tile(P, n, xt, tmp, bcB, bcC, st)
            ld("sp", xout[t0:t0 + n, :], xt[:n, :], xout, [xt])
    P.finish([xout])
    P.barrier()
    stk2.close()
    return P


def build_mod():
    P = Prog()
    IN = "ExternalInput"
    NCOL = 1536
    cT = P.dram("cT", [128, 16 * 3], F32, IN)
    w = P.dram("w", [2, D, NCOL], F32, IN)
    b = P.dram("b", [2, NCOL], F32, IN)
    out = P.dram("out", [2, 3, NCOL], F32, "ExternalOutput")
    ct = P.sb([128, 16, 3]); sg = P.sb([128, 16, 3])
    ring = [P.sb([128, NCOL]) for _ in range(3)]
    bb = P.sb([3, NCOL]); res = P.sb([3, NCOL])
    PS = [P.ps() for _ in range(3)]
    P.dma("sp", ct[:].rearrange("p k r -> p (k r)"), cT[:, :], r=[cT], w=[ct])
    P.op("act", lambda e: e.activation(out=sg[:], in_=ct[:], func=AF.Sigmoid), r=[ct], w=[sg])
    P.op("dve", lambda e: e.tensor_tensor(out=sg[:], in0=sg[:], in1=ct[:], op=ALU.mult), r=[sg, ct], w=[sg])
    i = 0
    for l in range(2):
        P.dma("sp", bb[:], bass.AP(b.t.tensor, b[l, :].offset, [[0, 3], [1, NCOL]]), r=[b], w=[bb])
        for kc in range(16):
            ws = ring[i % 3]; i += 1
            P.dma("sp", ws[:], w[l, kc * 128:(kc + 1) * 128, :], r=[w], w=[ws])
            for j in range(3):
                P.op("pe", lambda e, j=j, kc=kc, ws=ws: e.matmul(PS[j][0:3, :], sg[:, kc, :], ws[:, j * 512:(j + 1) * 512],
                                                                start=(kc == 0), stop=(kc == 15)), r=[sg, ws], w=[PS[j]])
        for j in range(3):
            P.op("dve", lambda e, j=j: e.tensor_tensor(out=res[:, j * 512:(j + 1) * 512], in0=PS[j][0:3, :],
                                                      in1=bb[:, j * 512:(j + 1) * 512], op=ALU.add), r=[PS[j], bb], w=[res])
        P.dma("sp", out[l, :, :], res[:], r=[res], w=[out])
    P.finish([out])
    return P


def build_ssd(LAT=16384, CTX=256, NU=2):
    P = Prog()
    IN = "ExternalInput"
    TB = CTX + LAT
    NCH = 6
    xT = P.dram("xT", [D, TB], F32, IN)
    modcol = P.dram("modcol", [2, 128, 32], F32, IN)
    wch = P.dram("wch", [NU, D, 768], F32, IN)
    wtm = P.dram("wtm", [NU, D, 528], F32, IN)
    cw = P.dram("cw", [NU, 128, NCH * 10], F32, IN)
    hp = P.dram("hp", [NU, 48], F32, IN)
    ng = P.dram("ng", [NU, 512], F32, IN)
    consts = P.dram("consts", [6, 128, 128], F32, IN)
    ymix = P.dram("ymix", [NU, LAT, 512], F32, "ExternalOutput")
    xbc_d = P.dram("xbc_d", [768, TB], F32)
    xc_d = P.dram("xc_d", [768, TB], F32)
    z_d = P.dram("z_d", [TB, 528], F32)
    yf_d = P.dram("yf_d", [LAT, 512], F32)
    sb = P.sb
    C = [sb([128, 128]) for _ in range(6)]
    for i in range(6):
        P.dma("sp", C[i][:], consts[i, :, :], r=[consts], w=[C[i]])
    ident, ones, TriF, TriB, SF, SB_ = C
    mcol = [sb([128, 32]) for _ in range(2)]
    scp1 = [sb([128, 16]) for _ in range(2)]
    for s in range(2):
        P.dma("sp", mcol[s][:], modcol[s, :, :], r=[modcol], w=[mcol[s]])
        P.op("dve", lambda e, s=s: e.tensor_scalar(out=scp1[s][:], in0=mcol[s][:, 16:32], scalar1=1.0, scalar2=None,
                                                    op0=ALU.add), r=[mcol[s]], w=[scp1[s]])
    PS = [P.ps() for _ in range(8)]
    wchs = sb([128, 16, 768], BF16)
    wtms = sb([128, 16, 528], BF16)
    xin32 = sb([128, 16, 512])
    hT = sb([128, 16, 512], BF16)
    stage = [sb([128, 528]) for _ in range(2)]
    cws = sb([128, NCH * 10])
    hpb = sb([128, 48]); aneg = sb([128, 16]); dsum = sb([128, 8]); ngb = sb([128, 512])
    R = min(32, LAT // 64)
    cin = sb([128, (R + 2) * 64]); cout = sb([128, max(R * 64, CTX)])

    for u in range(NU):
        P.dma("pool", wchs[:], wch.t[u].rearrange("(kc p) c -> p kc c", p=128), r=[wch], w=[wchs])
        P.dma("pool", wtms[:], wtm.t[u].rearrange("(kc p) c -> p kc c", p=128), r=[wtm], w=[wtms])
        P.dma("sp", cws[:], cw[u, :, :], r=[cw], w=[cws])
        P.dma("sp", hpb[:], bass.AP(hp.t.tensor, hp[u, :].offset, [[0, 128], [1, 48]]), r=[hp], w=[hpb])
        P.dma("sp", ngb[:], bass.AP(ng.t.tensor, ng[u, :].offset, [[0, 128], [1, 512]]), r=[ng], w=[ngb])
        P.op("act", lambda e: e.activation(out=aneg[:], in_=hpb[:, 16:32], func=AF.Exp), r=[hpb], w=[aneg])
        P.op("dve", lambda e: e.tensor_scalar(out=aneg[:], in0=aneg[:], scalar1=-1.0, scalar2=None, op0=ALU.mult), r=[aneg], w=[aneg])
        P.op("dve", lambda e: e.tensor_tensor(out=dsum[:], in0=hpb[:, 32:40], in1=hpb[:, 40:48], op=ALU.add), r=[hpb], w=[dsum])
        blocks = [(0, CTX, 1)] + [(CTX + i * 512, 512, 0) for i in range(LAT // 512)]
        si = 0
        for (t0, nb, s) in blocks:
            P.dma("sp", xin32[:, :, :nb], xT.t[:, t0:t0 + nb].rearrange("(kc p) t -> p kc t", p=128), r=[xT], w=[xin32])
            for kc in range(16):
                P.op("act", lambda e, kc=kc: e.activation(out=hT[:, kc, :nb], in_=xin32[:, kc, :nb], func=AF.Identity,
                                                          bias=mcol[s][:, kc:kc + 1], scale=scp1[s][:, kc:kc + 1]),
                     r=[xin32, mcol[s], scp1[s]], w=[hT])
            for m in range(NCH):
                pp = PS[m % 4]
                for kc in range(16):
                    P.op("pe", lambda e, m=m, kc=kc, pp=pp: e.matmul(pp[:, :nb], wchs[:, kc, m * 128:(m + 1) * 128], hT[:, kc, :nb],
                                                                   start=(kc == 0), stop=(kc == 15)), r=[wchs, hT], w=[pp])
                sg = stage[si % 2]; si += 1
                P.op("act", lambda e, pp=pp, sg=sg: e.activation(out=sg[:, :nb], in_=pp[:, :nb], func=AF.Copy), r=[pp], w=[sg])
                P.dma("sp", xbc_d[m * 128:(m + 1) * 128, t0:t0 + nb], sg[:, :nb], r=[sg], w=[xbc_d])
            for j0 in range(0, nb, 128):
                pz, pd = PS[4 + (j0 // 128) % 2 * 2], PS[5 + (j0 // 128) % 2 * 2]
                for kc in range(16):
                    P.op("pe", lambda e, kc=kc, pz=pz: e.matmul(pz[:, :], hT[:, kc, j0:j0 + 128], wtms[:, kc, 0:512],
                                                              start=(kc == 0), stop=(kc == 15)), r=[wtms, hT], w=[pz])
                for kc in range(16):
                    P.op("pe", lambda e, kc=kc, pd=pd: e.matmul(pd[:, 0:16], hT[:, kc, j0:j0 + 128], wtms[:, kc, 512:528],
                                                              start=(kc == 0), stop=(kc == 15)), r=[wtms, hT], w=[pd])
                sg = stage[si % 2]; si += 1
                P.op("act", lambda e, pz=pz, sg=sg: e.activation(out=sg[:, 0:512], in_=pz[:, :], func=AF.Copy), r=[pz], w=[sg])
                P.op("dve", lambda e, pd=pd, sg=sg: e.tensor_copy(out=sg[:, 512:528], in_=pd[:, 0:16]), r=[pd], w=[sg])
                P.dma("sp", z_d[t0 + j0:t0 + j0 + 128, :], sg[:, :], r=[sg], w=[z_d])
        for m in range(NCH):
            wv = lambda k: cws[:, m * 10 + k:m * 10 + k + 1]
            P.dma("sp", cin[:, 0:CTX], xbc_d[m * 128:(m + 1) * 128, 0:CTX], r=[xbc_d], w=[cin])
            P.op("dve", lambda e: e.tensor_scalar(out=cout[:, 0:CTX], in0=cin[:, 0:CTX], scalar1=wv(4), scalar2=wv(9),
                                                  op0=ALU.mult, op1=ALU.add), r=[cin, cws], w=[cout])
            P.op("dve", lambda e: e.scalar_tensor_tensor(out=cout[:, 1:CTX], in0=cin[:, 0:CTX - 1], scalar=wv(3), in1=cout[:, 1:CTX],
                                                         op0=ALU.mult, op1=ALU.add), r=[cin, cws, cout], w=[cout])
            P.op("dve", lambda e: e.scalar_tensor_tensor(out=cout[:, 0:CTX - 1], in0=cin[:, 1:CTX], scalar=wv(5), in1=cout[:, 0:CTX - 1],
                                                         op0=ALU.mult, op1=ALU.add), r=[cin, cws, cout], w=[cout])
            P.op("act", lambda e: e.activation(out=cout[:, 0:CTX], in_=cout[:, 0:CTX], func=AF.Silu), r=[cout], w=[cout])
            P.dma("sp", xc_d[m * 128:(m + 1) * 128, 0:CTX], cout[:, 0:CTX], r=[cout], w=[xc_d])
            NR = LAT // 64
            for r0 in range(0, NR, R):
                lo, hi = r0 - 1, r0 + R + 1
                if lo < 0:
                    P.op("dve", lambda e: e.memset(cin[:, 0:64], 0.0), w=[cin])
                if hi > NR:
                    P.op("dve", lambda e: e.memset(cin[:, (R + 1) * 64:(R + 2) * 64], 0.0), w=[cin])
                a, b_ = max(lo, 0), min(hi, NR)
                P.dma("sp", cin[:, (a - lo) * 64:(b_ - lo) * 64], xbc_d[m * 128:(m + 1) * 128, CTX + a * 64:CTX + b_ * 64],
                      r=[xbc_d], w=[cin])
                ci3 = cin[:, :].rearrange("p (r c) -> p r c", c=64)
                co3 = cout[:, :].rearrange("p (r c) -> p r c", c=64)
                P.op("dve", lambda e: e.tensor_scalar(out=co3[:, :, :], in0=ci3[:, 1:R + 1, :], scalar1=wv(4), scalar2=wv(9),
                                                      op0=ALU.mult, op1=ALU.add), r=[cin, cws], w=[cout])
                for i in range(3):
                    for j in range(3):
                        if i == 1 and j == 1:
                            continue
                        if j == 0:
                            o_, i_ = co3[:, :, 1:64], ci3[:, i:i + R, 0:63]
                        elif j == 1:
                            o_, i_ = co3[:, :, :], ci3[:, i:i + R, :]
                        else:
                            o_, i_ = co3[:, :, 0:63], ci3[:, i:i + R, 1:64]
                        P.op("dve", lambda e, o_=o_, i_=i_, k=i * 3 + j: e.scalar_tensor_tensor(out=o_, in0=i_, scalar=wv(k), in1=o_,
                                                                                           op0=ALU.mult, op1=ALU.add),
                             r=[cin, cws, cout], w=[cout])
                P.op("act", lambda e: e.activation(out=cout[:, :], in_=cout[:, :], func=AF.Silu), r=[cout], w=[cout])
                P.dma("sp", xc_d[m * 128:(m + 1) * 128, CTX + r0 * 64:CTX + (r0 + R) * 64], cout[:, :], r=[cout], w=[xc_d])
        ssd_scan(P, u, PS, (ident, ones, TriF, TriB, SF, SB_), xc_d, z_d, yf_d, ymix, hpb, aneg, dsum, ngb, LAT, CTX)
    P.finish([ymix])
    return P


def ssd_scan(P, u, PS, consts, xc_d, z_d, yf_d, ymix, hpb, aneg, dsum, ngb, LAT, CTX):
    ident, ones, TriF, TriB, SF, SB_ = consts
    sb = P.sb
    if not hasattr(P, "_ssd_bufs"):
        B = {}
        B["CT"] = sb([128, 128]); B["BT"] = sb([128, 128]); B["CTb"] = sb([128, 128], BF16); B["BTb"] = sb([128, 128], BF16)
        B["xcm"] = sb([128, 4, 128]); B["xtm"] = sb([128, 512]); B["Btm"] = sb([128, 128], BF16)
        B["zt"] = sb([128, 528]); B["dt"] = sb([128, 8]); B["a"] = sb([128, 8]); B["e8"] = sb([128, 8])
        B["acs"] = sb([128, 8]); B["eacs"] = sb([128, 8]); B["tail"] = sb([128, 8]); B["etot"] = sb([128, 8])
        B["xdt"] = sb([128, 512], BF16); B["xdtt"] = sb([128, 512], BF16)
        B["cbm"] = sb([128, 128]); B["lh"] = [sb([128, 128]) for _ in range(2)]; B["L"] = [sb([128, 128]) for _ in range(2)]
        B["M"] = [sb([128, 128], BF16) for _ in range(2)]
        B["S"] = sb([128, 512]); B["Sb"] = sb([128, 512], BF16); B["tS"] = sb([128, 512])
        B["y"] = sb([128, 512]); B["yf"] = sb([128, 512]); B["sz"] = sb([128, 512]); B["st"] = sb([128, 4])
        P._ssd_bufs = B
    B = P._ssd_bufs
    CT, BT, CTb, BTb, xcm, xtm, Btm, zt = (B[k] for k in ["CT", "BT", "CTb", "BTb", "xcm", "xtm", "Btm", "zt"])
    dt, a, e8, acs, eacs, tail, etot = (B[k] for k in ["dt", "a", "e8", "acs", "eacs", "tail", "etot"])
    xdt, xdtt, cbm, S, Sb, tS, y, yf, sz, st = (B[k] for k in ["xdt", "xdtt", "cbm", "S", "Sb", "tS", "y", "yf", "sz", "st"])
    bc8 = lambda t: t[:, 0:8].unsqueeze(2).to_broadcast([128, 8, 64])
    v3 = lambda t: t[:, :].rearrange("p (h q) -> p h q", h=8)
    nctx, nlat = CTX // 128, LAT // 128
    for d in range(2):
        Tri, SM = (TriF, SF) if d == 0 else (TriB, SB_)
        P.op("dve", lambda e: e.memset(S[:], 0.0), w=[S])
        P.op("dve", lambda e: e.memset(Sb[:], 0.0), w=[Sb])
        order = list(range(nctx)) + [nctx + i for i in range(nlat)]
        if d == 1:
            order = list(range(nctx))[::-1] + [nctx + i for i in range(nlat)][::-1]
        for ci, c in enumerate(order):
            p0 = c * 128
            lat = c >= nctx
            l0 = p0 - CTX
            last_state = (ci == len(order) - 1)
            P.dma("sp", CT[:], xc_d[640:768, p0:p0 + 128], r=[xc_d], w=[CT])
            P.dma("sp", BT[:], xc_d[512:640, p0:p0 + 128], r=[xc_d], w=[BT])
            P.dma("sp", xcm[:], xc_d.t[0:512, p0:p0 + 128].rearrange("(m p) t -> p m t", p=128), r=[xc_d], w=[xcm])
            P.dma("sp", zt[:], z_d[p0:p0 + 128, :], r=[z_d], w=[zt])
            P.op("act", lambda e: e.activation(out=CTb[:], in_=CT[:], func=AF.Copy), r=[CT], w=[CTb])
            P.op("act", lambda e: e.activation(out=BTb[:], in_=BT[:], func=AF.Copy), r=[BT], w=[BTb])
            for m in range(4):
                P.op("pe", lambda e, m=m: e.transpose(PS[0][:, m * 128:(m + 1) * 128], xcm[:, m, :], ident[:, :]), r=[xcm, ident], w=[PS[0]])
            P.op("act", lambda e: e.activation(out=xtm[:], in_=PS[0][:, :], func=AF.Copy), r=[PS[0]], w=[xtm])
            P.op("pe", lambda e: e.transpose(PS[1][:, 0:128], BT[:, :], ident[:, :]), r=[BT, ident], w=[PS[1]])
            P.op("act", lambda e: e.activation(out=Btm[:], in_=PS[1][:, 0:128], func=AF.Copy), r=[PS[1]], w=[Btm])
            P.op("dve", lambda e: e.tensor_tensor(out=e8[:], in0=zt[:, 512 + d * 8:520 + d * 8], in1=hpb[:, d * 8:d * 8 + 8], op=ALU.add),
                 r=[zt, hpb], w=[e8])
            P.op("act", lambda e: e.activation(out=e8[:], in_=e8[:], func=AF.Exp), r=[e8], w=[e8])
            P.op("act", lambda e: e.activation(out=dt[:], in_=e8[:], func=AF.Ln, bias=1.0, scale=1.0), r=[e8], w=[dt])
            P.op("dve", lambda e: e.tensor_tensor(out=a[:], in0=dt[:], in1=aneg[:, d * 8:d * 8 + 8], op=ALU.mult), r=[dt, aneg], w=[a])
            P.op("pe", lambda e: e.matmul(PS[2][:, 0:8], Tri[:, :], a[:, :], start=True, stop=True), r=[Tri, a], w=[PS[2]])
            P.op("pe", lambda e: e.matmul(PS[2][:, 8:16], ones[:, :], a[:, :], start=True, stop=True), r=[ones, a], w=[PS[2]])
            P.op("act", lambda e: e.activation(out=acs[:], in_=PS[2][:, 0:8], func=AF.Copy), r=[PS[2]], w=[acs])
            P.op("act", lambda e: e.activation(out=eacs[:], in_=PS[2][:, 0:8], func=AF.Exp), r=[PS[2]], w=[eacs])
            P.op("act", lambda e: e.activation(out=etot[:], in_=PS[2][:, 8:16], func=AF.Exp), r=[PS[2]], w=[etot])
            P.op("dve", lambda e: e.tensor_tensor(out=tail[:], in0=PS[2][:, 8:16], in1=acs[:], op=ALU.subtract), r=[PS[2], acs], w=[tail])
            P.op("act", lambda e: e.activation(out=tail[:], in_=tail[:], func=AF.Exp), r=[tail], w=[tail])
            P.op("dve", lambda e: e.tensor_tensor(out=v3(xdt), in0=v3(xtm), in1=bc8(dt), op=ALU.mult), r=[xtm, dt], w=[xdt])
            P.op("dve", lambda e: e.tensor_tensor(out=v3(xdtt), in0=v3(xdt), in1=bc8(tail), op=ALU.mult), r=[xdt, tail], w=[xdtt])
            if lat:
                P.op("pe", lambda e: e.matmul(PS[3][:, 0:128], BTb[:, :], CTb[:, :], start=True, stop=True), r=[BTb, CTb], w=[PS[3]])
                P.op("dve", lambda e: e.tensor_tensor(out=cbm[:], in0=PS[3][:, 0:128], in1=Tri[:, :], op=ALU.mult), r=[PS[3], Tri], w=[cbm])
                for h in range(8):
                    lh, L, M = B["lh"][h % 2], B["L"][h % 2], B["M"][h % 2]
                    pdf = PS[4 + h % 2]
                    P.op("dve", lambda e, h=h, lh=lh: e.tensor_scalar(out=lh[:], in0=SM[:, :], scalar1=a[:, h:h + 1], scalar2=None,
                                                                    op0=ALU.mult), r=[SM, a], w=[lh])
                    P.op("pe", lambda e, lh=lh, pdf=pdf: e.matmul(pdf[:, 0:128], lh[:, :], Tri[:, :], start=True, stop=True), r=[lh, Tri], w=[pdf])
                    P.op("act", lambda e, L=L, pdf=pdf: e.activation(out=L[:], in_=pdf[:, 0:128], func=AF.Exp), r=[pdf], w=[L])
                    P.op("dve", lambda e, L=L, M=M: e.tensor_tensor(out=M[:], in0=L[:], in1=cbm[:], op=ALU.mult), r=[L, cbm], w=[M])
                    P.op("pe", lambda e, h=h, M=M: e.matmul(PS[6][:, h * 64:(h + 1) * 64], M[:, :], xdt[:, h * 64:(h + 1) * 64],
                                                          start=True, stop=True), r=[M, xdt], w=[PS[6]])
                P.op("pe", lambda e: e.matmul(PS[7][:, :], CTb[:, :], Sb[:, :], start=True, stop=True), r=[CTb, Sb], w=[PS[7]])
                P.op("dve", lambda e: e.tensor_tensor(out=v3(y), in0=PS[7][:, :].rearrange("p (h q) -> p h q", h=8), in1=bc8(eacs), op=ALU.mult),
                     r=[PS[7], eacs], w=[y])
                P.op("dve", lambda e: e.tensor_tensor(out=y[:], in0=y[:], in1=PS[6][:, :], op=ALU.add), r=[y, PS[6]], w=[y])
                if d == 0:
                    P.dma("sp", yf_d[l0:l0 + 128, :], y[:], r=[y], w=[yf_d])
                else:
                    P.dma("sp", yf[:], yf_d[l0:l0 + 128, :], r=[yf_d], w=[yf])
                    P.op("dve", lambda e: e.tensor_tensor(out=y[:], in0=y[:], in1=yf[:], op=ALU.add), r=[y, yf], w=[y])
                    P.op("dve", lambda e: e.tensor_tensor(out=v3(yf), in0=v3(xtm), in1=bc8(dsum), op=ALU.mult), r=[xtm, dsum], w=[yf])
                    P.op("dve", lambda e: e.tensor_tensor(out=y[:], in0=y[:], in1=yf[:], op=ALU.add), r=[y, yf], w=[y])
                    P.op("act", lambda e: e.activation(out=sz[:], in_=zt[:, 0:512], func=AF.Silu), r=[zt], w=[sz])
                    P.op("dve", lambda e: e.tensor_tensor(out=y[:], in0=y[:], in1=sz[:], op=ALU.mult), r=[y, sz], w=[y])
                    P.op("act", lambda e: e.activation(out=sz[:], in_=y[:], func=AF.Square), r=[y], w=[sz])
                    P.op("dve", lambda e: e.reduce_sum(out=st[:, 0:1], in_=sz[:], axis=AX.X), r=[sz], w=[st])
                    P.op("dve", lambda e: e.tensor_scalar(out=st[:, 0:1], in0=st[:, 0:1], scalar1=1.0 / 512, scalar2=EPS,
                                                          op0=ALU.mult, op1=ALU.add), r=[st], w=[st])
                    P.op("act", lambda e: e.activation(out=st[:, 0:1], in_=st[:, 0:1], func=AF.Sqrt), r=[st], w=[st])
                    P.op("dve", lambda e: e.reciprocal(out=st[:, 1:2], in_=st[:, 0:1]), r=[st], w=[st])
                    P.op("dve", lambda e: e.scalar_tensor_tensor(out=y[:], in0=y[:], scalar=st[:, 1:2], in1=ngb[:], op0=ALU.mult,
                                                                 op1=ALU.mult), r=[y, st, ngb], w=[y])
                    P.dma("sp", ymix[u, l0:l0 + 128, :], y[:], r=[y], w=[ymix])
            if not last_state:
                P.op("pe", lambda e: e.matmul(PS[3][:, :], Btm[:, :], xdtt[:, :], start=True, stop=True), r=[Btm, xdtt], w=[PS[3]])
                P.op("dve", lambda e: e.tensor_tensor(out=v3(tS), in0=v3(S), in1=bc8(etot), op=ALU.mult), r=[S, etot], w=[tS])
                P.op("dve", lambda e: e.tensor_tensor(out=S[:], in0=tS[:], in1=PS[3][:, :], op=ALU.add), r=[tS, PS[3]], w=[S])
                P.op("act", lambda e: e.activation(out=Sb[:], in_=S[:], func=AF.Copy), r=[S], w=[Sb])


def build_mix0(LAT=16384, CTX=256, fft=None):
    P = Prog()
    IN = "ExternalInput"
    TB = CTX + LAT
    if fft is None:
        fft = (LAT == 16384)
    xT = P.dram("xT", [2, D, TB], F32, IN)
    modcol = P.dram("modcol", [3, 128, 32], F32, IN)
    whc = P.dram("whc", [D, 384], F32, IN)
    wgc = P.dram("wgc", [D, 288], F32, IN)
    wgt = P.dram("wgt", [D, 512], F32, IN)
    hcw = P.dram("hcw", [128, 12], F32, IN)
    hlb = P.dram("hlb", [128, 2], F32, IN)
    fw1 = P.dram("fw1", [33, 64], F32, IN)
    fcol = P.dram("fcol", [64, 4], F32, IN)
    fw2 = P.dram("fw2", [64, 64], F32, IN)
    fw3 = P.dram("fw3", [64, 512], F32, IN)
    zl = P.dram("zl", [33, LAT], F32, IN); zc = P.dram("zc", [33, CTX], F32, IN)
    El = P.dram("El", [128, LAT], F32, IN); Ec = P.dram("Ec", [128, CTX], F32, IN)
    gw2 = P.dram("gw2", [2, 16, 128], F32, IN)
    gbc = P.dram("gbc", [128, 2], F32, IN)
    gng = P.dram("gng", [256], F32, IN)
    consts = P.dram("consts", [4, 128, 512], F32, IN)
    hy_out = P.dram("hy_out", [2, 128, TB], F32, "ExternalOutput")
    gla_out = P.dram("gla_out", [TB, 256], F32, "ExternalOutput")
    hu_d = P.dram("hu_d", [2, 384, TB], F32)
    gq_d = P.dram("gq_d", [256, TB], F32)
    gg_d = P.dram("gg_d", [2, 16, TB], F32)
    gvr_d = P.dram("gvr_d", [TB, 512], F32)
    of_d = P.dram("of_d", [TB, 256], F32)
    hf_d = {CTX: P.dram("hf_c", [4, 128, CTX], F32)}
    if fft:
        zr = P.dram("zr", [33, LAT], F32, IN); Er = P.dram("Er", [128, LAT], F32, IN)
        fF1 = P.dram("fF1", [128, 256], F32, IN); fTW = P.dram("fTW", [128, 512], F32, IN); fCS = P.dram("fCS", [128, 1024], F32, IN)
        fNSC = P.dram("fNSC", [128, 1024], F32, IN); fTWI = P.dram("fTWI", [128, 512], F32, IN); fC1S = P.dram("fC1S", [128, 128], F32, IN)
        hfull_d = P.dram("hfull_d", [2, 128, 2 * LAT], F32)
        Hr_d = P.dram("Hr_d", [2, 2, 128, 128, 128], F32); Hi_d = P.dram("Hi_d", [2, 2, 128, 128, 128], F32)
        cur_d = [P.dram("cur0_d", [128, LAT], F32), P.dram("cur1_d", [128, LAT], F32)]
        conv_d = P.dram("conv_d", [128, LAT], F32)
    else:
        hf_d[LAT] = P.dram("hf_l", [4, 128, LAT], F32)
    sb = P.sb
    ident = sb([128, 128]); mF = sb([64, 64]); mB = sb([64, 64]); rmask = sb([128, 512])
    P.dma("sp", ident[:], consts[0, :, 0:128], r=[consts], w=[ident])
    P.dma("sp", mF[:], consts[1, 0:64, 0:64], r=[consts], w=[mF])
    P.dma("sp", mB[:], consts[2, 0:64, 0:64], r=[consts], w=[mB])
    P.dma("sp", rmask[:], consts[3, :, :], r=[consts], w=[rmask])
    mcol = [sb([128, 32]) for _ in range(3)]
    scp1 = [sb([128, 16]) for _ in range(3)]
    for s in range(3):
        P.dma("sp", mcol[s][:], modcol[s, :, :], r=[modcol], w=[mcol[s]])
        P.op("dve", lambda e, s=s: e.tensor_scalar(out=scp1[s][:], in0=mcol[s][:, 16:32], scalar1=1.0, scalar2=None,
                                                    op0=ALU.add), r=[mcol[s]], w=[scp1[s]])
    PS = [P.ps() for _ in range(8)]
    blocks = [(0, CTX)] + [(CTX + i * 512, 512) for i in range(LAT // 512)]

    with ExitStack() as stk:
        whs = P.sbs(stk, [128, 16, 384], BF16); wgs = P.sbs(stk, [128, 16, 288], BF16); wts = P.sbs(stk, [128, 16, 512], BF16)
        xin32 = P.sbs(stk, [128, 16, 512]); hT = P.sbs(stk, [128, 16, 512], BF16)
        stage = [P.sbs(stk, [128, 512]) for _ in range(3)]
        P.dma("pool", whs[:], whc.t.rearrange("(kc p) c -> p kc c", p=128), r=[whc], w=[whs])
        P.dma("pool", wgs[:], wgc.t.rearrange("(kc p) c -> p kc c", p=128), r=[wgc], w=[wgs])
        P.dma("pool", wts[:], wgt.t.rearrange("(kc p) c -> p kc c", p=128), r=[wgt], w=[wts])
        si = 0
        for bi in range(2):
            for (t0, nb) in blocks:
                s = 2 if t0 < CTX else bi
                P.dma("sp", xin32[:, :, :nb], xT.t[bi, :, t0:t0 + nb].rearrange("(kc p) t -> p kc t", p=128), r=[xT], w=[xin32])
                for kc in range(16):
                    P.op("act", lambda e, kc=kc, s=s: e.activation(out=hT[:, kc, :nb], in_=xin32[:, kc, :nb], func=AF.Identity,
                                                                   bias=mcol[s][:, kc:kc + 1], scale=scp1[s][:, kc:kc + 1]),
                         r=[xin32, mcol[s], scp1[s]], w=[hT])
                jobs = [(whs, m * 128, 128, hu_d, (bi, slice(m * 128, (m + 1) * 128))) for m in range(3)]
                if bi == 0:
                    jobs += [(wgs, m * 128, 128, gq_d, (slice(m * 128, (m + 1) * 128),)) for m in range(2)]
                    jobs += [(wgs, 256 + dd * 16, 16, gg_d, (dd, slice(0, 16))) for dd in range(2)]
                for ji, (wsb, c0, mw, dst, idx) in enumerate(jobs):
                    pp = PS[ji % 4]
                    for kc in range(16):
                        P.op("pe", lambda e, kc=kc, pp=pp, wsb=wsb, c0=c0, mw=mw: e.matmul(pp[:mw, :nb], wsb[:, kc, c0:c0 + mw], hT[:, kc, :nb],
                                                                                        start=(kc == 0), stop=(kc == 15)), r=[wsb, hT], w=[pp])
                    sg = stage[si % 3]; si += 1
                    P.op("act", lambda e, pp=pp, sg=sg, mw=mw: e.activation(out=sg[:mw, :nb], in_=pp[:mw, :nb], func=AF.Copy), r=[pp], w=[sg])
                    P.dma("sp", dst.t[idx + (slice(t0, t0 + nb),)], sg[:mw, :nb], r=[sg], w=[dst])
                if bi == 0:
                    for j0 in range(0, nb, 128):
                        pz = PS[4 + (j0 // 128) % 4]
                        for kc in range(16):
                            P.op("pe", lambda e, kc=kc, pz=pz, j0=j0: e.matmul(pz[:, :], hT[:, kc, j0:j0 + 128], wts[:, kc, :],
                                                                             start=(kc == 0), stop=(kc == 15)), r=[wts, hT], w=[pz])
                        sg = stage[si % 3]; si += 1
                        P.op("act", lambda e, pz=pz, sg=sg: e.activation(out=sg[:, :], in_=pz[:, :], func=AF.Copy), r=[pz], w=[sg])
                        P.dma("sp", gvr_d[t0 + j0:t0 + j0 + 128, :], sg[:, :], r=[sg], w=[gvr_d])
        P.barrier()

    with ExitStack() as stk:
        w1s = P.sbs(stk, [33, 64]); w2s = P.sbs(stk, [64, 64]); w3s = P.sbs(stk, [64, 512]); fc = P.sbs(stk, [64, 4])
        cws = P.sbs(stk, [128, 12]); lbs = P.sbs(stk, [128, 2])
        for dst, src in [(w1s, fw1), (w2s, fw2), (w3s, fw3), (fc, fcol), (cws, hcw), (lbs, hlb)]:
            P.dma("sp", dst[:], src[:, :], r=[src], w=[dst])
        zb = P.sbs(stk, [33, 512]); h1 = P.sbs(stk, [64, 512]); h2 = P.sbs(stk, [64, 512]); eb = P.sbs(stk, [128, 512])
        hst = [P.sbs(stk, [128, 512]) for _ in range(2)]
        rr = P.sbs(stk, [64, 512])
        si = 0
        gen = [(CTX, zc, Ec, [(od, hf_d[CTX], (od,), 0) for od in range(4)])]
        if fft:
            gen.append((LAT, zl, El, [(0, hfull_d, (0,), 0), (2, hfull_d, (1,), 0)]))
            gen.append((LAT, zr, Er, [(1, hfull_d, (0,), LAT), (3, hfull_d, (1,), LAT)]))
        else:
            gen.append((LAT, zl, El, [(od, hf_d[LAT], (od,), 0) for od in range(4)]))
        for (L, zsrc, Esrc, ods) in gen:
            for p0 in range(0, L, 512):
                nb = min(512, L - p0)
                P.dma("sp", zb[:, :nb], zsrc[:, p0:p0 + nb], r=[zsrc], w=[zb])
                P.dma("sp", eb[:, :nb], Esrc[:, p0:p0 + nb], r=[Esrc], w=[eb])
                for (wsb, kdim, src, dst, bcol, fcolm) in [(w1s, 33, zb, h1, 0, 1), (w2s, 64, h1, h2, 2, 3)]:
                    P.op("pe", lambda e, wsb=wsb, kdim=kdim, src=src: e.matmul(PS[0][:64, :nb], wsb[:kdim, :], src[:kdim, :nb], start=True, stop=True),
                         r=[wsb, src], w=[PS[0]])
                    P.op("dve", lambda e, dst=dst, bcol=bcol, fcolm=fcolm: e.tensor_scalar(out=dst[:, :nb], in0=PS[0][:64, :nb], scalar1=fc[:, bcol:bcol + 1],
                                                                                         scalar2=fc[:, fcolm:fcolm + 1], op0=ALU.add, op1=ALU.mult),
                         r=[PS[0], fc], w=[dst])
                    P.op("dve", lambda e, dst=dst: e.tensor_scalar(out=rr[:, :nb], in0=dst[:, :nb], scalar1=1.0 / (2 * math.pi), scalar2=12582912.0,
                                                                   op0=ALU.mult, op1=ALU.add), r=[dst], w=[rr])
                    P.op("dve", lambda e, dst=dst: e.tensor_scalar(out=rr[:, :nb], in0=rr[:, :nb], scalar1=12582912.0, scalar2=-2 * math.pi,
                                                                   op0=ALU.subtract, op1=ALU.mult), r=[rr], w=[rr])
                    P.op("dve", lambda e, dst=dst: e.tensor_tensor(out=dst[:, :nb], in0=dst[:, :nb], in1=rr[:, :nb], op=ALU.add), r=[dst, rr], w=[dst])
                    P.op("act", lambda e, dst=dst: e.activation(out=dst[:, :nb], in_=dst[:, :nb], func=AF.Sin), r=[dst], w=[dst])
                for (od, dbuf, didx, coff) in ods:
                    P.op("pe", lambda e, od=od: e.matmul(PS[1 + od % 2][:, :nb], w3s[:, od * 128:(od + 1) * 128], h2[:, :nb], start=True, stop=True),
                         r=[w3s, h2], w=[PS[1 + od % 2]])
                    sg = hst[si % 2]; si += 1
                    P.op("dve", lambda e, od=od, sg=sg: e.tensor_tensor(out=sg[:, :nb], in0=PS[1 + od % 2][:, :nb], in1=eb[:, :nb], op=ALU.mult),
                         r=[PS[1 + od % 2], eb], w=[sg])
                    P.dma("sp", dbuf.t[didx + (slice(None), slice(coff + p0, coff + p0 + nb))], sg[:, :nb], r=[sg], w=[dbuf])
        LD = CTX if fft else LAT
        LB = min(2048, LAT)
        u = P.sbs(stk, [128, LD]); acc = P.sbs(stk, [128, LD])
        raw = P.sbs(stk, [128, LB + 2]); xg = P.sbs(stk, [128, LB])
        hring = [P.sbs(stk, [128, min(LB, LD)]) for _ in range(2)]

        def short_conv_block(bi, ti, s0, L, b0, n, tgt, tb_):
            wv = lambda k: cws[:, ti * 4 + k:ti * 4 + k + 1]
            lo, hi = b0 - 1, b0 + n + 1
            if lo < 0:
                P.op("dve", lambda e: e.memset(raw[:, 0:1], 0.0), w=[raw])
            if hi > L:
                P.op("dve", lambda e: e.memset(raw[:, n + 1:n + 2], 0.0), w=[raw])
            a, b_ = max(lo, 0), min(hi, L)
            P.dma("sp", raw[:, a - lo:b_ - lo], hu_d[bi, ti * 128:(ti + 1) * 128, s0 + a:s0 + b_], r=[hu_d], w=[raw])
            P.op("dve", lambda e: e.tensor_scalar(out=tgt, in0=raw[:, 1:n + 1], scalar1=wv(1), scalar2=wv(3), op0=ALU.mult, op1=ALU.add),
                 r=[raw, cws], w=[tb_])
            P.op("dve", lambda e: e.scalar_tensor_tensor(out=tgt, in0=raw[:, 0:n], scalar=wv(0), in1=tgt, op0=ALU.mult, op1=ALU.add),
                 r=[raw, cws, tb_], w=[tb_])
            P.op("dve", lambda e: e.scalar_tensor_tensor(out=tgt, in0=raw[:, 2:n + 2], scalar=wv(2), in1=tgt, op0=ALU.mult, op1=ALU.add),
                 r=[raw, cws, tb_], w=[tb_])

        def short_conv(bi, ti, s0, L, dstbuf, mul_into=None):
            for b0 in range(0, L, LB):
                n = min(LB, L - b0)
                if mul_into is None:
                    short_conv_block(bi, ti, s0, L, b0, n, dstbuf[:, b0:b0 + n], dstbuf)
                else:
                    short_conv_block(bi, ti, s0, L, b0, n, xg[:, :n], xg)
                    P.op("dve", lambda e: e.tensor_tensor(out=mul_into[:, b0:b0 + n], in0=mul_into[:, b0:b0 + n], in1=xg[:, :n], op=ALU.mult),
                         r=[mul_into, xg], w=[mul_into])

        if fft:
            F1 = P.sbs(stk, [128, 256]); TW = P.sbs(stk, [128, 4, 128]); CS = P.sbs(stk, [128, 2, 512]); NSC = P.sbs(stk, [128, 2, 512])
            TWI = P.sbs(stk, [128, 2, 256]); C1S = P.sbs(stk, [128, 2, 64])
            for dst, src in [(F1, fF1), (TW, fTW), (CS, fCS), (NSC, fNSC), (TWI, fTWI), (C1S, fC1S)]:
                P.dma("sp", dst[:].rearrange("p a b -> p (a b)") if len(dst.t.shape) == 3 else dst[:], src[:, :], r=[src], w=[dst])
            X = P.sbs(stk, [128, 4, 256]); Apr = P.sbs(stk, [128, 2, 4, 128]); Api = P.sbs(stk, [128, 2, 4, 128])
            tm1 = P.sbs(stk, [128, 512]); tm2 = P.sbs(stk, [128, 512])
            Hr = P.sbs(stk, [128, 2, 4, 128]); Hi = P.sbs(stk, [128, 2, 4, 128])
            Yr = P.sbs(stk, [128, 2, 4, 128]); Yi = P.sbs(stk, [128, 2, 4, 128])
            Br = P.sbs(stk, [128, 4, 256]); Bi = P.sbs(stk, [128, 4, 256]); yt = P.sbs(stk, [64, 4, 256])
            cb = P.sbs(stk, [128, LB]); vb2 = P.sbs(stk, [128, LB])

            def cmul(outr, outi, ar, ai, br, bi_, tv, rb, wbr, wbi):
                t1, t2 = tv(tm1), tv(tm2)
                P.op("dve", lambda e: e.tensor_tensor(out=t1, in0=ar, in1=br, op=ALU.mult), r=rb, w=[tm1])
                P.op("dve", lambda e: e.tensor_tensor(out=t2, in0=ai, in1=bi_, op=ALU.mult), r=rb, w=[tm2])
                P.op("dve", lambda e: e.tensor_tensor(out=outr, in0=t1, in1=t2, op=ALU.subtract), r=[tm1, tm2], w=[wbr])
                P.op("dve", lambda e: e.tensor_tensor(out=t1, in0=ar, in1=bi_, op=ALU.mult), r=rb, w=[tm1])
                P.op("dve", lambda e: e.tensor_tensor(out=t2, in0=ai, in1=br, op=ALU.mult), r=rb, w=[tm2])
                P.op("dve", lambda e: e.tensor_tensor(out=outi, in0=t1, in1=t2, op=ALU.add), r=[tm1, tm2], w=[wbi])

            def fft_pass(src_d, src_ap, K, o, mode, dst_d=None):
                for gi in range(32):
                    ch0 = gi * 4
                    P.dma("sp", X[:K, :, :], src_ap[ch0:ch0 + 4, 0:K * 256].rearrange("c (n1 n2) -> n1 c n2", n2=256), r=[src_d], w=[X])
                    for c in range(4):
                        for h in range(2):
                            P.op("pe", lambda e, c=c, h=h: e.matmul(PS[c][:, h * 256:(h + 1) * 256], X[:K, c, h * 128:(h + 1) * 128], F1[:K, :],
                                                                    start=True, stop=True), r=[X, F1], w=[PS[c]])
                    tv3 = lambda t: t[:, 0:256].rearrange("p (h k) -> p h k", h=2)
                    for c in range(4):
                        bank = PS[c][:, :].rearrange("p (h r k) -> p h r k", h=2, r=2)
                        cmul(Apr[:, :, c, :], Api[:, :, c, :], bank[:, :, 0, :], bank[:, :, 1, :], TW[:, 0:2, :], TW[:, 2:4, :], tv3,
                             [PS[c], TW], Apr, Api)
                    fl = lambda ap: ap.rearrange("p c k -> p (c k)")
                    for q in range(2):
                        PR, PI = PS[4 + q * 2], PS[5 + q * 2]
                        seq_r = [(CS[:, h, q * 128:(q + 1) * 128], Apr[:, h, :, :]) for h in range(2)] + \
                                [(CS[:, h, 256 + q * 128:256 + (q + 1) * 128], Api[:, h, :, :]) for h in range(2)]
                        seq_i = [(CS[:, h, q * 128:(q + 1) * 128], Api[:, h, :, :]) for h in range(2)] + \
                                [(NSC[:, h, q * 128:(q + 1) * 128], Apr[:, h, :, :]) for h in range(2)]
                        for (pp, seq) in [(PR, seq_r), (PI, seq_i)]:
                            for k, (lt, rh) in enumerate(seq):
                                P.op("pe", lambda e, pp=pp, lt=lt, rh=rh, k=k: e.matmul(pp[:, :], lt, fl(rh), start=(k == 0), stop=(k == 3)),
                                     r=[CS, NSC, Apr, Api], w=[pp])
                    if mode == "filter":
                        for q in range(2):
                            P.op("act", lambda e, q=q: e.activation(out=fl(Yr[:, q, :, :]), in_=PS[4 + q * 2][:, :], func=AF.Copy), r=[PS[4 + q * 2]], w=[Yr])
                            P.op("act", lambda e, q=q: e.activation(out=fl(Yi[:, q, :, :]), in_=PS[5 + q * 2][:, :], func=AF.Copy), r=[PS[5 + q * 2]], w=[Yi])
                        P.dma("sp", Hr_d.t[o, :, :, ch0:ch0 + 4, :].rearrange("q p c k -> p q c k"), Yr[:, :, :, :], r=[Yr], w=[Hr_d])
                        P.dma("sp", Hi_d.t[o, :, :, ch0:ch0 + 4, :].rearrange("q p c k -> p q c k"), Yi[:, :, :, :], r=[Yi], w=[Hi_d])
                        continue
                    P.dma("sp", Hr[:, :, :, :], Hr_d.t[o, :, :, ch0:ch0 + 4, :].rearrange("q p c k -> p q c k"), r=[Hr_d], w=[Hr])
                    P.dma("sp", Hi[:, :, :, :], Hi_d.t[o, :, :, ch0:ch0 + 4, :].rearrange("q p c k -> p q c k"), r=[Hi_d], w=[Hi])
                    for q in range(2):
                        cmul(fl(Yr[:, q, :, :]), fl(Yi[:, q, :, :]), PS[4 + q * 2][:, :], PS[5 + q * 2][:, :], fl(Hr[:, q, :, :]), fl(Hi[:, q, :, :]),
                             lambda t: t[:, :], [PS[4 + q * 2], PS[5 + q * 2], Hr, Hi], Yr, Yi)
                    for c in range(4):
                        seq = []
                        for q in range(2):
                            seq += [(Yr[:, q, c, :], CS[:, q, :]), (Yi[:, q, c, :], NSC[:, q, :])]
                        for k, (lt, rh) in enumerate(seq):
                            P.op("pe", lambda e, c=c, lt=lt, rh=rh, k=k: e.matmul(PS[c][:, :], lt, rh, start=(k == 0), stop=(k == 3)),
                                 r=[Yr, Yi, CS, NSC], w=[PS[c]])
                    for c in range(4):
                        cmul(Br[:, c, :], Bi[:, c, :], PS[c][:, 0:256], PS[c][:, 256:512], TWI[:, 0, :], TWI[:, 1, :], lambda t: t[:, 0:256],
                             [PS[c], TWI], Br, Bi)
                    fl2 = lambda ap: ap.rearrange("p c m -> p (c m)")
                    for pr in range(2):
                        py = PS[4 + pr]
                        P.op("pe", lambda e, pr=pr, py=py: e.matmul(py[:64, :], C1S[:, 0, :], fl2(Br[:, 2 * pr:2 * pr + 2, :]), start=True, stop=False),
                             r=[C1S, Br], w=[py])
                        P.op("pe", lambda e, pr=pr, py=py: e.matmul(py[:64, :], C1S[:, 1, :], fl2(Bi[:, 2 * pr:2 * pr + 2, :]), start=False, stop=True),
                             r=[C1S, Bi], w=[py])
                        P.op("act", lambda e, pr=pr, py=py: e.activation(out=fl2(yt[:, 2 * pr:2 * pr + 2, :]), in_=py[:64, :], func=AF.Copy), r=[py], w=[yt])
                    P.dma("sp", dst_d.t[ch0:ch0 + 4, :].rearrange("c (n1 n2) -> n1 c n2", n2=256), yt[:, :, :], r=[yt], w=[dst_d])

            for o in range(2):
                fft_pass(hfull_d, hfull_d.t[o], 128, o, "filter")

        hi_ = 0
        for bi in range(2):
            for (s0, L) in [(0, CTX), (CTX, LAT)]:
                if fft and L == LAT:
                    for b0 in range(0, L, LB):
                        short_conv_block(bi, 0, s0, L, b0, LB, xg[:, :LB], xg)
                        P.dma("sp", cur_d[0][:, b0:b0 + LB], xg[:, :LB], r=[xg], w=[cur_d[0]])
                    for o in range(2):
                        fft_pass(cur_d[o % 2], cur_d[o % 2].t, 64, o, "conv", conv_d)
                        dst = cur_d[(o + 1) % 2]
                        for b0 in range(0, L, LB):
                            P.dma("sp", cb[:, :], cur_d[o % 2][:, b0:b0 + LB], r=[cur_d[o % 2]], w=[cb])
                            P.dma("sp", vb2[:, :], conv_d[:, b0:b0 + LB], r=[conv_d], w=[vb2])
                            P.op("dve", lambda e, o=o: e.scalar_tensor_tensor(out=cb[:, :], in0=cb[:, :], scalar=lbs[:, o:o + 1], in1=vb2[:, :],
                                                                             op0=ALU.mult, op1=ALU.add), r=[cb, lbs, vb2], w=[cb])
                            short_conv_block(bi, o + 1, s0, L, b0, LB, xg[:, :LB], xg)
                            P.op("dve", lambda e: e.tensor_tensor(out=cb[:, :], in0=cb[:, :], in1=xg[:, :LB], op=ALU.mult), r=[cb, xg], w=[cb])
                            if o == 0:
                                P.dma("sp", dst[:, b0:b0 + LB], cb[:, :], r=[cb], w=[dst])
                            else:
                                P.dma("sp", hy_out[bi, :, s0 + b0:s0 + b0 + LB], cb[:, :], r=[cb], w=[hy_out])
                    continue
                cur, nxt = u, acc
                short_conv(bi, 0, s0, L, cur)
                for o in range(2):
                    P.op("dve", lambda e, cur=cur, nxt=nxt, o=o: e.tensor_scalar(out=nxt[:, 0:L], in0=cur[:, 0:L], scalar1=lbs[:, o:o + 1], scalar2=None,
                                                                                op0=ALU.mult), r=[cur, lbs], w=[nxt])
                    for dr in range(2):
                        for l0 in range(0, L, LB):
                            n = min(LB, L - l0)
                            hb = hring[hi_ % 2]; hi_ += 1
                            P.dma("sp", hb[:, :n], hf_d[L][o * 2 + dr, :, l0:l0 + n], r=[hf_d[L]], w=[hb])
                            for m in range(l0, l0 + n):
                                if dr == 1 and m == 0:
                                    continue
                                if dr == 0:
                                    o_, i_ = nxt[:, m:L], cur[:, 0:L - m]
                                else:
                                    o_, i_ = nxt[:, 0:L - m], cur[:, m:L]
                                P.op("dve", lambda e, o_=o_, i_=i_, hb=hb, mm=m - l0: e.scalar_tensor_tensor(out=o_, in0=i_, scalar=hb[:, mm:mm + 1], in1=o_,
                                                                                                        op0=ALU.mult, op1=ALU.add),
                                     r=[cur, hb, nxt], w=[nxt])
                    short_conv(bi, o + 1, s0, L, None, mul_into=nxt)
                    cur, nxt = nxt, cur
                P.dma("sp", hy_out[bi, :, s0:s0 + L], cur[:, 0:L], r=[cur], w=[hy_out])
        P.barrier()

    w2s = [sb([16, 128]) for _ in range(2)]
    gb = sb([128, 2]); ngb = sb([64, 256])
    for dd in range(2):
        P.dma("sp", w2s[dd][:], gw2[dd, :, :], r=[gw2], w=[w2s[dd]])
    P.dma("sp", gb[:], gbc[:, :], r=[gbc], w=[gb])
    P.op("dve", lambda e: e.tensor_scalar(out=gb[:], in0=gb[:], scalar1=-1.0, scalar2=None, op0=ALU.mult), r=[gb], w=[gb])
    P.dma("sp", ngb[:], bass.AP(gng.t.tensor, gng.t.offset, [[0, 64], [1, 256]]), r=[gng], w=[ngb])
    g1 = sb([16, 512]); qk = sb([128, 2, 512]); g = sb([128, 512]); bb = sb([128, 512]); t5 = sb([128, 512])
    ebb = sb([128, 512]); qt = sb([128, 512], BF16); kt = sb([128, 512], BF16); ktl = sb([128, 512]); ebl = sb([128, 8])
    vr = sb([64, 512]); vb = sb([64, 256], BF16); attb = sb([64, 64], BF16); ktT = sb([64, 128], BF16)
    S = sb([128, 256]); Sb = sb([128, 256], BF16); o_ = sb([64, 256]); of = sb([64, 256]); sq = sb([64, 256]); st = sb([64, 2])
    QS = 128 ** -0.5
    for d in range(2):
        msk = mF if d == 0 else mB
        P.op("dve", lambda e: e.memset(S[:], 0.0), w=[S])
        P.op("dve", lambda e: e.memset(Sb[:], 0.0), w=[Sb])
        blks = [blocks[0]] + blocks[1:] if d == 0 else [blocks[0]] + blocks[1:][::-1]
        for (t0, nb) in blks:
            nch = nb // 64
            P.dma("sp", g1[:, :nb], gg_d[d, :, t0:t0 + nb], r=[gg_d], w=[g1])
            P.dma("sp", qk[:, :, :nb], gq_d.t[:, t0:t0 + nb].rearrange("(m p) t -> p m t", p=128), r=[gq_d], w=[qk])
            P.op("pe", lambda e: e.matmul(PS[0][:, :nb], w2s[d][:, :], g1[:, :nb], start=True, stop=True), r=[w2s[d], g1], w=[PS[0]])
            P.op("act", lambda e: e.activation(out=g[:, :nb], in_=PS[0][:, :nb], func=AF.Exp, bias=gb[:, d:d + 1], scale=-1.0), r=[PS[0], gb], w=[g])
            P.op("act", lambda e: e.activation(out=g[:, :nb], in_=g[:, :nb], func=AF.Ln, bias=1.0, scale=1.0), r=[g], w=[g])
            P.op("dve", lambda e: e.tensor_scalar(out=g[:, :nb], in0=g[:, :nb], scalar1=-1.0 / 16, scalar2=None, op0=ALU.mult), r=[g], w=[g])
            P.op("dve", lambda e: e.tensor_tensor_scan(out=bb[:, :nb], data0=rmask[:, :nb], data1=g[:, :nb], initial=0.0, op0=ALU.mult, op1=ALU.add),
                 r=[rmask, g], w=[bb])
            b3 = lambda t: t[:, :nb].rearrange("p (c q) -> p c q", q=64)
            if d == 1:
                P.op("dve", lambda e: e.tensor_tensor(out=t5[:, :nb], in0=g[:, :nb], in1=bb[:, :nb], op=ALU.subtract), r=[g, bb], w=[t5])
                P.op("dve", lambda e: e.tensor_tensor(out=b3(bb), in0=b3(t5), in1=b3(bb)[:, :, 63:64].to_broadcast([128, nch, 64]), op=ALU.add),
                     r=[t5, bb], w=[bb])
            lastcol = 63 if d == 0 else 0
            P.op("act", lambda e: e.activation(out=ebl[:, :nch], in_=b3(bb)[:, :, lastcol], func=AF.Exp), r=[bb], w=[ebl])
            P.op("dve", lambda e: e.tensor_tensor(out=b3(t5), in0=b3(bb)[:, :, lastcol:lastcol + 1].to_broadcast([128, nch, 64]), in1=b3(bb), op=ALU.subtract),
                 r=[bb], w=[t5])
            P.op("act", lambda e: e.activation(out=t5[:, :nb], in_=t5[:, :nb], func=AF.Exp), r=[t5], w=[t5])
            P.op("dve", lambda e: e.tensor_tensor(out=ktl[:, :nb], in0=qk[:, 1, :nb], in1=t5[:, :nb], op=ALU.mult), r=[qk, t5], w=[ktl])
            P.op("act", lambda e: e.activation(out=ebb[:, :nb], in_=bb[:, :nb], func=AF.Exp), r=[bb], w=[ebb])
            P.op("dve", lambda e: e.scalar_tensor_tensor(out=qt[:, :nb], in0=qk[:, 0, :nb], scalar=QS, in1=ebb[:, :nb], op0=ALU.mult, op1=ALU.mult),
                 r=[qk, ebb], w=[qt])
            P.op("act", lambda e: e.activation(out=ebb[:, :nb], in_=bb[:, :nb], func=AF.Exp, scale=-1.0), r=[bb], w=[ebb])
            P.op("dve", lambda e: e.tensor_tensor(out=kt[:, :nb], in0=qk[:, 1, :nb], in1=ebb[:, :nb], op=ALU.mult), r=[qk, ebb], w=[kt])
            chs = list(range(nch)) if d == 0 else list(range(nch))[::-1]
            for c in chs:
                c0 = c * 64
                p0 = t0 + c0
                P.dma("sp", vr[:], gvr_d[p0:p0 + 64, :], r=[gvr_d], w=[vr])
                P.op("act", lambda e: e.activation(out=vb[:], in_=vr[:, 0:256], func=AF.Copy), r=[vr], w=[vb])
                P.op("pe", lambda e: e.matmul(PS[1][:64, 0:64], kt[:, c0:c0 + 64], qt[:, c0:c0 + 64], start=True, stop=True), r=[kt, qt], w=[PS[1]])
                P.op("dve", lambda e: e.tensor_tensor(out=attb[:], in0=PS[1][:64, 0:64], in1=msk[:], op=ALU.mult), r=[PS[1], msk], w=[attb])
                P.op("pe", lambda e: e.matmul(PS[2][:64, 0:256], attb[:, :], vb[:, :], start=True, stop=False), r=[attb, vb], w=[PS[2]])
                P.op("pe", lambda e: e.matmul(PS[2][:64, 0:256], qt[:, c0:c0 + 64], Sb[:, :], start=False, stop=True), r=[qt, Sb], w=[PS[2]])
                if d == 0:
                    P.op("act", lambda e: e.activation(out=o_[:], in_=PS[2][:64, 0:256], func=AF.Copy), r=[PS[2]], w=[o_])
                    P.dma("sp", of_d[p0:p0 + 64, :], o_[:], r=[o_], w=[of_d])
                else:
                    P.dma("sp", of[:], of_d[p0:p0 + 64, :], r=[of_d], w=[of])
                    P.op("dve", lambda e: e.tensor_tensor(out=o_[:], in0=of[:], in1=PS[2][:64, 0:256], op=ALU.add), r=[of, PS[2]], w=[o_])
                    P.op("act", lambda e: e.activation(out=sq[:], in_=o_[:], func=AF.Square), r=[o_], w=[sq])
                    P.op("dve", lambda e: e.reduce_sum(out=st[:, 0:1], in_=sq[:], axis=AX.X), r=[sq], w=[st])
                    P.op("dve", lambda e: e.tensor_scalar(out=st[:, 0:1], in0=st[:, 0:1], scalar1=1.0 / 256, scalar2=EPS, op0=ALU.mult, op1=ALU.add),
                         r=[st], w=[st])
                    P.op("act", lambda e: e.activation(out=st[:, 0:1], in_=st[:, 0:1], func=AF.Sqrt), r=[st], w=[st])
                    P.op("dve", lambda e: e.reciprocal(out=st[:, 1:2], in_=st[:, 0:1]), r=[st], w=[st])
                    P.op("dve", lambda e: e.scalar_tensor_tensor(out=o_[:], in0=o_[:], scalar=st[:, 1:2], in1=ngb[:], op0=ALU.mult, op1=ALU.mult),
                         r=[o_, st, ngb], w=[o_])
                    P.op("act", lambda e: e.activation(out=sq[:], in_=vr[:, 256:512], func=AF.Silu), r=[vr], w=[sq])
                    P.op("dve", lambda e: e.tensor_tensor(out=o_[:], in0=o_[:], in1=sq[:], op=ALU.mult), r=[o_, sq], w=[o_])
                    P.dma("sp", gla_out[p0:p0 + 64, :], o_[:], r=[o_], w=[gla_out])
                P.op("pe", lambda e: e.transpose(PS[3][:64, 0:128], ktl[:, c0:c0 + 64], ident[:, :]), r=[ktl, ident], w=[PS[3]])
                P.op("act", lambda e: e.activation(out=ktT[:], in_=PS[3][:64, 0:128], func=AF.Copy), r=[PS[3]], w=[ktT])
                P.op("pe", lambda e: e.matmul(PS[4][:, 0:256], ktT[:, :], vb[:, :], start=True, stop=True), r=[ktT, vb], w=[PS[4]])
                P.op("dve", lambda e, c=c: e.scalar_tensor_tensor(out=S[:], in0=S[:], scalar=ebl[:, c:c + 1], in1=PS[4][:, 0:256], op0=ALU.mult, op1=ALU.add),
                     r=[S, ebl, PS[4]], w=[S])
                P.op("act", lambda e: e.activation(out=Sb[:], in_=S[:], func=AF.Copy), r=[S], w=[Sb])
    P.finish([hy_out, gla_out])
    return P


HY_MIN_DECAY = math.log(1e-2) / 1.5
HY_MAX_DECAY = math.log(1e-2) / 0.3


def _colpack(vecs):
    a = np.stack(vecs, 0).reshape(len(vecs), 16, 128)
    return np.ascontiguousarray(a.transpose(2, 0, 1).reshape(128, len(vecs) * 16)).astype(np.float32)


def _hy_tables(L):
    pos = np.arange(L, dtype=np.float64)[None, :]
    t = pos / max(L - 1, 1)
    bands = np.linspace(1e-4, 15, 16, dtype=np.float32).astype(np.float64)[:, None]
    ang = 2.0 * math.pi * bands * pos / L
    z = np.concatenate([t, np.cos(ang), -np.sin(ang)], 0).astype(np.float32)
    deltas = np.abs(np.linspace(HY_MIN_DECAY, HY_MAX_DECAY, 1024, dtype=np.float32)).astype(np.float64)
    E = np.exp(-t * deltas[:, None]).astype(np.float32)
    return z, E


def fft_consts():
    N = 32768
    p = np.arange(128, dtype=np.float64)[:, None]
    k128 = np.arange(128, dtype=np.float64)[None, :]
    a1 = 2 * np.pi * p * k128 / 128
    F1 = np.concatenate([np.cos(a1), -np.sin(a1)], 1)
    n2 = (np.arange(2)[None, :, None] * 128 + p[:, :, None])
    at = 2 * np.pi * n2 * np.arange(128)[None, None, :] / N
    TW = np.concatenate([np.cos(at), -np.sin(at)], 1).reshape(128, 512)
    a2 = 2 * np.pi * n2 * np.arange(256)[None, None, :] / 256
    CS = np.concatenate([np.cos(a2), np.sin(a2)], 2).reshape(128, 1024)
    NSC = np.concatenate([-np.sin(a2), np.cos(a2)], 2).reshape(128, 1024)
    ai = 2 * np.pi * p * np.arange(256, dtype=np.float64)[None, :] / N
    TWI = np.concatenate([np.cos(ai), np.sin(ai)], 1)
    a3 = 2 * np.pi * p * np.arange(64, dtype=np.float64)[None, :] / 128
    C1S = np.concatenate([np.cos(a3), -np.sin(a3)], 1) / N
    f = lambda a: np.ascontiguousarray(a, dtype=np.float32)
    return dict(fF1=f(F1), fTW=f(TW), fCS=f(CS), fNSC=f(NSC), fTWI=f(TWI), fC1S=f(C1S))


def _hy_tables_rev(L):
    pos = (L - np.arange(L, dtype=np.float64))[None, :]
    t = pos / max(L - 1, 1)
    bands = np.linspace(1e-4, 15, 16, dtype=np.float32).astype(np.float64)[:, None]
    ang = 2.0 * math.pi * bands * pos / L
    z = np.concatenate([t, np.cos(ang), -np.sin(ang)], 0).astype(np.float32)
    deltas = np.abs(np.linspace(HY_MIN_DECAY, HY_MAX_DECAY, 1024, dtype=np.float32)).astype(np.float64)
    E = np.exp(-t * deltas[:, None]).astype(np.float32)
    E[:, 0] = 0.0
    return z, E


def mix0_consts():
    c = np.zeros((4, 128, 512), np.float32)
    k = np.arange(128)[:, None]; i = np.arange(128)[None, :]
    c[0, :, :128] = np.eye(128); c[1, :, :128] = (k <= i); c[2, :, :128] = (k >= i)
    c[3] = 1.0; c[3, :, ::64] = 0.0
    return c


def mix0_core_inputs(ci, xTb, modl, modc, W, tabs, rev=None):
    b, head, ct = ci // 4, ci % 4, ci
    w_in = W["ab_w_in"]
    sl = lambda s, n: w_in[:, s:s + n]
    (zc, Ec), (zl, El) = tabs
    w3 = W["hy_f_w3"]
    extra = {}
    if rev is not None:
        zr, Er = rev
        extra = dict(zr=zr, Er=np.ascontiguousarray(Er[ct * 128:(ct + 1) * 128]), **fft_consts())
    return dict(
        **extra,
        xT=np.stack([xTb[b], xTb[1 - b]]),
        modcol=np.stack([_colpack([modl[b][0], modl[b][1]]), _colpack([modl[1 - b][0], modl[1 - b][1]]), _colpack([modc[0], modc[1]])]),
        whc=np.ascontiguousarray(np.concatenate([sl(ct * 128, 128), sl(1024 + ct * 128, 128), sl(2048 + ct * 128, 128)], 1)),
        wgc=np.ascontiguousarray(np.concatenate([sl(3072 + head * 128, 128), sl(3584 + head * 128, 128), sl(6144, 32)], 1)),
        wgt=np.ascontiguousarray(np.concatenate([sl(4096 + head * 256, 256), sl(5120 + head * 256, 256)], 1)),
        hcw=np.ascontiguousarray(np.stack([np.concatenate([W["hy_conv_w"][:, ti * 1024 + ct * 128:ti * 1024 + (ct + 1) * 128],
                                                           W["hy_conv_b"][None, ti * 1024 + ct * 128:ti * 1024 + (ct + 1) * 128]], 0).T
                                           for ti in range(3)], 1).reshape(128, 12)),
        hlb=np.ascontiguousarray(W["hy_long_bias"][:, ct * 128:(ct + 1) * 128].T),
        fw1=W["hy_f_w1"], fcol=np.ascontiguousarray(np.stack([W["hy_f_b1"], W["hy_f_fr1"], W["hy_f_b2"], W["hy_f_fr2"]], 1)),
        fw2=W["hy_f_w2"],
        fw3=np.ascontiguousarray(np.concatenate([w3[:, o * 2048 + dr * 1024 + ct * 128:o * 2048 + dr * 1024 + (ct + 1) * 128]
                                                 for o in range(2) for dr in range(2)], 1)),
        zl=zl, zc=zc, El=np.ascontiguousarray(El[ct * 128:(ct + 1) * 128]), Ec=np.ascontiguousarray(Ec[ct * 128:(ct + 1) * 128]),
        gw2=np.ascontiguousarray(W["gla_gate_w2"][:, :, head * 128:(head + 1) * 128]),
        gbc=np.ascontiguousarray(W["gla_gate_b"][:, head * 128:(head + 1) * 128].T),
        gng=np.ascontiguousarray(W["gla_norm_g"][head * 256:(head + 1) * 256]),
        consts=mix0_consts(),
    )


def ssd_unit_inputs(g, w_in, conv_w, conv_b, dt_bias, a_log, d_skip, norm_g):
    xs = 4096 + g * 512; bs = 4096 + 4096 + g * 128; cs = 4096 + 4096 + 1024 + g * 128
    wch = np.concatenate([w_in[:, xs:xs + 512], w_in[:, bs:bs + 128], w_in[:, cs:cs + 128]], 1)
    wtm = np.concatenate([w_in[:, g * 512:(g + 1) * 512], w_in[:, 10240 + g * 8:10240 + g * 8 + 8], w_in[:, 10304 + g * 8:10304 + g * 8 + 8]], 1)
    cidx = np.concatenate([np.arange(g * 512, (g + 1) * 512), 4096 + g * 128 + np.arange(128), 4096 + 1024 + g * 128 + np.arange(128)])
    cwf = np.concatenate([conv_w.reshape(9, 6144)[:, cidx], conv_b[None, cidx]], 0)
    cw = np.ascontiguousarray(cwf.reshape(10, 6, 128).transpose(2, 1, 0).reshape(128, 60))
    hp = np.concatenate([dt_bias[:, g * 8:(g + 1) * 8].reshape(-1), a_log[:, g * 8:(g + 1) * 8].reshape(-1), d_skip[:, g * 8:(g + 1) * 8].reshape(-1)])
    return wch, wtm, cw, hp.astype(np.float32), norm_g[g * 512:(g + 1) * 512]


def ssd_consts():
    k = np.arange(128)[:, None]; i = np.arange(128)[None, :]
    return np.stack([np.eye(128), np.ones((128, 128)), k <= i, k >= i, k > i, k < i]).astype(np.float32)


def _run(P, in_maps):
    in_maps = [{k: np.ascontiguousarray(v, dtype=np.float32) for k, v in m.items()} for m in in_maps]
    res = run_bass_kernel_spmd(P.nc, in_maps, core_ids=list(range(len(in_maps))))
    return res.results


def _post_launch(KC, T_l, T_c, yT_list, xres_list, modl, modc, w_out, lng, lnb, rw, rb, wg, wu, wd):
    ident = np.eye(128, dtype=np.float32)
    sel = np.zeros((16, 16, 128), np.float32)
    for e in range(16):
        sel[e, e, :] = 1
    sel = sel.reshape(16, 2048)
    P = build_post(T_l, T_c, KC)
    ins = []
    for ci in range(8):
        b = ci // 4
        ins.append(dict(yT=yT_list[ci], xres=xres_list[ci], w_out=w_out,
                        modrow=np.stack([modl[b], modc]), modcol=np.stack([_colpack(list(modl[b])), _colpack(list(modc))]),
                        lng=lng, lnb=lnb, rw=rw, rb=rb, wg=wg, wu=wu, wd=wd, ident=ident, sel=sel))
    return [r["xout"] for r in _run(P, ins)]


def kernel(**I):
    I = {k: np.asarray(v, dtype=np.float32) for k, v in I.items()}
    x, c, ctx, c_ctx = I["x"], I["c"], I["ctx"], I["c_ctx"]
    LAT, CTX = x.shape[1], ctx.shape[1]
    TB = LAT + CTX
    TPC = LAT // 4
    CPC = CTX // 4
    c_all = np.concatenate([c, c_ctx[None]], 0)
    cT = np.ascontiguousarray(c_all.reshape(3, 16, 128).transpose(2, 1, 0).reshape(128, 48))
    res = _run(build_mod(), [dict(cT=cT, w=I["mod_w"][:, :, i * 1536:(i + 1) * 1536], b=I["mod_b"][:, i * 1536:(i + 1) * 1536]) for i in range(8)])
    mod = np.concatenate([r["out"] for r in res], -1)
    modl = [[mod[l, b].reshape(6, D) for b in range(2)] for l in range(2)]
    modc = [mod[l, 2].reshape(6, D) for l in range(2)]
    xTb = [np.ascontiguousarray(np.concatenate([ctx[b], x[b]], 0).T) for b in range(2)]
    W0 = {k: I[k][0] for k in ["ab_w_in", "hy_conv_w", "hy_conv_b", "hy_f_w1", "hy_f_b1", "hy_f_fr1", "hy_f_w2", "hy_f_b2", "hy_f_fr2",
                               "hy_f_w3", "hy_long_bias", "gla_gate_w2", "gla_gate_b", "gla_norm_g"]}
    tabs = (_hy_tables(CTX), _hy_tables(LAT))
    use_fft = (LAT == 16384)
    rev = _hy_tables_rev(LAT) if use_fft else None
    res = _run(build_mix0(LAT, CTX, use_fft), [mix0_core_inputs(ci, xTb, modl[0], modc[0], W0, tabs, rev) for ci in range(8)])
    yT0 = [np.zeros((D, TB), np.float32) for _ in range(2)]
    for ci in range(8):
        b, head, ct = ci // 4, ci % 4, ci
        yT0[b][ct * 128:(ct + 1) * 128] = res[ci]["hy_out"][0]
        yT0[1 - b][ct * 128:(ct + 1) * 128] = res[ci]["hy_out"][1]
        yT0[b][1024 + head * 256:1024 + (head + 1) * 256] = res[ci]["gla_out"].T
    del xTb, res
    yl, xl = [], []
    for ci in range(8):
        b, j = ci // 4, ci % 4
        yl.append(np.concatenate([yT0[b][:, CTX + j * TPC:CTX + (j + 1) * TPC], yT0[b][:, j * CPC:(j + 1) * CPC]], 1))
        xl.append(np.concatenate([x[b, j * TPC:(j + 1) * TPC], ctx[b, j * CPC:(j + 1) * CPC]], 0))
    outs = _post_launch(16, TPC, CPC, yl, xl, modl[0], modc[0], I["ab_w_out"][0], I["ln_g"][0], I["ln_b"][0], I["router_w"], I["router_b"],
                        I["exp_w_gate"][0], I["exp_w_up"][0], I["exp_w_down"][0])
    x1 = np.zeros_like(x); ctx1 = np.zeros_like(ctx)
    for ci in range(8):
        b, j = ci // 4, ci % 4
        x1[b, j * TPC:(j + 1) * TPC] = outs[ci][:TPC]
        ctx1[b, j * CPC:(j + 1) * CPC] = outs[ci][TPC:]
    del yT0, yl, xl, outs
    xT1 = [np.ascontiguousarray(np.concatenate([ctx1[b], x1[b]], 0).T) for b in range(2)]
    cst = ssd_consts()
    ins = []
    for ci in range(8):
        b, gp = ci // 4, ci % 4
        us = [ssd_unit_inputs(2 * gp + u, I["ssd_w_in"][0], I["ssd_conv_w"][0], I["ssd_conv_b"][0], I["ssd_dt_bias"][0], I["ssd_a_log"][0],
                              I["ssd_d"][0], I["ssd_norm_g"][0]) for u in range(2)]
        ins.append(dict(xT=xT1[b], modcol=np.stack([_colpack([modl[1][b][0], modl[1][b][1]]), _colpack([modc[1][0], modc[1][1]])]),
                        wch=np.stack([u_[0] for u_ in us]), wtm=np.stack([u_[1] for u_ in us]), cw=np.stack([u_[2] for u_ in us]),
                        hp=np.stack([u_[3] for u_ in us]), ng=np.stack([u_[4] for u_ in us]), consts=cst))
    res = _run(build_ssd(LAT, CTX, 2), ins)
    yT1 = [np.zeros((4096, LAT), np.float32) for _ in range(2)]
    for ci in range(8):
        b, gp = ci // 4, ci % 4
        for u in range(2):
            g = 2 * gp + u
            yT1[b][g * 512:(g + 1) * 512] = res[ci]["ymix"][u].T
    del xT1, ins, res
    yl, xl = [], []
    for ci in range(8):
        b, j = ci // 4, ci % 4
        yl.append(yT1[b][:, j * TPC:(j + 1) * TPC])
        xl.append(x1[b, j * TPC:(j + 1) * TPC])
    outs = _post_launch(32, TPC, 0, yl, xl, modl[1], modc[1], I["ssd_w_out"][0], I["ln_g"][1], I["ln_b"][1], I["router_w"], I["router_b"],
                        I["exp_w_gate"][1], I["exp_w_up"][1], I["exp_w_down"][1])
    out = np.zeros_like(x)
    for ci in range(8):
        b, j = ci // 4, ci % 4
        out[b, j * TPC:(j + 1) * TPC] = outs[ci]
    return out
```

```python
import math
from contextlib import ExitStack
import numpy as np
import concourse.bass as bass
import concourse.mybir as mybir
from concourse.bass_utils import run_bass_kernel_spmd

F32 = mybir.dt.float32
BF16 = mybir.dt.bfloat16
AF = mybir.ActivationFunctionType
ALU = mybir.AluOpType
AX = mybir.AxisListType

D = 2048
NE = 16
DEXP = 1024
ALPHA = (2 * 2) ** 0.25
EPS = 1e-6
SEM_LIMIT = 30000


class Buf:
    def __init__(self, t=None, name=""):
        self.t = t
        self.name = name
        self.w = None
        self.r = []

    def __getitem__(self, idx):
        return self.t[idx]


class Prog:
    def __init__(self):
        self.nc = bass.Bass("TRN2", target_bir_lowering=False)
        nc = self.nc
        self.E = {"pe": nc.tensor, "dve": nc.vector, "act": nc.scalar, "pool": nc.gpsimd, "sp": nc.sync}
        self.nsem = 0
        self.sem = {}
        self.cnt = {}
        for e in self.E:
            self._new_eng_sem(e)
        self.waited = {e: {} for e in self.E}
        self.dsem = []
        self.dcnt = []
        for i in range(12):
            self.dsem.append(self._alloc_sem())
            self.dcnt.append(0)
        self.drr = 0
        self.ninstr = 0
        self.uid = 0

    def _alloc_sem(self):
        self.nsem += 1
        return (self.nsem, self.nc.alloc_semaphore(f"s{self.nsem}"))

    def _new_eng_sem(self, e):
        self.sem[e] = self._alloc_sem()
        self.cnt[e] = 0
        if not hasattr(self, "sem_owner"):
            self.sem_owner = {}
        self.sem_owner[self.sem[e][0]] = e

    def sb(self, shape, dt=F32, name=None):
        self.uid += 1
        return Buf(self.nc.alloc_sbuf_tensor(name or f"sb{self.uid}", list(shape), dt), name or f"sb{self.uid}")

    def ps(self, name=None):
        self.uid += 1
        return Buf(self.nc.alloc_psum_tensor(name or f"ps{self.uid}", [128, 512], F32), name or f"ps{self.uid}")

    def dram(self, name, shape, dt=F32, kind="Internal"):
        return Buf(self.nc.dram_tensor(name, list(shape), dt, kind=kind).ap(), name)

    def _wait(self, eng, tickets):
        wd = self.waited[eng]
        need = {}
        for tk in tickets:
            if tk is None:
                continue
            (sid, sh), v = tk
            if wd.get(sid, 0) >= v:
                continue
            if eng == "pe" and self.sem_owner.get(sid) == "pe":
                continue
            if sid not in need or need[sid][1] < v:
                need[sid] = (sh, v)
        for sid, (sh, v) in need.items():
            self.E[eng].wait_ge(sh, v)
            wd[sid] = v
            self.ninstr += 1

    def _deps(self, r, w):
        t = []
        for b in r:
            t.append(b.w)
        for b in w:
            t.append(b.w)
            t.extend(b.r)
        return t

    def _commit(self, tk, r, w):
        for b in r:
            b.r.append(tk)
            if len(b.r) > 24:
                b.r = b.r[-24:] if False else b.r
        for b in w:
            b.w = tk
            b.r = []

    def op(self, eng, fn, r=(), w=()):
        self._wait(eng, self._deps(r, w))
        if self.cnt[eng] >= SEM_LIMIT:
            self._new_eng_sem(eng)
        ins = fn(self.E[eng])
        self.cnt[eng] += 1
        s = self.sem[eng]
        ins.then_inc(s[1], 1)
        tk = (s, self.cnt[eng])
        self._commit(tk, r, w)
        self.ninstr += 1
        return tk

    def dma(self, q, out, in_, r=(), w=(), **kw):
        i = self.drr
        self.drr = (self.drr + 1) % len(self.dsem)
        s = self.dsem[i]
        self._wait(q, self._deps(r, w) + [(s, self.dcnt[i])] if self.dcnt[i] else self._deps(r, w))
        if self.dcnt[i] >= SEM_LIMIT * 16:
            self.dsem[i] = self._alloc_sem()
            self.dcnt[i] = 0
            s = self.dsem[i]
        ins = self.E[q].dma_start(out=out, in_=in_, **kw)
        self.dcnt[i] += 16
        ins.then_inc(s[1], 16)
        tk = (s, self.dcnt[i])
        self._commit(tk, r, w)
        self.ninstr += 1
        return tk

    def barrier(self):
        tks = [(self.sem[e], self.cnt[e]) for e in self.E if self.cnt[e]]
        tks += [(self.dsem[i], self.dcnt[i]) for i in range(len(self.dsem)) if self.dcnt[i]]
        for e in self.E:
            self._wait(e, tks)

    def sbs(self, stack, shape, dt=F32):
        self.uid += 1
        return Buf(stack.enter_context(self.nc.sbuf_tensor(f"sc{self.uid}", list(shape), dt)), f"sc{self.uid}")

    def finish(self, bufs):
        self._wait("sp", [b.w for b in bufs])


def make_ident(P, dt=F32):
    ident = P.sb([128, 128], dt, "ident_" + str(dt))
    P.op("pool", lambda e: e.memset(ident[:], 0.0), w=[ident])
    P.op("pool", lambda e: e.affine_select(out=ident[:], in_=ident[:], pattern=[[-1, 128]],
                                           compare_op=ALU.not_equal, fill=1.0, base=0, channel_multiplier=1),
         r=[ident], w=[ident])
    return ident


def bc_ap(ap1d, n):
    return bass.AP(ap1d.tensor, ap1d.offset, [[0, 128], [1, n]])


def layer_norm_tile(P, n, pre, tmp, gbc, bbc, st):
    P.op("dve", lambda e: e.reduce_sum(out=st[:n, 0:1], in_=pre[:n, :], axis=AX.X), r=[pre], w=[st])
    P.op("dve", lambda e: e.tensor_scalar(out=st[:n, 1:2], in0=st[:n, 0:1], scalar1=-1.0 / D, scalar2=None,
                                          op0=ALU.mult), r=[st], w=[st])
    P.op("act", lambda e: e.activation(out=tmp[:n, :], in_=pre[:n, :], func=AF.Square, bias=st[:n, 1:2], scale=1.0),
         r=[pre, st], w=[tmp])
    P.op("dve", lambda e: e.reduce_sum(out=st[:n, 2:3], in_=tmp[:n, :], axis=AX.X), r=[tmp], w=[st])
    P.op("dve", lambda e: e.tensor_scalar(out=st[:n, 2:3], in0=st[:n, 2:3], scalar1=1.0 / D, scalar2=EPS,
                                          op0=ALU.mult, op1=ALU.add), r=[st], w=[st])
    P.op("act", lambda e: e.activation(out=st[:n, 2:3], in_=st[:n, 2:3], func=AF.Sqrt), r=[st], w=[st])
    P.op("dve", lambda e: e.reciprocal(out=st[:n, 3:4], in_=st[:n, 2:3]), r=[st], w=[st])
    P.op("dve", lambda e: e.tensor_scalar(out=pre[:n, :], in0=pre[:n, :], scalar1=st[:n, 1:2], scalar2=st[:n, 3:4],
                                          op0=ALU.add, op1=ALU.mult), r=[pre, st], w=[pre])
    P.op("dve", lambda e: e.tensor_tensor(out=pre[:n, :], in0=pre[:n, :], in1=gbc[:n, :], op=ALU.mult),
         r=[pre, gbc], w=[pre])
    P.op("dve", lambda e: e.tensor_tensor(out=pre[:n, :], in0=pre[:n, :], in1=bbc[:n, :], op=ALU.add),
         r=[pre, bbc], w=[pre])


def build_post(T_l, T_c, KC, NEXP=NE):
    P = Prog()
    T = T_l + T_c
    IN = "ExternalInput"
    yT = P.dram("yT", [KC * 128, T], F32, IN)
    xres = P.dram("xres", [T, D], F32, IN)
    w_out = P.dram("w_out", [KC * 128, D], F32, IN)
    modrow = P.dram("modrow", [2, 6, D], F32, IN)
    modcol = P.dram("modcol", [2, 128, 6 * 16], F32, IN)
    lng = P.dram("lng", [2, D], F32, IN)
    lnb = P.dram("lnb", [2, D], F32, IN)
    rw = P.dram("rw", [D, NE], F32, IN)
    rb = P.dram("rb", [NE], F32, IN)
    wg = P.dram("wg", [NEXP, D, DEXP], F32, IN)
    wu = P.dram("wu", [NEXP, D, DEXP], F32, IN)
    wd = P.dram("wd", [NEXP, DEXP, D], F32, IN)
    ident_d = P.dram("ident", [128, 128], F32, IN)
    sel_d = P.dram("sel", [16, 16 * 128], F32, IN)
    xout = P.dram("xout", [T, D], F32, "ExternalOutput")
    x1_d = P.dram("x1_d", [T, D], F32)
    tokT_d = P.dram("tokT_d", [D, T], F32)

    sb, ps = P.sb, P.ps
    ident = sb([128, 128]); sel = sb([16, 16 * 128])
    rws = sb([128, 16, NE]); rbb = sb([128, NE])
    mcol = [sb([128, 96]) for _ in range(2)]
    sc2p1 = [sb([128, 16]) for _ in range(2)]
    bcB = sb([128, D]); bcC = sb([128, D])
    xt = sb([128, D]); tmp = sb([128, D]); st = sb([128, 4])
    PS = [ps() for _ in range(8)]
    sm = {k: sb([128, 16]) for k in ["sc", "sel", "eq", "s2", "t2", "msk", "gs"]}
    sm4 = {k: sb([128, 4]) for k in ["m1", "m2", "gs", "gm"]}
    sm1 = {k: sb([128, 1]) for k in ["gmax", "den"]}
    stk1 = ExitStack()
    bcA = P.sbs(stk1, [128, D])
    ytile = P.sbs(stk1, [128, KC, 128], BF16)
    wring = [P.sbs(stk1, [128, D], BF16) for _ in range(4)]
    tokT32 = P.sbs(stk1, [128, 16, 128])
    gtt = P.sbs(stk1, [16, 128])
    gt_d = P.dram("gt_d", [16, T], F32)
    wob_d = P.dram("wob_d", [KC, 128, D], BF16)
    for kc in range(KC):
        P.dma("pool", wob_d[kc, :, :], w_out[kc * 128:(kc + 1) * 128, :], r=[w_out], w=[wob_d])

    def ld(q, dst, src, wbuf, rbuf=()):
        P.dma(q, dst, src, r=list(rbuf), w=[wbuf])

    ld("sp", ident[:], ident_d[:, :], ident, [ident_d])
    ld("sp", sel[:], sel_d[:, :], sel, [sel_d])
    ld("sp", rws[:], rw.t.rearrange("(kc p) e -> p kc e", p=128), rws, [rw])
    ld("sp", rbb[:], bc_ap(rb.t, NE), rbb, [rb])
    for s in range(2):
        ld("sp", mcol[s][:], modcol[s, :, :], mcol[s], [modcol])
        P.op("dve", lambda e, s=s: e.tensor_scalar(out=sc2p1[s][:], in0=mcol[s][:, 64:80], scalar1=1.0, scalar2=None,
                                                    op0=ALU.add), r=[mcol[s]], w=[sc2p1[s]])
    ld("sp", bcB[:], bc_ap(lng[0, :], D), bcB, [lng])
    ld("sp", bcC[:], bc_ap(lnb[0, :], D), bcC, [lnb])

    tiles = [(t0, 128, 0) for t0 in range(0, T_l, 128)]
    if T_c:
        tiles.append((T_l, T_c, 1))
    cur_set = [-1]

    for ti, (t0, n, s) in enumerate(tiles):
        if cur_set[0] != s:
            ld("sp", bcA[:], bc_ap(modrow[s, 2, :], D), bcA, [modrow])
            cur_set[0] = s
        ld("pool", ytile[:, :, :n], yT.t[:, t0:t0 + n].rearrange("(kc p) t -> p kc t", p=128), ytile, [yT])
        ld("sp", xt[:n, :], xres[t0:t0 + n, :], xt, [xres])
        for kc in range(KC):
            ws = wring[kc % 4]
            ld("sp" if kc % 2 == 0 else "act", ws[:], wob_d[kc, :, :], ws, [wob_d])
            for j in range(4):
                P.op("pe", lambda e, j=j, kc=kc, ws=ws: e.matmul(PS[j][:n, :], ytile[:, kc, :n], ws[:, j * 512:(j + 1) * 512],
                                                                start=(kc == 0), stop=(kc == KC - 1)),
                     r=[ytile, ws], w=[PS[j]])
        for j in range(4):
            P.op("dve", lambda e, j=j: e.tensor_tensor(out=tmp[:n, j * 512:(j + 1) * 512], in0=PS[j][:n, :],
                                                      in1=bcA[:n, j * 512:(j + 1) * 512], op=ALU.mult),
                 r=[PS[j], bcA], w=[tmp])
        P.op("dve", lambda e: e.scalar_tensor_tensor(out=xt[:n, :], in0=xt[:n, :], scalar=ALPHA, in1=tmp[:n, :],
                                                     op0=ALU.mult, op1=ALU.add), r=[xt, tmp], w=[xt])
        layer_norm_tile(P, n, xt, tmp, bcB, bcC, st)
        ld("sp", x1_d[t0:t0 + n, :], xt[:n, :], x1_d, [xt])
        for kc in range(16):
            P.op("pe", lambda e, kc=kc: e.transpose(PS[4 + kc // 4][:, (kc % 4) * 128:(kc % 4) * 128 + n],
                                                    xt[:n, kc * 128:(kc + 1) * 128], ident[:n, :n]),
                 r=[xt, ident], w=[PS[4 + kc // 4]])
        for kc in range(16):
            P.op("act", lambda e, kc=kc: e.activation(out=tokT32[:, kc, :n],
                                                      in_=PS[4 + kc // 4][:, (kc % 4) * 128:(kc % 4) * 128 + n],
                                                      func=AF.Identity, bias=mcol[s][:, 48 + kc:49 + kc],
                                                      scale=sc2p1[s][:, kc:kc + 1]),
                 r=[PS[4 + kc // 4], mcol[s], sc2p1[s]], w=[tokT32])
        ld("sp", tokT_d.t[:, t0:t0 + n].rearrange("(kc p) t -> p kc t", p=128), tokT32[:, :, :n], tokT_d, [tokT32])
        for kc in range(16):
            P.op("pe", lambda e, kc=kc: e.matmul(PS[0][:n, 0:NE], tokT32[:, kc, :n], rws[:, kc, :],
                                                 start=(kc == 0), stop=(kc == 15)), r=[tokT32, rws], w=[PS[0]])
        sc, sl, eq, s2, t2, msk, gsel = (sm[k] for k in ["sc", "sel", "eq", "s2", "t2", "msk", "gs"])
        m1, m2, gs, gm = (sm4[k] for k in ["m1", "m2", "gs", "gm"])
        gmax, den = sm1["gmax"], sm1["den"]
        v3 = lambda b: b[:n, :].rearrange("p (g e) -> p g e", g=4)
        b4 = lambda b: b[:n, :].unsqueeze(2).to_broadcast([n, 4, 4])
        P.op("act", lambda e: e.activation(out=sc[:n, :], in_=PS[0][:n, 0:NE], func=AF.Sigmoid), r=[PS[0]], w=[sc])
        P.op("dve", lambda e: e.tensor_tensor(out=sl[:n, :], in0=sc[:n, :], in1=rbb[:n, :], op=ALU.add), r=[sc, rbb], w=[sl])
        P.op("dve", lambda e: e.tensor_reduce(out=m1[:n, :], in_=v3(sl), axis=AX.X, op=ALU.max), r=[sl], w=[m1])
        P.op("dve", lambda e: e.tensor_tensor(out=v3(eq), in0=v3(sl), in1=b4(m1), op=ALU.is_equal), r=[sl, m1], w=[eq])
        P.op("dve", lambda e: e.scalar_tensor_tensor(out=s2[:n, :], in0=eq[:n, :], scalar=-1e9, in1=sl[:n, :],
                                                     op0=ALU.mult, op1=ALU.add), r=[eq, sl], w=[s2])
        P.op("dve", lambda e: e.tensor_reduce(out=m2[:n, :], in_=v3(s2), axis=AX.X, op=ALU.max), r=[s2], w=[m2])
        P.op("dve", lambda e: e.tensor_tensor(out=gs[:n, :], in0=m1[:n, :], in1=m2[:n, :], op=ALU.add), r=[m1, m2], w=[gs])
        P.op("dve", lambda e: e.tensor_reduce(out=gmax[:n, :], in_=gs[:n, :], axis=AX.X, op=ALU.max), r=[gs], w=[gmax])
        P.op("dve", lambda e: e.tensor_scalar(out=gm[:n, :], in0=gs[:n, :], scalar1=gmax[:n, 0:1], scalar2=None,
                                              op0=ALU.is_equal), r=[gs, gmax], w=[gm])
        P.op("dve", lambda e: e.tensor_tensor(out=v3(t2), in0=v3(sl), in1=b4(m2), op=ALU.is_ge), r=[sl, m2], w=[t2])
        P.op("dve", lambda e: e.tensor_tensor(out=v3(msk), in0=v3(t2), in1=b4(gm), op=ALU.mult), r=[t2, gm], w=[msk])
        P.op("dve", lambda e: e.tensor_tensor(out=gsel[:n, :], in0=sc[:n, :], in1=msk[:n, :], op=ALU.mult), r=[sc, msk], w=[gsel])
        P.op("dve", lambda e: e.reduce_sum(out=den[:n, :], in_=gsel[:n, :], axis=AX.X), r=[gsel], w=[den])
        P.op("dve", lambda e: e.reciprocal(out=den[:n, :], in_=den[:n, :]), r=[den], w=[den])
        P.op("dve", lambda e: e.tensor_scalar(out=gsel[:n, :], in0=gsel[:n, :], scalar1=den[:n, 0:1], scalar2=None,
                                              op0=ALU.mult), r=[gsel, den], w=[gsel])
        P.op("pe", lambda e: e.transpose(PS[1][0:16, 0:n], gsel[:n, :], ident[:n, :n]), r=[gsel, ident], w=[PS[1]])
        P.op("act", lambda e: e.activation(out=gtt[:, 0:n], in_=PS[1][0:16, 0:n], func=AF.Copy), r=[PS[1]], w=[gtt])
        ld("sp", gt_d[:, t0:t0 + n], gtt[:, 0:n], gt_d, [gtt])

    P.barrier()
    stk1.close()
    stk2 = ExitStack()
    tb = P.sbs(stk2, [128, 16, 512], BF16)
    acc = P.sbs(stk2, [128, 16, 512])
    hTs = [P.sbs(stk2, [128, 8, 512], BF16) for _ in range(2)]
    sg = [P.sbs(stk2, [128, 512]) for _ in range(2)]
    t1 = [P.sbs(stk2, [128, 512]) for _ in range(2)]
    gb = P.sbs(stk2, [128, 512])
    GTb = P.sbs(stk2, [16, 512])
    wgs = [P.sbs(stk2, [128, 16, 128], BF16) for _ in range(2)]
    wus = [P.sbs(stk2, [128, 16, 128], BF16) for _ in range(2)]
    wdss = [P.sbs(stk2, [128, 8, D], BF16) for _ in range(2)]
    wgb_d = P.dram("wgb_d", [NEXP, 8, 128, 16 * 128], BF16)
    wub_d = P.dram("wub_d", [NEXP, 8, 128, 16 * 128], BF16)
    wdb_d = P.dram("wdb_d", [NEXP, 128, 8 * D], BF16)
    for ex in range(NEXP):
        for hc in range(8):
            P.dma("pool", wgb_d.t[ex, hc].rearrange("p (kc h) -> p kc h", h=128),
                  wg.t[ex][:, hc * 128:(hc + 1) * 128].rearrange("(kc p) h -> p kc h", p=128), r=[wg], w=[wgb_d])
            P.dma("pool", wub_d.t[ex, hc].rearrange("p (kc h) -> p kc h", h=128),
                  wu.t[ex][:, hc * 128:(hc + 1) * 128].rearrange("(kc p) h -> p kc h", p=128), r=[wu], w=[wub_d])
        P.dma("pool", wdb_d.t[ex].rearrange("p (hc d) -> p hc d", d=D), wd.t[ex].rearrange("(hc p) d -> p hc d", p=128), r=[wd], w=[wdb_d])
    ld("sp", bcB[:], bc_ap(lng[1, :], D), bcB, [lng])
    ld("sp", bcC[:], bc_ap(lnb[1, :], D), bcC, [lnb])
    blocks = [(b0, 512, 0) for b0 in range(0, T_l, 512)]
    if T_c:
        blocks.append((T_l, T_c, 1))
    pc = 0
    for (b0, nb, s) in blocks:
        ld("pool", tb[:, :, :nb], tokT_d.t[:, b0:b0 + nb].rearrange("(kc p) t -> p kc t", p=128), tb, [tokT_d])
        ld("sp", GTb[:, :nb], gt_d[:, b0:b0 + nb], GTb, [gt_d])
        for ex in range(NEXP):
            hT = hTs[ex % 2]; wds = wdss[ex % 2]
            ld("sp", wds[:].rearrange("p hc d -> p (hc d)"), wdb_d[ex, :, :], wds, [wdb_d])
            P.op("pe", lambda e, ex=ex: e.matmul(PS[2][:, :nb], sel[:, ex * 128:(ex + 1) * 128], GTb[:, :nb],
                                                 start=True, stop=True), r=[sel, GTb], w=[PS[2]])
            P.op("act", lambda e: e.activation(out=gb[:, :nb], in_=PS[2][:, :nb], func=AF.Copy), r=[PS[2]], w=[gb])
            for hc in range(8):
                a, b = wgs[pc % 2], wus[pc % 2]
                sgi, t1i = sg[pc % 2], t1[pc % 2]
                pg, pu = PS[(pc % 2) * 2], PS[(pc % 2) * 2 + 1]
                pc += 1
                ld("sp", a[:].rearrange("p kc h -> p (kc h)"), wgb_d[ex, hc, :, :], a, [wgb_d])
                ld("act", b[:].rearrange("p kc h -> p (kc h)"), wub_d[ex, hc, :, :], b, [wub_d])
                for kc in range(16):
                    P.op("pe", lambda e, kc=kc, a=a, pg=pg: e.matmul(pg[:, :nb], a[:, kc, :], tb[:, kc, :nb], start=(kc == 0),
                                                                   stop=(kc == 15)), r=[a, tb], w=[pg])
                for kc in range(16):
                    P.op("pe", lambda e, kc=kc, b=b, pu=pu: e.matmul(pu[:, :nb], b[:, kc, :], tb[:, kc, :nb], start=(kc == 0),
                                                                   stop=(kc == 15)), r=[b, tb], w=[pu])
                P.op("act", lambda e, pg=pg, sgi=sgi: e.activation(out=sgi[:, :nb], in_=pg[:, :nb], func=AF.Silu), r=[pg], w=[sgi])
                P.op("dve", lambda e, pu=pu, t1i=t1i: e.tensor_tensor(out=t1i[:, :nb], in0=pu[:, :nb], in1=gb[:, :nb], op=ALU.mult),
                     r=[pu, gb], w=[t1i])
                P.op("dve", lambda e, hc=hc, sgi=sgi, t1i=t1i, hT=hT: e.tensor_tensor(out=hT[:, hc, :nb], in0=sgi[:, :nb], in1=t1i[:, :nb],
                                                                              op=ALU.mult), r=[sgi, t1i], w=[hT])
            for dc in range(16):
                pd = PS[4 + dc % 4]
                for hc in range(8):
                    P.op("pe", lambda e, dc=dc, hc=hc, pd=pd, wds=wds, hT=hT: e.matmul(pd[:, :nb], wds[:, hc, dc * 128:(dc + 1) * 128], hT[:, hc, :nb],
                                                                     start=(hc == 0), stop=(hc == 7)), r=[wds, hT], w=[pd])
                if ex == 0:
                    P.op("act", lambda e, dc=dc, pd=pd: e.activation(out=acc[:, dc, :nb], in_=pd[:, :nb], func=AF.Copy), r=[pd], w=[acc])
                else:
                    P.op("dve", lambda e, dc=dc, pd=pd: e.tensor_tensor(out=acc[:, dc, :nb], in0=acc[:, dc, :nb], in1=pd[:, :nb],
                                                                       op=ALU.add), r=[pd, acc], w=[acc])
        for dc in range(16):
            P.op("dve", lambda e, dc=dc: e.tensor_scalar(out=acc[:, dc, :nb], in0=acc[:, dc, :nb], scalar1=mcol[s][:, 80 + dc:81 + dc],
                                                        scalar2=None, op0=ALU.mult), r=[acc, mcol[s]], w=[acc])
        for j0 in range(0, nb, 128):
            n = min(128, nb - j0)
            t0 = b0 + j0
            for dc in range(16):
                P.op("pe", lambda e, dc=dc: e.transpose(PS[dc // 4][:n, (dc % 4) * 128:(dc % 4 + 1) * 128],
                                                        acc[:, dc, j0:j0 + n], ident[:, :]), r=[acc, ident], w=[PS[dc // 4]])
            ld("sp", xt[:n, :], x1_d[t0:t0 + n, :], xt, [x1_d])
            for j in range(4):
                P.op("dve", lambda e, j=j: e.scalar_tensor_tensor(out=xt[:n, j * 512:(j + 1) * 512], in0=xt[:n, j * 512:(j + 1) * 512],
                                                                 scalar=ALPHA, in1=PS[j][:n, :], op0=ALU.mult, op1=ALU.add),
                     r=[xt, PS[j]], w=[xt])
            layer_norm_tile(P, n, xt, tmp, bcB, bcC, st)
            ld("sp", xout[t0:t0 + n, :], xt[:n, :], xout, [xt])
    P.finish([xout])
    P.barrier()
    stk2.close()
    return P


def build_mod():
    P = Prog()
    IN = "ExternalInput"
    NCOL = 1536
    cT = P.dram("cT", [128, 16 * 3], F32, IN)
    w = P.dram("w", [2, D, NCOL], F32, IN)
    b = P.dram("b", [2, NCOL], F32, IN)
    out = P.dram("out", [2, 3, NCOL], F32, "ExternalOutput")
    ct = P.sb([128, 16, 3]); sg = P.sb([128, 16, 3])
    ring = [P.sb([128, NCOL]) for _ in range(3)]
    bb = P.sb([3, NCOL]); res = P.sb([3, NCOL])
    PS = [P.ps() for _ in range(3)]
    P.dma("sp", ct[:].rearrange("p k r -> p (k r)"), cT[:, :], r=[cT], w=[ct])
    P.op("act", lambda e: e.activation(out=sg[:], in_=ct[:], func=AF.Sigmoid), r=[ct], w=[sg])
    P.op("dve", lambda e: e.tensor_tensor(out=sg[:], in0=sg[:], in1=ct[:], op=ALU.mult), r=[sg, ct], w=[sg])
    i = 0
    for l in range(2):
        P.dma("sp", bb[:], bass.AP(b.t.tensor, b[l, :].offset, [[0, 3], [1, NCOL]]), r=[b], w=[bb])
        for kc in range(16):
            ws = ring[i % 3]; i += 1
            P.dma("sp", ws[:], w[l, kc * 128:(kc + 1) * 128, :], r=[w], w=[ws])
            for j in range(3):
                P.op("pe", lambda e, j=j, kc=kc, ws=ws: e.matmul(PS[j][0:3, :], sg[:, kc, :], ws[:, j * 512:(j + 1) * 512],
                                                                start=(kc == 0), stop=(kc == 15)), r=[sg, ws], w=[PS[j]])
        for j in range(3):
            P.op("dve", lambda e, j=j: e.tensor_tensor(out=res[:, j * 512:(j + 1) * 512], in0=PS[j][0:3, :],
                                                      in1=bb[:, j * 512:(j + 1) * 512], op=ALU.add), r=[PS[j], bb], w=[res])
        P.dma("sp", out[l, :, :], res[:], r=[res], w=[out])
    P.finish([out])
    return P


def build_ssd(LAT=16384, CTX=256, NU=2):
    P = Prog()
    IN = "ExternalInput"
    TB = CTX + LAT
    NCH = 6
    xT = P.dram("xT", [D, TB], F32, IN)
    modcol = P.dram("modcol", [2, 128, 32], F32, IN)
    wch = P.dram("wch", [NU, D, 768], F32, IN)
    wtm = P.dram("wtm", [NU, D, 528], F32, IN)
    cw = P.dram("cw", [NU, 128, NCH * 10], F32, IN)
    hp = P.dram("hp", [NU, 48], F32, IN)
    ng = P.dram("ng", [NU, 512], F32, IN)
    consts = P.dram("consts", [6, 128, 128], F32, IN)
    ymix = P.dram("ymix", [NU, LAT, 512], F32, "ExternalOutput")
    xbc_d = P.dram("xbc_d", [768, TB], F32)
    xc_d = P.dram("xc_d", [768, TB], F32)
    z_d = P.dram("z_d", [TB, 528], F32)
    yf_d = P.dram("yf_d", [LAT, 512], F32)
    sb = P.sb
    C = [sb([128, 128]) for _ in range(6)]
    for i in range(6):
        P.dma("sp", C[i][:], consts[i, :, :], r=[consts], w=[C[i]])
    ident, ones, TriF, TriB, SF, SB_ = C
    mcol = [sb([128, 32]) for _ in range(2)]
    scp1 = [sb([128, 16]) for _ in range(2)]
    for s in range(2):
        P.dma("sp", mcol[s][:], modcol[s, :, :], r=[modcol], w=[mcol[s]])
        P.op("dve", lambda e, s=s: e.tensor_scalar(out=scp1[s][:], in0=mcol[s][:, 16:32], scalar1=1.0, scalar2=None,
                                                    op0=ALU.add), r=[mcol[s]], w=[scp1[s]])
    PS = [P.ps() for _ in range(8)]
    wchs = sb([128, 16, 768], BF16)
    wtms = sb([128, 16, 528], BF16)
    xin32 = sb([128, 16, 512])
    hT = sb([128, 16, 512], BF16)
    stage = [sb([128, 528]) for _ in range(2)]
    cws = sb([128, NCH * 10])
    hpb = sb([128, 48]); aneg = sb([128, 16]); dsum = sb([128, 8]); ngb = sb([128, 512])
    R = min(32, LAT // 64)
    cin = sb([128, (R + 2) * 64]); cout = sb([128, max(R * 64, CTX)])

    for u in range(NU):
        P.dma("pool", wchs[:], wch.t[u].rearrange("(kc p) c -> p kc c", p=128), r=[wch], w=[wchs])
        P.dma("pool", wtms[:], wtm.t[u].rearrange("(kc p) c -> p kc c", p=128), r=[wtm], w=[wtms])
        P.dma("sp", cws[:], cw[u, :, :], r=[cw], w=[cws])
        P.dma("sp", hpb[:], bass.AP(hp.t.tensor, hp[u, :].offset, [[0, 128], [1, 48]]), r=[hp], w=[hpb])
        P.dma("sp", ngb[:], bass.AP(ng.t.tensor, ng[u, :].offset, [[0, 128], [1, 512]]), r=[ng], w=[ngb])
        P.op("act", lambda e: e.activation(out=aneg[:], in_=hpb[:, 16:32], func=AF.Exp), r=[hpb], w=[aneg])
        P.op("dve", lambda e: e.tensor_scalar(out=aneg[:], in0=aneg[:], scalar1=-1.0, scalar2=None, op0=ALU.mult), r=[aneg], w=[aneg])
        P.op("dve", lambda e: e.tensor_tensor(out=dsum[:], in0=hpb[:, 32:40], in1=hpb[:, 40:48], op=ALU.add), r=[hpb], w=[dsum])
        blocks = [(0, CTX, 1)] + [(CTX + i * 512, 512, 0) for i in range(LAT // 512)]
        si = 0
        for (t0, nb, s) in blocks:
            P.dma("sp", xin32[:, :, :nb], xT.t[:, t0:t0 + nb].rearrange("(kc p) t -> p kc t", p=128), r=[xT], w=[xin32])
            for kc in range(16):
                P.op("act", lambda e, kc=kc: e.activation(out=hT[:, kc, :nb], in_=xin32[:, kc, :nb], func=AF.Identity,
                                                          bias=mcol[s][:, kc:kc + 1], scale=scp1[s][:, kc:kc + 1]),
                     r=[xin32, mcol[s], scp1[s]], w=[hT])
            for m in range(NCH):
                pp = PS[m % 4]
                for kc in range(16):
                    P.op("pe", lambda e, m=m, kc=kc, pp=pp: e.matmul(pp[:, :nb], wchs[:, kc, m * 128:(m + 1) * 128], hT[:, kc, :nb],
                                                                   start=(kc == 0), stop=(kc == 15)), r=[wchs, hT], w=[pp])
                sg = stage[si % 2]; si += 1
                P.op("act", lambda e, pp=pp, sg=sg: e.activation(out=sg[:, :nb], in_=pp[:, :nb], func=AF.Copy), r=[pp], w=[sg])
                P.dma("sp", xbc_d[m * 128:(m + 1) * 128, t0:t0 + nb], sg[:, :nb], r=[sg], w=[xbc_d])
            for j0 in range(0, nb, 128):
                pz, pd = PS[4 + (j0 // 128) % 2 * 2], PS[5 + (j0 // 128) % 2 * 2]
                for kc in range(16):
                    P.op("pe", lambda e, kc=kc, pz=pz: e.matmul(pz[:, :], hT[:, kc, j0:j0 + 128], wtms[:, kc, 0:512],
                                                              start=(kc == 0), stop=(kc == 15)), r=[wtms, hT], w=[pz])
                for kc in range(16):
                    P.op("pe", lambda e, kc=kc, pd=pd: e.matmul(pd[:, 0:16], hT[:, kc, j0:j0 + 128], wtms[:, kc, 512:528],
                                                              start=(kc == 0), stop=(kc == 15)), r=[wtms, hT], w=[pd])
                sg = stage[si % 2]; si += 1
                P.op("act", lambda e, pz=pz, sg=sg: e.activation(out=sg[:, 0:512], in_=pz[:, :], func=AF.Copy), r=[pz], w=[sg])
                P.op("dve", lambda e, pd=pd, sg=sg: e.tensor_copy(out=sg[:, 512:528], in_=pd[:, 0:16]), r=[pd], w=[sg])
                P.dma("sp", z_d[t0 + j0:t0 + j0 + 128, :], sg[:, :], r=[sg], w=[z_d])
        for m in range(NCH):
            wv = lambda k: cws[:, m * 10 + k:m * 10 + k + 1]
            P.dma("sp", cin[:, 0:CTX], xbc_d[m * 128:(m + 1) * 128, 0:CTX], r=[xbc_d], w=[cin])
            P.op("dve", lambda e: e.tensor_scalar(out=cout[:, 0:CTX], in0=cin[:, 0:CTX], scalar1=wv(4), scalar2=wv(9),
                                                  op0=ALU.mult, op1=ALU.add), r=[cin, cws], w=[cout])
            P.op("dve", lambda e: e.scalar_tensor_tensor(out=cout[:, 1:CTX], in0=cin[:, 0:CTX - 1], scalar=wv(3), in1=cout[:, 1:CTX],
                                                         op0=ALU.mult, op1=ALU.add), r=[cin, cws, cout], w=[cout])
            P.op("dve", lambda e: e.scalar_tensor_tensor(out=cout[:, 0:CTX - 1], in0=cin[:, 1:CTX], scalar=wv(5), in1=cout[:, 0:CTX - 1],
                                                         op0=ALU.mult, op1=ALU.add), r=[cin, cws, cout], w=[cout])
            P.op("act", lambda e: e.activation(out=cout[:, 0:CTX], in_=cout[:, 0:CTX], func=AF.Silu), r=[cout], w=[cout])
            P.dma("sp", xc_d[m * 128:(m + 1) * 128, 0:CTX], cout[:, 0:CTX], r=[cout], w=[xc_d])
            NR = LAT // 64
            for r0 in range(0, NR, R):
                lo, hi = r0 - 1, r0 + R + 1
                if lo < 0:
                    P.op("dve", lambda e: e.memset(cin[:, 0:64], 0.0), w=[cin])
                if hi > NR:
                    P.op("dve", lambda e: e.memset(cin[:, (R + 1) * 64:(R + 2) * 64], 0.0), w=[cin])
                a, b_ = max(lo, 0), min(hi, NR)
                P.dma("sp", cin[:, (a - lo) * 64:(b_ - lo) * 64], xbc_d[m * 128:(m + 1) * 128, CTX + a * 64:CTX + b_ * 64],
                      r=[xbc_d], w=[cin])
                ci3 = cin[:, :].rearrange("p (r c) -> p r c", c=64)
                co3 = cout[:, :].rearrange("p (r c) -> p r c", c=64)
                P.op("dve", lambda e: e.tensor_scalar(out=co3[:, :, :], in0=ci3[:, 1:R + 1, :], scalar1=wv(4), scalar2=wv(9),
                                                      op0=ALU.mult, op1=ALU.add), r=[cin, cws], w=[cout])
                for i in range(3):
                    for j in range(3):
                        if i == 1 and j == 1:
                            continue
                        if j == 0:
                            o_, i_ = co3[:, :, 1:64], ci3[:, i:i + R, 0:63]
                        elif j == 1:
                            o_, i_ = co3[:, :, :], ci3[:, i:i + R, :]
                        else:
                            o_, i_ = co3[:, :, 0:63], ci3[:, i:i + R, 1:64]
                        P.op("dve", lambda e, o_=o_, i_=i_, k=i * 3 + j: e.scalar_tensor_tensor(out=o_, in0=i_, scalar=wv(k), in1=o_,
                                                                                           op0=ALU.mult, op1=ALU.add),
                             r=[cin, cws, cout], w=[cout])
                P.op("act", lambda e: e.activation(out=cout[:, :], in_=cout[:, :], func=AF.Silu), r=[cout], w=[cout])
                P.dma("sp", xc_d[m * 128:(m + 1) * 128, CTX + r0 * 64:CTX + (r0 + R) * 64], cout[:, :], r=[cout], w=[xc_d])
        ssd_scan(P, u, PS, (ident, ones, TriF, TriB, SF, SB_), xc_d, z_d, yf_d, ymix, hpb, aneg, dsum, ngb, LAT, CTX)
    P.finish([ymix])
    return P


def ssd_scan(P, u, PS, consts, xc_d, z_d, yf_d, ymix, hpb, aneg, dsum, ngb, LAT, CTX):
    ident, ones, TriF, TriB, SF, SB_ = consts
    sb = P.sb
    if not hasattr(P, "_ssd_bufs"):
        B = {}
        B["CT"] = sb([128, 128]); B["BT"] = sb([128, 128]); B["CTb"] = sb([128, 128], BF16); B["BTb"] = sb([128, 128], BF16)
        B["xcm"] = sb([128, 4, 128]); B["xtm"] = sb([128, 512]); B["Btm"] = sb([128, 128], BF16)
        B["zt"] = sb([128, 528]); B["dt"] = sb([128, 8]); B["a"] = sb([128, 8]); B["e8"] = sb([128, 8])
        B["acs"] = sb([128, 8]); B["eacs"] = sb([128, 8]); B["tail"] = sb([128, 8]); B["etot"] = sb([128, 8])
        B["xdt"] = sb([128, 512], BF16); B["xdtt"] = sb([128, 512], BF16)
        B["cbm"] = sb([128, 128]); B["lh"] = [sb([128, 128]) for _ in range(2)]; B["L"] = [sb([128, 128]) for _ in range(2)]
        B["M"] = [sb([128, 128], BF16) for _ in range(2)]
        B["S"] = sb([128, 512]); B["Sb"] = sb([128, 512], BF16); B["tS"] = sb([128, 512])
        B["y"] = sb([128, 512]); B["yf"] = sb([128, 512]); B["sz"] = sb([128, 512]); B["st"] = sb([128, 4])
        P._ssd_bufs = B
    B = P._ssd_bufs
    CT, BT, CTb, BTb, xcm, xtm, Btm, zt = (B[k] for k in ["CT", "BT", "CTb", "BTb", "xcm", "xtm", "Btm", "zt"])
    dt, a, e8, acs, eacs, tail, etot = (B[k] for k in ["dt", "a", "e8", "acs", "eacs", "tail", "etot"])
    xdt, xdtt, cbm, S, Sb, tS, y, yf, sz, st = (B[k] for k in ["xdt", "xdtt", "cbm", "S", "Sb", "tS", "y", "yf", "sz", "st"])
    bc8 = lambda t: t[:, 0:8].unsqueeze(2).to_broadcast([128, 8, 64])
    v3 = lambda t: t[:, :].rearrange("p (h q) -> p h q", h=8)
    nctx, nlat = CTX // 128, LAT // 128
    for d in range(2):
        Tri, SM = (TriF, SF) if d == 0 else (TriB, SB_)
        P.op("dve", lambda e: e.memset(S[:], 0.0), w=[S])
        P.op("dve", lambda e: e.memset(Sb[:], 0.0), w=[Sb])
        order = list(range(nctx)) + [nctx + i for i in range(nlat)]
        if d == 1:
            order = list(range(nctx))[::-1] + [nctx + i for i in range(nlat)][::-1]
        for ci, c in enumerate(order):
            p0 = c * 128
            lat = c >= nctx
            l0 = p0 - CTX
            last_state = (ci == len(order) - 1)
            P.dma("sp", CT[:], xc_d[640:768, p0:p0 + 128], r=[xc_d], w=[CT])
            P.dma("sp", BT[:], xc_d[512:640, p0:p0 + 128], r=[xc_d], w=[BT])
            P.dma("sp", xcm[:], xc_d.t[0:512, p0:p0 + 128].rearrange("(m p) t -> p m t", p=128), r=[xc_d], w=[xcm])
            P.dma("sp", zt[:], z_d[p0:p0 + 128, :], r=[z_d], w=[zt])
            P.op("act", lambda e: e.activation(out=CTb[:], in_=CT[:], func=AF.Copy), r=[CT], w=[CTb])
            P.op("act", lambda e: e.activation(out=BTb[:], in_=BT[:], func=AF.Copy), r=[BT], w=[BTb])
            for m in range(4):
                P.op("pe", lambda e, m=m: e.transpose(PS[0][:, m * 128:(m + 1) * 128], xcm[:, m, :], ident[:, :]), r=[xcm, ident], w=[PS[0]])
            P.op("act", lambda e: e.activation(out=xtm[:], in_=PS[0][:, :], func=AF.Copy), r=[PS[0]], w=[xtm])
            P.op("pe", lambda e: e.transpose(PS[1][:, 0:128], BT[:, :], ident[:, :]), r=[BT, ident], w=[PS[1]])
            P.op("act", lambda e: e.activation(out=Btm[:], in_=PS[1][:, 0:128], func=AF.Copy), r=[PS[1]], w=[Btm])
            P.op("dve", lambda e: e.tensor_tensor(out=e8[:], in0=zt[:, 512 + d * 8:520 + d * 8], in1=hpb[:, d * 8:d * 8 + 8], op=ALU.add),
                 r=[zt, hpb], w=[e8])
            P.op("act", lambda e: e.activation(out=e8[:], in_=e8[:], func=AF.Exp), r=[e8], w=[e8])
            P.op("act", lambda e: e.activation(out=dt[:], in_=e8[:], func=AF.Ln, bias=1.0, scale=1.0), r=[e8], w=[dt])
            P.op("dve", lambda e: e.tensor_tensor(out=a[:], in0=dt[:], in1=aneg[:, d * 8:d * 8 + 8], op=ALU.mult), r=[dt, aneg], w=[a])
            P.op("pe", lambda e: e.matmul(PS[2][:, 0:8], Tri[:, :], a[:, :], start=True, stop=True), r=[Tri, a], w=[PS[2]])
            P.op("pe", lambda e: e.matmul(PS[2][:, 8:16], ones[:, :], a[:, :], start=True, stop=True), r=[ones, a], w=[PS[2]])
            P.op("act", lambda e: e.activation(out=acs[:], in_=PS[2][:, 0:8], func=AF.Copy), r=[PS[2]], w=[acs])
            P.op("act", lambda e: e.activation(out=eacs[:], in_=PS[2][:, 0:8], func=AF.Exp), r=[PS[2]], w=[eacs])
            P.op("act", lambda e: e.activation(out=etot[:], in_=PS[2][:, 8:16], func=AF.Exp), r=[PS[2]], w=[etot])
            P.op("dve", lambda e: e.tensor_tensor(out=tail[:], in0=PS[2][:, 8:16], in1=acs[:], op=ALU.subtract), r=[PS[2], acs], w=[tail])
            P.op("act", lambda e: e.activation(out=tail[:], in_=tail[:], func=AF.Exp), r=[tail], w=[tail])
            P.op("dve", lambda e: e.tensor_tensor(out=v3(xdt), in0=v3(xtm), in1=bc8(dt), op=ALU.mult), r=[xtm, dt], w=[xdt])
            P.op("dve", lambda e: e.tensor_tensor(out=v3(xdtt), in0=v3(xdt), in1=bc8(tail), op=ALU.mult), r=[xdt, tail], w=[xdtt])
            if lat:
                P.op("pe", lambda e: e.matmul(PS[3][:, 0:128], BTb[:, :], CTb[:, :], start=True, stop=True), r=[BTb, CTb], w=[PS[3]])
                P.op("dve", lambda e: e.tensor_tensor(out=cbm[:], in0=PS[3][:, 0:128], in1=Tri[:, :], op=ALU.mult), r=[PS[3], Tri], w=[cbm])
                for h in range(8):
                    lh, L, M = B["lh"][h % 2], B["L"][h % 2], B["M"][h % 2]
                    pdf = PS[4 + h % 2]
                    P.op("dve", lambda e, h=h, lh=lh: e.tensor_scalar(out=lh[:], in0=SM[:, :], scalar1=a[:, h:h + 1], scalar2=None,
                                                                    op0=ALU.mult), r=[SM, a], w=[lh])
                    P.op("pe", lambda e, lh=lh, pdf=pdf: e.matmul(pdf[:, 0:128], lh[:, :], Tri[:, :], start=True, stop=True), r=[lh, Tri], w=[pdf])
                    P.op("act", lambda e, L=L, pdf=pdf: e.activation(out=L[:], in_=pdf[:, 0:128], func=AF.Exp), r=[pdf], w=[L])
                    P.op("dve", lambda e, L=L, M=M: e.tensor_tensor(out=M[:], in0=L[:], in1=cbm[:], op=ALU.mult), r=[L, cbm], w=[M])
                    P.op("pe", lambda e, h=h, M=M: e.matmul(PS[6][:, h * 64:(h + 1) * 64], M[:, :], xdt[:, h * 64:(h + 1) * 64],
                                                          start=True, stop=True), r=[M, xdt], w=[PS[6]])
                P.op("pe", lambda e: e.matmul(PS[7][:, :], CTb[:, :], Sb[:, :], start=True, stop=True), r=[CTb, Sb], w=[PS[7]])
                P.op("dve", lambda e: e.tensor_tensor(out=v3(y), in0=PS[7][:, :].rearrange("p (h q) -> p h q", h=8), in1=bc8(eacs), op=ALU.mult),
                     r=[PS[7], eacs], w=[y])
                P.op("dve", lambda e: e.tensor_tensor(out=y[:], in0=y[:], in1=PS[6][:, :], op=ALU.add), r=[y, PS[6]], w=[y])
                if d == 0:
                    P.dma("sp", yf_d[l0:l0 + 128, :], y[:], r=[y], w=[yf_d])
                else:
                    P.dma("sp", yf[:], yf_d[l0:l0 + 128, :], r=[yf_d], w=[yf])
                    P.op("dve", lambda e: e.tensor_tensor(out=y[:], in0=y[:], in1=yf[:], op=ALU.add), r=[y, yf], w=[y])
                    P.op("dve", lambda e: e.tensor_tensor(out=v3(yf), in0=v3(xtm), in1=bc8(dsum), op=ALU.mult), r=[xtm, dsum], w=[yf])
                    P.op("dve", lambda e: e.tensor_tensor(out=y[:], in0=y[:], in1=yf[:], op=ALU.add), r=[y, yf], w=[y])
                    P.op("act", lambda e: e.activation(out=sz[:], in_=zt[:, 0:512], func=AF.Silu), r=[zt], w=[sz])
                    P.op("dve", lambda e: e.tensor_tensor(out=y[:], in0=y[:], in1=sz[:], op=ALU.mult), r=[y, sz], w=[y])
                    P.op("act", lambda e: e.activation(out=sz[:], in_=y[:], func=AF.Square), r=[y], w=[sz])
                    P.op("dve", lambda e: e.reduce_sum(out=st[:, 0:1], in_=sz[:], axis=AX.X), r=[sz], w=[st])
                    P.op("dve", lambda e: e.tensor_scalar(out=st[:, 0:1], in0=st[:, 0:1], scalar1=1.0 / 512, scalar2=EPS,
                                                          op0=ALU.mult, op1=ALU.add), r=[st], w=[st])
                    P.op("act", lambda e: e.activation(out=st[:, 0:1], in_=st[:, 0:1], func=AF.Sqrt), r=[st], w=[st])
                    P.op("dve", lambda e: e.reciprocal(out=st[:, 1:2], in_=st[:, 0:1]), r=[st], w=[st])
                    P.op("dve", lambda e: e.scalar_tensor_tensor(out=y[:], in0=y[:], scalar=st[:, 1:2], in1=ngb[:], op0=ALU.mult,
                                                                 op1=ALU.mult), r=[y, st, ngb], w=[y])
                    P.dma("sp", ymix[u, l0:l0 + 128, :], y[:], r=[y], w=[ymix])
            if not last_state:
                P.op("pe", lambda e: e.matmul(PS[3][:, :], Btm[:, :], xdtt[:, :], start=True, stop=True), r=[Btm, xdtt], w=[PS[3]])
                P.op("dve", lambda e: e.tensor_tensor(out=v3(tS), in0=v3(S), in1=bc8(etot), op=ALU.mult), r=[S, etot], w=[tS])
                P.op("dve", lambda e: e.tensor_tensor(out=S[:], in0=tS[:], in1=PS[3][:, :], op=ALU.add), r=[tS, PS[3]], w=[S])
                P.op("act", lambda e: e.activation(out=Sb[:], in_=S[:], func=AF.Copy), r=[S], w=[Sb])


def build_mix0(LAT=16384, CTX=256, fft=None):
    P = Prog()
    IN = "ExternalInput"
    TB = CTX + LAT
    if fft is None:
        fft = (LAT == 16384)
    xT = P.dram("xT", [2, D, TB], F32, IN)
    modcol = P.dram("modcol", [3, 128, 32], F32, IN)
    whc = P.dram("whc", [D, 384], F32, IN)
    wgc = P.dram("wgc", [D, 288], F32, IN)
    wgt = P.dram("wgt", [D, 512], F32, IN)
    hcw = P.dram("hcw", [128, 12], F32, IN)
    hlb = P.dram("hlb", [128, 2], F32, IN)
    fw1 = P.dram("fw1", [33, 64], F32, IN)
    fcol = P.dram("fcol", [64, 4], F32, IN)
    fw2 = P.dram("fw2", [64, 64], F32, IN)
    fw3 = P.dram("fw3", [64, 512], F32, IN)
    zl = P.dram("zl", [33, LAT], F32, IN); zc = P.dram("zc", [33, CTX], F32, IN)
    El = P.dram("El", [128, LAT], F32, IN); Ec = P.dram("Ec", [128, CTX], F32, IN)
    gw2 = P.dram("gw2", [2, 16, 128], F32, IN)
    gbc = P.dram("gbc", [128, 2], F32, IN)
    gng = P.dram("gng", [256], F32, IN)
    consts = P.dram("consts", [4, 128, 512], F32, IN)
    hy_out = P.dram("hy_out", [2, 128, TB], F32, "ExternalOutput")
    gla_out = P.dram("gla_out", [TB, 256], F32, "ExternalOutput")
    hu_d = P.dram("hu_d", [2, 384, TB], F32)
    gq_d = P.dram("gq_d", [256, TB], F32)
    gg_d = P.dram("gg_d", [2, 16, TB], F32)
    gvr_d = P.dram("gvr_d", [TB, 512], F32)
    of_d = P.dram("of_d", [TB, 256], F32)
    hf_d = {CTX: P.dram("hf_c", [4, 128, CTX], F32)}
    if fft:
        zr = P.dram("zr", [33, LAT], F32, IN); Er = P.dram("Er", [128, LAT], F32, IN)
        fF1 = P.dram("fF1", [128, 256], F32, IN); fTW = P.dram("fTW", [128, 512], F32, IN); fCS = P.dram("fCS", [128, 1024], F32, IN)
        fNSC = P.dram("fNSC", [128, 1024], F32, IN); fTWI = P.dram("fTWI", [128, 512], F32, IN); fC1S = P.dram("fC1S", [128, 128], F32, IN)
        hfull_d = P.dram("hfull_d", [2, 128, 2 * LAT], F32)
        Hr_d = P.dram("Hr_d", [2, 2, 128, 128, 128], F32); Hi_d = P.dram("Hi_d", [2, 2, 128, 128, 128], F32)
        cur_d = [P.dram("cur0_d", [128, LAT], F32), P.dram("cur1_d", [128, LAT], F32)]
        conv_d = P.dram("conv_d", [128, LAT], F32)
    else:
        hf_d[LAT] = P.dram("hf_l", [4, 128, LAT], F32)
    sb = P.sb
    ident = sb([128, 128]); mF = sb([64, 64]); mB = sb([64, 64]); rmask = sb([128, 512])
    P.dma("sp", ident[:], consts[0, :, 0:128], r=[consts], w=[ident])
    P.dma("sp", mF[:], consts[1, 0:64, 0:64], r=[consts], w=[mF])
    P.dma("sp", mB[:], consts[2, 0:64, 0:64], r=[consts], w=[mB])
    P.dma("sp", rmask[:], consts[3, :, :], r=[consts], w=[rmask])
    mcol = [sb([128, 32]) for _ in range(3)]
    scp1 = [sb([128, 16]) for _ in range(3)]
    for s in range(3):
        P.dma("sp", mcol[s][:], modcol[s, :, :], r=[modcol], w=[mcol[s]])
        P.op("dve", lambda e, s=s: e.tensor_scalar(out=scp1[s][:], in0=mcol[s][:, 16:32], scalar1=1.0, scalar2=None,
                                                    op0=ALU.add), r=[mcol[s]], w=[scp1[s]])
    PS = [P.ps() for _ in range(8)]
    blocks = [(0, CTX)] + [(CTX + i * 512, 512) for i in range(LAT // 512)]

    with ExitStack() as stk:
        whs = P.sbs(stk, [128, 16, 384], BF16); wgs = P.sbs(stk, [128, 16, 288], BF16); wts = P.sbs(stk, [128, 16, 512], BF16)
        xin32 = P.sbs(stk, [128, 16, 512]); hT = P.sbs(stk, [128, 16, 512], BF16)
        stage = [P.sbs(stk, [128, 512]) for _ in range(3)]
        P.dma("pool", whs[:], whc.t.rearrange("(kc p) c -> p kc c", p=128), r=[whc], w=[whs])
        P.dma("pool", wgs[:], wgc.t.rearrange("(kc p) c -> p kc c", p=128), r=[wgc], w=[wgs])
        P.dma("pool", wts[:], wgt.t.rearrange("(kc p) c -> p kc c", p=128), r=[wgt], w=[wts])
        si = 0
        for bi in range(2):
            for (t0, nb) in blocks:
                s = 2 if t0 < CTX else bi
                P.dma("sp", xin32[:, :, :nb], xT.t[bi, :, t0:t0 + nb].rearrange("(kc p) t -> p kc t", p=128), r=[xT], w=[xin32])
                for kc in range(16):
                    P.op("act", lambda e, kc=kc, s=s: e.activation(out=hT[:, kc, :nb], in_=xin32[:, kc, :nb], func=AF.Identity,
                                                                   bias=mcol[s][:, kc:kc + 1], scale=scp1[s][:, kc:kc + 1]),
                         r=[xin32, mcol[s], scp1[s]], w=[hT])
                jobs = [(whs, m * 128, 128, hu_d, (bi, slice(m * 128, (m + 1) * 128))) for m in range(3)]
                if bi == 0:
                    jobs += [(wgs, m * 128, 128, gq_d, (slice(m * 128, (m + 1) * 128),)) for m in range(2)]
                    jobs += [(wgs, 256 + dd * 16, 16, gg_d, (dd, slice(0, 16))) for dd in range(2)]
                for ji, (wsb, c0, mw, dst, idx) in enumerate(jobs):
                    pp = PS[ji % 4]
                    for kc in range(16):
                        P.op("pe", lambda e, kc=kc, pp=pp, wsb=wsb, c0=c0, mw=mw: e.matmul(pp[:mw, :nb], wsb[:, kc, c0:c0 + mw], hT[:, kc, :nb],
                                                                                        start=(kc == 0), stop=(kc == 15)), r=[wsb, hT], w=[pp])
                    sg = stage[si % 3]; si += 1
                    P.op("act", lambda e, pp=pp, sg=sg, mw=mw: e.activation(out=sg[:mw, :nb], in_=pp[:mw, :nb], func=AF.Copy), r=[pp], w=[sg])
                    P.dma("sp", dst.t[idx + (slice(t0, t0 + nb),)], sg[:mw, :nb], r=[sg], w=[dst])
                if bi == 0:
                    for j0 in range(0, nb, 128):
                        pz = PS[4 + (j0 // 128) % 4]
                        for kc in range(16):
                            P.op("pe", lambda e, kc=kc, pz=pz, j0=j0: e.matmul(pz[:, :], hT[:, kc, j0:j0 + 128], wts[:, kc, :],
                                                                             start=(kc == 0), stop=(kc == 15)), r=[wts, hT], w=[pz])
                        sg = stage[si % 3]; si += 1
                        P.op("act", lambda e, pz=pz, sg=sg: e.activation(out=sg[:, :], in_=pz[:, :], func=AF.Copy), r=[pz], w=[sg])
                        P.dma("sp", gvr_d[t0 + j0:t0 + j0 + 128, :], sg[:, :], r=[sg], w=[gvr_d])
        P.barrier()

    with ExitStack() as stk:
        w1s = P.sbs(stk, [33, 64]); w2s = P.sbs(stk, [64, 64]); w3s = P.sbs(stk, [64, 512]); fc = P.sbs(stk, [64, 4])
        cws = P.sbs(stk, [128, 12]); lbs = P.sbs(stk, [128, 2])
        for dst, src in [(w1s, fw1), (w2s, fw2), (w3s, fw3), (fc, fcol), (cws, hcw), (lbs, hlb)]:
            P.dma("sp", dst[:], src[:, :], r=[src], w=[dst])
        zb = P.sbs(stk, [33, 512]); h1 = P.sbs(stk, [64, 512]); h2 = P.sbs(stk, [64, 512]); eb = P.sbs(stk, [128, 512])
        hst = [P.sbs(stk, [128, 512]) for _ in range(2)]
        rr = P.sbs(stk, [64, 512])
        si = 0
        gen = [(CTX, zc, Ec, [(od, hf_d[CTX], (od,), 0) for od in range(4)])]
        if fft:
            gen.append((LAT, zl, El, [(0, hfull_d, (0,), 0), (2, hfull_d, (1,), 0)]))
            gen.append((LAT, zr, Er, [(1, hfull_d, (0,), LAT), (3, hfull_d, (1,), LAT)]))
        else:
            gen.append((LAT, zl, El, [(od, hf_d[LAT], (od,), 0) for od in range(4)]))
        for (L, zsrc, Esrc, ods) in gen:
            for p0 in range(0, L, 512):
                nb = min(512, L - p0)
                P.dma("sp", zb[:, :nb], zsrc[:, p0:p0 + nb], r=[zsrc], w=[zb])
                P.dma("sp", eb[:, :nb], Esrc[:, p0:p0 + nb], r=[Esrc], w=[eb])
                for (wsb, kdim, src, dst, bcol, fcolm) in [(w1s, 33, zb, h1, 0, 1), (w2s, 64, h1, h2, 2, 3)]:
                    P.op("pe", lambda e, wsb=wsb, kdim=kdim, src=src: e.matmul(PS[0][:64, :nb], wsb[:kdim, :], src[:kdim, :nb], start=True, stop=True),
                         r=[wsb, src], w=[PS[0]])
                    P.op("dve", lambda e, dst=dst, bcol=bcol, fcolm=fcolm: e.tensor_scalar(out=dst[:, :nb], in0=PS[0][:64, :nb], scalar1=fc[:, bcol:bcol + 1],
                                                                                         scalar2=fc[:, fcolm:fcolm + 1], op0=ALU.add, op1=ALU.mult),
                         r=[PS[0], fc], w=[dst])
                    P.op("dve", lambda e, dst=dst: e.tensor_scalar(out=rr[:, :nb], in0=dst[:, :nb], scalar1=1.0 / (2 * math.pi), scalar2=12582912.0,
                                                                   op0=ALU.mult, op1=ALU.add), r=[dst], w=[rr])
                    P.op("dve", lambda e, dst=dst: e.tensor_scalar(out=rr[:, :nb], in0=rr[:, :nb], scalar1=12582912.0, scalar2=-2 * math.pi,
                                                                   op0=ALU.subtract, op1=ALU.mult), r=[rr], w=[rr])
                    P.op("dve", lambda e, dst=dst: e.tensor_tensor(out=dst[:, :nb], in0=dst[:, :nb], in1=rr[:, :nb], op=ALU.add), r=[dst, rr], w=[dst])
                    P.op("act", lambda e, dst=dst: e.activation(out=dst[:, :nb], in_=dst[:, :nb], func=AF.Sin), r=[dst], w=[dst])
                for (od, dbuf, didx, coff) in ods:
                    P.op("pe", lambda e, od=od: e.matmul(PS[1 + od % 2][:, :nb], w3s[:, od * 128:(od + 1) * 128], h2[:, :nb], start=True, stop=True),
                         r=[w3s, h2], w=[PS[1 + od % 2]])
                    sg = hst[si % 2]; si += 1
                    P.op("dve", lambda e, od=od, sg=sg: e.tensor_tensor(out=sg[:, :nb], in0=PS[1 + od % 2][:, :nb], in1=eb[:, :nb], op=ALU.mult),
                         r=[PS[1 + od % 2], eb], w=[sg])
                    P.dma("sp", dbuf.t[didx + (slice(None), slice(coff + p0, coff + p0 + nb))], sg[:, :nb], r=[sg], w=[dbuf])
        LD = CTX if fft else LAT
        LB = min(2048, LAT)
        u = P.sbs(stk, [128, LD]); acc = P.sbs(stk, [128, LD])
        raw = P.sbs(stk, [128, LB + 2]); xg = P.sbs(stk, [128, LB])
        hring = [P.sbs(stk, [128, min(LB, LD)]) for _ in range(2)]

        def short_conv_block(bi, ti, s0, L, b0, n, tgt, tb_):
            wv = lambda k: cws[:, ti * 4 + k:ti * 4 + k + 1]
            lo, hi = b0 - 1, b0 + n + 1
            if lo < 0:
                P.op("dve", lambda e: e.memset(raw[:, 0:1], 0.0), w=[raw])
            if hi > L:
                P.op("dve", lambda e: e.memset(raw[:, n + 1:n + 2], 0.0), w=[raw])
            a, b_ = max(lo, 0), min(hi, L)
            P.dma("sp", raw[:, a - lo:b_ - lo], hu_d[bi, ti * 128:(ti + 1) * 128, s0 + a:s0 + b_], r=[hu_d], w=[raw])
            P.op("dve", lambda e: e.tensor_scalar(out=tgt, in0=raw[:, 1:n + 1], scalar1=wv(1), scalar2=wv(3), op0=ALU.mult, op1=ALU.add),
                 r=[raw, cws], w=[tb_])
            P.op("dve", lambda e: e.scalar_tensor_tensor(out=tgt, in0=raw[:, 0:n], scalar=wv(0), in1=tgt, op0=ALU.mult, op1=ALU.add),
                 r=[raw, cws, tb_], w=[tb_])
            P.op("dve", lambda e: e.scalar_tensor_tensor(out=tgt, in0=raw[:, 2:n + 2], scalar=wv(2), in1=tgt, op0=ALU.mult, op1=ALU.add),
                 r=[raw, cws, tb_], w=[tb_])

        def short_conv(bi, ti, s0, L, dstbuf, mul_into=None):
            for b0 in range(0, L, LB):
                n = min(LB, L - b0)
                if mul_into is None:
                    short_conv_block(bi, ti, s0, L, b0, n, dstbuf[:, b0:b0 + n], dstbuf)
                else:
                    short_conv_block(bi, ti, s0, L, b0, n, xg[:, :n], xg)
                    P.op("dve", lambda e: e.tensor_tensor(out=mul_into[:, b0:b0 + n], in0=mul_into[:, b0:b0 + n], in1=xg[:, :n], op=ALU.mult),
                         r=[mul_into, xg], w=[mul_into])

        if fft:
            F1 = P.sbs(stk, [128, 256]); TW = P.sbs(stk, [128, 4, 128]); CS = P.sbs(stk, [128, 2, 512]); NSC = P.sbs(stk, [128, 2, 512])
            TWI = P.sbs(stk, [128, 2, 256]); C1S = P.sbs(stk, [128, 2, 64])
            for dst, src in [(F1, fF1), (TW, fTW), (CS, fCS), (NSC, fNSC), (TWI, fTWI), (C1S, fC1S)]:
                P.dma("sp", dst[:].rearrange("p a b -> p (a b)") if len(dst.t.shape) == 3 else dst[:], src[:, :], r=[src], w=[dst])
            X = P.sbs(stk, [128, 4, 256]); Apr = P.sbs(stk, [128, 2, 4, 128]); Api = P.sbs(stk, [128, 2, 4, 128])
            tm1 = P.sbs(stk, [128, 512]); tm2 = P.sbs(stk, [128, 512])
            Hr = P.sbs(stk, [128, 2, 4, 128]); Hi = P.sbs(stk, [128, 2, 4, 128])
            Yr = P.sbs(stk, [128, 2, 4, 128]); Yi = P.sbs(stk, [128, 2, 4, 128])
            Br = P.sbs(stk, [128, 4, 256]); Bi = P.sbs(stk, [128, 4, 256]); yt = P.sbs(stk, [64, 4, 256])
            cb = P.sbs(stk, [128, LB]); vb2 = P.sbs(stk, [128, LB])

            def cmul(outr, outi, ar, ai, br, bi_, tv, rb, wbr, wbi):
                t1, t2 = tv(tm1), tv(tm2)
                P.op("dve", lambda e: e.tensor_tensor(out=t1, in0=ar, in1=br, op=ALU.mult), r=rb, w=[tm1])
                P.op("dve", lambda e: e.tensor_tensor(out=t2, in0=ai, in1=bi_, op=ALU.mult), r=rb, w=[tm2])
                P.op("dve", lambda e: e.tensor_tensor(out=outr, in0=t1, in1=t2, op=ALU.subtract), r=[tm1, tm2], w=[wbr])
                P.op("dve", lambda e: e.tensor_tensor(out=t1, in0=ar, in1=bi_, op=ALU.mult), r=rb, w=[tm1])
                P.op("dve", lambda e: e.tensor_tensor(out=t2, in0=ai, in1=br, op=ALU.mult), r=rb, w=[tm2])
                P.op("dve", lambda e: e.tensor_tensor(out=outi, in0=t1, in1=t2, op=ALU.add), r=[tm1, tm2], w=[wbi])

            def fft_pass(src_d, src_ap, K, o, mode, dst_d=None):
                for gi in range(32):
                    ch0 = gi * 4
                    P.dma("sp", X[:K, :, :], src_ap[ch0:ch0 + 4, 0:K * 256].rearrange("c (n1 n2) -> n1 c n2", n2=256), r=[src_d], w=[X])
                    for c in range(4):
                        for h in range(2):
                            P.op("pe", lambda e, c=c, h=h: e.matmul(PS[c][:, h * 256:(h + 1) * 256], X[:K, c, h * 128:(h + 1) * 128], F1[:K, :],
                                                                    start=True, stop=True), r=[X, F1], w=[PS[c]])
                    tv3 = lambda t: t[:, 0:256].rearrange("p (h k) -> p h k", h=2)
                    for c in range(4):
                        bank = PS[c][:, :].rearrange("p (h r k) -> p h r k", h=2, r=2)
                        cmul(Apr[:, :, c, :], Api[:, :, c, :], bank[:, :, 0, :], bank[:, :, 1, :], TW[:, 0:2, :], TW[:, 2:4, :], tv3,
                             [PS[c], TW], Apr, Api)
                    fl = lambda ap: ap.rearrange("p c k -> p (c k)")
                    for q in range(2):
                        PR, PI = PS[4 + q * 2], PS[5 + q * 2]
                        seq_r = [(CS[:, h, q * 128:(q + 1) * 128], Apr[:, h, :, :]) for h in range(2)] + \
                                [(CS[:, h, 256 + q * 128:256 + (q + 1) * 128], Api[:, h, :, :]) for h in range(2)]
                        seq_i = [(CS[:, h, q * 128:(q + 1) * 128], Api[:, h, :, :]) for h in range(2)] + \
                                [(NSC[:, h, q * 128:(q + 1) * 128], Apr[:, h, :, :]) for h in range(2)]
                        for (pp, seq) in [(PR, seq_r), (PI, seq_i)]:
                            for k, (lt, rh) in enumerate(seq):
                                P.op("pe", lambda e, pp=pp, lt=lt, rh=rh, k=k: e.matmul(pp[:, :], lt, fl(rh), start=(k == 0), stop=(k == 3)),
                                     r=[CS, NSC, Apr, Api], w=[pp])
                    if mode == "filter":
                        for q in range(2):
                            P.op("act", lambda e, q=q: e.activation(out=fl(Yr[:, q, :, :]), in_=PS[4 + q * 2][:, :], func=AF.Copy), r=[PS[4 + q * 2]], w=[Yr])
                            P.op("act", lambda e, q=q: e.activation(out=fl(Yi[:, q, :, :]), in_=PS[5 + q * 2][:, :], func=AF.Copy), r=[PS[5 + q * 2]], w=[Yi])
                        P.dma("sp", Hr_d.t[o, :, :, ch0:ch0 + 4, :].rearrange("q p c k -> p q c k"), Yr[:, :, :, :], r=[Yr], w=[Hr_d])
                        P.dma("sp", Hi_d.t[o, :, :, ch0:ch0 + 4, :].rearrange("q p c k -> p q c k"), Yi[:, :, :, :], r=[Yi], w=[Hi_d])
                        continue
                    P.dma("sp", Hr[:, :, :, :], Hr_d.t[o, :, :, ch0:ch0 + 4, :].rearrange("q p c k -> p q c k"), r=[Hr_d], w=[Hr])
                    P.dma("sp", Hi[:, :, :, :], Hi_d.t[o, :, :, ch0:ch0 + 4, :].rearrange("q p c k -> p q c k"), r=[Hi_d], w=[Hi])
                    for q in range(2):
                        cmul(fl(Yr[:, q, :, :]), fl(Yi[:, q, :, :]), PS[4 + q * 2][:, :], PS[5 + q * 2][:, :], fl(Hr[:, q, :, :]), fl(Hi[:, q, :, :]),
                             lambda t: t[:, :], [PS[4 + q * 2], PS[5 + q * 2], Hr, Hi], Yr, Yi)
                    for c in range(4):
                        seq = []
                        for q in range(2):
                            seq += [(Yr[:, q, c, :], CS[:, q, :]), (Yi[:, q, c, :], NSC[:, q, :])]
                        for k, (lt, rh) in enumerate(seq):
                            P.op("pe", lambda e, c=c, lt=lt, rh=rh, k=k: e.matmul(PS[c][:, :], lt, rh, start=(k == 0), stop=(k == 3)),
                                 r=[Yr, Yi, CS, NSC], w=[PS[c]])
                    for c in range(4):
                        cmul(Br[:, c, :], Bi[:, c, :], PS[c][:, 0:256], PS[c][:, 256:512], TWI[:, 0, :], TWI[:, 1, :], lambda t: t[:, 0:256],
                             [PS[c], TWI], Br, Bi)
                    fl2 = lambda ap: ap.rearrange("p c m -> p (c m)")
                    for pr in range(2):
                        py = PS[4 + pr]
                        P.op("pe", lambda e, pr=pr, py=py: e.matmul(py[:64, :], C1S[:, 0, :], fl2(Br[:, 2 * pr:2 * pr + 2, :]), start=True, stop=False),
                             r=[C1S, Br], w=[py])
                        P.op("pe", lambda e, pr=pr, py=py: e.matmul(py[:64, :], C1S[:, 1, :], fl2(Bi[:, 2 * pr:2 * pr + 2, :]), start=False, stop=True),
                             r=[C1S, Bi], w=[py])
                        P.op("act", lambda e, pr=pr, py=py: e.activation(out=fl2(yt[:, 2 * pr:2 * pr + 2, :]), in_=py[:64, :], func=AF.Copy), r=[py], w=[yt])
                    P.dma("sp", dst_d.t[ch0:ch0 + 4, :].rearrange("c (n1 n2) -> n1 c n2", n2=256), yt[:, :, :], r=[yt], w=[dst_d])

            for o in range(2):
                fft_pass(hfull_d, hfull_d.t[o], 128, o, "filter")

        hi_ = 0
        for bi in range(2):
            for (s0, L) in [(0, CTX), (CTX, LAT)]:
                if fft and L == LAT:
                    for b0 in range(0, L, LB):
                        short_conv_block(bi, 0, s0, L, b0, LB, xg[:, :LB], xg)
                        P.dma("sp", cur_d[0][:, b0:b0 + LB], xg[:, :LB], r=[xg], w=[cur_d[0]])
                    for o in range(2):
                        fft_pass(cur_d[o % 2], cur_d[o % 2].t, 64, o, "conv", conv_d)
                        dst = cur_d[(o + 1) % 2]
                        for b0 in range(0, L, LB):
                            P.dma("sp", cb[:, :], cur_d[o % 2][:, b0:b0 + LB], r=[cur_d[o % 2]], w=[cb])
                            P.dma("sp", vb2[:, :], conv_d[:, b0:b0 + LB], r=[conv_d], w=[vb2])
                            P.op("dve", lambda e, o=o: e.scalar_tensor_tensor(out=cb[:, :], in0=cb[:, :], scalar=lbs[:, o:o + 1], in1=vb2[:, :],
                                                                             op0=ALU.mult, op1=ALU.add), r=[cb, lbs, vb2], w=[cb])
                            short_conv_block(bi, o + 1, s0, L, b0, LB, xg[:, :LB], xg)
                            P.op("dve", lambda e: e.tensor_tensor(out=cb[:, :], in0=cb[:, :], in1=xg[:, :LB], op=ALU.mult), r=[cb, xg], w=[cb])
                            if o == 0:
                                P.dma("sp", dst[:, b0:b0 + LB], cb[:, :], r=[cb], w=[dst])
                            else:
                                P.dma("sp", hy_out[bi, :, s0 + b0:s0 + b0 + LB], cb[:, :], r=[cb], w=[hy_out])
                    continue
                cur, nxt = u, acc
                short_conv(bi, 0, s0, L, cur)
                for o in range(2):
                    P.op("dve", lambda e, cur=cur, nxt=nxt, o=o: e.tensor_scalar(out=nxt[:, 0:L], in0=cur[:, 0:L], scalar1=lbs[:, o:o + 1], scalar2=None,
                                                                                op0=ALU.mult), r=[cur, lbs], w=[nxt])
                    for dr in range(2):
                        for l0 in range(0, L, LB):
                            n = min(LB, L - l0)
                            hb = hring[hi_ % 2]; hi_ += 1
                            P.dma("sp", hb[:, :n], hf_d[L][o * 2 + dr, :, l0:l0 + n], r=[hf_d[L]], w=[hb])
                            for m in range(l0, l0 + n):
                                if dr == 1 and m == 0:
                                    continue
                                if dr == 0:
                                    o_, i_ = nxt[:, m:L], cur[:, 0:L - m]
                                else:
                                    o_, i_ = nxt[:, 0:L - m], cur[:, m:L]
                                P.op("dve", lambda e, o_=o_, i_=i_, hb=hb, mm=m - l0: e.scalar_tensor_tensor(out=o_, in0=i_, scalar=hb[:, mm:mm + 1], in1=o_,
                                                                                                        op0=ALU.mult, op1=ALU.add),
                                     r=[cur, hb, nxt], w=[nxt])
                    short_conv(bi, o + 1, s0, L, None, mul_into=nxt)
                    cur, nxt = nxt, cur
                P.dma("sp", hy_out[bi, :, s0:s0 + L], cur[:, 0:L], r=[cur], w=[hy_out])
        P.barrier()

    w2s = [sb([16, 128]) for _ in range(2)]
    gb = sb([128, 2]); ngb = sb([64, 256])
    for dd in range(2):
        P.dma("sp", w2s[dd][:], gw2[dd, :, :], r=[gw2], w=[w2s[dd]])
    P.dma("sp", gb[:], gbc[:, :], r=[gbc], w=[gb])
    P.op("dve", lambda e: e.tensor_scalar(out=gb[:], in0=gb[:], scalar1=-1.0, scalar2=None, op0=ALU.mult), r=[gb], w=[gb])
    P.dma("sp", ngb[:], bass.AP(gng.t.tensor, gng.t.offset, [[0, 64], [1, 256]]), r=[gng], w=[ngb])
    g1 = sb([16, 512]); qk = sb([128, 2, 512]); g = sb([128, 512]); bb = sb([128, 512]); t5 = sb([128, 512])
    ebb = sb([128, 512]); qt = sb([128, 512], BF16); kt = sb([128, 512], BF16); ktl = sb([128, 512]); ebl = sb([128, 8])
    vr = sb([64, 512]); vb = sb([64, 256], BF16); attb = sb([64, 64], BF16); ktT = sb([64, 128], BF16)
    S = sb([128, 256]); Sb = sb([128, 256], BF16); o_ = sb([64, 256]); of = sb([64, 256]); sq = sb([64, 256]); st = sb([64, 2])
    QS = 128 ** -0.5
    for d in range(2):
        msk = mF if d == 0 else mB
        P.op("dve", lambda e: e.memset(S[:], 0.0), w=[S])
        P.op("dve", lambda e: e.memset(Sb[:], 0.0), w=[Sb])
        blks = [blocks[0]] + blocks[1:] if d == 0 else [blocks[0]] + blocks[1:][::-1]
        for (t0, nb) in blks:
            nch = nb // 64
            P.dma("sp", g1[:, :nb], gg_d[d, :, t0:t0 + nb], r=[gg_d], w=[g1])
            P.dma("sp", qk[:, :, :nb], gq_d.t[:, t0:t0 + nb].rearrange("(m p) t -> p m t", p=128), r=[gq_d], w=[qk])
            P.op("pe", lambda e: e.matmul(PS[0][:, :nb], w2s[d][:, :], g1[:, :nb], start=True, stop=True), r=[w2s[d], g1], w=[PS[0]])
            P.op("act", lambda e: e.activation(out=g[:, :nb], in_=PS[0][:, :nb], func=AF.Exp, bias=gb[:, d:d + 1], scale=-1.0), r=[PS[0], gb], w=[g])
            P.op("act", lambda e: e.activation(out=g[:, :nb], in_=g[:, :nb], func=AF.Ln, bias=1.0, scale=1.0), r=[g], w=[g])
            P.op("dve", lambda e: e.tensor_scalar(out=g[:, :nb], in0=g[:, :nb], scalar1=-1.0 / 16, scalar2=None, op0=ALU.mult), r=[g], w=[g])
            P.op("dve", lambda e: e.tensor_tensor_scan(out=bb[:, :nb], data0=rmask[:, :nb], data1=g[:, :nb], initial=0.0, op0=ALU.mult, op1=ALU.add),
                 r=[rmask, g], w=[bb])
            b3 = lambda t: t[:, :nb].rearrange("p (c q) -> p c q", q=64)
            if d == 1:
                P.op("dve", lambda e: e.tensor_tensor(out=t5[:, :nb], in0=g[:, :nb], in1=bb[:, :nb], op=ALU.subtract), r=[g, bb], w=[t5])
                P.op("dve", lambda e: e.tensor_tensor(out=b3(bb), in0=b3(t5), in1=b3(bb)[:, :, 63:64].to_broadcast([128, nch, 64]), op=ALU.add),
                     r=[t5, bb], w=[bb])
            lastcol = 63 if d == 0 else 0
            P.op("act", lambda e: e.activation(out=ebl[:, :nch], in_=b3(bb)[:, :, lastcol], func=AF.Exp), r=[bb], w=[ebl])
            P.op("dve", lambda e: e.tensor_tensor(out=b3(t5), in0=b3(bb)[:, :, lastcol:lastcol + 1].to_broadcast([128, nch, 64]), in1=b3(bb), op=ALU.subtract),
                 r=[bb], w=[t5])
            P.op("act", lambda e: e.activation(out=t5[:, :nb], in_=t5[:, :nb], func=AF.Exp), r=[t5], w=[t5])
            P.op("dve", lambda e: e.tensor_tensor(out=ktl[:, :nb], in0=qk[:, 1, :nb], in1=t5[:, :nb], op=ALU.mult), r=[qk, t5], w=[ktl])
            P.op("act", lambda e: e.activation(out=ebb[:, :nb], in_=bb[:, :nb], func=AF.Exp), r=[bb], w=[ebb])
            P.op("dve", lambda e: e.scalar_tensor_tensor(out=qt[:, :nb], in0=qk[:, 0, :nb], scalar=QS, in1=ebb[:, :nb], op0=ALU.mult, op1=ALU.mult),
                 r=[qk, ebb], w=[qt])
            P.op("act", lambda e: e.activation(out=ebb[:, :nb], in_=bb[:, :nb], func=AF.Exp, scale=-1.0), r=[bb], w=[ebb])
            P.op("dve", lambda e: e.tensor_tensor(out=kt[:, :nb], in0=qk[:, 1, :nb], in1=ebb[:, :nb], op=ALU.mult), r=[qk, ebb], w=[kt])
            chs = list(range(nch)) if d == 0 else list(range(nch))[::-1]
            for c in chs:
                c0 = c * 64
                p0 = t0 + c0
                P.dma("sp", vr[:], gvr_d[p0:p0 + 64, :], r=[gvr_d], w=[vr])
                P.op("act", lambda e: e.activation(out=vb[:], in_=vr[:, 0:256], func=AF.Copy), r=[vr], w=[vb])
                P.op("pe", lambda e: e.matmul(PS[1][:64, 0:64], kt[:, c0:c0 + 64], qt[:, c0:c0 + 64], start=True, stop=True), r=[kt, qt], w=[PS[1]])
                P.op("dve", lambda e: e.tensor_tensor(out=attb[:], in0=PS[1][:64, 0:64], in1=msk[:], op=ALU.mult), r=[PS[1], msk], w=[attb])
                P.op("pe", lambda e: e.matmul(PS[2][:64, 0:256], attb[:, :], vb[:, :], start=True, stop=False), r=[attb, vb], w=[PS[2]])
                P.op("pe", lambda e: e.matmul(PS[2][:64, 0:256], qt[:, c0:c0 + 64], Sb[:, :], start=False, stop=True), r=[qt, Sb], w=[PS[2]])
                if d == 0:
                    P.op("act", lambda e: e.activation(out=o_[:], in_=PS[2][:64, 0:256], func=AF.Copy), r=[PS[2]], w=[o_])
                    P.dma("sp", of_d[p0:p0 + 64, :], o_[:], r=[o_], w=[of_d])
                else:
                    P.dma("sp", of[:], of_d[p0:p0 + 64, :], r=[of_d], w=[of])
                    P.op("dve", lambda e: e.tensor_tensor(out=o_[:], in0=of[:], in1=PS[2][:64, 0:256], op=ALU.add), r=[of, PS[2]], w=[o_])
                    P.op("act", lambda e: e.activation(out=sq[:], in_=o_[:], func=AF.Square), r=[o_], w=[sq])
                    P.op("dve", lambda e: e.reduce_sum(out=st[:, 0:1], in_=sq[:], axis=AX.X), r=[sq], w=[st])
                    P.op("dve", lambda e: e.tensor_scalar(out=st[:, 0:1], in0=st[:, 0:1], scalar1=1.0 / 256, scalar2=EPS, op0=ALU.mult, op1=ALU.add),
                         r=[st], w=[st])
                    P.op("act", lambda e: e.activation(out=st[:, 0:1], in_=st[:, 0:1], func=AF.Sqrt), r=[st], w=[st])
                    P.op("dve", lambda e: e.reciprocal(out=st[:, 1:2], in_=st[:, 0:1]), r=[st], w=[st])
                    P.op("dve", lambda e: e.scalar_tensor_tensor(out=o_[:], in0=o_[:], scalar=st[:, 1:2], in1=ngb[:], op0=ALU.mult, op1=ALU.mult),
                         r=[o_, st, ngb], w=[o_])
                    P.op("act", lambda e: e.activation(out=sq[:], in_=vr[:, 256:512], func=AF.Silu), r=[vr], w=[sq])
                    P.op("dve", lambda e: e.tensor_tensor(out=o_[:], in0=o_[:], in1=sq[:], op=ALU.mult), r=[o_, sq], w=[o_])
                    P.dma("sp", gla_out[p0:p0 + 64, :], o_[:], r=[o_], w=[gla_out])
                P.op("pe", lambda e: e.transpose(PS[3][:64, 0:128], ktl[:, c0:c0 + 64], ident[:, :]), r=[ktl, ident], w=[PS[3]])
                P.op("act", lambda e: e.activation(out=ktT[:], in_=PS[3][:64, 0:128], func=AF.Copy), r=[PS[3]], w=[ktT])
                P.op("pe", lambda e: e.matmul(PS[4][:, 0:256], ktT[:, :], vb[:, :], start=True, stop=True), r=[ktT, vb], w=[PS[4]])
                P.op("dve", lambda e, c=c: e.scalar_tensor_tensor(out=S[:], in0=S[:], scalar=ebl[:, c:c + 1], in1=PS[4][:, 0:256], op0=ALU.mult, op1=ALU.add),
                     r=[S, ebl, PS[4]], w=[S])
                P.op("act", lambda e: e.activation(out=Sb[:], in_=S[:], func=AF.Copy), r=[S], w=[Sb])
    P.finish([hy_out, gla_out])
    return P


HY_MIN_DECAY = math.log(1e-2) / 1.5
HY_MAX_DECAY = math.log(1e-2) / 0.3


def _colpack(vecs):
    a = np.stack(vecs, 0).reshape(len(vecs), 16, 128)
    return np.ascontiguousarray(a.transpose(2, 0, 1).reshape(128, len(vecs) * 16)).astype(np.float32)


def _hy_tables(L):
    pos = np.arange(L, dtype=np.float64)[None, :]
    t = pos / max(L - 1, 1)
    bands = np.linspace(1e-4, 15, 16, dtype=np.float32).astype(np.float64)[:, None]
    ang = 2.0 * math.pi * bands * pos / L
    z = np.concatenate([t, np.cos(ang), -np.sin(ang)], 0).astype(np.float32)
    deltas = np.abs(np.linspace(HY_MIN_DECAY, HY_MAX_DECAY, 1024, dtype=np.float32)).astype(np.float64)
    E = np.exp(-t * deltas[:, None]).astype(np.float32)
    return z, E


def fft_consts():
    N = 32768
    p = np.arange(128, dtype=np.float64)[:, None]
    k128 = np.arange(128, dtype=np.float64)[None, :]
    a1 = 2 * np.pi * p * k128 / 128
    F1 = np.concatenate([np.cos(a1), -np.sin(a1)], 1)
    n2 = (np.arange(2)[None, :, None] * 128 + p[:, :, None])
    at = 2 * np.pi * n2 * np.arange(128)[None, None, :] / N
    TW = np.concatenate([np.cos(at), -np.sin(at)], 1).reshape(128, 512)
    a2 = 2 * np.pi * n2 * np.arange(256)[None, None, :] / 256
    CS = np.concatenate([np.cos(a2), np.sin(a2)], 2).reshape(128, 1024)
    NSC = np.concatenate([-np.sin(a2), np.cos(a2)], 2).reshape(128, 1024)
    ai = 2 * np.pi * p * np.arange(256, dtype=np.float64)[None, :] / N
    TWI = np.concatenate([np.cos(ai), np.sin(ai)], 1)
    a3 = 2 * np.pi * p * np.arange(64, dtype=np.float64)[None, :] / 128
    C1S = np.concatenate([np.cos(a3), -np.sin(a3)], 1) / N
    f = lambda a: np.ascontiguousarray(a, dtype=np.float32)
    return dict(fF1=f(F1), fTW=f(TW), fCS=f(CS), fNSC=f(NSC), fTWI=f(TWI), fC1S=f(C1S))


def _hy_tables_rev(L):
    pos = (L - np.arange(L, dtype=np.float64))[None, :]
    t = pos / max(L - 1, 1)
    bands = np.linspace(1e-4, 15, 16, dtype=np.float32).astype(np.float64)[:, None]
    ang = 2.0 * math.pi * bands * pos / L
    z = np.concatenate([t, np.cos(ang), -np.sin(ang)], 0).astype(np.float32)
    deltas = np.abs(np.linspace(HY_MIN_DECAY, HY_MAX_DECAY, 1024, dtype=np.float32)).astype(np.float64)
    E = np.exp(-t * deltas[:, None]).astype(np.float32)
    E[:, 0] = 0.0
    return z, E


def mix0_consts():
    c = np.zeros((4, 128, 512), np.float32)
    k = np.arange(128)[:, None]; i = np.arange(128)[None, :]
    c[0, :, :128] = np.eye(128); c[1, :, :128] = (k <= i); c[2, :, :128] = (k >= i)
    c[3] = 1.0; c[3, :, ::64] = 0.0
    return c


def mix0_core_inputs(ci, xTb, modl, modc, W, tabs, rev=None):
    b, head, ct = ci // 4, ci % 4, ci
    w_in = W["ab_w_in"]
    sl = lambda s, n: w_in[:, s:s + n]
    (zc, Ec), (zl, El) = tabs
    w3 = W["hy_f_w3"]
    extra = {}
    if rev is not None:
        zr, Er = rev
        extra = dict(zr=zr, Er=np.ascontiguousarray(Er[ct * 128:(ct + 1) * 128]), **fft_consts())
    return dict(
        **extra,
        xT=np.stack([xTb[b], xTb[1 - b]]),
        modcol=np.stack([_colpack([modl[b][0], modl[b][1]]), _colpack([modl[1 - b][0], modl[1 - b][1]]), _colpack([modc[0], modc[1]])]),
        whc=np.ascontiguousarray(np.concatenate([sl(ct * 128, 128), sl(1024 + ct * 128, 128), sl(2048 + ct * 128, 128)], 1)),
        wgc=np.ascontiguousarray(np.concatenate([sl(3072 + head * 128, 128), sl(3584 + head * 128, 128), sl(6144, 32)], 1)),
        wgt=np.ascontiguousarray(np.concatenate([sl(4096 + head * 256, 256), sl(5120 + head * 256, 256)], 1)),
        hcw=np.ascontiguousarray(np.stack([np.concatenate([W["hy_conv_w"][:, ti * 1024 + ct * 128:ti * 1024 + (ct + 1) * 128],
                                                           W["hy_conv_b"][None, ti * 1024 + ct * 128:ti * 1024 + (ct + 1) * 128]], 0).T
                                           for ti in range(3)], 1).reshape(128, 12)),
        hlb=np.ascontiguousarray(W["hy_long_bias"][:, ct * 128:(ct + 1) * 128].T),
        fw1=W["hy_f_w1"], fcol=np.ascontiguousarray(np.stack([W["hy_f_b1"], W["hy_f_fr1"], W["hy_f_b2"], W["hy_f_fr2"]], 1)),
        fw2=W["hy_f_w2"],
        fw3=np.ascontiguousarray(np.concatenate([w3[:, o * 2048 + dr * 1024 + ct * 128:o * 2048 + dr * 1024 + (ct + 1) * 128]
                                                 for o in range(2) for dr in range(2)], 1)),
        zl=zl, zc=zc, El=np.ascontiguousarray(El[ct * 128:(ct + 1) * 128]), Ec=np.ascontiguousarray(Ec[ct * 128:(ct + 1) * 128]),
        gw2=np.ascontiguousarray(W["gla_gate_w2"][:, :, head * 128:(head + 1) * 128]),
        gbc=np.ascontiguousarray(W["gla_gate_b"][:, head * 128:(head + 1) * 128].T),
        gng=np.ascontiguousarray(W["gla_norm_g"][head * 256:(head + 1) * 256]),
        consts=mix0_consts(),
    )


def ssd_unit_inputs(g, w_in, conv_w, conv_b, dt_bias, a_log, d_skip, norm_g):
    xs = 4096 + g * 512; bs = 4096 + 4096 + g * 128; cs = 4096 + 4096 + 1024 + g * 128
    wch = np.concatenate([w_in[:, xs:xs + 512], w_in[:, bs:bs + 128], w_in[:, cs:cs + 128]], 1)
    wtm = np.concatenate([w_in[:, g * 512:(g + 1) * 512], w_in[:, 10240 + g * 8:10240 + g * 8 + 8], w_in[:, 10304 + g * 8:10304 + g * 8 + 8]], 1)
    cidx = np.concatenate([np.arange(g * 512, (g + 1) * 512), 4096 + g * 128 + np.arange(128), 4096 + 1024 + g * 128 + np.arange(128)])
    cwf = np.concatenate([conv_w.reshape(9, 6144)[:, cidx], conv_b[None, cidx]], 0)
    cw = np.ascontiguousarray(cwf.reshape(10, 6, 128).transpose(2, 1, 0).reshape(128, 60))
    hp = np.concatenate([dt_bias[:, g * 8:(g + 1) * 8].reshape(-1), a_log[:, g * 8:(g + 1) * 8].reshape(-1), d_skip[:, g * 8:(g + 1) * 8].reshape(-1)])
    return wch, wtm, cw, hp.astype(np.float32), norm_g[g * 512:(g + 1) * 512]


def ssd_consts():
    k = np.arange(128)[:, None]; i = np.arange(128)[None, :]
    return np.stack([np.eye(128), np.ones((128, 128)), k <= i, k >= i, k > i, k < i]).astype(np.float32)


def _run(P, in_maps):
    in_maps = [{k: np.ascontiguousarray(v, dtype=np.float32) for k, v in m.items()} for m in in_maps]
    res = run_bass_kernel_spmd(P.nc, in_maps, core_ids=list(range(len(in_maps))))
    return res.results


def _post_launch(KC, T_l, T_c, yT_list, xres_list, modl, modc, w_out, lng, lnb, rw, rb, wg, wu, wd):
    ident = np.eye(128, dtype=np.float32)
    sel = np.zeros((16, 16, 128), np.float32)
    for e in range(16):
        sel[e, e, :] = 1
    sel = sel.reshape(16, 2048)
    P = build_post(T_l, T_c, KC)
    ins = []
    for ci in range(8):
        b = ci // 4
        ins.append(dict(yT=yT_list[ci], xres=xres_list[ci], w_out=w_out,
                        modrow=np.stack([modl[b], modc]), modcol=np.stack([_colpack(list(modl[b])), _colpack(list(modc))]),
                        lng=lng, lnb=lnb, rw=rw, rb=rb, wg=wg, wu=wu, wd=wd, ident=ident, sel=sel))
    return [r["xout"] for r in _run(P, ins)]


def kernel(**I):
    I = {k: np.asarray(v, dtype=np.float32) for k, v in I.items()}
    x, c, ctx, c_ctx = I["x"], I["c"], I["ctx"], I["c_ctx"]
    LAT, CTX = x.shape[1], ctx.shape[1]
    TB = LAT + CTX
    TPC = LAT // 4
    CPC = CTX // 4
    c_all = np.concatenate([c, c_ctx[None]], 0)
    cT = np.ascontiguousarray(c_all.reshape(3, 16, 128).transpose(2, 1, 0).reshape(128, 48))
    res = _run(build_mod(), [dict(cT=cT, w=I["mod_w"][:, :, i * 1536:(i + 1) * 1536], b=I["mod_b"][:, i * 1536:(i + 1) * 1536]) for i in range(8)])
    mod = np.concatenate([r["out"] for r in res], -1)
    modl = [[mod[l, b].reshape(6, D) for b in range(2)] for l in range(2)]
    modc = [mod[l, 2].reshape(6, D) for l in range(2)]
    xTb = [np.ascontiguousarray(np.concatenate([ctx[b], x[b]], 0).T) for b in range(2)]
    W0 = {k: I[k][0] for k in ["ab_w_in", "hy_conv_w", "hy_conv_b", "hy_f_w1", "hy_f_b1", "hy_f_fr1", "hy_f_w2", "hy_f_b2", "hy_f_fr2",
                               "hy_f_w3", "hy_long_bias", "gla_gate_w2", "gla_gate_b", "gla_norm_g"]}
    tabs = (_hy_tables(CTX), _hy_tables(LAT))
    use_fft = (LAT == 16384)
    rev = _hy_tables_rev(LAT) if use_fft else None
    res = _run(build_mix0(LAT, CTX, use_fft), [mix0_core_inputs(ci, xTb, modl[0], modc[0], W0, tabs, rev) for ci in range(8)])
    yT0 = [np.zeros((D, TB), np.float32) for _ in range(2)]
    for ci in range(8):
        b, head, ct = ci // 4, ci % 4, ci
        yT0[b][ct * 128:(ct + 1) * 128] = res[ci]["hy_out"][0]
        yT0[1 - b][ct * 128:(ct + 1) * 128] = res[ci]["hy_out"][1]
        yT0[b][1024 + head * 256:1024 + (head + 1) * 256] = res[ci]["gla_out"].T
    del xTb, res
    yl, xl = [], []
    for ci in range(8):
        b, j = ci // 4, ci % 4
        yl.append(np.concatenate([yT0[b][:, CTX + j * TPC:CTX + (j + 1) * TPC], yT0[b][:, j * CPC:(j + 1) * CPC]], 1))
        xl.append(np.concatenate([x[b, j * TPC:(j + 1) * TPC], ctx[b, j * CPC:(j + 1) * CPC]], 0))
    outs = _post_launch(16, TPC, CPC, yl, xl, modl[0], modc[0], I["ab_w_out"][0], I["ln_g"][0], I["ln_b"][0], I["router_w"], I["router_b"],
                        I["exp_w_gate"][0], I["exp_w_up"][0], I["exp_w_down"][0])
    x1 = np.zeros_like(x); ctx1 = np.zeros_like(ctx)
    for ci in range(8):
        b, j = ci // 4, ci % 4
        x1[b, j * TPC:(j + 1) * TPC] = outs[ci][:TPC]
        ctx1[b, j * CPC:(j + 1) * CPC] = outs[ci][TPC:]
    del yT0, yl, xl, outs
    xT1 = [np.ascontiguousarray(np.concatenate([ctx1[b], x1[b]], 0).T) for b in range(2)]
    cst = ssd_consts()
    ins = []
    for ci in range(8):
        b, gp = ci // 4, ci % 4
        us = [ssd_unit_inputs(2 * gp + u, I["ssd_w_in"][0], I["ssd_conv_w"][0], I["ssd_conv_b"][0], I["ssd_dt_bias"][0], I["ssd_a_log"][0],
                              I["ssd_d"][0], I["ssd_norm_g"][0]) for u in range(2)]
        ins.append(dict(xT=xT1[b], modcol=np.stack([_colpack([modl[1][b][0], modl[1][b][1]]), _colpack([modc[1][0], modc[1][1]])]),
                        wch=np.stack([u_[0] for u_ in us]), wtm=np.stack([u_[1] for u_ in us]), cw=np.stack([u_[2] for u_ in us]),
                        hp=np.stack([u_[3] for u_ in us]), ng=np.stack([u_[4] for u_ in us]), consts=cst))
    res = _run(build_ssd(LAT, CTX, 2), ins)
    yT1 = [np.zeros((4096, LAT), np.float32) for _ in range(2)]
    for ci in range(8):
        b, gp = ci // 4, ci % 4
        for u in range(2):
            g = 2 * gp + u
            yT1[b][g * 512:(g + 1) * 512] = res[ci]["ymix"][u].T
    del xT1, ins, res
    yl, xl = [], []
    for ci in range(8):
        b, j = ci // 4, ci % 4
        yl.append(yT1[b][:, j * TPC:(j + 1) * TPC])
        xl.append(x1[b, j * TPC:(j + 1) * TPC])
    outs = _post_launch(32, TPC, 0, yl, xl, modl[1], modc[1], I["ssd_w_out"][0], I["ln_g"][1], I["ln_b"][1], I["router_w"], I["router_b"],
                        I["exp_w_gate"][1], I["exp_w_up"][1], I["exp_w_down"][1])
    out = np.zeros_like(x)
    for ci in range(8):
        b, j = ci // 4, ci % 4
        out[b, j * TPC:(j + 1) * TPC] = outs[ci]
    return out
```
